# Optimizing a Trainium2 kernel written in Bass

```python
import math
import jax
import jax.numpy as jnp
from jax import lax
import numpy as np

D_MODEL = 1024
BATCH = 8
SEQ = 2048
DEPTH = 4

CTX_LEN = 256
GRID_W = 64
HEAD_DIM = 64
NA_HEADS = 4
NA_ROWS_MAX = 8
NA_COLS = 16
GA_HEADS = 4
GA_KV_HEADS = 2
SSM_GROUPS = 16
SSM_GROUP_CH = 16
SSM_STATE = 64
SW_HEADS = 4
SW_KV_HEADS = 2
SW_WINDOW = 128
Q_BLOCK = 128
NA_WIDTH = NA_HEADS * HEAD_DIM
GA_WIDTH = GA_HEADS * HEAD_DIM
GA_KV_WIDTH = GA_KV_HEADS * HEAD_DIM
SSM_WIDTH = SSM_GROUPS * SSM_GROUP_CH
SW_WIDTH = SW_HEADS * HEAD_DIM
SW_KV_WIDTH = SW_KV_HEADS * HEAD_DIM
MIX_WIDTH = NA_WIDTH + GA_WIDTH + SSM_WIDTH + SW_WIDTH
IN_SIZES = (NA_WIDTH, NA_WIDTH, NA_WIDTH, GA_WIDTH, GA_KV_WIDTH, GA_KV_WIDTH, SSM_WIDTH, SW_WIDTH, SW_KV_WIDTH, SW_KV_WIDTH)
IN_WIDTH = sum(IN_SIZES)
D_FF = 2816
N_EXPERTS = 8
TOP_K = 2
D_FF_EXPERT = 3584
ADA_CHUNKS = 6
ROPE_THETA = 10000.0
LN_EPS = 1e-6
RMS_EPS = 1e-6
NEG_INF = -1e30
DEEPNORM_ALPHA = (2 * DEPTH) ** 0.25
DEEPNORM_BETA = (8 * DEPTH) ** -0.25
F32 = jnp.float32

kernel_name = 'hybrid_dit_parallel_mixer_trunk'


def _layer_norm(x, g, b):
    xf = x.astype(F32)
    xc = xf - jnp.mean(xf, -1, keepdims=True)
    y = xc * lax.rsqrt(jnp.mean(xc * xc, -1, keepdims=True) + LN_EPS)
    return (y * g.astype(F32) + b.astype(F32)).astype(x.dtype)


def _rms_norm(x, g):
    xf = x.astype(F32)
    y = xf * lax.rsqrt(jnp.mean(xf * xf, -1, keepdims=True) + RMS_EPS)
    return (y * g.astype(F32)).astype(x.dtype)


def _heads(t):
    return t.reshape(t.shape[:-1] + (t.shape[-1] // HEAD_DIM, HEAD_DIM))


def _split_cols(p):
    return jnp.split(p, np.cumsum(IN_SIZES)[:-1].tolist(), axis=-1)


def _axial_rope_tables(n_tokens):
    pos = jnp.arange(n_tokens, dtype=jnp.int32)
    row = (pos // GRID_W).astype(F32)
    col = (pos % GRID_W).astype(F32)
    n_freq = HEAD_DIM // 4
    inv_freq = ROPE_THETA ** (-jnp.arange(n_freq, dtype=F32) / n_freq)
    ang = jnp.concatenate([row[:, None] * inv_freq, col[:, None] * inv_freq], axis=-1)
    return jnp.cos(ang), jnp.sin(ang)


def _apply_rope(x, cos, sin):
    xf = x.astype(F32)
    x1, x2 = jnp.split(xf, 2, axis=-1)
    c = cos[None, :, None, :]
    s = sin[None, :, None, :]
    return jnp.concatenate([x1 * c - x2 * s, x1 * s + x2 * c], axis=-1).astype(x.dtype)


def _sink_softmax(s, sink_b):
    m = jnp.maximum(jnp.max(s, -1, keepdims=True), sink_b)
    e = jnp.exp(s - m)
    return e / (jnp.sum(e, -1, keepdims=True) + jnp.exp(sink_b - m))


def _context_attention(q, k, v, sink=None):
    b, n, hq, dh = q.shape
    g = k.shape[2]
    r = hq // g
    qg = q.reshape(b, n, g, r, dh)
    s = jnp.einsum('bqgrd,bkgd->bgrqk', qg, k, preferred_element_type=F32) * (dh ** -0.5)
    if sink is None:
        p = jax.nn.softmax(s, -1)
    else:
        p = _sink_softmax(s, sink.astype(F32).reshape(1, g, r, 1, 1))
    o = jnp.einsum('bgrqk,bkgd->bqgrd', p.astype(v.dtype), v)
    return o.reshape(b, n, hq * dh)


def _neighbourhood_attention(q, k, v, k_ctx, v_ctx, rpb):
    b, n, h, dh = q.shape
    rows = n // GRID_W
    kh = min(NA_ROWS_MAX, rows)
    r_idx = jnp.arange(rows)
    row_start = jnp.clip(r_idx - kh // 2, 0, rows - kh)
    band_rows = row_start[:, None] + jnp.arange(kh)[None, :]
    qg = q.reshape(b, rows, GRID_W, h, dh)
    kg = k.reshape(b, rows, GRID_W, h, dh)[:, band_rows]
    vg = v.reshape(b, rows, GRID_W, h, dh)[:, band_rows]
    c_idx = jnp.arange(GRID_W)
    col_start = jnp.clip(c_idx - NA_COLS // 2, 0, GRID_W - NA_COLS)
    col_valid = (c_idx[None, :] >= col_start[:, None]) & (c_idx[None, :] < col_start[:, None] + NA_COLS)
    row_rel = band_rows - r_idx[:, None] + NA_ROWS_MAX - 1
    col_rel = jnp.clip(c_idx[None, :] - c_idx[:, None], 1 - NA_COLS, NA_COLS - 1) + NA_COLS - 1
    bias = rpb.astype(F32)[:, row_rel[:, None, :, None], col_rel[None, :, None, :]]
    scale = dh ** -0.5
    s_loc = jnp.einsum('brqhd,brikhd->bhrqik', qg, kg, preferred_element_type=F32) * scale + bias
    s_loc = jnp.where(col_valid[:, None, :], s_loc, NEG_INF).reshape(b, h, rows, GRID_W, kh * GRID_W)
    s_ctx = jnp.einsum('brqhd,bchd->bhrqc', qg, k_ctx, preferred_element_type=F32) * scale
    p = jax.nn.softmax(jnp.concatenate([s_loc, s_ctx], -1), -1).astype(v.dtype)
    n_loc = kh * GRID_W
    p_loc = p[..., :n_loc].reshape(b, h, rows, GRID_W, kh, GRID_W)
    o = (jnp.einsum('bhrqik,brikhd->brqhd', p_loc, vg)
         + jnp.einsum('bhrqc,bchd->brqhd', p[..., n_loc:], v_ctx))
    return o.reshape(b, n, h * dh)


def _global_gqa(q, k, v, k_ctx, v_ctx):
    b, n, hq, dh = q.shape
    g = k.shape[2]
    r = hq // g
    k_all = jnp.concatenate([k_ctx, k], axis=1)
    v_all = jnp.concatenate([v_ctx, v], axis=1)
    qb = jnp.moveaxis(q.reshape(b, n // Q_BLOCK, Q_BLOCK, g, r, dh), 1, 0)

    def attend(qi):
        s = jnp.einsum('bqgrd,bkgd->bgrqk', qi, k_all, preferred_element_type=F32) * (dh ** -0.5)
        p = jax.nn.softmax(s, -1).astype(v_all.dtype)
        return jnp.einsum('bgrqk,bkgd->bqgrd', p, v_all)

    o = lax.map(attend, qb)
    return jnp.moveaxis(o, 0, 1).reshape(b, n, hq * dh)


def _window_gqa(q, k, v, k_ctx, v_ctx, sink):
    b, n, hq, dh = q.shape
    g = k.shape[2]
    r = hq // g
    nb = n // Q_BLOCK
    side = SW_WINDOW // Q_BLOCK
    span = (2 * side + 1) * Q_BLOCK

    def band(t):
        tp = jnp.pad(t, ((0, 0), (SW_WINDOW, SW_WINDOW), (0, 0), (0, 0))).reshape(b, nb + 2 * side, Q_BLOCK, g, dh)
        return jnp.concatenate([tp[:, j:j + nb] for j in range(2 * side + 1)], axis=2)

    kw = band(k)
    vw = band(v)
    blk = jnp.arange(nb)[:, None, None] * Q_BLOCK
    qpos = blk + jnp.arange(Q_BLOCK)[None, :, None]
    kpos = blk - SW_WINDOW + jnp.arange(span)[None, None, :]
    valid = (kpos >= 0) & (kpos < n) & (jnp.abs(kpos - qpos) <= SW_WINDOW)
    qg = q.reshape(b, nb, Q_BLOCK, g, r, dh)
    scale = dh ** -0.5
    s_loc = jnp.einsum('bnqgrd,bnkgd->bgrnqk', qg, kw, preferred_element_type=F32) * scale
    s_loc = jnp.where(valid, s_loc, NEG_INF)
    s_ctx = jnp.einsum('bnqgrd,bcgd->bgrnqc', qg, k_ctx, preferred_element_type=F32) * scale
    p = _sink_softmax(jnp.concatenate([s_loc, s_ctx], -1), sink.astype(F32).reshape(1, g, r, 1, 1, 1)).astype(v.dtype)
    o = (jnp.einsum('bgrnqk,bnkgd->bnqgrd', p[..., :span], vw)
         + jnp.einsum('bgrnqc,bcgd->bnqgrd', p[..., span:], v_ctx))
    return o.reshape(b, n, hq * dh)


def _s5_discretise(lam_re, lam_im, log_step, b_re, b_im):
    lam_re = lam_re.astype(F32)
    lam_im = lam_im.astype(F32)
    step = jnp.exp(log_step.astype(F32))[:, None]
    mag = jnp.exp(lam_re * step)
    ab_re = mag * jnp.cos(lam_im * step)
    ab_im = mag * jnp.sin(lam_im * step)
    den = lam_re * lam_re + lam_im * lam_im
    num_re = ab_re - 1.0
    f_re = ((num_re * lam_re + ab_im * lam_im) / den)[..., None]
    f_im = ((ab_im * lam_re - num_re * lam_im) / den)[..., None]
    b_re = b_re.astype(F32)
    b_im = b_im.astype(F32)
    return ab_re, ab_im, f_re * b_re - f_im * b_im, f_re * b_im + f_im * b_re


def _complex_linear_scan(u, ab_re, ab_im, bb_re, bb_im, reverse):
    bu_re = jnp.einsum('blgh,gph->blgp', u, bb_re)
    bu_im = jnp.einsum('blgh,gph->blgp', u, bb_im)
    a_re = jnp.broadcast_to(ab_re, bu_re.shape)
    a_im = jnp.broadcast_to(ab_im, bu_re.shape)

    def combine(e1, e2):
        a1r, a1i, b1r, b1i = e1
        a2r, a2i, b2r, b2i = e2
        return (a2r * a1r - a2i * a1i, a2r * a1i + a2i * a1r,
                a2r * b1r - a2i * b1i + b2r, a2r * b1i + a2i * b1r + b2i)

    return lax.associative_scan(combine, (a_re, a_im, bu_re, bu_im), axis=1, reverse=reverse)


def _s5_readout(h_re, h_im, c_re, c_im):
    return (jnp.einsum('blgp,ghp->blgh', h_re, c_re.astype(F32))
            - jnp.einsum('blgp,ghp->blgh', h_im, c_im.astype(F32)))


def _s5_mixer(u_lat, u_ctx, lam_re, lam_im, log_step, b_re, b_im, c_re, c_im, d_skip, w_glu, need_ctx):
    b, n, _ = u_lat.shape
    nc = u_ctx.shape[1]
    ul = u_lat.astype(F32).reshape(b, n, SSM_GROUPS, SSM_GROUP_CH)
    uc = u_ctx.astype(F32).reshape(b, nc, SSM_GROUPS, SSM_GROUP_CH)
    d = d_skip.astype(F32).reshape(SSM_GROUPS, SSM_GROUP_CH)
    y_lat = d * ul
    y_ctx = d * uc if need_ctx else None
    for direction in range(2):
        reverse = direction == 1
        ab_re, ab_im, bb_re, bb_im = _s5_discretise(lam_re[direction], lam_im[direction], log_step[direction],
                                                    b_re[direction], b_im[direction])
        _, _, hc_re, hc_im = _complex_linear_scan(uc, ab_re, ab_im, bb_re, bb_im, reverse)
        end = 0 if reverse else nc - 1
        h0_re = hc_re[:, end][:, None]
        h0_im = hc_im[:, end][:, None]
        ac_re, ac_im, hl_re, hl_im = _complex_linear_scan(ul, ab_re, ab_im, bb_re, bb_im, reverse)
        hl_re, hl_im = (hl_re + ac_re * h0_re - ac_im * h0_im,
                        hl_im + ac_re * h0_im + ac_im * h0_re)
        y_lat = y_lat + _s5_readout(hl_re, hl_im, c_re[direction], c_im[direction])
        if need_ctx:
            y_ctx = y_ctx + _s5_readout(hc_re, hc_im, c_re[direction], c_im[direction])

    def glu(y, m):
        y = jax.nn.gelu(y.reshape(b, m, SSM_WIDTH))
        return (y * jax.nn.sigmoid(y @ w_glu.astype(F32))).astype(u_lat.dtype)

    return glu(y_lat, n), (glu(y_ctx, nc) if need_ctx else None)


def _parallel_mixer(a_lat, a_ctx, cos, sin, w_in, w_out, na_rpb, ga_q_norm, ga_k_norm,
                    lam_re, lam_im, log_step, b_re, b_im, c_re, c_im, d_skip, w_glu, sw_sink, need_ctx):
    qa, ka, va, qb, kb, vb, u, qd, kd, vd = _split_cols(a_lat @ w_in)
    qa_c, ka_c, va_c, qb_c, kb_c, vb_c, u_c, qd_c, kd_c, vd_c = _split_cols(a_ctx @ w_in)
    ka_c, va_c = _heads(ka_c), _heads(va_c)
    out_a = _neighbourhood_attention(_heads(qa), _heads(ka), _heads(va), ka_c, va_c, na_rpb)
    kb_c, vb_c = _rms_norm(_heads(kb_c), ga_k_norm), _heads(vb_c)
    qb = _apply_rope(_rms_norm(_heads(qb), ga_q_norm), cos, sin)
    kb = _apply_rope(_rms_norm(_heads(kb), ga_k_norm), cos, sin)
    out_b = _global_gqa(qb, kb, _heads(vb), kb_c, vb_c)
    out_c, ctx_c = _s5_mixer(u, u_c, lam_re, lam_im, log_step, b_re, b_im, c_re, c_im, d_skip, w_glu, need_ctx)
    kd_c, vd_c = _heads(kd_c), _heads(vd_c)
    qd = _apply_rope(_heads(qd), cos, sin)
    kd = _apply_rope(_heads(kd), cos, sin)
    out_d = _window_gqa(qd, kd, _heads(vd), kd_c, vd_c, sw_sink)
    y_lat = jnp.concatenate([out_a, out_b, out_c, out_d], axis=-1) @ w_out
    if not need_ctx:
        return y_lat, None
    ctx_a = _context_attention(_heads(qa_c), ka_c, va_c)
    ctx_b = _context_attention(_rms_norm(_heads(qb_c), ga_q_norm), kb_c, vb_c)
    ctx_d = _context_attention(_heads(qd_c), kd_c, vd_c, sw_sink)
    y_ctx = jnp.concatenate([ctx_a, ctx_b, ctx_c, ctx_d], axis=-1) @ w_out
    return y_lat, y_ctx


def _swiglu(h, w_gate, w_up, w_down):
    return (jax.nn.silu(h @ w_gate) * (h @ w_up)) @ w_down


def _moe_swiglu(h, w_router, b_router, w_gate, w_up, w_down):
    logits = jnp.einsum('bld,de->ble', h, w_router, preferred_element_type=F32) + b_router.astype(F32)
    top_val, top_idx = lax.top_k(logits, TOP_K)
    top_w = jax.nn.softmax(top_val, -1)
    gate = jnp.sum(jax.nn.one_hot(top_idx, N_EXPERTS, dtype=F32) * top_w[..., None], axis=-2)
    out = jnp.zeros(h.shape, F32)
    for e in range(N_EXPERTS):
        out = out + gate[..., e:e + 1] * _swiglu(h, w_gate[e], w_up[e], w_down[e]).astype(F32)
    return out.astype(h.dtype)


def setup_inputs(seed: int = 0) -> dict:
    key = jax.random.key(seed)
    k = jax.random.split(key, 33)
    n_dense = (DEPTH + 1) // 2
    n_moe = DEPTH // 2

    def nrm(kk, shape, std):
        return std * jax.random.normal(kk, shape, F32)

    lam_im_base = jnp.pi * jnp.arange(SSM_STATE, dtype=F32)
    return {
        'x': nrm(k[0], (BATCH, SEQ, D_MODEL), 1.0),
        'c': nrm(k[1], (BATCH, D_MODEL), 1.0),
        'ctx': nrm(k[2], (BATCH, CTX_LEN, D_MODEL), 1.0),
        'c_ctx': nrm(k[3], (D_MODEL,), 1.0),
        'ada_w': nrm(k[4], (DEPTH, D_MODEL, ADA_CHUNKS * D_MODEL), 0.5 * D_MODEL ** -0.5),
        'ada_b': nrm(k[5], (DEPTH, ADA_CHUNKS * D_MODEL), 0.02),
        'w_in': nrm(k[6], (DEPTH, D_MODEL, IN_WIDTH), D_MODEL ** -0.5),
        'w_out': nrm(k[7], (DEPTH, MIX_WIDTH, D_MODEL), DEEPNORM_BETA * MIX_WIDTH ** -0.5),
        'na_rpb': nrm(k[8], (DEPTH, NA_HEADS, 2 * NA_ROWS_MAX - 1, 2 * NA_COLS - 1), 0.02),
        'ga_q_norm': 1.0 + nrm(k[9], (DEPTH, HEAD_DIM), 0.02),
        'ga_k_norm': 1.0 + nrm(k[10], (DEPTH, HEAD_DIM), 0.02),
        'ssm_lambda_re': -0.5 + nrm(k[11], (DEPTH, 2, SSM_GROUPS, SSM_STATE), 0.01),
        'ssm_lambda_im': lam_im_base + nrm(k[12], (DEPTH, 2, SSM_GROUPS, SSM_STATE), 0.01),
        'ssm_log_step': jax.random.uniform(k[13], (DEPTH, 2, SSM_GROUPS), F32, math.log(1e-3), math.log(1e-1)),
        'ssm_b_re': nrm(k[14], (DEPTH, 2, SSM_GROUPS, SSM_STATE, SSM_GROUP_CH), (2 * SSM_GROUP_CH) ** -0.5),
        'ssm_b_im': nrm(k[15], (DEPTH, 2, SSM_GROUPS, SSM_STATE, SSM_GROUP_CH), (2 * SSM_GROUP_CH) ** -0.5),
        'ssm_c_re': nrm(k[16], (DEPTH, 2, SSM_GROUPS, SSM_GROUP_CH, SSM_STATE), SSM_STATE ** -0.5),
        'ssm_c_im': nrm(k[17], (DEPTH, 2, SSM_GROUPS, SSM_GROUP_CH, SSM_STATE), SSM_STATE ** -0.5),
        'ssm_d': nrm(k[18], (DEPTH, SSM_WIDTH), 1.0),
        'ssm_w_glu': nrm(k[19], (DEPTH, SSM_WIDTH, SSM_WIDTH), SSM_WIDTH ** -0.5),
        'sw_sink': nrm(k[20], (DEPTH, SW_HEADS), 0.5),
        'ln1_g': 1.0 + nrm(k[21], (DEPTH, D_MODEL), 0.02),
        'ln1_b': nrm(k[22], (DEPTH, D_MODEL), 0.02),
        'ln2_g': 1.0 + nrm(k[23], (DEPTH, D_MODEL), 0.02),
        'ln2_b': nrm(k[24], (DEPTH, D_MODEL), 0.02),
        'ffn_w_gate': nrm(k[25], (n_dense, D_MODEL, D_FF), D_MODEL ** -0.5),
        'ffn_w_up': nrm(k[26], (n_dense, D_MODEL, D_FF), D_MODEL ** -0.5),
        'ffn_w_down': nrm(k[27], (n_dense, D_FF, D_MODEL), DEEPNORM_BETA * D_FF ** -0.5),
        'moe_w_router': nrm(k[28], (n_moe, D_MODEL, N_EXPERTS), D_MODEL ** -0.5),
        'moe_b_router': nrm(k[29], (n_moe, N_EXPERTS), 0.01),
        'moe_w_gate': nrm(k[30], (n_moe, N_EXPERTS, D_MODEL, D_FF_EXPERT), D_MODEL ** -0.5),
        'moe_w_up': nrm(k[31], (n_moe, N_EXPERTS, D_MODEL, D_FF_EXPERT), D_MODEL ** -0.5),
        'moe_w_down': nrm(k[32], (n_moe, N_EXPERTS, D_FF_EXPERT, D_MODEL), DEEPNORM_BETA * D_FF_EXPERT ** -0.5),
    }


def reference(x, c, ctx, c_ctx, ada_w, ada_b, w_in, w_out, na_rpb, ga_q_norm, ga_k_norm,
              ssm_lambda_re, ssm_lambda_im, ssm_log_step, ssm_b_re, ssm_b_im, ssm_c_re, ssm_c_im,
              ssm_d, ssm_w_glu, sw_sink, ln1_g, ln1_b, ln2_g, ln2_b,
              ffn_w_gate, ffn_w_up, ffn_w_down,
              moe_w_router, moe_b_router, moe_w_gate, moe_w_up, moe_w_down):
    n = x.shape[1]
    cos, sin = _axial_rope_tables(n)
    act_c = jax.nn.silu(c)
    act_cc = jax.nn.silu(c_ctx)
    h, hc = x, ctx
    for layer in range(DEPTH):
        need_ctx = layer < DEPTH - 1
        shift1, scale1, gate1, shift2, scale2, gate2 = [
            m[:, None, :] for m in jnp.split(act_c @ ada_w[layer] + ada_b[layer], ADA_CHUNKS, axis=-1)]
        shift1c, scale1c, gate1c, shift2c, scale2c, gate2c = jnp.split(
            act_cc @ ada_w[layer] + ada_b[layer], ADA_CHUNKS, axis=-1)
        y, yc = _parallel_mixer(
            h * (1.0 + scale1) + shift1, hc * (1.0 + scale1c) + shift1c, cos, sin,
            w_in[layer], w_out[layer], na_rpb[layer], ga_q_norm[layer], ga_k_norm[layer],
            ssm_lambda_re[layer], ssm_lambda_im[layer], ssm_log_step[layer],
            ssm_b_re[layer], ssm_b_im[layer], ssm_c_re[layer], ssm_c_im[layer],
            ssm_d[layer], ssm_w_glu[layer], sw_sink[layer], need_ctx)
        h = _layer_norm(DEEPNORM_ALPHA * h + gate1 * y, ln1_g[layer], ln1_b[layer])
        if need_ctx:
            hc = _layer_norm(DEEPNORM_ALPHA * hc + gate1c * yc, ln1_g[layer], ln1_b[layer])
        f_in = h * (1.0 + scale2) + shift2
        if need_ctx:
            f_in = jnp.concatenate([f_in, hc * (1.0 + scale2c) + shift2c], axis=1)
        i = layer // 2
        if layer % 2 == 0:
            f = _swiglu(f_in, ffn_w_gate[i], ffn_w_up[i], ffn_w_down[i])
        else:
            f = _moe_swiglu(f_in, moe_w_router[i], moe_b_router[i], moe_w_gate[i], moe_w_up[i], moe_w_down[i])
        h = _layer_norm(DEEPNORM_ALPHA * h + gate2 * f[:, :n], ln2_g[layer], ln2_b[layer])
        if need_ctx:
            hc = _layer_norm(DEEPNORM_ALPHA * hc + gate2c * f[:, n:], ln2_g[layer], ln2_b[layer])
    return h
```

```python
import numpy as np
import ml_dtypes
import concourse.bass as bass
import concourse.mybir as mybir
from concourse.bass_utils import run_bass_kernel_spmd

F32 = mybir.dt.float32
BF16 = mybir.dt.bfloat16
I32 = mybir.dt.int32
ALU = mybir.AluOpType
AF = mybir.ActivationFunctionType
AX = mybir.AxisListType


class Tk:
    __slots__ = ("w", "r")

    def __init__(self):
        self.w = None
        self.r = []


class Sched:
    ENG = ("pe", "act", "dve", "pool", "sp")
    NDMA = {"sp": 12, "pool": 8, "act": 4}

    def __init__(self, nc):
        self.nc = nc
        self.prog = {e: [] for e in self.ENG}
        self.n = {e: 0 for e in self.ENG}
        self.seen = {e: {} for e in self.ENG}
        self.signaled = {e: set() for e in self.ENG}
        self.dma_rr = {q: 0 for q in self.NDMA}
        self.dma_tot = {}
        self.lastc = {e: 0 for e in self.ENG}
        self.cur_fence = {}

    def fence(self):
        for e in self.ENG:
            if self.lastc[e]:
                self.cur_fence[e] = self.lastc[e]
        for key, tot in self.dma_tot.items():
            self.cur_fence[key] = tot

    def _deps(self, eng, reads, writes):
        deps = dict(self.cur_fence)
        def add(t):
            if t is None:
                return
            k, v = t
            if deps.get(k, 0) < v:
                deps[k] = v
        for t in reads:
            add(t.w)
        for t in writes:
            add(t.w)
            for r in t.r:
                add(r)
        waits = []
        for k, v in deps.items():
            if k == "pe" and eng == "pe":
                continue
            if self.seen[eng].get(k, 0) < v:
                self.seen[eng][k] = v
                waits.append((k, v))
                if k in self.signaled:
                    self.signaled[k].add(v)
        return waits

    def _commit(self, ticket, reads, writes):
        for t in reads:
            t.r.append(ticket)
        for t in writes:
            t.w = ticket
            t.r = []

    def op(self, eng, fn, R=(), W=()):
        waits = self._deps(eng, R, W)
        self.n[eng] += 1
        self.lastc[eng] = self.n[eng]
        ticket = (eng, self.n[eng])
        self.prog[eng].append((waits, fn, ticket, None))
        self._commit(ticket, R, W)

    def dma(self, q, out, in_, R=(), W=(), slow=False):
        j = self.dma_rr[q]
        self.dma_rr[q] = (j + 1) % self.NDMA[q]
        key = ("d", q, j)
        waits = self._deps(q, R, W)
        prev = self.dma_tot.get(key, 0)
        if prev and self.seen[q].get(key, 0) < prev:
            self.seen[q][key] = prev
            waits.append((key, prev))
        self.dma_tot[key] = prev + 16
        ticket = (key, prev + 16)
        self.n[q] += 1
        if slow:
            fn = lambda e, o=out, i=in_: e.dma_start(out=o, in_=i, allow_slow_non_contiguous=True)
        else:
            fn = lambda e, o=out, i=in_: e.dma_start(out=o, in_=i)
        self.prog[q].append((waits, fn, (q, self.n[q]), key))
        self._commit(ticket, R, W)

    def emit(self, block, final_waits):
        nc = self.nc
        sems = {e: nc.alloc_semaphore("s_" + e) for e in self.ENG}
        for key in self.dma_tot:
            sems[key] = nc.alloc_semaphore("d_%s%d" % (key[1], key[2]))
        rank = {}
        for e in self.ENG:
            rank[e] = {v: i + 1 for i, v in enumerate(sorted(self.signaled[e]))}

        def val(k, v):
            return rank[k][v] if k in rank else v

        def run(ename, eng):
            for waits, fn, ticket, dkey in self.prog[ename]:
                for k, v in waits:
                    eng.wait_ge(sems[k], val(k, v))
                ins = fn(eng)
                if dkey is not None:
                    ins.then_inc(sems[dkey], 16)
                elif ticket[1] in self.signaled[ename]:
                    ins.then_inc(sems[ename], 1)
            if ename == "sp":
                for k, v in final_waits:
                    eng.wait_ge(sems[k], val(k, v))

        for k, v in final_waits:
            if k in self.signaled:
                self.signaled[k].add(v)
        for e in self.ENG:
            rank[e] = {v: i + 1 for i, v in enumerate(sorted(self.signaled[e]))}
        block.tensor(lambda e: run("pe", e))
        block.scalar(lambda e: run("act", e))
        block.vector(lambda e: run("dve", e))
        block.gpsimd(lambda e: run("pool", e))
        block.sync(lambda e: run("sp", e))


class Arena:
    def __init__(self, nc, nbytes, S=None):
        self.S = S
        self.t = nc.alloc_sbuf_tensor("arena", [128, nbytes // 4], F32)
        self.nbytes = nbytes
        self.top = 0
        self.peak = 0

    def alloc(self, free_shape, dtype):
        esz = 2 if dtype == BF16 else 4
        n = int(np.prod(free_shape))
        nb = (n * esz + 63) // 64 * 64
        off = self.top
        self.top += nb
        self.peak = max(self.peak, self.top)
        assert self.top <= self.nbytes, ("arena overflow", self.top, self.nbytes)
        ap = self.t[:, off // 4:(off + nb) // 4]
        if dtype != F32:
            ap = ap.bitcast(dtype)
        ap = ap[:, 0:n]
        if len(free_shape) == 2:
            ap = ap.rearrange("p (a b) -> p a b", b=free_shape[1])
        elif len(free_shape) == 3:
            ap = ap.rearrange("p (a b c) -> p a b c", b=free_shape[1], c=free_shape[2])
        return ap

    def mark(self):
        return self.top

    def release(self, m):
        self.top = m
        self.S.fence()


D = 1024
NT = 18
NL = 16
DEPTH = 4
DFF = 2816
DFE = 3584
NE = 8
ALPHA = float((2 * DEPTH) ** 0.25)
NEG = -1.0e30
SLAB = 512


class B:
    def __init__(self, layers, dbg=None):
        self.layers = layers
        self.dbg = dbg or {}
        nc = self.nc = bass.Bass("TRN2", target_bir_lowering=False)
        self.S = Sched(nc)
        self.A = Arena(nc, 212800, self.S)
        self.fin = []
        self.din = {}
        self.psall = nc.alloc_psum_tensor("psall", [128, 8 * 512], F32)
        self.ps = [self.psall[:, i * 512:(i + 1) * 512] for i in range(8)]
        self.tp = [Tk() for _ in range(8)]

    SHAPES = {
        "hx": ([NT * 128, D], F32), "cv": ([128, 8, 2], F32), "c_ident_f": ([128, 128], F32), "c_ident_b": ([128, 128], BF16),
        "c_rope": ([128, NT, 96], F32), "c_tri": ([128, 2, 128], BF16), "c_namask": ([128, 2688], BF16), "c_tau": ([128, 130], F32),
        "ada_w": ([DEPTH, D, 6 * D], F32), "ada_b": ([DEPTH, 6 * D], F32), "w_in": ([DEPTH, D, 2048], F32), "w_out": ([DEPTH, D, D], F32),
        "rpbh": ([DEPTH, 4, 18, 128], F32), "gqk": ([DEPTH, 2, 64], F32), "ssm_vec": ([DEPTH, 128, 16, 3], F32),
        "ssm_bs": ([DEPTH, 128, 16, 2, 16], F32), "ssm_cs": ([DEPTH, 128, 16, 2, 32], F32), "ssm_dd": ([DEPTH, 128, 8, 32], F32),
        "ssm_wglu": ([DEPTH, 256, 256], F32), "sink": ([DEPTH, 4], F32), "lngb": ([DEPTH, 4, D], F32),
        "ffn_g": ([2, D, DFF], F32), "ffn_u": ([2, D, DFF], F32), "ffn_d": ([2, DFF, D], F32),
        "moe_r": ([2, D, NE], F32), "moe_rb": ([2, NE], F32), "moe_g": ([2, NE, D, DFE], F32), "moe_u": ([2, NE, D, DFE], F32),
        "moe_d": ([2, NE, DFE, D], F32),
    }

    def __getattr__(self, name):
        sh = B.SHAPES.get(name)
        if sh is None:
            raise AttributeError(name)
        ap = self.inp(name, sh[0], sh[1])
        self.__dict__[name] = ap
        return ap

    def dt_(self, name):
        getattr(self, name)
        return self.din[name]

    def inp(self, name, shape, dt=F32):
        t = self.nc.dram_tensor(name, list(shape), dt, kind="ExternalInput")
        self.din[name] = t
        return t.ap()

    def outp(self, name, shape, dt=F32):
        return self.nc.dram_tensor(name, list(shape), dt, kind="ExternalOutput").ap()

    def store(self, dst, src, R):
        S = self.S
        S.dma("sp", dst, src, R=R)
        j = (S.dma_rr["sp"] - 1) % S.NDMA["sp"]
        key = ("d", "sp", j)
        self.fin.append((key, S.dma_tot[key]))

    def pe(self, fn, R=(), W=()): self.S.op("pe", fn, R, W)
    def act(self, fn, R=(), W=()): self.S.op("act", fn, R, W)
    def dve(self, fn, R=(), W=()): self.S.op("dve", fn, R, W)
    def pool(self, fn, R=(), W=()): self.S.op("pool", fn, R, W)

    def mm(self, out, lhsT, rhs, start, stop, R, W):
        self.pe(lambda e: e.matmul(out, lhsT=lhsT, rhs=rhs, start=start, stop=stop), R, W)

    def tr(self, out, in_, R, W):
        self.pe(lambda e: e.transpose(out=out, in_=in_, identity=self.ident_f), R + [self.tconst], W)

    def rstd_from(self, out, var_ap, scale, eps, R, tk):
        self.act(lambda e: e.activation(out=out, in_=var_ap, func=AF.Sqrt, bias=self.eps_ap(eps), scale=scale), R, [tk])
        self.dve(lambda e: e.reciprocal(out=out, in_=out), [tk], [tk])

    def eps_ap(self, eps):
        return self.epsc[:, 0:1]

    def setup(self):
        A = self.A
        inp = self.inp
        self.out = self.outp("out", [NL * 128, D])

        S = self.S
        self.tconst = Tk()
        self.H = A.alloc([NT, D], F32)
        self.tH = [Tk() for _ in range(NT)]
        self.ident_f = A.alloc([128], F32)
        self.ident_b = A.alloc([128], BF16)
        self.epsc = A.alloc([2], F32)
        self.csil = A.alloc([8, 2], F32)
        self.modc = A.alloc([DEPTH, 32, 2], F32)
        self.tmodc = Tk()
        for dst, src in ((self.ident_f, self.c_ident_f), (self.ident_b, self.c_ident_b), (self.csil, self.cv)):
            S.dma("sp", dst, src, W=[self.tconst])
        self.dve(lambda e: e.memset(self.epsc, 1e-6), W=[self.tconst])
        for i in range(NT):
            S.dma("sp" if i % 2 == 0 else "act", self.H[:, i, :], self.hx[i * 128:(i + 1) * 128, :], W=[self.tH[i]])
        self.act(lambda e: e.activation(out=self.csil, in_=self.csil, func=AF.Silu), [self.tconst], [self.tconst])

    def ada_cols(self, l):
        A, S = self.A, self.S
        m = A.mark()
        blocks = [0, 1, 3, 4]
        wst = [A.alloc([8, 128], F32) for _ in range(3)]
        tw = [Tk() for _ in range(3)]
        bcol = A.alloc([32], F32)
        tb = Tk()
        for j, blk in enumerate(blocks):
            S.dma("sp", bcol[:, j * 8:(j + 1) * 8], self.ada_b[l, blk * D:(blk + 1) * D].rearrange("(kc p) -> p kc", p=128), W=[tb], slow=True)
        n = 0
        for j, blk in enumerate(blocks):
            for fc in range(8):
                b = n % 3
                col0 = blk * D + fc * 128
                S.dma("sp" if n % 2 == 0 else "act", wst[b], self.ada_w[l, :, col0:col0 + 128].rearrange("(kc p) n -> p kc n", p=128), W=[tw[b]])
                pb = 7
                for kc in range(8):
                    self.mm(self.ps[pb][:, 0:2], wst[b][:, kc, :], self.csil[:, kc, :], kc == 0, kc == 7, [tw[b], self.tconst], [self.tp[pb]])
                idx = j * 8 + fc
                add = 1.0 if blk in (1, 4) else 0.0
                self.dve(lambda e, idx=idx, add=add, pb=pb: e.tensor_scalar(out=self.modc[:, l, idx, :], in0=self.ps[pb][:, 0:2], scalar1=bcol[:, idx:idx + 1],
                                                                           scalar2=add, op0=ALU.add, op1=ALU.add), [self.tp[pb], tb], [self.tmodc])
                n += 1
        A.release(m)

    def ada_gate(self, l, which, G, tG):
        A, S = self.A, self.S
        m = A.mark()
        blk = 2 if which == 0 else 5
        crep = A.alloc([2, 8, 128], F32); tcr = Tk()
        for v in range(2):
            for kc in range(8):
                self.dve(lambda e, v=v, kc=kc: e.tensor_copy(out=crep[:, v, kc, :], in_=self.csil[:, kc, v:v + 1].to_broadcast([128, 128])),
                         [self.tconst], [tcr])
        wst = [A.alloc([8, 512], F32) for _ in range(2)]
        tw = [Tk() for _ in range(2)]
        bb = A.alloc([D], F32)
        tb = Tk()
        S.dma("sp", bb, self.ada_b[l:l + 1, blk * D:(blk + 1) * D].partition_broadcast(128) if False else
              bass.AP(self.dt_("ada_b"), l * 6 * D + blk * D, [[0, 128], [1, D]]), W=[tb])
        for nb in range(2):
            col0 = blk * D + nb * 512
            S.dma("sp", wst[nb], self.ada_w[l, :, col0:col0 + 512].rearrange("(kc p) n -> p kc n", p=128), W=[tw[nb]])
            for v in range(2):
                pb = 5 + v
                for kc in range(8):
                    self.mm(self.ps[pb][:, :], crep[:, v, kc, :], wst[nb][:, kc, :], kc == 0, kc == 7, [tw[nb], tcr], [self.tp[pb]])
                self.dve(lambda e, v=v, nb=nb, pb=pb: e.tensor_tensor(out=G[:, v, nb * 512:(nb + 1) * 512], in0=self.ps[pb][:, :], in1=bb[:, nb * 512:(nb + 1) * 512], op=ALU.add),
                         [self.tp[pb], tb], [tG])
        A.release(m)

    def load_ln(self, l, which, LN, tLN):
        for j in range(2):
            self.S.dma("sp", LN[:, j, :], bass.AP(self.dt_("lngb"), (l * 4 + which * 2 + j) * D, [[0, 128], [1, D]]), W=[tLN])

    def make_aT(self, l, i, which, aT, taT, aT32=None, taT32=None):
        v = 1 if i >= NL else 0
        for half in range(2):
            pb = 5 + half
            for q in range(4):
                kc = half * 4 + q
                self.tr(self.ps[pb][:, q * 128:(q + 1) * 128], self.H[:, i, kc * 128:(kc + 1) * 128], [self.tH[i]], [self.tp[pb]])
            for q in range(4):
                kc = half * 4 + q
                sc = self.modc[:, l, (which * 2 + 1) * 8 + kc, v:v + 1]
                sh = self.modc[:, l, (which * 2) * 8 + kc, v:v + 1]
                self.act(lambda e, kc=kc, q=q, pb=pb, sc=sc, sh=sh: e.activation(out=aT[:, kc, :], in_=self.ps[pb][:, q * 128:(q + 1) * 128], func=AF.Identity, bias=sh, scale=sc),
                         [self.tp[pb], self.tmodc], [taT])
                if aT32 is not None:
                    self.act(lambda e, kc=kc, q=q, pb=pb, sc=sc, sh=sh: e.activation(out=aT32[:, kc, :], in_=self.ps[pb][:, q * 128:(q + 1) * 128], func=AF.Identity, bias=sh, scale=sc),
                             [self.tp[pb], self.tmodc], [taT32])

    def resid_ln(self, i, ys, G, tG, LN, tLN, tmp, ttmp, st, tst):
        v = 1 if i >= NL else 0
        for hf, (yap, ty) in enumerate(ys):
            self.dve(lambda e, hf=hf, yap=yap: e.tensor_tensor(out=tmp[:, hf * 512:(hf + 1) * 512], in0=yap, in1=G[:, v, hf * 512:(hf + 1) * 512], op=ALU.mult),
                     [ty, tG], [ttmp])
        self.ln_tail(i, tmp, ttmp, LN, tLN, st, tst)

    def ln_tail(self, i, tmp, ttmp, LN, tLN, st, tst):
        self.dve(lambda e: e.scalar_tensor_tensor(out=tmp, in0=self.H[:, i, :], scalar=ALPHA, in1=tmp, op0=ALU.mult, op1=ALU.add), [self.tH[i], ttmp], [ttmp])
        for hf in range(2):
            self.dve(lambda e, hf=hf: e.bn_stats(out=st[:, hf * 6:(hf + 1) * 6], in_=tmp[:, hf * 512:(hf + 1) * 512]), [ttmp], [tst])
        self.dve(lambda e: e.bn_aggr(out=st[:, 12:14], in_=st[:, 0:12]), [tst], [tst])
        self.rstd_from(st[:, 14:15], st[:, 13:14], 1.0, 1e-6, [tst], tst)
        self.dve(lambda e: e.tensor_scalar(out=tmp, in0=tmp, scalar1=st[:, 12:13], scalar2=st[:, 14:15], op0=ALU.subtract, op1=ALU.mult), [ttmp, tst], [ttmp])
        self.pool(lambda e: e.tensor_tensor(out=tmp, in0=tmp, in1=LN[:, 0, :], op=ALU.mult), [ttmp, tLN], [ttmp])
        self.pool(lambda e: e.tensor_tensor(out=self.H[:, i, :], in0=tmp, in1=LN[:, 1, :], op=ALU.add), [ttmp, tLN], [self.tH[i]])

    def ffn_phase(self, l):
        A, S = self.A, self.S
        last = (l == DEPTH - 1)
        nt = NL if last else NT
        moe = (l % 2 == 1)
        li = l // 2
        m = A.mark()
        G = A.alloc([2, D], F32); tG = Tk()
        LN = A.alloc([2, D], F32); tLN = Tk()
        self.ada_gate(l, 1, G, tG)
        self.load_ln(l, 1, LN, tLN)
        FT = A.alloc([8, nt * 128], BF16)
        tFT = [Tk() for _ in range(nt)]
        moe_ = (l % 2 == 1)
        if moe_:
            gate = A.alloc([nt, NE], F32); tgate = Tk()
            a32 = A.alloc([8, 128], F32); ta32 = Tk()
            rt = self.router_setup(l // 2)
        for i in range(nt):
            if moe_:
                self.make_aT(l, i, 1, FT[:, :, i * 128:(i + 1) * 128], tFT[i], a32, ta32)
                self.router_tile(rt, i, a32, ta32, gate, tgate)
            else:
                self.make_aT(l, i, 1, FT[:, :, i * 128:(i + 1) * 128], tFT[i])
        experts = range(NE) if moe else [0]
        dff = DFE if moe else DFF
        nsl = dff // SLAB + (1 if dff % SLAB else 0)
        facc = A.alloc([nt, D], F32) if False else None
        tmp = A.alloc([D], F32); ttmp = Tk()
        st = A.alloc([16], F32); tst = Tk()
        for i in range(nt):
            self.pool(lambda e, i=i: e.tensor_scalar(out=self.H[:, i, :], in0=self.H[:, i, :], scalar1=ALPHA, scalar2=None, op0=ALU.mult), [self.tH[i]], [self.tH[i]])
        NB = 2
        wg = [A.alloc([8, SLAB], BF16) for _ in range(NB)]
        wu = [A.alloc([8, SLAB], BF16) for _ in range(NB)]
        wd = [A.alloc([SLAB // 128, D], BF16) for _ in range(NB)]
        tw = [Tk() for _ in range(NB)]
        twu = [Tk() for _ in range(NB)]
        twd = [Tk() for _ in range(NB)]
        h1 = [A.alloc([SLAB // 128, 512], BF16) for _ in range(2)]
        th1 = [Tk() for _ in range(2)]
        sg = [A.alloc([512], F32) for _ in range(2)]
        tsg = [Tk() for _ in range(2)]
        yt = [A.alloc([D], F32)] * 2
        tyt = [Tk()] * 2
        nblk = (nt * 128 + 511) // 512
        cnt = 0
        hcnt = 0
        items = [(e_, s) for e_ in experts for s in range(nsl)]

        def issue(k):
            e_, s = items[k]
            Wg = self.moe_g[li, e_] if moe else self.ffn_g[li]
            Wu = self.moe_u[li, e_] if moe else self.ffn_u[li]
            Wd = self.moe_d[li, e_] if moe else self.ffn_d[li]
            b = k % NB
            c0 = s * SLAB
            w = min(SLAB, dff - c0)
            nch = w // 128
            S.dma("pool", wg[b][:, :, 0:w], Wg[:, c0:c0 + w].rearrange("(kc p) n -> p kc n", p=128), W=[tw[b]])
            S.dma("pool", wu[b][:, :, 0:w], Wu[:, c0:c0 + w].rearrange("(kc p) n -> p kc n", p=128), W=[twu[b]])
            S.dma("pool", wd[b][:, 0:nch, :], Wd[c0:c0 + w, :].rearrange("(fc p) n -> p fc n", p=128), W=[twd[b]])

        issue(0)
        for k, (e_, s) in enumerate(items):
            if True:
                if k + 1 < len(items):
                    issue(k + 1)
                b = k % NB
                c0 = s * SLAB
                w = min(SLAB, dff - c0)
                nch = w // 128
                def gu(tb, hb, b=b, nch=nch):
                    t0 = tb * 512
                    ntok = min(512, nt * 128 - t0)
                    tiles = list(range(t0 // 128, (t0 + ntok) // 128))
                    for fc in range(nch):
                        pg, pu = 0 + (fc % 2) * 2, 1 + (fc % 2) * 2
                        for kc in range(8):
                            self.mm(self.ps[pg][:, 0:ntok], wg[b][:, kc, fc * 128:(fc + 1) * 128], FT[:, kc, t0:t0 + ntok], kc == 0, kc == 7,
                                    [tw[b]] + [tFT[i] for i in tiles], [self.tp[pg]])
                        for kc in range(8):
                            self.mm(self.ps[pu][:, 0:ntok], wu[b][:, kc, fc * 128:(fc + 1) * 128], FT[:, kc, t0:t0 + ntok], kc == 0, kc == 7,
                                    [twu[b]] + [tFT[i] for i in tiles], [self.tp[pu]])
                        sb = fc % 2
                        self.act(lambda e, pg=pg, sb=sb, ntok=ntok: e.activation(out=sg[sb][:, 0:ntok], in_=self.ps[pg][:, 0:ntok], func=AF.Silu), [self.tp[pg]], [tsg[sb]])
                        self.dve(lambda e, pu=pu, sb=sb, hb=hb, fc=fc, ntok=ntok: e.tensor_tensor(out=h1[hb][:, fc, 0:ntok], in0=self.ps[pu][:, 0:ntok], in1=sg[sb][:, 0:ntok], op=ALU.mult),
                                 [self.tp[pu], tsg[sb]], [th1[hb]])

                def down(tb, hb, b=b, nch=nch, e_=e_):
                    t0 = tb * 512
                    ntok = min(512, nt * 128 - t0)
                    tiles = list(range(t0 // 128, (t0 + ntok) // 128))
                    for ti, i in enumerate(tiles):
                        v = 1 if i >= NL else 0
                        yb = i % 2
                        for hf in range(2):
                            pb = 4 + hf + 2 * (i % 2)
                            for fc in range(nch):
                                self.mm(self.ps[pb][:, :], h1[hb][:, fc, ti * 128:(ti + 1) * 128], wd[b][:, fc, hf * 512:(hf + 1) * 512], fc == 0, fc == nch - 1,
                                        [th1[hb], twd[b]], [self.tp[pb]])
                            if moe:
                                self.act(lambda e, pb=pb, hf=hf, yb=yb, i=i, e_=e_: e.activation(out=yt[yb][:, hf * 512:(hf + 1) * 512], in_=self.ps[pb][:, :], func=AF.Copy, scale=gate[:, i, e_:e_ + 1]),
                                         [self.tp[pb], tgate], [tyt[yb]])
                            else:
                                self.dve(lambda e, pb=pb, hf=hf, v=v, yb=yb: e.tensor_tensor(out=yt[yb][:, hf * 512:(hf + 1) * 512], in0=self.ps[pb][:, :], in1=G[:, v, hf * 512:(hf + 1) * 512], op=ALU.mult),
                                         [self.tp[pb], tG], [tyt[yb]])
                        if moe:
                            self.dve(lambda e, v=v, yb=yb: e.tensor_tensor(out=yt[yb], in0=yt[yb], in1=G[:, v, :], op=ALU.mult), [tyt[yb], tG], [tyt[yb]])
                        self.pool(lambda e, i=i, yb=yb: e.tensor_tensor(out=self.H[:, i, :], in0=yt[yb], in1=self.H[:, i, :], op=ALU.add), [tyt[yb], self.tH[i]], [self.tH[i]])

                hbs = []
                for tb in range(nblk):
                    hbs.append(hcnt % 2)
                    hcnt += 1
                gu(0, hbs[0])
                for tb in range(nblk):
                    if tb + 1 < nblk:
                        gu(tb + 1, hbs[tb + 1])
                    down(tb, hbs[tb])
        for i in range(nt):
            self.ln_only(i, LN, tLN, tmp, ttmp, st, tst)
        A.release(m)

    def ln_only(self, i, LN, tLN, tmp, ttmp, st, tst):
        for hf in range(2):
            self.dve(lambda e, hf=hf: e.bn_stats(out=st[:, hf * 6:(hf + 1) * 6], in_=self.H[:, i, hf * 512:(hf + 1) * 512]), [self.tH[i]], [tst])
        self.dve(lambda e: e.bn_aggr(out=st[:, 12:14], in_=st[:, 0:12]), [tst], [tst])
        self.rstd_from(st[:, 14:15], st[:, 13:14], 1.0, 1e-6, [tst], tst)
        self.dve(lambda e: e.tensor_scalar(out=tmp, in0=self.H[:, i, :], scalar1=st[:, 12:13], scalar2=st[:, 14:15], op0=ALU.subtract, op1=ALU.mult), [self.tH[i], tst], [ttmp])
        self.pool(lambda e: e.tensor_tensor(out=tmp, in0=tmp, in1=LN[:, 0, :], op=ALU.mult), [ttmp, tLN], [ttmp])
        self.pool(lambda e: e.tensor_tensor(out=self.H[:, i, :], in0=tmp, in1=LN[:, 1, :], op=ALU.add), [ttmp, tLN], [self.tH[i]])

    def router_setup(self, li):
        A, S = self.A, self.S
        wr = A.alloc([8, NE], F32); twr = Tk()
        rb = A.alloc([NE], F32)
        S.dma("sp", wr, self.moe_r[li].rearrange("(kc p) n -> p kc n", p=128), W=[twr])
        S.dma("sp", rb, bass.AP(self.dt_("moe_rb"), li * NE, [[0, 128], [1, NE]]), W=[twr])
        return dict(wr=wr, twr=twr, rb=rb, lg=A.alloc([NE], F32), tlg=Tk(), m8=A.alloc([8], F32), wk=A.alloc([2, NE], F32))

    def router_tile(self, rt, i, a32, ta32, gate, tgate):
        wr, twr, rb, lg, tlg, m8, wk = rt["wr"], rt["twr"], rt["rb"], rt["lg"], rt["tlg"], rt["m8"], rt["wk"]
        pb = 7
        for kc in range(8):
            self.mm(self.ps[pb][:, 0:NE], a32[:, kc, :], wr[:, kc, :], kc == 0, kc == 7, [ta32, twr], [self.tp[pb]])
        self.dve(lambda e: e.tensor_tensor(out=lg, in0=self.ps[pb][:, 0:NE], in1=rb, op=ALU.add), [self.tp[pb], twr], [tlg])
        self.dve(lambda e: e.max(out=m8, in_=lg), [tlg], [tlg])
        self.dve(lambda e: e.tensor_scalar(out=wk[:, 0, :], in0=lg, scalar1=m8[:, 1:2], scalar2=None, op0=ALU.is_ge), [tlg], [tlg])
        self.dve(lambda e: e.tensor_scalar(out=wk[:, 1, :], in0=lg, scalar1=m8[:, 0:1], scalar2=None, op0=ALU.subtract), [tlg], [tlg])
        self.act(lambda e: e.activation(out=wk[:, 1, :], in_=wk[:, 1, :], func=AF.Exp), [tlg], [tlg])
        self.dve(lambda e: e.tensor_tensor(out=wk[:, 1, :], in0=wk[:, 1, :], in1=wk[:, 0, :], op=ALU.mult), [tlg], [tlg])
        self.dve(lambda e: e.reduce_sum(out=m8[:, 2:3], in_=wk[:, 1, :], axis=AX.X), [tlg], [tlg])
        self.dve(lambda e: e.reciprocal(out=m8[:, 2:3], in_=m8[:, 2:3]), [tlg], [tlg])
        self.dve(lambda e: e.tensor_scalar(out=gate[:, i, :], in0=wk[:, 1, :], scalar1=m8[:, 2:3], scalar2=None, op0=ALU.mult), [tlg], [tgate])


    def rms_rope(self, src, tsrc, nh, dst, tdst, tile, rw, g=None, perm=False, qs=1.0):
        rope, trope = self.rope, self.tmc
        s3 = src.rearrange("p (h d) -> p h d", d=64)
        x, ss, t, tw = rw["x"], rw["ss"], rw["t"], rw["tw"]
        x3 = x[:, 0:nh * 64].rearrange("p (h d) -> p h d", d=64)
        if g is not None:
            for h in range(nh):
                self.act(lambda e, h=h: e.activation(out=x3[:, h, :], in_=s3[:, h, :], func=AF.Square, accum_out=ss[:, h:h + 1]), [tsrc], [tw])
            self.act(lambda e: e.activation(out=ss[:, 0:nh], in_=ss[:, 0:nh], func=AF.Sqrt, bias=self.epsc[:, 0:1], scale=1.0 / 64), [tw, self.tconst], [tw])
            self.dve(lambda e: e.reciprocal(out=ss[:, 0:nh], in_=ss[:, 0:nh]), [tw], [tw])
            for h in range(nh):
                self.dve(lambda e, h=h: e.scalar_tensor_tensor(out=x3[:, h, :], in0=s3[:, h, :], scalar=ss[:, h:h + 1], in1=g, op0=ALU.mult, op1=ALU.mult), [tsrc, tw, self.tmc], [tw])
            cur, tcur, qs = x3, tw, 1.0
        else:
            cur, tcur = s3, tsrc
        C = rope[:, tile, 0:32].unsqueeze(1).unsqueeze(1).to_broadcast([128, nh, 2, 32])
        Sg = rope[:, tile, 32:96].rearrange("p (a d) -> p a d", d=32).unsqueeze(1).to_broadcast([128, nh, 2, 32])
        c4 = cur.rearrange("p h (a d) -> p h a d", d=32)
        sw = c4[:, :, ::-1, :]
        t1 = t[:, 0, 0:nh * 64].rearrange("p (h a d) -> p h a d", a=2, d=32)
        t2 = t[:, 1, 0:nh * 64].rearrange("p (h a d) -> p h a d", a=2, d=32)
        if qs != 1.0:
            self.dve(lambda e: e.scalar_tensor_tensor(out=t1, in0=c4, scalar=qs, in1=C, op0=ALU.mult, op1=ALU.mult), [tcur, trope], [tw])
            self.dve(lambda e: e.scalar_tensor_tensor(out=t2, in0=sw, scalar=qs, in1=Sg, op0=ALU.mult, op1=ALU.mult), [tcur, trope], [tw])
        else:
            self.dve(lambda e: e.tensor_tensor(out=t1, in0=c4, in1=C, op=ALU.mult), [tcur, trope], [tw])
            self.dve(lambda e: e.tensor_tensor(out=t2, in0=sw, in1=Sg, op=ALU.mult), [tcur, trope], [tw])
        f1 = t[:, 0, 0:nh * 64].rearrange("p (h d) -> p h d", d=64)
        f2 = t[:, 1, 0:nh * 64].rearrange("p (h d) -> p h d", d=64)
        if perm:
            dv = dst.rearrange("p (b s d) -> p s b d", b=2, s=2, d=64)
            f1 = f1.rearrange("p (s b) d -> p s b d", b=2)
            f2 = f2.rearrange("p (s b) d -> p s b d", b=2)
        else:
            dv = dst.rearrange("p (h d) -> p h d", d=64)
        self.dve(lambda e: e.tensor_tensor(out=dv, in0=f1, in1=f2, op=ALU.add), [tw], [tdst])

    def mixer_phase(self, l):
        A, S = self.A, self.S
        last = (l == DEPTH - 1)
        nq = NL if last else NT
        m_all = A.mark()
        self.rope = A.alloc([NT, 96], F32)
        gqk = A.alloc([2, 64], F32)
        self.tmc = Tk()
        S.dma("sp", self.rope, self.c_rope, W=[self.tmc])
        S.dma("sp", gqk, bass.AP(self.dt_("gqk"), l * 128, [[0, 128], [1, 128]]), W=[self.tmc])
        KT = A.alloc([4, NT * 128], BF16); tKT = [Tk() for _ in range(NT)]
        V = A.alloc([NT, 512], BF16); tV = [Tk() for _ in range(NT)]
        OC = A.alloc([NT, 256], BF16); tOC = [Tk() for _ in range(NT)]
        m_s5 = A.mark()
        UT = A.alloc([2, NT * 128], BF16); tUT = [Tk() for _ in range(NT)]
        m0 = A.mark()
        rw = dict(x=A.alloc([256], F32), ss=A.alloc([4], F32), t=A.alloc([2, 256], F32), tw=Tk())
        aT = [A.alloc([8, 128], BF16) for _ in range(2)]; taT = [Tk() for _ in range(2)]
        W = A.alloc([8, 1280], BF16); tW = [Tk() for _ in range(7)]
        srcs = [(256, 256), (1024, 128), (1792, 128), (512, 256), (1152, 128), (1920, 128), (1280, 256)]
        o = 0
        for k, (c0, w) in enumerate(srcs):
            S.dma("pool", W[:, :, o:o + w], self.w_in[l, :, c0:c0 + w].rearrange("(kc p) n -> p kc n", p=128), W=[tW[k]])
            o += w
        kbf = A.alloc([512], BF16); tkb = Tk()
        for i in range(NT):
            b = i % 2
            self.make_aT(l, i, 0, aT[b], taT[b])
            for kc in range(8):
                self.mm(self.ps[0][:, :], aT[b][:, kc, :], W[:, kc, 0:512], kc == 0, kc == 7, [taT[b]] + tW[0:3], [self.tp[0]])
            for kc in range(8):
                self.mm(self.ps[1][:, :], aT[b][:, kc, :], W[:, kc, 512:1024], kc == 0, kc == 7, [taT[b]] + tW[3:6], [self.tp[1]])
            for c in range(2):
                for kc in range(8):
                    self.mm(self.ps[2][:, c * 128:(c + 1) * 128], W[:, kc, 1024 + c * 128:1152 + c * 128], aT[b][:, kc, :], kc == 0, kc == 7, [taT[b], tW[6]], [self.tp[2]])
            self.act(lambda e, i=i: e.activation(out=V[:, i, :], in_=self.ps[1][:, :], func=AF.Copy), [self.tp[1]], [tV[i]])
            self.act(lambda e, i=i: e.activation(out=UT[:, :, i * 128:(i + 1) * 128], in_=self.ps[2][:, 0:256].rearrange("p (c t) -> p c t", t=128), func=AF.Copy), [self.tp[2]], [tUT[i]])
            self.act(lambda e: e.activation(out=kbf[:, 0:256], in_=self.ps[0][:, 0:256], func=AF.Copy), [self.tp[0]], [tkb])
            self.rms_rope(self.ps[0][:, 256:384], self.tp[0], 2, kbf[:, 256:384], tkb, i, rw, g=gqk[:, 1, :])
            self.rms_rope(self.ps[0][:, 384:512], self.tp[0], 2, kbf[:, 384:512], tkb, i, rw)
            for c in range(4):
                self.mm(self.ps[3][:, c * 128:(c + 1) * 128], kbf[:, c * 128:(c + 1) * 128], self.ident_b, True, True, [tkb, self.tconst], [self.tp[3]])
            self.dve(lambda e, i=i: e.tensor_copy(out=KT[:, :, i * 128:(i + 1) * 128], in_=self.ps[3][:, :].rearrange("p (c t) -> p c t", t=128)), [self.tp[3]], [tKT[i]])
        A.release(m0)
        self.s5_phase(l, UT, tUT, OC, tOC, nq)
        A.release(m_s5)
        rw = dict(x=A.alloc([256], F32), ss=A.alloc([4], F32), t=A.alloc([2, 256], F32), tw=Tk())
        aT = [A.alloc([8, 128], BF16)] * 2; taT = [Tk()] * 2
        tri = A.alloc([2, 128], BF16); namask = A.alloc([2688], BF16); tmk = Tk()
        S.dma("sp", tri, self.c_tri, W=[tmk]); S.dma("sp", namask, self.c_namask, W=[tmk])
        G = A.alloc([2, D], F32); tG = Tk()
        LN = A.alloc([2, D], F32); tLN = Tk()
        self.ada_gate(l, 0, G, tG)
        self.load_ln(l, 0, LN, tLN)
        Wq = A.alloc([8, 768], BF16); tWq = [Tk() for _ in range(3)]
        for k, c0 in enumerate((0, 768, 1536)):
            S.dma("pool", Wq[:, :, k * 256:(k + 1) * 256], self.w_in[l, :, c0:c0 + 256].rearrange("(kc p) n -> p kc n", p=128), W=[tWq[k]])
        Wo = A.alloc([8, D], BF16); tWo = Tk()
        S.dma("pool", Wo, self.w_out[l].rearrange("(kc p) n -> p kc n", p=128), W=[tWo])
        Traw = A.alloc([4, 14, 64], BF16); tTr = Tk()
        mh = A.mark()
        hk = [A.alloc([16, 64], F32) for _ in range(2)]; thk = [Tk() for _ in range(2)]
        for h in range(4):
            for rl in range(2):
                S.dma("sp", hk[h % 2][rl * 64:(rl + 1) * 64, :, :], bass.AP(self.dt_("rpbh"), ((l * 4 + h) * 18 + (1 - rl)) * 128, [[1, 64], [128, 16], [1, 64]]), W=[thk[h % 2]])
            self.dve(lambda e, h=h: e.tensor_copy(out=Traw[:, h, :, :], in_=hk[h % 2][:, 1:15, ::-1]), [thk[h % 2]], [tTr])
        A.release(mh)
        sk = A.alloc([8], F32); tsk = Tk()
        S.dma("sp", sk[:, 0:4], bass.AP(self.dt_("sink"), l * 4, [[0, 128], [1, 4]]), W=[tsk])
        self.dve(lambda e: e.tensor_scalar(out=sk[:, 4:8], in0=sk[:, 0:4], scalar1=-1.0, scalar2=None, op0=ALU.mult), [tsk], [tsk])
        qbf = A.alloc([768], BF16); tqb = Tk()
        qT = A.alloc([6, 128], BF16); tqT = Tk()
        Ps = [A.alloc([NT * 128], BF16) for _ in range(2)]; tPs = [Tk() for _ in range(2)]
        P, tP = Ps[0], tPs[0]
        PT = [A.alloc([512], BF16) for _ in range(2)]; tPT = [Tk() for _ in range(2)]
        cc = A.alloc([D], BF16); tcc = Tk()
        ccT = A.alloc([8, 128], BF16); tccT = Tk()
        tmp = P[:, 0:2 * D].bitcast(F32); ttmp = tP
        st = A.alloc([16], F32); tst = Tk()
        sms = [A.alloc([16], F32) for _ in range(4)]; tsms = [Tk() for _ in range(4)]
        print('M2 arena top', A.top)
        ocnt = [0]
        acnt = [0]

        jobs = []

        def attention(*a, **kw):
            jobs.append((a, kw, {}))

        def att1(ctx_, i, blk, pb0, segs, vcol, oc0, sc, bias_h=None, s0=0, sink_h=None):
            nseg = len(segs)
            nb = (nseg + 3) // 4
            acnt[0] += 1
            sm, tsm = sms[acnt[0] % 4], tsms[acnt[0] % 4]
            P, tP = Ps[acnt[0] % 2], tPs[acnt[0] % 2]
            ctx_.update(sm=sm, tsm=tsm, P=P, tP=tP)
            b0 = 0 if nb > 2 else 2 * (acnt[0] % 2)
            banks = list(range(b0, b0 + nb))
            tb_ = [self.tp[k] for k in banks]
            ntot = nseg * 128
            Sall = self.psall[:, b0 * 512:b0 * 512 + ntot]
            q_ap = qT[pb0:pb0 + 64, blk, :]
            for t, (kt, c, mask) in enumerate(segs):
                bk, cb = b0 + t // 4, (t % 4) * 128
                self.mm(self.ps[bk][:, cb:cb + 128], q_ap, KT[pb0:pb0 + 64, c, kt * 128:(kt + 1) * 128], True, mask is None, [tqT, tKT[kt]], [self.tp[bk]])
                if mask is not None:
                    self.mm(self.ps[bk][:, cb:cb + 128], self.ident_b, mask, False, True, [self.tconst, tmk], [self.tp[bk]])
            if bias_h is not None:
                nloc = nseg - 2
                self.dve(lambda e: e.tensor_tensor(out=self.psall[:, b0 * 512:b0 * 512 + nloc * 128], in0=self.psall[:, b0 * 512:b0 * 512 + nloc * 128],
                                                   in1=Traw[:, bias_h, s0 - 1:s0 - 1 + 2 * nloc, :].rearrange("p s k -> p (s k)"), op=ALU.add), tb_ + [tTr], tb_)
            self.dve(lambda e: e.reduce_max(out=sm[:, 9:10], in_=Sall, axis=AX.X, negate=True), tb_, [tsm])
            if sink_h is not None:
                self.dve(lambda e: e.tensor_tensor(out=sm[:, 9:10], in0=sm[:, 9:10], in1=sk[:, 4 + sink_h:5 + sink_h], op=ALU.min), [tsm, tsk], [tsm])
            self.act(lambda e: e.activation(out=P[:, 0:ntot], in_=Sall, func=AF.Exp, bias=sm[:, 9:10], scale=1.0, accum_out=sm[:, 10:11]), tb_ + [tsm], [tP, tsm])
            if sink_h is not None:
                self.act(lambda e: e.activation(out=sm[:, 12:13], in_=sk[:, sink_h:sink_h + 1], func=AF.Exp, bias=sm[:, 9:10], scale=1.0), [tsk, tsm], [tsm])
                self.dve(lambda e: e.tensor_tensor(out=sm[:, 10:11], in0=sm[:, 10:11], in1=sm[:, 12:13], op=ALU.add), [tsm], [tsm])
            self.dve(lambda e: e.reciprocal(out=sm[:, 11:12], in_=sm[:, 10:11]), [tsm], [tsm])

        def att2(ctx_, i, blk, pb0, segs, vcol, oc0, sc, bias_h=None, s0=0, sink_h=None):
            sm, tsm, P, tP = ctx_["sm"], ctx_["tsm"], ctx_["P"], ctx_["tP"]
            nseg = len(segs)
            ob = oc0 % 512
            for g0 in range(0, nseg, 4):
                gi = ocnt[0] % 2
                ocnt[0] += 1
                pt = 5 + gi
                ng = min(4, nseg - g0)
                for t in range(g0, g0 + ng):
                    self.mm(self.ps[pt][:, (t - g0) * 128:(t - g0 + 1) * 128], P[:, t * 128:(t + 1) * 128], self.ident_b, True, True, [tP, self.tconst], [self.tp[pt]])
                self.dve(lambda e, pt=pt, gi=gi, ng=ng: e.tensor_copy(out=PT[gi][:, 0:ng * 128], in_=self.ps[pt][:, 0:ng * 128]), [self.tp[pt]], [tPT[gi]])
                for t in range(g0, g0 + ng):
                    kt = segs[t][0]
                    self.mm(self.ps[7][:, ob:ob + 64], PT[gi][:, (t - g0) * 128:(t - g0 + 1) * 128], V[:, kt, vcol:vcol + 64], t == 0, t == nseg - 1, [tPT[gi], tV[kt]], [self.tp[7]])
            self.act(lambda e: e.activation(out=cc[:, oc0:oc0 + 64], in_=self.ps[7][:, ob:ob + 64], func=AF.Copy, scale=sm[:, 11:12]), [self.tp[7], tsm], [tcc])

        for i in range(nq):
            b = i % 2
            isctx = i >= NL
            self.make_aT(l, i, 0, aT[b], taT[b])
            for kc in range(8):
                self.mm(self.ps[0][:, :], aT[b][:, kc, :], Wq[:, kc, 0:512], kc == 0, kc == 7, [taT[b]] + tWq[0:2], [self.tp[0]])
            for kc in range(8):
                self.mm(self.ps[1][:, 0:256], aT[b][:, kc, :], Wq[:, kc, 512:768], kc == 0, kc == 7, [taT[b], tWq[2]], [self.tp[1]])
            self.act(lambda e: e.activation(out=qbf[:, 0:256], in_=self.ps[0][:, 0:256], func=AF.Copy), [self.tp[0]], [tqb])
            self.rms_rope(self.ps[0][:, 256:512], self.tp[0], 4, qbf[:, 256:512], tqb, i, rw, g=gqk[:, 0, :], perm=True)
            self.rms_rope(self.ps[1][:, 0:256], self.tp[1], 4, qbf[:, 512:768], tqb, i, rw, perm=True)
            for c in range(6):
                bk = 2 + c // 4
                self.mm(self.ps[bk][:, (c % 4) * 128:(c % 4 + 1) * 128], qbf[:, c * 128:(c + 1) * 128], self.ident_b, True, True, [tqb, self.tconst], [self.tp[bk]])
            self.dve(lambda e: e.tensor_scalar(out=qT[:, 0:4, :], in0=self.ps[2][:, :].rearrange("p (c t) -> p c t", t=128), scalar1=0.125, scalar2=None, op0=ALU.mult), [self.tp[2]], [tqT])
            self.dve(lambda e: e.tensor_scalar(out=qT[:, 4:6, :], in0=self.ps[3][:, 0:256].rearrange("p (c t) -> p c t", t=128), scalar1=0.125, scalar2=None, op0=ALU.mult), [self.tp[3]], [tqT])
            ctxs = [(16, None), (17, None)]
            for h in range(4):
                blk, pb0, c = h // 2, (h % 2) * 64, h // 2
                if isctx:
                    segs = [(kt, c, None) for kt, _ in ctxs]
                    attention(i, blk, pb0, segs, h * 64, h * 64, 0.125)
                else:
                    j = i
                    if 2 <= j <= 13:
                        var, t0, ntl, s0 = 0, j - 2, 5, 3
                    elif j == 0:
                        var, t0, ntl, s0 = 1, 0, 4, 7
                    elif j == 1:
                        var, t0, ntl, s0 = 2, 0, 4, 5
                    elif j == 14:
                        var, t0, ntl, s0 = 3, 12, 4, 3
                    else:
                        var, t0, ntl, s0 = 4, 12, 4, 1
                    mo = 0 if var == 0 else 640 + (var - 1) * 512
                    segs = [(t0 + k, c, namask[:, mo + k * 128:mo + (k + 1) * 128]) for k in range(ntl)] + [(kt, c, None) for kt, _ in ctxs]
                    attention(i, blk, pb0, segs, h * 64, h * 64, 1.0, bias_h=h, s0=s0)
            for h in range(4):
                blk, pb0, kvh = 2 + (h % 2), (h // 2) * 64, h // 2
                kts = [16, 17] if isctx else list(range(NT))
                attention(i, blk, pb0, [(kt, 2, None) for kt in kts], 256 + kvh * 64, 256 + h * 64, 0.125)
            self.pool(lambda e, i=i: e.tensor_copy(out=cc[:, 512:768], in_=OC[:, i, :]), [tOC[i]], [tcc])
            for h in range(4):
                blk, pb0, kvh = 4 + (h % 2), (h // 2) * 64, h // 2
                if isctx:
                    segs = [(16, 3, None), (17, 3, None)]
                else:
                    segs = []
                    if i - 1 >= 0: segs.append((i - 1, 3, tri[:, 0, :]))
                    segs.append((i, 3, None))
                    if i + 1 < NL: segs.append((i + 1, 3, tri[:, 1, :]))
                    segs += [(16, 3, None), (17, 3, None)]
                attention(i, blk, pb0, segs, 384 + kvh * 64, 768 + h * 64, 0.125, sink_h=h)
            att1(jobs[0][2], *jobs[0][0], **jobs[0][1])
            for k_ in range(len(jobs)):
                if k_ + 1 < len(jobs):
                    att1(jobs[k_ + 1][2], *jobs[k_ + 1][0], **jobs[k_ + 1][1])
                att2(jobs[k_][2], *jobs[k_][0], **jobs[k_][1])
            del jobs[:]
            for c in range(8):
                bk = 5 + c // 4
                self.mm(self.ps[bk][:, (c % 4) * 128:(c % 4 + 1) * 128], cc[:, c * 128:(c + 1) * 128], self.ident_b, True, True, [tcc, self.tconst], [self.tp[bk]])
            for hf in range(2):
                self.act(lambda e, hf=hf: e.activation(out=ccT[:, hf * 4:(hf + 1) * 4, :], in_=self.ps[5 + hf][:, :].rearrange("p (c t) -> p c t", t=128), func=AF.Copy), [self.tp[5 + hf]], [tccT])
            for hf in range(2):
                for kc in range(8):
                    self.mm(self.ps[hf][:, :], ccT[:, kc, :], Wo[:, kc, hf * 512:(hf + 1) * 512], kc == 0, kc == 7, [tccT, tWo], [self.tp[hf]])
            self.resid_ln(i, [(self.ps[0][:, :], self.tp[0]), (self.ps[1][:, :], self.tp[1])], G, tG, LN, tLN, tmp, ttmp, st, tst)
        A.release(m_all)

    def sin_of(self, out, x, shift, wk, tk, R):
        TWO_PI = 2.0 * np.pi
        a, k = wk
        ki = k.bitcast(I32)
        self.dve(lambda e: e.tensor_scalar(out=a, in0=x, scalar1=1.0 / TWO_PI, scalar2=shift / TWO_PI + 0.5, op0=ALU.mult, op1=ALU.add), R, [tk])
        self.dve(lambda e: e.tensor_copy(out=ki, in_=a), [tk], [tk])
        self.dve(lambda e: e.tensor_copy(out=a, in_=ki), [tk], [tk])
        self.dve(lambda e: e.tensor_scalar(out=k, in0=x, scalar1=shift, scalar2=None, op0=ALU.add), R + [tk], [tk])
        self.dve(lambda e: e.scalar_tensor_tensor(out=k, in0=a, scalar=-TWO_PI, in1=k, op0=ALU.mult, op1=ALU.add), [tk], [tk])
        self.dve(lambda e: e.tensor_scalar(out=a, in0=k, scalar1=float(np.pi), scalar2=None, op0=ALU.is_gt), [tk], [tk])
        self.dve(lambda e: e.scalar_tensor_tensor(out=k, in0=a, scalar=-TWO_PI, in1=k, op0=ALU.mult, op1=ALU.add), [tk], [tk])
        self.dve(lambda e: e.tensor_scalar(out=a, in0=k, scalar1=-float(np.pi), scalar2=None, op0=ALU.is_lt), [tk], [tk])
        self.dve(lambda e: e.scalar_tensor_tensor(out=k, in0=a, scalar=TWO_PI, in1=k, op0=ALU.mult, op1=ALU.add), [tk], [tk])
        self.dve(lambda e: e.tensor_scalar(out=k, in0=k, scalar1=3.1415925, scalar2=-3.1415925, op0=ALU.min, op1=ALU.max), [tk], [tk])
        self.act(lambda e: e.activation(out=out, in_=k, func=AF.Sin), [tk], [tk])

    def s5_phase(self, l, UT, tUT, OC, tOC, nq):
        A, S = self.A, self.S
        dve, act, pool = self.dve, self.act, self.pool
        tp_ = Tk()
        vec = A.alloc([16, 3], F32)
        tau = A.alloc([130], F32)
        bs = A.alloc([16, 2, 16], F32)
        S.dma("sp", vec, self.ssm_vec[l], W=[tp_])
        S.dma("sp", tau, self.c_tau, W=[tp_])
        S.dma("sp", bs, self.ssm_bs[l], W=[tp_])
        CSb = A.alloc([16, 2, 32], BF16); tcs = Tk()
        DDb = A.alloc([8, 32], BF16)
        Wg = A.alloc([2, 256], BF16)
        S.dma("pool", CSb, self.ssm_cs[l], W=[tcs])
        S.dma("pool", DDb, self.ssm_dd[l], W=[tcs])
        S.dma("pool", Wg, self.ssm_wglu[l].rearrange("(c p) n -> p c n", p=128), W=[tcs])
        dve(lambda e: e.tensor_scalar(out=CSb[:, :, 1, :], in0=CSb[:, :, 1, :], scalar1=-1.0, scalar2=None, op0=ALU.mult), [tcs], [tcs])
        pv = A.alloc([16, 16], F32)
        def q(k): return pv[:, k, :]
        lre, lim, lst = vec[:, :, 0], vec[:, :, 1], vec[:, :, 2]
        STEP, LR, ANG, MAG, SN, CN, ABR, ABI, DEN, NUM, FRE, FIM, T0, T1 = range(14)
        act(lambda e: e.activation(out=q(STEP), in_=lst, func=AF.Exp), [tp_], [tp_])
        dve(lambda e: e.tensor_tensor(out=q(LR), in0=lre, in1=q(STEP), op=ALU.mult), [tp_], [tp_])
        dve(lambda e: e.tensor_tensor(out=q(ANG), in0=lim, in1=q(STEP), op=ALU.mult), [tp_], [tp_])
        act(lambda e: e.activation(out=q(MAG), in_=q(LR), func=AF.Exp), [tp_], [tp_])
        self.sin_of(q(SN), q(ANG), 0.0, (q(T0), q(T1)), tp_, [tp_])
        self.sin_of(q(CN), q(ANG), float(np.pi / 2), (q(T0), q(T1)), tp_, [tp_])
        dve(lambda e: e.tensor_tensor(out=q(ABR), in0=q(MAG), in1=q(CN), op=ALU.mult), [tp_], [tp_])
        dve(lambda e: e.tensor_tensor(out=q(ABI), in0=q(MAG), in1=q(SN), op=ALU.mult), [tp_], [tp_])
        dve(lambda e: e.tensor_tensor(out=q(DEN), in0=lre, in1=lre, op=ALU.mult), [tp_], [tp_])
        dve(lambda e: e.tensor_tensor(out=q(T0), in0=lim, in1=lim, op=ALU.mult), [tp_], [tp_])
        dve(lambda e: e.tensor_tensor(out=q(DEN), in0=q(DEN), in1=q(T0), op=ALU.add), [tp_], [tp_])
        dve(lambda e: e.reciprocal(out=q(DEN), in_=q(DEN)), [tp_], [tp_])
        dve(lambda e: e.tensor_scalar(out=q(NUM), in0=q(ABR), scalar1=-1.0, scalar2=None, op0=ALU.add), [tp_], [tp_])
        dve(lambda e: e.tensor_tensor(out=q(T0), in0=q(NUM), in1=lre, op=ALU.mult), [tp_], [tp_])
        dve(lambda e: e.tensor_tensor(out=q(T1), in0=q(ABI), in1=lim, op=ALU.mult), [tp_], [tp_])
        dve(lambda e: e.tensor_tensor(out=q(FRE), in0=q(T0), in1=q(T1), op=ALU.add), [tp_], [tp_])
        dve(lambda e: e.tensor_tensor(out=q(FRE), in0=q(FRE), in1=q(DEN), op=ALU.mult), [tp_], [tp_])
        dve(lambda e: e.tensor_tensor(out=q(T0), in0=q(ABI), in1=lre, op=ALU.mult), [tp_], [tp_])
        dve(lambda e: e.tensor_tensor(out=q(T1), in0=q(NUM), in1=lim, op=ALU.mult), [tp_], [tp_])
        dve(lambda e: e.tensor_tensor(out=q(FIM), in0=q(T0), in1=q(T1), op=ALU.subtract), [tp_], [tp_])
        dve(lambda e: e.tensor_tensor(out=q(FIM), in0=q(FIM), in1=q(DEN), op=ALU.mult), [tp_], [tp_])
        EC = A.alloc([16, 129], F32); ES = A.alloc([16, 129], F32); tE = Tk()
        mtab = A.mark()
        X = A.alloc([16, 129], F32); wa = A.alloc([16, 129], F32); wb = A.alloc([16, 129], F32); tX = Tk()
        dve(lambda e: e.tensor_tensor(out=X, in0=q(ANG).unsqueeze(2).to_broadcast([128, 16, 129]), in1=tau[:, 0:129].unsqueeze(1).to_broadcast([128, 16, 129]), op=ALU.mult), [tp_], [tX])
        self.sin_of(ES, X, 0.0, (wa, wb), tE, [tX])
        self.sin_of(EC, X, float(np.pi / 2), (wa, wb), tE, [tX])
        A.release(mtab)
        BT = A.alloc([16, 2, 128], BF16); tBT = Tk()
        mb = A.mark()
        bb = A.alloc([16, 2, 16], F32); tbb = Tk()
        w4 = A.alloc([4, 16, 16], F32)
        fre_b = q(FRE).unsqueeze(2).to_broadcast([128, 16, 16]); fim_b = q(FIM).unsqueeze(2).to_broadcast([128, 16, 16])
        dve(lambda e: e.tensor_tensor(out=w4[:, 0], in0=bs[:, :, 0, :], in1=fre_b, op=ALU.mult), [tp_], [tbb])
        dve(lambda e: e.tensor_tensor(out=w4[:, 1], in0=bs[:, :, 1, :], in1=fim_b, op=ALU.mult), [tp_], [tbb])
        dve(lambda e: e.tensor_tensor(out=w4[:, 2], in0=bs[:, :, 1, :], in1=fre_b, op=ALU.mult), [tp_], [tbb])
        dve(lambda e: e.tensor_tensor(out=w4[:, 3], in0=bs[:, :, 0, :], in1=fim_b, op=ALU.mult), [tp_], [tbb])
        dve(lambda e: e.tensor_tensor(out=bb[:, :, 0, :], in0=w4[:, 0], in1=w4[:, 1], op=ALU.subtract), [tbb], [tbb])
        dve(lambda e: e.tensor_tensor(out=bb[:, :, 1, :], in0=w4[:, 2], in1=w4[:, 3], op=ALU.add), [tbb], [tbb])
        Bp = A.alloc([4, 128], BF16); tBp = [Tk() for _ in range(4)]
        dve(lambda e: e.memset(Bp, 0.0), [], tBp)
        n = 0
        for dg in range(16):
            gc = dg % 8
            band = gc % 4
            for ri in range(2):
                for gl in range(2):
                    dve(lambda e, dg=dg, ri=ri, gl=gl, band=band: e.tensor_copy(out=Bp[gl * 64:(gl + 1) * 64, band, band * 32 + gl * 16:band * 32 + gl * 16 + 16],
                                                                            in_=bb[gl * 64:(gl + 1) * 64, dg, ri, :]), [tbb], [tBp[band]])
                bk = 5 + (n // 4) % 2
                self.mm(self.ps[bk][:, (n % 4) * 128:(n % 4 + 1) * 128], Bp[:, band, :], self.ident_b, True, True, [tBp[band], self.tconst], [self.tp[bk]])
                if n % 4 == 3:
                    dg0 = (n - 3) // 2
                    act(lambda e, bk=bk, dg0=dg0: e.activation(out=BT[:, dg0:dg0 + 2, :, :], in_=self.ps[bk][:, :].rearrange("p (a b t) -> p a b t", a=2, b=2), func=AF.Copy), [self.tp[bk]], [tBT])
                n += 1
        A.release(mb)
        Y = A.alloc([NT, 256], F32); tY = [Tk() for _ in range(NT)]
        g = A.alloc([8, 2, 128], F32); tg = Tk()
        gi = A.alloc([8, 2], F32); tgi = Tk()
        giw = A.alloc([4, 8], F32)
        tt = [A.alloc([2, 128], F32) for _ in range(4)]; ttt = Tk()
        w = A.alloc([2, 2, 128], F32); tw = Tk()
        hs = A.alloc([8, 2, 128], BF16); ths = Tk()
        z = A.alloc([256], F32); z2 = A.alloc([256], F32); tz = Tk()
        zb = A.alloc([256], BF16); zT = A.alloc([2, 128], BF16); tzT = Tk()
        for d in range(2):
            order = [16, 17] + list(range(16)) if d == 0 else [17, 16] + list(range(15, -1, -1))
            rv = (lambda ap: ap) if d == 0 else (lambda ap: ap[:, ::-1])
            for n_, i in enumerate(order):
                tk = slice(i * 128, (i + 1) * 128)
                for gc in range(8):
                    for ri in range(2):
                        bk = gc // 2
                        c0 = (gc % 2) * 256 + ri * 128
                        self.mm(self.ps[bk][:, c0:c0 + 128], BT[:, d * 8 + gc, ri, :], UT[:, gc // 4, tk], True, True, [tBT, tUT[i]], [self.tp[bk]])
                if n_ > 0:
                    last = 127 if d == 0 else 0
                    glr, gli = g[:, :, 0, last], g[:, :, 1, last]
                    cT, sT = EC[:, d * 8:(d + 1) * 8, 128], ES[:, d * 8:(d + 1) * 8, 128]
                    dve(lambda e, glr=glr, cT=cT: e.tensor_tensor(out=giw[:, 0], in0=glr, in1=cT, op=ALU.mult), [tg, tE], [tgi])
                    dve(lambda e, gli=gli, sT=sT: e.tensor_tensor(out=giw[:, 1], in0=gli, in1=sT, op=ALU.mult), [tg, tE], [tgi])
                    dve(lambda e, glr=glr, sT=sT: e.tensor_tensor(out=giw[:, 2], in0=glr, in1=sT, op=ALU.mult), [tg, tE], [tgi])
                    dve(lambda e, gli=gli, cT=cT: e.tensor_tensor(out=giw[:, 3], in0=gli, in1=cT, op=ALU.mult), [tg, tE], [tgi])
                    dve(lambda e: e.tensor_tensor(out=gi[:, :, 0], in0=giw[:, 0], in1=giw[:, 1], op=ALU.subtract), [tgi], [tgi])
                    dve(lambda e: e.tensor_tensor(out=gi[:, :, 1], in0=giw[:, 2], in1=giw[:, 3], op=ALU.add), [tgi], [tgi])
                for bk in range(4):
                    pv4 = self.ps[bk][:, :].rearrange("p (a b t) -> p a b t", a=2, b=2)
                    bre, bim = pv4[:, :, 0, :], pv4[:, :, 1, :]
                    dg0 = d * 8 + bk * 2
                    cs_, sn_ = rv_tab(EC, dg0, d), rv_tab(ES, dg0, d)
                    dve(lambda e, bre=bre, cs_=cs_: e.tensor_tensor(out=tt[0], in0=bre, in1=cs_, op=ALU.mult), [self.tp[bk], tE], [ttt])
                    dve(lambda e, bim=bim, sn_=sn_: e.tensor_tensor(out=tt[1], in0=bim, in1=sn_, op=ALU.mult), [self.tp[bk], tE], [ttt])
                    dve(lambda e, bim=bim, cs_=cs_: e.tensor_tensor(out=tt[2], in0=bim, in1=cs_, op=ALU.mult), [self.tp[bk], tE], [ttt])
                    dve(lambda e, bre=bre, sn_=sn_: e.tensor_tensor(out=tt[3], in0=bre, in1=sn_, op=ALU.mult), [self.tp[bk], tE], [ttt])
                    pool(lambda e: e.tensor_tensor(out=w[:, :, 0, :], in0=tt[0], in1=tt[1], op=ALU.add), [ttt], [tw])
                    pool(lambda e: e.tensor_tensor(out=w[:, :, 1, :], in0=tt[2], in1=tt[3], op=ALU.subtract), [ttt], [tw])
                    for g2 in range(2):
                        gc = bk * 2 + g2
                        for ri in range(2):
                            init = 0.0 if n_ == 0 else gi[:, gc, ri:ri + 1]
                            dve(lambda e, gc=gc, ri=ri, g2=g2, init=init: e.tensor_tensor_scan(out=rv(g[:, gc, ri, :]), data0=q(MAG)[:, d * 8 + gc:d * 8 + gc + 1].to_broadcast([128, 128]),
                                                                                          data1=rv(w[:, g2, ri, :]), initial=init, op0=ALU.mult, op1=ALU.add),
                                [tw, tp_, tgi], [tg])
                for hh in range(4):
                    gre, gim = g[:, hh * 2:(hh + 1) * 2, 0, :], g[:, hh * 2:(hh + 1) * 2, 1, :]
                    dg0 = d * 8 + hh * 2
                    cs_, sn_ = rv_tab(EC, dg0, d), rv_tab(ES, dg0, d)
                    dve(lambda e, gre=gre, cs_=cs_: e.tensor_tensor(out=tt[0], in0=gre, in1=cs_, op=ALU.mult), [tg, tE], [ttt])
                    dve(lambda e, gim=gim, sn_=sn_: e.tensor_tensor(out=tt[1], in0=gim, in1=sn_, op=ALU.mult), [tg, tE], [ttt])
                    dve(lambda e, gre=gre, sn_=sn_: e.tensor_tensor(out=tt[2], in0=gre, in1=sn_, op=ALU.mult), [tg, tE], [ttt])
                    dve(lambda e, gim=gim, cs_=cs_: e.tensor_tensor(out=tt[3], in0=gim, in1=cs_, op=ALU.mult), [tg, tE], [ttt])
                    pool(lambda e, hh=hh: e.tensor_tensor(out=hs[:, hh * 2:(hh + 1) * 2, 0, :], in0=tt[0], in1=tt[1], op=ALU.subtract), [ttt], [ths])
                    pool(lambda e, hh=hh: e.tensor_tensor(out=hs[:, hh * 2:(hh + 1) * 2, 1, :], in0=tt[2], in1=tt[3], op=ALU.add), [ttt], [ths])
                for gc in range(8):
                    terms = [(hs[:, gc, 0, :], CSb[:, d * 8 + gc, 0, :], [ths, tcs]), (hs[:, gc, 1, :], CSb[:, d * 8 + gc, 1, :], [ths, tcs])]
                    if d == 0:
                        terms.append((UT[:, gc // 4, tk], DDb[:, gc, :], [tUT[i], tcs]))
                    for k, (lt, rh, R_) in enumerate(terms):
                        self.mm(self.ps[4][:, gc * 32:(gc + 1) * 32], lt, rh, k == 0, k == len(terms) - 1, R_, [self.tp[4]])
                if d == 0:
                    act(lambda e, i=i: e.activation(out=Y[:, i, :], in_=self.ps[4][:, 0:256], func=AF.Copy), [self.tp[4]], [tY[i]])
                    continue
                if i >= nq:
                    continue
                dve(lambda e, i=i: e.tensor_tensor(out=z, in0=self.ps[4][:, 0:256], in1=Y[:, i, :], op=ALU.add), [self.tp[4], tY[i]], [tz])
                pool(lambda e: e.tensor_tensor(out=z2, in0=z, in1=z, op=ALU.mult), [tz], [tz])
                pool(lambda e: e.tensor_scalar(out=z2, in0=z2, scalar1=0.044715, scalar2=1.0, op0=ALU.mult, op1=ALU.add), [tz], [tz])
                pool(lambda e: e.tensor_tensor(out=z2, in0=z2, in1=z, op=ALU.mult), [tz], [tz])
                act(lambda e: e.activation(out=z2, in_=z2, func=AF.Sigmoid, scale=1.5957691216057308), [tz], [tz])
                dve(lambda e: e.tensor_tensor(out=z, in0=z, in1=z2, op=ALU.mult), [tz], [tz])
                dve(lambda e: e.tensor_copy(out=zb, in_=z), [tz], [tz])
                for c in range(2):
                    self.mm(self.ps[5][:, c * 128:(c + 1) * 128], zb[:, c * 128:(c + 1) * 128], self.ident_b, True, True, [tz, self.tconst], [self.tp[5]])
                act(lambda e: e.activation(out=zT, in_=self.ps[5][:, 0:256].rearrange("p (c t) -> p c t", t=128), func=AF.Copy), [self.tp[5]], [tzT])
                for c in range(2):
                    self.mm(self.ps[6][:, 0:256], zT[:, c, :], Wg[:, c, :], c == 0, c == 1, [tzT, tcs], [self.tp[6]])
                act(lambda e: e.activation(out=z2, in_=self.ps[6][:, 0:256], func=AF.Sigmoid), [self.tp[6]], [tz])
                dve(lambda e, i=i: e.tensor_tensor(out=OC[:, i, :], in0=z, in1=z2, op=ALU.mult), [tz], [tOC[i]])


def rv_tab(E, dg0, d, n=2):
    if d == 0:
        return E[:, dg0:dg0 + n, 0:128]
    return E[:, dg0:dg0 + n, 127::-1]


def _consts():
    c = {}
    c["c_ident_f"] = np.eye(128, dtype=np.float32)
    c["c_ident_b"] = np.eye(128, dtype=np.float32).astype(ml_dtypes.bfloat16)
    pos = np.arange(NL * 128)
    row = (pos // 64).astype(np.float32)
    col = (pos % 64).astype(np.float32)
    inv = (10000.0 ** (-np.arange(16, dtype=np.float32) / 16)).astype(np.float32)
    ang = np.concatenate([row[:, None] * inv, col[:, None] * inv], -1).astype(np.float32)
    cs = np.concatenate([np.cos(ang), -np.sin(ang), np.sin(ang)], -1).astype(np.float32)
    ctxcs = np.concatenate([np.ones((256, 32), np.float32), np.zeros((256, 64), np.float32)], -1)
    cs = np.concatenate([cs, ctxcs], 0).reshape(NT, 128, 96).transpose(1, 0, 2)
    c["c_rope"] = np.ascontiguousarray(cs)
    q = np.arange(128)[:, None]
    k = np.arange(128)[None, :]
    tri = np.stack([np.where(k >= q, 0.0, NEG), np.where(k <= q, 0.0, NEG)], 1).astype(np.float32)
    c["c_tri"] = tri.astype(ml_dtypes.bfloat16)
    nm = np.full((5, 128, 10, 64), NEG, np.float32)
    qc = np.arange(64)
    cstart = np.clip(qc - 8, 0, 48)
    kc = np.arange(64)
    colv = (kc[None, :] >= cstart[:, None]) & (kc[None, :] < cstart[:, None] + 16)
    def fill(var, j, i0, nr):
        for rl in range(2):
            r = 2 * j + rl
            rs = int(np.clip(r - 4, 0, 24))
            for ii in range(nr):
                i = i0 + ii
                if rs <= i < rs + 8:
                    blk = np.where(colv, 0.0, NEG)
                    nm[var, rl * 64:(rl + 1) * 64, ii, :] = blk
    fill(0, 5, 6, 10)
    fill(1, 0, 0, 8); fill(2, 1, 0, 8); fill(3, 14, 24, 8); fill(4, 15, 24, 8)
    nm2 = nm.transpose(1, 0, 2, 3).reshape(128, 5, 640)
    c["c_namask"] = np.ascontiguousarray(np.concatenate([nm2[:, 0, :]] + [nm2[:, v, 0:512] for v in range(1, 5)], 1)).astype(ml_dtypes.bfloat16)
    tau = np.zeros((128, 130), np.float32)
    tau[:, :] = np.arange(130, dtype=np.float32)[None, :]
    c["c_tau"] = tau
    return c


def _prep_shared(I):
    f = np.float32
    d = dict(_consts())
    for k in ("ada_w", "ada_b", "w_in", "w_out"):
        d[k] = np.ascontiguousarray(I[k], dtype=f)
    rp = np.zeros((DEPTH, 4, 18, 128), f)
    rp[:, :, 1:16, 48:79] = I["na_rpb"][:, :, :, ::-1]
    d["rpbh"] = rp
    d["gqk"] = np.ascontiguousarray(np.stack([I["ga_q_norm"], I["ga_k_norm"]], 1), dtype=f)
    def st(a):
        L = a.shape[0]
        rest = a.shape[4:]
        a = a.reshape((L, 2, 8, 2, 64) + rest)
        a = np.moveaxis(a, (3, 4), (1, 2))
        return np.ascontiguousarray(a.reshape((L, 128, 16) + rest))
    ls = np.broadcast_to(I["ssm_log_step"][..., None], I["ssm_lambda_re"].shape)
    d["ssm_vec"] = np.ascontiguousarray(np.stack([st(I["ssm_lambda_re"]), st(I["ssm_lambda_im"]), st(np.ascontiguousarray(ls))], -1), dtype=f)
    d["ssm_bs"] = np.ascontiguousarray(np.stack([st(I["ssm_b_re"]), st(I["ssm_b_im"])], -2), dtype=f)
    cre = st(np.swapaxes(I["ssm_c_re"], -1, -2))
    cim = st(np.swapaxes(I["ssm_c_im"], -1, -2))
    cs = np.zeros((DEPTH, 128, 16, 2, 32), f)
    for gl in range(2):
        cs[:, gl * 64:(gl + 1) * 64, :, 0, gl * 16:(gl + 1) * 16] = cre[:, gl * 64:(gl + 1) * 64]
        cs[:, gl * 64:(gl + 1) * 64, :, 1, gl * 16:(gl + 1) * 16] = cim[:, gl * 64:(gl + 1) * 64]
    d["ssm_cs"] = cs
    dd = np.zeros((DEPTH, 128, 8, 32), f)
    sd = I["ssm_d"].reshape(DEPTH, 2, 128)
    for gc in range(8):
        for j in range(32):
            pl = (gc % 4) * 32 + j
            dd[:, pl, gc, j] = sd[:, gc // 4, pl]
    d["ssm_dd"] = dd
    d["ssm_wglu"] = np.ascontiguousarray(I["ssm_w_glu"], dtype=f)
    d["sink"] = np.ascontiguousarray(I["sw_sink"], dtype=f)
    d["lngb"] = np.ascontiguousarray(np.stack([I["ln1_g"], I["ln1_b"], I["ln2_g"], I["ln2_b"]], 1), dtype=f)
    d["ffn_g"] = I["ffn_w_gate"]; d["ffn_u"] = I["ffn_w_up"]; d["ffn_d"] = I["ffn_w_down"]
    d["moe_r"] = I["moe_w_router"]; d["moe_rb"] = I["moe_b_router"]
    d["moe_g"] = I["moe_w_gate"]; d["moe_u"] = I["moe_w_up"]; d["moe_d"] = I["moe_w_down"]
    return d


def _prep_core(I, b, hx=None):
    if hx is None:
        hx = np.concatenate([I["x"][b], I["ctx"][b]], 0)
    cv = np.stack([I["c"][b].reshape(8, 128).T, I["c_ctx"].reshape(8, 128).T], -1)
    return {"hx": np.ascontiguousarray(hx, dtype=np.float32), "cv": np.ascontiguousarray(cv, dtype=np.float32)}


def build_program(layers=(0, 1, 2, 3)):
    b = B(list(layers))
    b.setup()
    for l in layers:
        b.ada_cols(l)
        b.mixer_phase(l)
        b.ffn_phase(l)
    for i in range(NL):
        b.store(b.out[i * 128:(i + 1) * 128, :], b.H[:, i, :], [b.tH[i]])
    with b.nc.Block() as blk:
        b.S.emit(blk, b.fin)
    return b


def kernel(**inputs):
    I = {k: np.asarray(v) for k, v in inputs.items()}
    n = 8
    b = build_program()
    shared = _prep_shared(I)
    shared = {k: v for k, v in shared.items() if k in b.din}
    in_maps = []
    for c in range(n):
        m = dict(shared)
        m.update(_prep_core(I, c))
        in_maps.append(m)
    res = run_bass_kernel_spmd(b.nc, in_maps, core_ids=list(range(n)))
    out = np.stack([np.asarray(r["out"], dtype=np.float32).reshape(NL * 128, D) for r in res.results], 0)
    return out
```

```python
import numpy as np
import ml_dtypes
import concourse.bass as bass
import concourse.mybir as mybir
from concourse.bass_utils import run_bass_kernel_spmd

F32 = mybir.dt.float32
BF16 = mybir.dt.bfloat16
I32 = mybir.dt.int32
ALU = mybir.AluOpType
AF = mybir.ActivationFunctionType
AX = mybir.AxisListType


class Tk:
    __slots__ = ("w", "r")

    def __init__(self):
        self.w = None
        self.r = []


class Sched:
    ENG = ("pe", "act", "dve", "pool", "sp")
    NDMA = {"sp": 12, "pool": 8, "act": 4}

    def __init__(self, nc):
        self.nc = nc
        self.prog = {e: [] for e in self.ENG}
        self.n = {e: 0 for e in self.ENG}
        self.seen = {e: {} for e in self.ENG}
        self.signaled = {e: set() for e in self.ENG}
        self.dma_rr = {q: 0 for q in self.NDMA}
        self.dma_tot = {}
        self.lastc = {e: 0 for e in self.ENG}
        self.cur_fence = {}

    def fence(self):
        for e in self.ENG:
            if self.lastc[e]:
                self.cur_fence[e] = self.lastc[e]
        for key, tot in self.dma_tot.items():
            self.cur_fence[key] = tot

    def _deps(self, eng, reads, writes):
        deps = dict(self.cur_fence)
        def add(t):
            if t is None:
                return
            k, v = t
            if deps.get(k, 0) < v:
                deps[k] = v
        for t in reads:
            add(t.w)
        for t in writes:
            add(t.w)
            for r in t.r:
                add(r)
        waits = []
        for k, v in deps.items():
            if k == "pe" and eng == "pe":
                continue
            if self.seen[eng].get(k, 0) < v:
                self.seen[eng][k] = v
                waits.append((k, v))
                if k in self.signaled:
                    self.signaled[k].add(v)
        return waits

    def _commit(self, ticket, reads, writes):
        for t in reads:
            t.r.append(ticket)
        for t in writes:
            t.w = ticket
            t.r = []

    def op(self, eng, fn, R=(), W=()):
        waits = self._deps(eng, R, W)
        self.n[eng] += 1
        self.lastc[eng] = self.n[eng]
        ticket = (eng, self.n[eng])
        self.prog[eng].append((waits, fn, ticket, None))
        self._commit(ticket, R, W)

    def dma(self, q, out, in_, R=(), W=(), slow=False):
        j = self.dma_rr[q]
        self.dma_rr[q] = (j + 1) % self.NDMA[q]
        key = ("d", q, j)
        waits = self._deps(q, R, W)
        prev = self.dma_tot.get(key, 0)
        if prev and self.seen[q].get(key, 0) < prev:
            self.seen[q][key] = prev
            waits.append((key, prev))
        self.dma_tot[key] = prev + 16
        ticket = (key, prev + 16)
        self.n[q] += 1
        if slow:
            fn = lambda e, o=out, i=in_: e.dma_start(out=o, in_=i, allow_slow_non_contiguous=True)
        else:
            fn = lambda e, o=out, i=in_: e.dma_start(out=o, in_=i)
        self.prog[q].append((waits, fn, (q, self.n[q]), key))
        self._commit(ticket, R, W)

    def emit(self, block, final_waits):
        nc = self.nc
        sems = {e: nc.alloc_semaphore("s_" + e) for e in self.ENG}
        for key in self.dma_tot:
            sems[key] = nc.alloc_semaphore("d_%s%d" % (key[1], key[2]))
        rank = {}
        for e in self.ENG:
            rank[e] = {v: i + 1 for i, v in enumerate(sorted(self.signaled[e]))}

        def val(k, v):
            return rank[k][v] if k in rank else v

        def run(ename, eng):
            for waits, fn, ticket, dkey in self.prog[ename]:
                for k, v in waits:
                    eng.wait_ge(sems[k], val(k, v))
                ins = fn(eng)
                if dkey is not None:
                    ins.then_inc(sems[dkey], 16)
                elif ticket[1] in self.signaled[ename]:
                    ins.then_inc(sems[ename], 1)
            if ename == "sp":
                for k, v in final_waits:
                    eng.wait_ge(sems[k], val(k, v))

        for k, v in final_waits:
            if k in self.signaled:
                self.signaled[k].add(v)
        for e in self.ENG:
            rank[e] = {v: i + 1 for i, v in enumerate(sorted(self.signaled[e]))}
        block.tensor(lambda e: run("pe", e))
        block.scalar(lambda e: run("act", e))
        block.vector(lambda e: run("dve", e))
        block.gpsimd(lambda e: run("pool", e))
        block.sync(lambda e: run("sp", e))


class Arena:
    def __init__(self, nc, nbytes, S=None):
        self.S = S
        self.t = nc.alloc_sbuf_tensor("arena", [128, nbytes // 4], F32)
        self.nbytes = nbytes
        self.top = 0
        self.peak = 0

    def alloc(self, free_shape, dtype):
        esz = 2 if dtype == BF16 else 4
        n = int(np.prod(free_shape))
        nb = (n * esz + 63) // 64 * 64
        off = self.top
        self.top += nb
        self.peak = max(self.peak, self.top)
        assert self.top <= self.nbytes, ("arena overflow", self.top, self.nbytes)
        ap = self.t[:, off // 4:(off + nb) // 4]
        if dtype != F32:
            ap = ap.bitcast(dtype)
        ap = ap[:, 0:n]
        if len(free_shape) == 2:
            ap = ap.rearrange("p (a b) -> p a b", b=free_shape[1])
        elif len(free_shape) == 3:
            ap = ap.rearrange("p (a b c) -> p a b c", b=free_shape[1], c=free_shape[2])
        return ap

    def mark(self):
        return self.top

    def release(self, m):
        self.top = m
        self.S.fence()


D = 1024
NT = 18
NL = 16
DEPTH = 4
DFF = 2816
DFE = 3584
NE = 8
ALPHA = float((2 * DEPTH) ** 0.25)
NEG = -1.0e30
SLAB = 512


class B:
    def __init__(self, layers, dbg=None):
        self.layers = layers
        self.dbg = dbg or {}
        nc = self.nc = bass.Bass("TRN2", target_bir_lowering=False)
        self.S = Sched(nc)
        self.A = Arena(nc, 212800, self.S)
        self.fin = []
        self.din = {}
        self.psall = nc.alloc_psum_tensor("psall", [128, 8 * 512], F32)
        self.ps = [self.psall[:, i * 512:(i + 1) * 512] for i in range(8)]
        self.tp = [Tk() for _ in range(8)]

    SHAPES = {
        "hx": ([NT * 128, D], F32), "cv": ([128, 8, 2], F32), "c_ident_f": ([128, 128], F32), "c_ident_b": ([128, 128], BF16),
        "c_rope": ([128, NT, 96], F32), "c_tri": ([128, 2, 128], BF16), "c_namask": ([128, 2688], BF16), "c_tau": ([128, 130], F32),
        "ada_w": ([DEPTH, D, 6 * D], F32), "ada_b": ([DEPTH, 6 * D], F32), "w_in": ([DEPTH, D, 2048], F32), "w_out": ([DEPTH, D, D], F32),
        "rpbh": ([DEPTH, 4, 18, 128], F32), "gqk": ([DEPTH, 2, 64], F32), "ssm_vec": ([DEPTH, 128, 16, 3], F32),
        "ssm_bs": ([DEPTH, 128, 16, 2, 16], F32), "ssm_cs": ([DEPTH, 128, 16, 2, 32], F32), "ssm_dd": ([DEPTH, 128, 8, 32], F32),
        "ssm_wglu": ([DEPTH, 256, 256], F32), "sink": ([DEPTH, 4], F32), "lngb": ([DEPTH, 4, D], F32),
        "ffn_g": ([2, D, DFF], F32), "ffn_u": ([2, D, DFF], F32), "ffn_d": ([2, DFF, D], F32),
        "moe_r": ([2, D, NE], F32), "moe_rb": ([2, NE], F32), "moe_g": ([2, NE, D, DFE], F32), "moe_u": ([2, NE, D, DFE], F32),
        "moe_d": ([2, NE, DFE, D], F32),
    }

    def __getattr__(self, name):
        sh = B.SHAPES.get(name)
        if sh is None:
            raise AttributeError(name)
        ap = self.inp(name, sh[0], sh[1])
        self.__dict__[name] = ap
        return ap

    def dt_(self, name):
        getattr(self, name)
        return self.din[name]

    def inp(self, name, shape, dt=F32):
        t = self.nc.dram_tensor(name, list(shape), dt, kind="ExternalInput")
        self.din[name] = t
        return t.ap()

    def outp(self, name, shape, dt=F32):
        return self.nc.dram_tensor(name, list(shape), dt, kind="ExternalOutput").ap()

    def store(self, dst, src, R):
        S = self.S
        S.dma("sp", dst, src, R=R)
        j = (S.dma_rr["sp"] - 1) % S.NDMA["sp"]
        key = ("d", "sp", j)
        self.fin.append((key, S.dma_tot[key]))

    def pe(self, fn, R=(), W=()): self.S.op("pe", fn, R, W)
    def act(self, fn, R=(), W=()): self.S.op("act", fn, R, W)
    def dve(self, fn, R=(), W=()): self.S.op("dve", fn, R, W)
    def pool(self, fn, R=(), W=()): self.S.op("pool", fn, R, W)

    def mm(self, out, lhsT, rhs, start, stop, R, W):
        self.pe(lambda e: e.matmul(out, lhsT=lhsT, rhs=rhs, start=start, stop=stop), R, W)

    def tr(self, out, in_, R, W):
        self.pe(lambda e: e.transpose(out=out, in_=in_, identity=self.ident_f), R + [self.tconst], W)

    def rstd_from(self, out, var_ap, scale, eps, R, tk):
        self.act(lambda e: e.activation(out=out, in_=var_ap, func=AF.Sqrt, bias=self.eps_ap(eps), scale=scale), R, [tk])
        self.dve(lambda e: e.reciprocal(out=out, in_=out), [tk], [tk])

    def eps_ap(self, eps):
        return self.epsc[:, 0:1]

    def setup(self):
        A = self.A
        inp = self.inp
        self.out = self.outp("out", [NL * 128, D])

        S = self.S
        self.tconst = Tk()
        self.H = A.alloc([NT, D], F32)
        self.tH = [Tk() for _ in range(NT)]
        self.ident_f = A.alloc([128], F32)
        self.ident_b = A.alloc([128], BF16)
        self.epsc = A.alloc([2], F32)
        self.csil = A.alloc([8, 2], F32)
        self.modc = A.alloc([DEPTH, 32, 2], F32)
        self.tmodc = Tk()
        for dst, src in ((self.ident_f, self.c_ident_f), (self.ident_b, self.c_ident_b), (self.csil, self.cv)):
            S.dma("sp", dst, src, W=[self.tconst])
        self.dve(lambda e: e.memset(self.epsc, 1e-6), W=[self.tconst])
        for i in range(NT):
            S.dma("sp" if i % 2 == 0 else "act", self.H[:, i, :], self.hx[i * 128:(i + 1) * 128, :], W=[self.tH[i]])
        self.act(lambda e: e.activation(out=self.csil, in_=self.csil, func=AF.Silu), [self.tconst], [self.tconst])

    def ada_cols(self, l):
        A, S = self.A, self.S
        m = A.mark()
        blocks = [0, 1, 3, 4]
        wst = [A.alloc([8, 128], F32) for _ in range(3)]
        tw = [Tk() for _ in range(3)]
        bcol = A.alloc([32], F32)
        tb = Tk()
        for j, blk in enumerate(blocks):
            S.dma("sp", bcol[:, j * 8:(j + 1) * 8], self.ada_b[l, blk * D:(blk + 1) * D].rearrange("(kc p) -> p kc", p=128), W=[tb], slow=True)
        n = 0
        for j, blk in enumerate(blocks):
            for fc in range(8):
                b = n % 3
                col0 = blk * D + fc * 128
                S.dma("sp" if n % 2 == 0 else "act", wst[b], self.ada_w[l, :, col0:col0 + 128].rearrange("(kc p) n -> p kc n", p=128), W=[tw[b]])
                pb = 7
                for kc in range(8):
                    self.mm(self.ps[pb][:, 0:2], wst[b][:, kc, :], self.csil[:, kc, :], kc == 0, kc == 7, [tw[b], self.tconst], [self.tp[pb]])
                idx = j * 8 + fc
                add = 1.0 if blk in (1, 4) else 0.0
                self.dve(lambda e, idx=idx, add=add, pb=pb: e.tensor_scalar(out=self.modc[:, l, idx, :], in0=self.ps[pb][:, 0:2], scalar1=bcol[:, idx:idx + 1],
                                                                           scalar2=add, op0=ALU.add, op1=ALU.add), [self.tp[pb], tb], [self.tmodc])
                n += 1
        A.release(m)

    def ada_gate(self, l, which, G, tG):
        A, S = self.A, self.S
        m = A.mark()
        blk = 2 if which == 0 else 5
        crep = A.alloc([2, 8, 128], F32); tcr = Tk()
        for v in range(2):
            for kc in range(8):
                self.dve(lambda e, v=v, kc=kc: e.tensor_copy(out=crep[:, v, kc, :], in_=self.csil[:, kc, v:v + 1].to_broadcast([128, 128])),
                         [self.tconst], [tcr])
        wst = [A.alloc([8, 512], F32) for _ in range(2)]
        tw = [Tk() for _ in range(2)]
        bb = A.alloc([D], F32)
        tb = Tk()
        S.dma("sp", bb, self.ada_b[l:l + 1, blk * D:(blk + 1) * D].partition_broadcast(128) if False else
              bass.AP(self.dt_("ada_b"), l * 6 * D + blk * D, [[0, 128], [1, D]]), W=[tb])
        for nb in range(2):
            col0 = blk * D + nb * 512
            S.dma("sp", wst[nb], self.ada_w[l, :, col0:col0 + 512].rearrange("(kc p) n -> p kc n", p=128), W=[tw[nb]])
            for v in range(2):
                pb = 5 + v
                for kc in range(8):
                    self.mm(self.ps[pb][:, :], crep[:, v, kc, :], wst[nb][:, kc, :], kc == 0, kc == 7, [tw[nb], tcr], [self.tp[pb]])
                self.dve(lambda e, v=v, nb=nb, pb=pb: e.tensor_tensor(out=G[:, v, nb * 512:(nb + 1) * 512], in0=self.ps[pb][:, :], in1=bb[:, nb * 512:(nb + 1) * 512], op=ALU.add),
                         [self.tp[pb], tb], [tG])
        A.release(m)

    def load_ln(self, l, which, LN, tLN):
        for j in range(2):
            self.S.dma("sp", LN[:, j, :], bass.AP(self.dt_("lngb"), (l * 4 + which * 2 + j) * D, [[0, 128], [1, D]]), W=[tLN])

    def make_aT(self, l, i, which, aT, taT, aT32=None, taT32=None):
        v = 1 if i >= NL else 0
        for half in range(2):
            pb = 5 + half
            for q in range(4):
                kc = half * 4 + q
                self.tr(self.ps[pb][:, q * 128:(q + 1) * 128], self.H[:, i, kc * 128:(kc + 1) * 128], [self.tH[i]], [self.tp[pb]])
            for q in range(4):
                kc = half * 4 + q
                sc = self.modc[:, l, (which * 2 + 1) * 8 + kc, v:v + 1]
                sh = self.modc[:, l, (which * 2) * 8 + kc, v:v + 1]
                self.act(lambda e, kc=kc, q=q, pb=pb, sc=sc, sh=sh: e.activation(out=aT[:, kc, :], in_=self.ps[pb][:, q * 128:(q + 1) * 128], func=AF.Identity, bias=sh, scale=sc),
                         [self.tp[pb], self.tmodc], [taT])
                if aT32 is not None:
                    self.act(lambda e, kc=kc, q=q, pb=pb, sc=sc, sh=sh: e.activation(out=aT32[:, kc, :], in_=self.ps[pb][:, q * 128:(q + 1) * 128], func=AF.Identity, bias=sh, scale=sc),
                             [self.tp[pb], self.tmodc], [taT32])

    def resid_ln(self, i, ys, G, tG, LN, tLN, tmp, ttmp, st, tst):
        v = 1 if i >= NL else 0
        for hf, (yap, ty) in enumerate(ys):
            self.dve(lambda e, hf=hf, yap=yap: e.tensor_tensor(out=tmp[:, hf * 512:(hf + 1) * 512], in0=yap, in1=G[:, v, hf * 512:(hf + 1) * 512], op=ALU.mult),
                     [ty, tG], [ttmp])
        self.ln_tail(i, tmp, ttmp, LN, tLN, st, tst)

    def ln_tail(self, i, tmp, ttmp, LN, tLN, st, tst):
        self.dve(lambda e: e.scalar_tensor_tensor(out=tmp, in0=self.H[:, i, :], scalar=ALPHA, in1=tmp, op0=ALU.mult, op1=ALU.add), [self.tH[i], ttmp], [ttmp])
        for hf in range(2):
            self.dve(lambda e, hf=hf: e.bn_stats(out=st[:, hf * 6:(hf + 1) * 6], in_=tmp[:, hf * 512:(hf + 1) * 512]), [ttmp], [tst])
        self.dve(lambda e: e.bn_aggr(out=st[:, 12:14], in_=st[:, 0:12]), [tst], [tst])
        self.rstd_from(st[:, 14:15], st[:, 13:14], 1.0, 1e-6, [tst], tst)
        self.dve(lambda e: e.tensor_scalar(out=tmp, in0=tmp, scalar1=st[:, 12:13], scalar2=st[:, 14:15], op0=ALU.subtract, op1=ALU.mult), [ttmp, tst], [ttmp])
        self.pool(lambda e: e.tensor_tensor(out=tmp, in0=tmp, in1=LN[:, 0, :], op=ALU.mult), [ttmp, tLN], [ttmp])
        self.pool(lambda e: e.tensor_tensor(out=self.H[:, i, :], in0=tmp, in1=LN[:, 1, :], op=ALU.add), [ttmp, tLN], [self.tH[i]])

    def ffn_phase(self, l):
        A, S = self.A, self.S
        last = (l == DEPTH - 1)
        nt = NL if last else NT
        moe = (l % 2 == 1)
        li = l // 2
        m = A.mark()
        G = A.alloc([2, D], F32); tG = Tk()
        LN = A.alloc([2, D], F32); tLN = Tk()
        self.ada_gate(l, 1, G, tG)
        self.load_ln(l, 1, LN, tLN)
        FT = A.alloc([8, nt * 128], BF16)
        tFT = [Tk() for _ in range(nt)]
        moe_ = (l % 2 == 1)
        if moe_:
            gate = A.alloc([nt, NE], F32); tgate = Tk()
            a32 = A.alloc([8, 128], F32); ta32 = Tk()
            rt = self.router_setup(l // 2)
        for i in range(nt):
            if moe_:
                self.make_aT(l, i, 1, FT[:, :, i * 128:(i + 1) * 128], tFT[i], a32, ta32)
                self.router_tile(rt, i, a32, ta32, gate, tgate)
            else:
                self.make_aT(l, i, 1, FT[:, :, i * 128:(i + 1) * 128], tFT[i])
        experts = range(NE) if moe else [0]
        dff = DFE if moe else DFF
        nsl = dff // SLAB + (1 if dff % SLAB else 0)
        facc = A.alloc([nt, D], F32) if False else None
        tmp = A.alloc([D], F32); ttmp = Tk()
        st = A.alloc([16], F32); tst = Tk()
        for i in range(nt):
            self.pool(lambda e, i=i: e.tensor_scalar(out=self.H[:, i, :], in0=self.H[:, i, :], scalar1=ALPHA, scalar2=None, op0=ALU.mult), [self.tH[i]], [self.tH[i]])
        NB = 2
        wg = [A.alloc([8, SLAB], BF16) for _ in range(NB)]
        wu = [A.alloc([8, SLAB], BF16) for _ in range(NB)]
        wd = [A.alloc([SLAB // 128, D], BF16) for _ in range(NB)]
        tw = [Tk() for _ in range(NB)]
        twu = [Tk() for _ in range(NB)]
        twd = [Tk() for _ in range(NB)]
        h1 = [A.alloc([SLAB // 128, 512], BF16) for _ in range(2)]
        th1 = [Tk() for _ in range(2)]
        sg = [A.alloc([512], F32) for _ in range(2)]
        tsg = [Tk() for _ in range(2)]
        yt = [A.alloc([D], F32)] * 2
        tyt = [Tk()] * 2
        nblk = (nt * 128 + 511) // 512
        cnt = 0
        hcnt = 0
        items = [(e_, s) for e_ in experts for s in range(nsl)]

        def issue(k):
            e_, s = items[k]
            Wg = self.moe_g[li, e_] if moe else self.ffn_g[li]
            Wu = self.moe_u[li, e_] if moe else self.ffn_u[li]
            Wd = self.moe_d[li, e_] if moe else self.ffn_d[li]
            b = k % NB
            c0 = s * SLAB
            w = min(SLAB, dff - c0)
            nch = w // 128
            S.dma("pool", wg[b][:, :, 0:w], Wg[:, c0:c0 + w].rearrange("(kc p) n -> p kc n", p=128), W=[tw[b]])
            S.dma("pool", wu[b][:, :, 0:w], Wu[:, c0:c0 + w].rearrange("(kc p) n -> p kc n", p=128), W=[twu[b]])
            S.dma("pool", wd[b][:, 0:nch, :], Wd[c0:c0 + w, :].rearrange("(fc p) n -> p fc n", p=128), W=[twd[b]])

        issue(0)
        for k, (e_, s) in enumerate(items):
            if True:
                if k + 1 < len(items):
                    issue(k + 1)
                b = k % NB
                c0 = s * SLAB
                w = min(SLAB, dff - c0)
                nch = w // 128
                def gu(tb, hb, b=b, nch=nch):
                    t0 = tb * 512
                    ntok = min(512, nt * 128 - t0)
                    tiles = list(range(t0 // 128, (t0 + ntok) // 128))
                    for fc in range(nch):
                        pg, pu = 0 + (fc % 2) * 2, 1 + (fc % 2) * 2
                        for kc in range(8):
                            self.mm(self.ps[pg][:, 0:ntok], wg[b][:, kc, fc * 128:(fc + 1) * 128], FT[:, kc, t0:t0 + ntok], kc == 0, kc == 7,
                                    [tw[b]] + [tFT[i] for i in tiles], [self.tp[pg]])
                        for kc in range(8):
                            self.mm(self.ps[pu][:, 0:ntok], wu[b][:, kc, fc * 128:(fc + 1) * 128], FT[:, kc, t0:t0 + ntok], kc == 0, kc == 7,
                                    [twu[b]] + [tFT[i] for i in tiles], [self.tp[pu]])
                        sb = fc % 2
                        self.act(lambda e, pg=pg, sb=sb, ntok=ntok: e.activation(out=sg[sb][:, 0:ntok], in_=self.ps[pg][:, 0:ntok], func=AF.Silu), [self.tp[pg]], [tsg[sb]])
                        self.dve(lambda e, pu=pu, sb=sb, hb=hb, fc=fc, ntok=ntok: e.tensor_tensor(out=h1[hb][:, fc, 0:ntok], in0=self.ps[pu][:, 0:ntok], in1=sg[sb][:, 0:ntok], op=ALU.mult),
                                 [self.tp[pu], tsg[sb]], [th1[hb]])

                def down(tb, hb, b=b, nch=nch, e_=e_):
                    t0 = tb * 512
                    ntok = min(512, nt * 128 - t0)
                    tiles = list(range(t0 // 128, (t0 + ntok) // 128))
                    for ti, i in enumerate(tiles):
                        v = 1 if i >= NL else 0
                        yb = i % 2
                        for hf in range(2):
                            pb = 4 + hf + 2 * (i % 2)
                            for fc in range(nch):
                                self.mm(self.ps[pb][:, :], h1[hb][:, fc, ti * 128:(ti + 1) * 128], wd[b][:, fc, hf * 512:(hf + 1) * 512], fc == 0, fc == nch - 1,
                                        [th1[hb], twd[b]], [self.tp[pb]])
                            if moe:
                                self.act(lambda e, pb=pb, hf=hf, yb=yb, i=i, e_=e_: e.activation(out=yt[yb][:, hf * 512:(hf + 1) * 512], in_=self.ps[pb][:, :], func=AF.Copy, scale=gate[:, i, e_:e_ + 1]),
                                         [self.tp[pb], tgate], [tyt[yb]])
                            else:
                                self.dve(lambda e, pb=pb, hf=hf, v=v, yb=yb: e.tensor_tensor(out=yt[yb][:, hf * 512:(hf + 1) * 512], in0=self.ps[pb][:, :], in1=G[:, v, hf * 512:(hf + 1) * 512], op=ALU.mult),
                                         [self.tp[pb], tG], [tyt[yb]])
                        if moe:
                            self.dve(lambda e, v=v, yb=yb: e.tensor_tensor(out=yt[yb], in0=yt[yb], in1=G[:, v, :], op=ALU.mult), [tyt[yb], tG], [tyt[yb]])
                        self.pool(lambda e, i=i, yb=yb: e.tensor_tensor(out=self.H[:, i, :], in0=yt[yb], in1=self.H[:, i, :], op=ALU.add), [tyt[yb], self.tH[i]], [self.tH[i]])

                hbs = []
                for tb in range(nblk):
                    hbs.append(hcnt % 2)
                    hcnt += 1
                gu(0, hbs[0])
                for tb in range(nblk):
                    if tb + 1 < nblk:
                        gu(tb + 1, hbs[tb + 1])
                    down(tb, hbs[tb])
        for i in range(nt):
            self.ln_only(i, LN, tLN, tmp, ttmp, st, tst)
        A.release(m)

    def ln_only(self, i, LN, tLN, tmp, ttmp, st, tst):
        for hf in range(2):
            self.dve(lambda e, hf=hf: e.bn_stats(out=st[:, hf * 6:(hf + 1) * 6], in_=self.H[:, i, hf * 512:(hf + 1) * 512]), [self.tH[i]], [tst])
        self.dve(lambda e: e.bn_aggr(out=st[:, 12:14], in_=st[:, 0:12]), [tst], [tst])
        self.rstd_from(st[:, 14:15], st[:, 13:14], 1.0, 1e-6, [tst], tst)
        self.dve(lambda e: e.tensor_scalar(out=tmp, in0=self.H[:, i, :], scalar1=st[:, 12:13], scalar2=st[:, 14:15], op0=ALU.subtract, op1=ALU.mult), [self.tH[i], tst], [ttmp])
        self.pool(lambda e: e.tensor_tensor(out=tmp, in0=tmp, in1=LN[:, 0, :], op=ALU.mult), [ttmp, tLN], [ttmp])
        self.pool(lambda e: e.tensor_tensor(out=self.H[:, i, :], in0=tmp, in1=LN[:, 1, :], op=ALU.add), [ttmp, tLN], [self.tH[i]])

    def router_setup(self, li):
        A, S = self.A, self.S
        wr = A.alloc([8, NE], F32); twr = Tk()
        rb = A.alloc([NE], F32)
        S.dma("sp", wr, self.moe_r[li].rearrange("(kc p) n -> p kc n", p=128), W=[twr])
        S.dma("sp", rb, bass.AP(self.dt_("moe_rb"), li * NE, [[0, 128], [1, NE]]), W=[twr])
        return dict(wr=wr, twr=twr, rb=rb, lg=A.alloc([NE], F32), tlg=Tk(), m8=A.alloc([8], F32), wk=A.alloc([2, NE], F32))

    def router_tile(self, rt, i, a32, ta32, gate, tgate):
        wr, twr, rb, lg, tlg, m8, wk = rt["wr"], rt["twr"], rt["rb"], rt["lg"], rt["tlg"], rt["m8"], rt["wk"]
        pb = 7
        for kc in range(8):
            self.mm(self.ps[pb][:, 0:NE], a32[:, kc, :], wr[:, kc, :], kc == 0, kc == 7, [ta32, twr], [self.tp[pb]])
        self.dve(lambda e: e.tensor_tensor(out=lg, in0=self.ps[pb][:, 0:NE], in1=rb, op=ALU.add), [self.tp[pb], twr], [tlg])
        self.dve(lambda e: e.max(out=m8, in_=lg), [tlg], [tlg])
        self.dve(lambda e: e.tensor_scalar(out=wk[:, 0, :], in0=lg, scalar1=m8[:, 1:2], scalar2=None, op0=ALU.is_ge), [tlg], [tlg])
        self.dve(lambda e: e.tensor_scalar(out=wk[:, 1, :], in0=lg, scalar1=m8[:, 0:1], scalar2=None, op0=ALU.subtract), [tlg], [tlg])
        self.act(lambda e: e.activation(out=wk[:, 1, :], in_=wk[:, 1, :], func=AF.Exp), [tlg], [tlg])
        self.dve(lambda e: e.tensor_tensor(out=wk[:, 1, :], in0=wk[:, 1, :], in1=wk[:, 0, :], op=ALU.mult), [tlg], [tlg])
        self.dve(lambda e: e.reduce_sum(out=m8[:, 2:3], in_=wk[:, 1, :], axis=AX.X), [tlg], [tlg])
        self.dve(lambda e: e.reciprocal(out=m8[:, 2:3], in_=m8[:, 2:3]), [tlg], [tlg])
        self.dve(lambda e: e.tensor_scalar(out=gate[:, i, :], in0=wk[:, 1, :], scalar1=m8[:, 2:3], scalar2=None, op0=ALU.mult), [tlg], [tgate])


    def rms_rope(self, src, tsrc, nh, dst, tdst, tile, rw, g=None, perm=False, qs=1.0):
        rope, trope = self.rope, self.tmc
        s3 = src.rearrange("p (h d) -> p h d", d=64)
        x, ss, t, tw = rw["x"], rw["ss"], rw["t"], rw["tw"]
        x3 = x[:, 0:nh * 64].rearrange("p (h d) -> p h d", d=64)
        if g is not None:
            for h in range(nh):
                self.act(lambda e, h=h: e.activation(out=x3[:, h, :], in_=s3[:, h, :], func=AF.Square, accum_out=ss[:, h:h + 1]), [tsrc], [tw])
            self.act(lambda e: e.activation(out=ss[:, 0:nh], in_=ss[:, 0:nh], func=AF.Sqrt, bias=self.epsc[:, 0:1], scale=1.0 / 64), [tw, self.tconst], [tw])
            self.dve(lambda e: e.reciprocal(out=ss[:, 0:nh], in_=ss[:, 0:nh]), [tw], [tw])
            for h in range(nh):
                self.dve(lambda e, h=h: e.scalar_tensor_tensor(out=x3[:, h, :], in0=s3[:, h, :], scalar=ss[:, h:h + 1], in1=g, op0=ALU.mult, op1=ALU.mult), [tsrc, tw, self.tmc], [tw])
            cur, tcur, qs = x3, tw, 1.0
        else:
            cur, tcur = s3, tsrc
        C = rope[:, tile, 0:32].unsqueeze(1).unsqueeze(1).to_broadcast([128, nh, 2, 32])
        Sg = rope[:, tile, 32:96].rearrange("p (a d) -> p a d", d=32).unsqueeze(1).to_broadcast([128, nh, 2, 32])
        c4 = cur.rearrange("p h (a d) -> p h a d", d=32)
        sw = c4[:, :, ::-1, :]
        t1 = t[:, 0, 0:nh * 64].rearrange("p (h a d) -> p h a d", a=2, d=32)
        t2 = t[:, 1, 0:nh * 64].rearrange("p (h a d) -> p h a d", a=2, d=32)
        if qs != 1.0:
            self.dve(lambda e: e.scalar_tensor_tensor(out=t1, in0=c4, scalar=qs, in1=C, op0=ALU.mult, op1=ALU.mult), [tcur, trope], [tw])
            self.dve(lambda e: e.scalar_tensor_tensor(out=t2, in0=sw, scalar=qs, in1=Sg, op0=ALU.mult, op1=ALU.mult), [tcur, trope], [tw])
        else:
            self.dve(lambda e: e.tensor_tensor(out=t1, in0=c4, in1=C, op=ALU.mult), [tcur, trope], [tw])
            self.dve(lambda e: e.tensor_tensor(out=t2, in0=sw, in1=Sg, op=ALU.mult), [tcur, trope], [tw])
        f1 = t[:, 0, 0:nh * 64].rearrange("p (h d) -> p h d", d=64)
        f2 = t[:, 1, 0:nh * 64].rearrange("p (h d) -> p h d", d=64)
        if perm:
            dv = dst.rearrange("p (b s d) -> p s b d", b=2, s=2, d=64)
            f1 = f1.rearrange("p (s b) d -> p s b d", b=2)
            f2 = f2.rearrange("p (s b) d -> p s b d", b=2)
        else:
            dv = dst.rearrange("p (h d) -> p h d", d=64)
        self.dve(lambda e: e.tensor_tensor(out=dv, in0=f1, in1=f2, op=ALU.add), [tw], [tdst])

    def mixer_phase(self, l):
        A, S = self.A, self.S
        last = (l == DEPTH - 1)
        nq = NL if last else NT
        m_all = A.mark()
        self.rope = A.alloc([NT, 96], F32)
        gqk = A.alloc([2, 64], F32)
        self.tmc = Tk()
        S.dma("sp", self.rope, self.c_rope, W=[self.tmc])
        S.dma("sp", gqk, bass.AP(self.dt_("gqk"), l * 128, [[0, 128], [1, 128]]), W=[self.tmc])
        OC = A.alloc([NT, 256], BF16); tOC = [Tk() for _ in range(NT)]
        m_s5 = A.mark()
        UT = A.alloc([2, NT * 128], BF16); tUT = [Tk() for _ in range(NT)]
        m0 = A.mark()
        aT = [A.alloc([8, 128], BF16) for _ in range(2)]; taT = [Tk() for _ in range(2)]
        Wu_ = A.alloc([8, 256], BF16); tWu = Tk()
        S.dma("pool", Wu_, self.w_in[l, :, 1280:1536].rearrange("(kc p) n -> p kc n", p=128), W=[tWu])
        for i in range(NT):
            b = i % 2
            self.make_aT(l, i, 0, aT[b], taT[b])
            for c in range(2):
                for kc in range(8):
                    self.mm(self.ps[2 + b][:, c * 128:(c + 1) * 128], Wu_[:, kc, c * 128:(c + 1) * 128], aT[b][:, kc, :], kc == 0, kc == 7, [taT[b], tWu], [self.tp[2 + b]])
            self.act(lambda e, i=i, b=b: e.activation(out=UT[:, :, i * 128:(i + 1) * 128], in_=self.ps[2 + b][:, 0:256].rearrange("p (c t) -> p c t", t=128), func=AF.Copy), [self.tp[2 + b]], [tUT[i]])
        A.release(m0)
        self.s5_phase(l, UT, tUT, OC, tOC, nq)
        A.release(m_s5)
        KT = A.alloc([4, NT * 128], BF16); tKT = [Tk() for _ in range(NT)]
        V = A.alloc([NT, 512], BF16); tV = [Tk() for _ in range(NT)]
        m0 = A.mark()
        rw = dict(x=A.alloc([256], F32), ss=A.alloc([4], F32), t=A.alloc([2, 256], F32), tw=Tk())
        aT = [A.alloc([8, 128], BF16) for _ in range(2)]; taT = [Tk() for _ in range(2)]
        W = A.alloc([8, 1024], BF16); tW = [Tk() for _ in range(6)]
        srcs = [(256, 256), (1024, 128), (1792, 128), (512, 256), (1152, 128), (1920, 128)]
        o = 0
        for k, (c0, w) in enumerate(srcs):
            S.dma("pool", W[:, :, o:o + w], self.w_in[l, :, c0:c0 + w].rearrange("(kc p) n -> p kc n", p=128), W=[tW[k]])
            o += w
        kbf = A.alloc([512], BF16); tkb = Tk()
        for i in range(NT):
            b = i % 2
            self.make_aT(l, i, 0, aT[b], taT[b])
            for kc in range(8):
                self.mm(self.ps[0][:, :], aT[b][:, kc, :], W[:, kc, 0:512], kc == 0, kc == 7, [taT[b]] + tW[0:3], [self.tp[0]])
            for kc in range(8):
                self.mm(self.ps[1][:, :], aT[b][:, kc, :], W[:, kc, 512:1024], kc == 0, kc == 7, [taT[b]] + tW[3:6], [self.tp[1]])
            self.act(lambda e, i=i: e.activation(out=V[:, i, :], in_=self.ps[1][:, :], func=AF.Copy), [self.tp[1]], [tV[i]])
            self.act(lambda e: e.activation(out=kbf[:, 0:256], in_=self.ps[0][:, 0:256], func=AF.Copy), [self.tp[0]], [tkb])
            self.rms_rope(self.ps[0][:, 256:384], self.tp[0], 2, kbf[:, 256:384], tkb, i, rw, g=gqk[:, 1, :])
            self.rms_rope(self.ps[0][:, 384:512], self.tp[0], 2, kbf[:, 384:512], tkb, i, rw)
            for c in range(4):
                self.mm(self.ps[3][:, c * 128:(c + 1) * 128], kbf[:, c * 128:(c + 1) * 128], self.ident_b, True, True, [tkb, self.tconst], [self.tp[3]])
            self.dve(lambda e, i=i: e.tensor_copy(out=KT[:, :, i * 128:(i + 1) * 128], in_=self.ps[3][:, :].rearrange("p (c t) -> p c t", t=128)), [self.tp[3]], [tKT[i]])
        A.release(m0)
        rw = dict(x=A.alloc([256], F32), ss=A.alloc([4], F32), t=A.alloc([2, 256], F32), tw=Tk())
        aT = [A.alloc([8, 128], BF16)] * 2; taT = [Tk()] * 2
        tri = A.alloc([2, 128], BF16); namask = A.alloc([2688], BF16); tmk = Tk()
        S.dma("sp", tri, self.c_tri, W=[tmk]); S.dma("sp", namask, self.c_namask, W=[tmk])
        G = A.alloc([2, D], F32); tG = Tk()
        LN = A.alloc([2, D], F32); tLN = Tk()
        self.ada_gate(l, 0, G, tG)
        self.load_ln(l, 0, LN, tLN)
        Wq = A.alloc([8, 768], BF16); tWq = [Tk() for _ in range(3)]
        for k, c0 in enumerate((0, 768, 1536)):
            S.dma("pool", Wq[:, :, k * 256:(k + 1) * 256], self.w_in[l, :, c0:c0 + 256].rearrange("(kc p) n -> p kc n", p=128), W=[tWq[k]])
        Wo = A.alloc([8, D], BF16); tWo = Tk()
        S.dma("pool", Wo, self.w_out[l].rearrange("(kc p) n -> p kc n", p=128), W=[tWo])
        Traw = A.alloc([4, 14, 64], BF16); tTr = Tk()
        mh = A.mark()
        hk = [A.alloc([16, 64], F32) for _ in range(2)]; thk = [Tk() for _ in range(2)]
        for h in range(4):
            for rl in range(2):
                S.dma("sp", hk[h % 2][rl * 64:(rl + 1) * 64, :, :], bass.AP(self.dt_("rpbh"), ((l * 4 + h) * 18 + (1 - rl)) * 128, [[1, 64], [128, 16], [1, 64]]), W=[thk[h % 2]])
            self.dve(lambda e, h=h: e.tensor_copy(out=Traw[:, h, :, :], in_=hk[h % 2][:, 1:15, ::-1]), [thk[h % 2]], [tTr])
        A.release(mh)
        sk = A.alloc([8], F32); tsk = Tk()
        S.dma("sp", sk[:, 0:4], bass.AP(self.dt_("sink"), l * 4, [[0, 128], [1, 4]]), W=[tsk])
        self.dve(lambda e: e.tensor_scalar(out=sk[:, 4:8], in0=sk[:, 0:4], scalar1=-1.0, scalar2=None, op0=ALU.mult), [tsk], [tsk])
        qbf = A.alloc([768], BF16); tqb = Tk()
        qT = A.alloc([6, 128], BF16); tqT = Tk()
        Ps = [A.alloc([NT * 128], BF16) for _ in range(2)]; tPs = [Tk() for _ in range(2)]
        P, tP = Ps[0], tPs[0]
        PT = [A.alloc([512], BF16) for _ in range(2)]; tPT = [Tk() for _ in range(2)]
        cc = A.alloc([D], BF16); tcc = Tk()
        ccT = A.alloc([8, 128], BF16); tccT = Tk()
        tmp = P[:, 0:2 * D].bitcast(F32); ttmp = tP
        st = A.alloc([16], F32); tst = Tk()
        sms = [A.alloc([16], F32) for _ in range(4)]; tsms = [Tk() for _ in range(4)]
        print('M2 arena top', A.top)
        ocnt = [0]
        acnt = [0]

        jobs = []

        def attention(*a, **kw):
            jobs.append((a, kw, {}))

        def att1(ctx_, i, blk, pb0, segs, vcol, oc0, sc, bias_h=None, s0=0, sink_h=None):
            nseg = len(segs)
            nb = (nseg + 3) // 4
            acnt[0] += 1
            sm, tsm = sms[acnt[0] % 4], tsms[acnt[0] % 4]
            P, tP = Ps[acnt[0] % 2], tPs[acnt[0] % 2]
            ctx_.update(sm=sm, tsm=tsm, P=P, tP=tP)
            b0 = 0 if nb > 2 else 2 * (acnt[0] % 2)
            banks = list(range(b0, b0 + nb))
            tb_ = [self.tp[k] for k in banks]
            ntot = nseg * 128
            Sall = self.psall[:, b0 * 512:b0 * 512 + ntot]
            q_ap = qT[pb0:pb0 + 64, blk, :]
            for t, (kt, c, mask) in enumerate(segs):
                bk, cb = b0 + t // 4, (t % 4) * 128
                self.mm(self.ps[bk][:, cb:cb + 128], q_ap, KT[pb0:pb0 + 64, c, kt * 128:(kt + 1) * 128], True, mask is None, [tqT, tKT[kt]], [self.tp[bk]])
                if mask is not None:
                    self.mm(self.ps[bk][:, cb:cb + 128], self.ident_b, mask, False, True, [self.tconst, tmk], [self.tp[bk]])
            if bias_h is not None:
                nloc = nseg - 2
                self.dve(lambda e: e.tensor_tensor(out=self.psall[:, b0 * 512:b0 * 512 + nloc * 128], in0=self.psall[:, b0 * 512:b0 * 512 + nloc * 128],
                                                   in1=Traw[:, bias_h, s0 - 1:s0 - 1 + 2 * nloc, :].rearrange("p s k -> p (s k)"), op=ALU.add), tb_ + [tTr], tb_)
            self.dve(lambda e: e.reduce_max(out=sm[:, 9:10], in_=Sall, axis=AX.X, negate=True), tb_, [tsm])
            if sink_h is not None:
                self.dve(lambda e: e.tensor_tensor(out=sm[:, 9:10], in0=sm[:, 9:10], in1=sk[:, 4 + sink_h:5 + sink_h], op=ALU.min), [tsm, tsk], [tsm])
            self.act(lambda e: e.activation(out=P[:, 0:ntot], in_=Sall, func=AF.Exp, bias=sm[:, 9:10], scale=1.0, accum_out=sm[:, 10:11]), tb_ + [tsm], [tP, tsm])
            if sink_h is not None:
                self.act(lambda e: e.activation(out=sm[:, 12:13], in_=sk[:, sink_h:sink_h + 1], func=AF.Exp, bias=sm[:, 9:10], scale=1.0), [tsk, tsm], [tsm])

        def att1b(ctx_, i, blk, pb0, segs, vcol, oc0, sc, bias_h=None, s0=0, sink_h=None):
            sm, tsm = ctx_["sm"], ctx_["tsm"]
            if sink_h is not None:
                self.dve(lambda e: e.tensor_tensor(out=sm[:, 10:11], in0=sm[:, 10:11], in1=sm[:, 12:13], op=ALU.add), [tsm], [tsm])
            self.dve(lambda e: e.reciprocal(out=sm[:, 11:12], in_=sm[:, 10:11]), [tsm], [tsm])

        def att2(ctx_, i, blk, pb0, segs, vcol, oc0, sc, bias_h=None, s0=0, sink_h=None):
            sm, tsm, P, tP = ctx_["sm"], ctx_["tsm"], ctx_["P"], ctx_["tP"]
            nseg = len(segs)
            ob = oc0 % 512
            for g0 in range(0, nseg, 4):
                gi = ocnt[0] % 2
                ocnt[0] += 1
                pt = 5 + gi
                ng = min(4, nseg - g0)
                for t in range(g0, g0 + ng):
                    self.mm(self.ps[pt][:, (t - g0) * 128:(t - g0 + 1) * 128], P[:, t * 128:(t + 1) * 128], self.ident_b, True, True, [tP, self.tconst], [self.tp[pt]])
                self.dve(lambda e, pt=pt, gi=gi, ng=ng: e.tensor_copy(out=PT[gi][:, 0:ng * 128], in_=self.ps[pt][:, 0:ng * 128]), [self.tp[pt]], [tPT[gi]])
                for t in range(g0, g0 + ng):
                    kt = segs[t][0]
                    self.mm(self.ps[7][:, ob:ob + 64], PT[gi][:, (t - g0) * 128:(t - g0 + 1) * 128], V[:, kt, vcol:vcol + 64], t == 0, t == nseg - 1, [tPT[gi], tV[kt]], [self.tp[7]])
            self.act(lambda e: e.activation(out=cc[:, oc0:oc0 + 64], in_=self.ps[7][:, ob:ob + 64], func=AF.Copy, scale=sm[:, 11:12]), [self.tp[7], tsm], [tcc])

        for i in range(nq):
            b = i % 2
            isctx = i >= NL
            self.make_aT(l, i, 0, aT[b], taT[b])
            for kc in range(8):
                self.mm(self.ps[0][:, :], aT[b][:, kc, :], Wq[:, kc, 0:512], kc == 0, kc == 7, [taT[b]] + tWq[0:2], [self.tp[0]])
            for kc in range(8):
                self.mm(self.ps[1][:, 0:256], aT[b][:, kc, :], Wq[:, kc, 512:768], kc == 0, kc == 7, [taT[b], tWq[2]], [self.tp[1]])
            self.act(lambda e: e.activation(out=qbf[:, 0:256], in_=self.ps[0][:, 0:256], func=AF.Copy), [self.tp[0]], [tqb])
            self.rms_rope(self.ps[0][:, 256:512], self.tp[0], 4, qbf[:, 256:512], tqb, i, rw, g=gqk[:, 0, :], perm=True)
            self.rms_rope(self.ps[1][:, 0:256], self.tp[1], 4, qbf[:, 512:768], tqb, i, rw, perm=True)
            for c in range(6):
                bk = 2 + c // 4
                self.mm(self.ps[bk][:, (c % 4) * 128:(c % 4 + 1) * 128], qbf[:, c * 128:(c + 1) * 128], self.ident_b, True, True, [tqb, self.tconst], [self.tp[bk]])
            self.dve(lambda e: e.tensor_scalar(out=qT[:, 0:4, :], in0=self.ps[2][:, :].rearrange("p (c t) -> p c t", t=128), scalar1=0.125, scalar2=None, op0=ALU.mult), [self.tp[2]], [tqT])
            self.dve(lambda e: e.tensor_scalar(out=qT[:, 4:6, :], in0=self.ps[3][:, 0:256].rearrange("p (c t) -> p c t", t=128), scalar1=0.125, scalar2=None, op0=ALU.mult), [self.tp[3]], [tqT])
            ctxs = [(16, None), (17, None)]
            for h in range(4):
                blk, pb0, c = h // 2, (h % 2) * 64, h // 2
                if isctx:
                    segs = [(kt, c, None) for kt, _ in ctxs]
                    attention(i, blk, pb0, segs, h * 64, h * 64, 0.125)
                else:
                    j = i
                    if 2 <= j <= 13:
                        var, t0, ntl, s0 = 0, j - 2, 5, 3
                    elif j == 0:
                        var, t0, ntl, s0 = 1, 0, 4, 7
                    elif j == 1:
                        var, t0, ntl, s0 = 2, 0, 4, 5
                    elif j == 14:
                        var, t0, ntl, s0 = 3, 12, 4, 3
                    else:
                        var, t0, ntl, s0 = 4, 12, 4, 1
                    mo = 0 if var == 0 else 640 + (var - 1) * 512
                    segs = [(t0 + k, c, namask[:, mo + k * 128:mo + (k + 1) * 128]) for k in range(ntl)] + [(kt, c, None) for kt, _ in ctxs]
                    attention(i, blk, pb0, segs, h * 64, h * 64, 1.0, bias_h=h, s0=s0)
            for h in range(4):
                blk, pb0, kvh = 2 + (h % 2), (h // 2) * 64, h // 2
                kts = [16, 17] if isctx else list(range(NT))
                attention(i, blk, pb0, [(kt, 2, None) for kt in kts], 256 + kvh * 64, 256 + h * 64, 0.125)
            self.pool(lambda e, i=i: e.tensor_copy(out=cc[:, 512:768], in_=OC[:, i, :]), [tOC[i]], [tcc])
            for h in range(4):
                blk, pb0, kvh = 4 + (h % 2), (h // 2) * 64, h // 2
                if isctx:
                    segs = [(16, 3, None), (17, 3, None)]
                else:
                    segs = []
                    if i - 1 >= 0: segs.append((i - 1, 3, tri[:, 0, :]))
                    segs.append((i, 3, None))
                    if i + 1 < NL: segs.append((i + 1, 3, tri[:, 1, :]))
                    segs += [(16, 3, None), (17, 3, None)]
                attention(i, blk, pb0, segs, 384 + kvh * 64, 768 + h * 64, 0.125, sink_h=h)
            att1(jobs[0][2], *jobs[0][0], **jobs[0][1])
            att1b(jobs[0][2], *jobs[0][0], **jobs[0][1])
            for k_ in range(len(jobs)):
                if k_ + 1 < len(jobs):
                    att1(jobs[k_ + 1][2], *jobs[k_ + 1][0], **jobs[k_ + 1][1])
                att2(jobs[k_][2], *jobs[k_][0], **jobs[k_][1])
                if k_ + 1 < len(jobs):
                    att1b(jobs[k_ + 1][2], *jobs[k_ + 1][0], **jobs[k_ + 1][1])
            del jobs[:]
            for c in range(8):
                bk = 5 + c // 4
                self.mm(self.ps[bk][:, (c % 4) * 128:(c % 4 + 1) * 128], cc[:, c * 128:(c + 1) * 128], self.ident_b, True, True, [tcc, self.tconst], [self.tp[bk]])
            for hf in range(2):
                self.act(lambda e, hf=hf: e.activation(out=ccT[:, hf * 4:(hf + 1) * 4, :], in_=self.ps[5 + hf][:, :].rearrange("p (c t) -> p c t", t=128), func=AF.Copy), [self.tp[5 + hf]], [tccT])
            for hf in range(2):
                for kc in range(8):
                    self.mm(self.ps[hf][:, :], ccT[:, kc, :], Wo[:, kc, hf * 512:(hf + 1) * 512], kc == 0, kc == 7, [tccT, tWo], [self.tp[hf]])
            self.resid_ln(i, [(self.ps[0][:, :], self.tp[0]), (self.ps[1][:, :], self.tp[1])], G, tG, LN, tLN, tmp, ttmp, st, tst)
        A.release(m_all)

    def sin_of(self, out, x, shift, wk, tk, R):
        TWO_PI = 2.0 * np.pi
        a, k = wk
        ki = k.bitcast(I32)
        self.dve(lambda e: e.tensor_scalar(out=a, in0=x, scalar1=1.0 / TWO_PI, scalar2=shift / TWO_PI + 0.5, op0=ALU.mult, op1=ALU.add), R, [tk])
        self.dve(lambda e: e.tensor_copy(out=ki, in_=a), [tk], [tk])
        self.dve(lambda e: e.tensor_copy(out=a, in_=ki), [tk], [tk])
        self.dve(lambda e: e.tensor_scalar(out=k, in0=x, scalar1=shift, scalar2=None, op0=ALU.add), R + [tk], [tk])
        self.dve(lambda e: e.scalar_tensor_tensor(out=k, in0=a, scalar=-TWO_PI, in1=k, op0=ALU.mult, op1=ALU.add), [tk], [tk])
        self.dve(lambda e: e.tensor_scalar(out=a, in0=k, scalar1=float(np.pi), scalar2=None, op0=ALU.is_gt), [tk], [tk])
        self.dve(lambda e: e.scalar_tensor_tensor(out=k, in0=a, scalar=-TWO_PI, in1=k, op0=ALU.mult, op1=ALU.add), [tk], [tk])
        self.dve(lambda e: e.tensor_scalar(out=a, in0=k, scalar1=-float(np.pi), scalar2=None, op0=ALU.is_lt), [tk], [tk])
        self.dve(lambda e: e.scalar_tensor_tensor(out=k, in0=a, scalar=TWO_PI, in1=k, op0=ALU.mult, op1=ALU.add), [tk], [tk])
        self.dve(lambda e: e.tensor_scalar(out=k, in0=k, scalar1=3.1415925, scalar2=-3.1415925, op0=ALU.min, op1=ALU.max), [tk], [tk])
        self.act(lambda e: e.activation(out=out, in_=k, func=AF.Sin), [tk], [tk])

    def s5_phase(self, l, UT, tUT, OC, tOC, nq):
        A, S = self.A, self.S
        dve, act, pool = self.dve, self.act, self.pool
        tp_ = Tk()
        vec = A.alloc([16, 3], F32)
        tau = A.alloc([130], F32)
        bs = A.alloc([16, 2, 16], F32)
        S.dma("sp", vec, self.ssm_vec[l], W=[tp_])
        S.dma("sp", tau, self.c_tau, W=[tp_])
        S.dma("sp", bs, self.ssm_bs[l], W=[tp_])
        CSb = A.alloc([16, 2, 32], BF16); tcs = Tk()
        DDb = A.alloc([8, 32], BF16)
        Wg = A.alloc([2, 256], BF16)
        S.dma("pool", CSb, self.ssm_cs[l], W=[tcs])
        S.dma("pool", DDb, self.ssm_dd[l], W=[tcs])
        S.dma("pool", Wg, self.ssm_wglu[l].rearrange("(c p) n -> p c n", p=128), W=[tcs])
        dve(lambda e: e.tensor_scalar(out=CSb[:, :, 1, :], in0=CSb[:, :, 1, :], scalar1=-1.0, scalar2=None, op0=ALU.mult), [tcs], [tcs])
        pv = A.alloc([16, 16], F32)
        def q(k): return pv[:, k, :]
        lre, lim, lst = vec[:, :, 0], vec[:, :, 1], vec[:, :, 2]
        STEP, LR, ANG, MAG, SN, CN, ABR, ABI, DEN, NUM, FRE, FIM, T0, T1 = range(14)
        act(lambda e: e.activation(out=q(STEP), in_=lst, func=AF.Exp), [tp_], [tp_])
        dve(lambda e: e.tensor_tensor(out=q(LR), in0=lre, in1=q(STEP), op=ALU.mult), [tp_], [tp_])
        dve(lambda e: e.tensor_tensor(out=q(ANG), in0=lim, in1=q(STEP), op=ALU.mult), [tp_], [tp_])
        act(lambda e: e.activation(out=q(MAG), in_=q(LR), func=AF.Exp), [tp_], [tp_])
        self.sin_of(q(SN), q(ANG), 0.0, (q(T0), q(T1)), tp_, [tp_])
        self.sin_of(q(CN), q(ANG), float(np.pi / 2), (q(T0), q(T1)), tp_, [tp_])
        dve(lambda e: e.tensor_tensor(out=q(ABR), in0=q(MAG), in1=q(CN), op=ALU.mult), [tp_], [tp_])
        dve(lambda e: e.tensor_tensor(out=q(ABI), in0=q(MAG), in1=q(SN), op=ALU.mult), [tp_], [tp_])
        dve(lambda e: e.tensor_tensor(out=q(DEN), in0=lre, in1=lre, op=ALU.mult), [tp_], [tp_])
        dve(lambda e: e.tensor_tensor(out=q(T0), in0=lim, in1=lim, op=ALU.mult), [tp_], [tp_])
        dve(lambda e: e.tensor_tensor(out=q(DEN), in0=q(DEN), in1=q(T0), op=ALU.add), [tp_], [tp_])
        dve(lambda e: e.reciprocal(out=q(DEN), in_=q(DEN)), [tp_], [tp_])
        dve(lambda e: e.tensor_scalar(out=q(NUM), in0=q(ABR), scalar1=-1.0, scalar2=None, op0=ALU.add), [tp_], [tp_])
        dve(lambda e: e.tensor_tensor(out=q(T0), in0=q(NUM), in1=lre, op=ALU.mult), [tp_], [tp_])
        dve(lambda e: e.tensor_tensor(out=q(T1), in0=q(ABI), in1=lim, op=ALU.mult), [tp_], [tp_])
        dve(lambda e: e.tensor_tensor(out=q(FRE), in0=q(T0), in1=q(T1), op=ALU.add), [tp_], [tp_])
        dve(lambda e: e.tensor_tensor(out=q(FRE), in0=q(FRE), in1=q(DEN), op=ALU.mult), [tp_], [tp_])
        dve(lambda e: e.tensor_tensor(out=q(T0), in0=q(ABI), in1=lre, op=ALU.mult), [tp_], [tp_])
        dve(lambda e: e.tensor_tensor(out=q(T1), in0=q(NUM), in1=lim, op=ALU.mult), [tp_], [tp_])
        dve(lambda e: e.tensor_tensor(out=q(FIM), in0=q(T0), in1=q(T1), op=ALU.subtract), [tp_], [tp_])
        dve(lambda e: e.tensor_tensor(out=q(FIM), in0=q(FIM), in1=q(DEN), op=ALU.mult), [tp_], [tp_])
        EC = A.alloc([16, 129], F32); ES = A.alloc([16, 129], F32); tE = Tk()
        mtab = A.mark()
        X = A.alloc([16, 129], F32); wa = A.alloc([16, 129], F32); wb = A.alloc([16, 129], F32); tX = Tk()
        dve(lambda e: e.tensor_tensor(out=X, in0=q(ANG).unsqueeze(2).to_broadcast([128, 16, 129]), in1=tau[:, 0:129].unsqueeze(1).to_broadcast([128, 16, 129]), op=ALU.mult), [tp_], [tX])
        self.sin_of(ES, X, 0.0, (wa, wb), tE, [tX])
        self.sin_of(EC, X, float(np.pi / 2), (wa, wb), tE, [tX])
        A.release(mtab)
        BT = A.alloc([16, 2, 128], BF16); tBT = Tk()
        mb = A.mark()
        bb = A.alloc([16, 2, 16], F32); tbb = Tk()
        w4 = A.alloc([4, 16, 16], F32)
        fre_b = q(FRE).unsqueeze(2).to_broadcast([128, 16, 16]); fim_b = q(FIM).unsqueeze(2).to_broadcast([128, 16, 16])
        dve(lambda e: e.tensor_tensor(out=w4[:, 0], in0=bs[:, :, 0, :], in1=fre_b, op=ALU.mult), [tp_], [tbb])
        dve(lambda e: e.tensor_tensor(out=w4[:, 1], in0=bs[:, :, 1, :], in1=fim_b, op=ALU.mult), [tp_], [tbb])
        dve(lambda e: e.tensor_tensor(out=w4[:, 2], in0=bs[:, :, 1, :], in1=fre_b, op=ALU.mult), [tp_], [tbb])
        dve(lambda e: e.tensor_tensor(out=w4[:, 3], in0=bs[:, :, 0, :], in1=fim_b, op=ALU.mult), [tp_], [tbb])
        dve(lambda e: e.tensor_tensor(out=bb[:, :, 0, :], in0=w4[:, 0], in1=w4[:, 1], op=ALU.subtract), [tbb], [tbb])
        dve(lambda e: e.tensor_tensor(out=bb[:, :, 1, :], in0=w4[:, 2], in1=w4[:, 3], op=ALU.add), [tbb], [tbb])
        Bp = A.alloc([4, 128], BF16); tBp = [Tk() for _ in range(4)]
        dve(lambda e: e.memset(Bp, 0.0), [], tBp)
        n = 0
        for dg in range(16):
            gc = dg % 8
            band = gc % 4
            for ri in range(2):
                for gl in range(2):
                    dve(lambda e, dg=dg, ri=ri, gl=gl, band=band: e.tensor_copy(out=Bp[gl * 64:(gl + 1) * 64, band, band * 32 + gl * 16:band * 32 + gl * 16 + 16],
                                                                            in_=bb[gl * 64:(gl + 1) * 64, dg, ri, :]), [tbb], [tBp[band]])
                bk = 5 + (n // 4) % 2
                self.mm(self.ps[bk][:, (n % 4) * 128:(n % 4 + 1) * 128], Bp[:, band, :], self.ident_b, True, True, [tBp[band], self.tconst], [self.tp[bk]])
                if n % 4 == 3:
                    dg0 = (n - 3) // 2
                    act(lambda e, bk=bk, dg0=dg0: e.activation(out=BT[:, dg0:dg0 + 2, :, :], in_=self.ps[bk][:, :].rearrange("p (a b t) -> p a b t", a=2, b=2), func=AF.Copy), [self.tp[bk]], [tBT])
                n += 1
        A.release(mb)
        Y = A.alloc([NT, 256], F32); tY = [Tk() for _ in range(NT)]
        z = A.alloc([256], F32); z2 = A.alloc([256], F32); tz = Tk()
        zb = A.alloc([256], BF16); zT = A.alloc([2, 128], BF16); tzT = Tk()
        DB = []
        for d in range(2):
            DB.append(dict(g=A.alloc([8, 2, 128], F32), tg=Tk(), gi=A.alloc([8, 2], F32), tgi=Tk(), giw=A.alloc([4, 8], F32),
                           tt=[A.alloc([2, 128], F32) for _ in range(4)], ttt=Tk(), w=A.alloc([2, 2, 128], F32), tw=Tk(),
                           hs=A.alloc([8, 2, 128], BF16), ths=Tk()))
        orders = [[16, 17] + list(range(16)), [17, 16] + list(range(15, -1, -1))]
        seen_tile = set()
        for n_ in range(NT):
          for d in range(2):
            i = orders[d][n_]
            X_ = DB[d]
            g, tg, gi, tgi, giw, tt, ttt, w, tw, hs, ths = (X_[k] for k in ("g", "tg", "gi", "tgi", "giw", "tt", "ttt", "w", "tw", "hs", "ths"))
            rv = (lambda ap: ap) if d == 0 else (lambda ap: ap[:, ::-1])
            tk = slice(i * 128, (i + 1) * 128)
            if n_ > 0:
                last = 127 if d == 0 else 0
                glr, gli = g[:, :, 0, last], g[:, :, 1, last]
                cT, sT = EC[:, d * 8:(d + 1) * 8, 128], ES[:, d * 8:(d + 1) * 8, 128]
                dve(lambda e, glr=glr, cT=cT, giw=giw: e.tensor_tensor(out=giw[:, 0], in0=glr, in1=cT, op=ALU.mult), [tg, tE], [tgi])
                dve(lambda e, gli=gli, sT=sT, giw=giw: e.tensor_tensor(out=giw[:, 1], in0=gli, in1=sT, op=ALU.mult), [tg, tE], [tgi])
                dve(lambda e, glr=glr, sT=sT, giw=giw: e.tensor_tensor(out=giw[:, 2], in0=glr, in1=sT, op=ALU.mult), [tg, tE], [tgi])
                dve(lambda e, gli=gli, cT=cT, giw=giw: e.tensor_tensor(out=giw[:, 3], in0=gli, in1=cT, op=ALU.mult), [tg, tE], [tgi])
                dve(lambda e, gi=gi, giw=giw: e.tensor_tensor(out=gi[:, :, 0], in0=giw[:, 0], in1=giw[:, 1], op=ALU.subtract), [tgi], [tgi])
                dve(lambda e, gi=gi, giw=giw: e.tensor_tensor(out=gi[:, :, 1], in0=giw[:, 2], in1=giw[:, 3], op=ALU.add), [tgi], [tgi])
            for bq in range(4):
                bk = d * 2 + bq % 2
                for g2 in range(2):
                    gc = bq * 2 + g2
                    for ri in range(2):
                        c0 = g2 * 256 + ri * 128
                        self.mm(self.ps[bk][:, c0:c0 + 128], BT[:, d * 8 + gc, ri, :], UT[:, gc // 4, tk], True, True, [tBT, tUT[i]], [self.tp[bk]])
                pv4 = self.ps[bk][:, :].rearrange("p (a b t) -> p a b t", a=2, b=2)
                bre, bim = pv4[:, :, 0, :], pv4[:, :, 1, :]
                dg0 = d * 8 + bq * 2
                cs_, sn_ = rv_tab(EC, dg0, d), rv_tab(ES, dg0, d)
                dve(lambda e, bre=bre, cs_=cs_, tt=tt: e.tensor_tensor(out=tt[0], in0=bre, in1=cs_, op=ALU.mult), [self.tp[bk], tE], [ttt])
                dve(lambda e, bim=bim, sn_=sn_, tt=tt: e.tensor_tensor(out=tt[1], in0=bim, in1=sn_, op=ALU.mult), [self.tp[bk], tE], [ttt])
                dve(lambda e, bim=bim, cs_=cs_, tt=tt: e.tensor_tensor(out=tt[2], in0=bim, in1=cs_, op=ALU.mult), [self.tp[bk], tE], [ttt])
                dve(lambda e, bre=bre, sn_=sn_, tt=tt: e.tensor_tensor(out=tt[3], in0=bre, in1=sn_, op=ALU.mult), [self.tp[bk], tE], [ttt])
                pool(lambda e, w=w, tt=tt: e.tensor_tensor(out=w[:, :, 0, :], in0=tt[0], in1=tt[1], op=ALU.add), [ttt], [tw])
                pool(lambda e, w=w, tt=tt: e.tensor_tensor(out=w[:, :, 1, :], in0=tt[2], in1=tt[3], op=ALU.subtract), [ttt], [tw])
                for g2 in range(2):
                    gc = bq * 2 + g2
                    for ri in range(2):
                        init = 0.0 if n_ == 0 else gi[:, gc, ri:ri + 1]
                        dve(lambda e, gc=gc, ri=ri, g2=g2, init=init, g=g, w=w, rv=rv, d=d: e.tensor_tensor_scan(out=rv(g[:, gc, ri, :]), data0=q(MAG)[:, d * 8 + gc:d * 8 + gc + 1].to_broadcast([128, 128]),
                                                                                      data1=rv(w[:, g2, ri, :]), initial=init, op0=ALU.mult, op1=ALU.add),
                            [tw, tp_, tgi], [tg])
            for hh in range(4):
                gre, gim = g[:, hh * 2:(hh + 1) * 2, 0, :], g[:, hh * 2:(hh + 1) * 2, 1, :]
                dg0 = d * 8 + hh * 2
                cs_, sn_ = rv_tab(EC, dg0, d), rv_tab(ES, dg0, d)
                dve(lambda e, gre=gre, cs_=cs_, tt=tt: e.tensor_tensor(out=tt[0], in0=gre, in1=cs_, op=ALU.mult), [tg, tE], [ttt])
                dve(lambda e, gim=gim, sn_=sn_, tt=tt: e.tensor_tensor(out=tt[1], in0=gim, in1=sn_, op=ALU.mult), [tg, tE], [ttt])
                dve(lambda e, gre=gre, sn_=sn_, tt=tt: e.tensor_tensor(out=tt[2], in0=gre, in1=sn_, op=ALU.mult), [tg, tE], [ttt])
                dve(lambda e, gim=gim, cs_=cs_, tt=tt: e.tensor_tensor(out=tt[3], in0=gim, in1=cs_, op=ALU.mult), [tg, tE], [ttt])
                pool(lambda e, hh=hh, hs=hs, tt=tt: e.tensor_tensor(out=hs[:, hh * 2:(hh + 1) * 2, 0, :], in0=tt[0], in1=tt[1], op=ALU.subtract), [ttt], [ths])
                pool(lambda e, hh=hh, hs=hs, tt=tt: e.tensor_tensor(out=hs[:, hh * 2:(hh + 1) * 2, 1, :], in0=tt[2], in1=tt[3], op=ALU.add), [ttt], [ths])
            yb_ = 4 + d
            for gc in range(8):
                terms = [(hs[:, gc, 0, :], CSb[:, d * 8 + gc, 0, :], [ths, tcs]), (hs[:, gc, 1, :], CSb[:, d * 8 + gc, 1, :], [ths, tcs])]
                if d == 0:
                    terms.append((UT[:, gc // 4, tk], DDb[:, gc, :], [tUT[i], tcs]))
                for k, (lt, rh, R_) in enumerate(terms):
                    self.mm(self.ps[yb_][:, gc * 32:(gc + 1) * 32], lt, rh, k == 0, k == len(terms) - 1, R_, [self.tp[yb_]])
            if i not in seen_tile:
                seen_tile.add(i)
                act(lambda e, i=i, yb_=yb_: e.activation(out=Y[:, i, :], in_=self.ps[yb_][:, 0:256], func=AF.Copy), [self.tp[yb_]], [tY[i]])
                continue
            if i >= nq:
                continue
            dve(lambda e, i=i, yb_=yb_: e.tensor_tensor(out=z, in0=self.ps[yb_][:, 0:256], in1=Y[:, i, :], op=ALU.add), [self.tp[yb_], tY[i]], [tz])
            pool(lambda e: e.tensor_tensor(out=z2, in0=z, in1=z, op=ALU.mult), [tz], [tz])
            pool(lambda e: e.tensor_scalar(out=z2, in0=z2, scalar1=0.044715, scalar2=1.0, op0=ALU.mult, op1=ALU.add), [tz], [tz])
            pool(lambda e: e.tensor_tensor(out=z2, in0=z2, in1=z, op=ALU.mult), [tz], [tz])
            act(lambda e: e.activation(out=z2, in_=z2, func=AF.Sigmoid, scale=1.5957691216057308), [tz], [tz])
            dve(lambda e: e.tensor_tensor(out=z, in0=z, in1=z2, op=ALU.mult), [tz], [tz])
            dve(lambda e: e.tensor_copy(out=zb, in_=z), [tz], [tz])
            for c in range(2):
                self.mm(self.ps[6][:, c * 128:(c + 1) * 128], zb[:, c * 128:(c + 1) * 128], self.ident_b, True, True, [tz, self.tconst], [self.tp[6]])
            act(lambda e: e.activation(out=zT, in_=self.ps[6][:, 0:256].rearrange("p (c t) -> p c t", t=128), func=AF.Copy), [self.tp[6]], [tzT])
            for c in range(2):
                self.mm(self.ps[7][:, 0:256], zT[:, c, :], Wg[:, c, :], c == 0, c == 1, [tzT, tcs], [self.tp[7]])
            act(lambda e: e.activation(out=z2, in_=self.ps[7][:, 0:256], func=AF.Sigmoid), [self.tp[7]], [tz])
            dve(lambda e, i=i: e.tensor_tensor(out=OC[:, i, :], in0=z, in1=z2, op=ALU.mult), [tz], [tOC[i]])


def rv_tab(E, dg0, d, n=2):
    if d == 0:
        return E[:, dg0:dg0 + n, 0:128]
    return E[:, dg0:dg0 + n, 127::-1]


def _consts():
    c = {}
    c["c_ident_f"] = np.eye(128, dtype=np.float32)
    c["c_ident_b"] = np.eye(128, dtype=np.float32).astype(ml_dtypes.bfloat16)
    pos = np.arange(NL * 128)
    row = (pos // 64).astype(np.float32)
    col = (pos % 64).astype(np.float32)
    inv = (10000.0 ** (-np.arange(16, dtype=np.float32) / 16)).astype(np.float32)
    ang = np.concatenate([row[:, None] * inv, col[:, None] * inv], -1).astype(np.float32)
    cs = np.concatenate([np.cos(ang), -np.sin(ang), np.sin(ang)], -1).astype(np.float32)
    ctxcs = np.concatenate([np.ones((256, 32), np.float32), np.zeros((256, 64), np.float32)], -1)
    cs = np.concatenate([cs, ctxcs], 0).reshape(NT, 128, 96).transpose(1, 0, 2)
    c["c_rope"] = np.ascontiguousarray(cs)
    q = np.arange(128)[:, None]
    k = np.arange(128)[None, :]
    tri = np.stack([np.where(k >= q, 0.0, NEG), np.where(k <= q, 0.0, NEG)], 1).astype(np.float32)
    c["c_tri"] = tri.astype(ml_dtypes.bfloat16)
    nm = np.full((5, 128, 10, 64), NEG, np.float32)
    qc = np.arange(64)
    cstart = np.clip(qc - 8, 0, 48)
    kc = np.arange(64)
    colv = (kc[None, :] >= cstart[:, None]) & (kc[None, :] < cstart[:, None] + 16)
    def fill(var, j, i0, nr):
        for rl in range(2):
            r = 2 * j + rl
            rs = int(np.clip(r - 4, 0, 24))
            for ii in range(nr):
                i = i0 + ii
                if rs <= i < rs + 8:
                    blk = np.where(colv, 0.0, NEG)
                    nm[var, rl * 64:(rl + 1) * 64, ii, :] = blk
    fill(0, 5, 6, 10)
    fill(1, 0, 0, 8); fill(2, 1, 0, 8); fill(3, 14, 24, 8); fill(4, 15, 24, 8)
    nm2 = nm.transpose(1, 0, 2, 3).reshape(128, 5, 640)
    c["c_namask"] = np.ascontiguousarray(np.concatenate([nm2[:, 0, :]] + [nm2[:, v, 0:512] for v in range(1, 5)], 1)).astype(ml_dtypes.bfloat16)
    tau = np.zeros((128, 130), np.float32)
    tau[:, :] = np.arange(130, dtype=np.float32)[None, :]
    c["c_tau"] = tau
    return c


def _prep_shared(I):
    f = np.float32
    d = dict(_consts())
    for k in ("ada_w", "ada_b", "w_in", "w_out"):
        d[k] = np.ascontiguousarray(I[k], dtype=f)
    rp = np.zeros((DEPTH, 4, 18, 128), f)
    rp[:, :, 1:16, 48:79] = I["na_rpb"][:, :, :, ::-1]
    d["rpbh"] = rp
    d["gqk"] = np.ascontiguousarray(np.stack([I["ga_q_norm"], I["ga_k_norm"]], 1), dtype=f)
    def st(a):
        L = a.shape[0]
        rest = a.shape[4:]
        a = a.reshape((L, 2, 8, 2, 64) + rest)
        a = np.moveaxis(a, (3, 4), (1, 2))
        return np.ascontiguousarray(a.reshape((L, 128, 16) + rest))
    ls = np.broadcast_to(I["ssm_log_step"][..., None], I["ssm_lambda_re"].shape)
    d["ssm_vec"] = np.ascontiguousarray(np.stack([st(I["ssm_lambda_re"]), st(I["ssm_lambda_im"]), st(np.ascontiguousarray(ls))], -1), dtype=f)
    d["ssm_bs"] = np.ascontiguousarray(np.stack([st(I["ssm_b_re"]), st(I["ssm_b_im"])], -2), dtype=f)
    cre = st(np.swapaxes(I["ssm_c_re"], -1, -2))
    cim = st(np.swapaxes(I["ssm_c_im"], -1, -2))
    cs = np.zeros((DEPTH, 128, 16, 2, 32), f)
    for gl in range(2):
        cs[:, gl * 64:(gl + 1) * 64, :, 0, gl * 16:(gl + 1) * 16] = cre[:, gl * 64:(gl + 1) * 64]
        cs[:, gl * 64:(gl + 1) * 64, :, 1, gl * 16:(gl + 1) * 16] = cim[:, gl * 64:(gl + 1) * 64]
    d["ssm_cs"] = cs
    dd = np.zeros((DEPTH, 128, 8, 32), f)
    sd = I["ssm_d"].reshape(DEPTH, 2, 128)
    for gc in range(8):
        for j in range(32):
            pl = (gc % 4) * 32 + j
            dd[:, pl, gc, j] = sd[:, gc // 4, pl]
    d["ssm_dd"] = dd
    d["ssm_wglu"] = np.ascontiguousarray(I["ssm_w_glu"], dtype=f)
    d["sink"] = np.ascontiguousarray(I["sw_sink"], dtype=f)
    d["lngb"] = np.ascontiguousarray(np.stack([I["ln1_g"], I["ln1_b"], I["ln2_g"], I["ln2_b"]], 1), dtype=f)
    d["ffn_g"] = I["ffn_w_gate"]; d["ffn_u"] = I["ffn_w_up"]; d["ffn_d"] = I["ffn_w_down"]
    d["moe_r"] = I["moe_w_router"]; d["moe_rb"] = I["moe_b_router"]
    d["moe_g"] = I["moe_w_gate"]; d["moe_u"] = I["moe_w_up"]; d["moe_d"] = I["moe_w_down"]
    return d


def _prep_core(I, b, hx=None):
    if hx is None:
        hx = np.concatenate([I["x"][b], I["ctx"][b]], 0)
    cv = np.stack([I["c"][b].reshape(8, 128).T, I["c_ctx"].reshape(8, 128).T], -1)
    return {"hx": np.ascontiguousarray(hx, dtype=np.float32), "cv": np.ascontiguousarray(cv, dtype=np.float32)}


def build_program(layers=(0, 1, 2, 3)):
    b = B(list(layers))
    b.setup()
    for l in layers:
        b.ada_cols(l)
        b.mixer_phase(l)
        b.ffn_phase(l)
    for i in range(NL):
        b.store(b.out[i * 128:(i + 1) * 128, :], b.H[:, i, :], [b.tH[i]])
    with b.nc.Block() as blk:
        b.S.emit(blk, b.fin)
    return b


def kernel(**inputs):
    I = {k: np.asarray(v) for k, v in inputs.items()}
    n = 8
    b = build_program()
    shared = _prep_shared(I)
    shared = {k: v for k, v in shared.items() if k in b.din}
    in_maps = []
    for c in range(n):
        m = dict(shared)
        m.update(_prep_core(I, c))
        in_maps.append(m)
    res = run_bass_kernel_spmd(b.nc, in_maps, core_ids=list(range(n)))
    out = np.stack([np.asarray(r["out"], dtype=np.float32).reshape(NL * 128, D) for r in res.results], 0)
    return out
```

```python
import numpy as np
import ml_dtypes
import concourse.bass as bass
import concourse.mybir as mybir
from concourse.bass_utils import run_bass_kernel_spmd

F32 = mybir.dt.float32
BF16 = mybir.dt.bfloat16
I32 = mybir.dt.int32
ALU = mybir.AluOpType
AF = mybir.ActivationFunctionType
AX = mybir.AxisListType


class Tk:
    __slots__ = ("w", "r")

    def __init__(self):
        self.w = None
        self.r = []


class Sched:
    ENG = ("pe", "act", "dve", "pool", "sp")
    NDMA = {"sp": 12, "pool": 8, "act": 4}

    def __init__(self, nc):
        self.nc = nc
        self.prog = {e: [] for e in self.ENG}
        self.n = {e: 0 for e in self.ENG}
        self.seen = {e: {} for e in self.ENG}
        self.signaled = {e: set() for e in self.ENG}
        self.dma_rr = {q: 0 for q in self.NDMA}
        self.dma_tot = {}
        self.lastc = {e: 0 for e in self.ENG}
        self.cur_fence = {}

    def fence(self):
        for e in self.ENG:
            if self.lastc[e]:
                self.cur_fence[e] = self.lastc[e]
        for key, tot in self.dma_tot.items():
            self.cur_fence[key] = tot

    def _deps(self, eng, reads, writes):
        deps = dict(self.cur_fence)
        def add(t):
            if t is None:
                return
            k, v = t
            if deps.get(k, 0) < v:
                deps[k] = v
        for t in reads:
            add(t.w)
        for t in writes:
            add(t.w)
            for r in t.r:
                add(r)
        waits = []
        for k, v in deps.items():
            if k == "pe" and eng == "pe":
                continue
            if self.seen[eng].get(k, 0) < v:
                self.seen[eng][k] = v
                waits.append((k, v))
                if k in self.signaled:
                    self.signaled[k].add(v)
        return waits

    def _commit(self, ticket, reads, writes):
        for t in reads:
            t.r.append(ticket)
        for t in writes:
            t.w = ticket
            t.r = []

    def op(self, eng, fn, R=(), W=()):
        waits = self._deps(eng, R, W)
        self.n[eng] += 1
        self.lastc[eng] = self.n[eng]
        ticket = (eng, self.n[eng])
        self.prog[eng].append((waits, fn, ticket, None))
        self._commit(ticket, R, W)

    def dma(self, q, out, in_, R=(), W=(), slow=False):
        j = self.dma_rr[q]
        self.dma_rr[q] = (j + 1) % self.NDMA[q]
        key = ("d", q, j)
        waits = self._deps(q, R, W)
        prev = self.dma_tot.get(key, 0)
        if prev and self.seen[q].get(key, 0) < prev:
            self.seen[q][key] = prev
            waits.append((key, prev))
        self.dma_tot[key] = prev + 16
        ticket = (key, prev + 16)
        self.n[q] += 1
        if slow:
            fn = lambda e, o=out, i=in_: e.dma_start(out=o, in_=i, allow_slow_non_contiguous=True)
        else:
            fn = lambda e, o=out, i=in_: e.dma_start(out=o, in_=i)
        self.prog[q].append((waits, fn, (q, self.n[q]), key))
        self._commit(ticket, R, W)

    def emit(self, block, final_waits):
        nc = self.nc
        sems = {e: nc.alloc_semaphore("s_" + e) for e in self.ENG}
        for key in self.dma_tot:
            sems[key] = nc.alloc_semaphore("d_%s%d" % (key[1], key[2]))
        rank = {}
        for e in self.ENG:
            rank[e] = {v: i + 1 for i, v in enumerate(sorted(self.signaled[e]))}

        def val(k, v):
            return rank[k][v] if k in rank else v

        def run(ename, eng):
            for waits, fn, ticket, dkey in self.prog[ename]:
                for k, v in waits:
                    eng.wait_ge(sems[k], val(k, v))
                ins = fn(eng)
                if dkey is not None:
                    ins.then_inc(sems[dkey], 16)
                elif ticket[1] in self.signaled[ename]:
                    ins.then_inc(sems[ename], 1)
            if ename == "sp":
                for k, v in final_waits:
                    eng.wait_ge(sems[k], val(k, v))

        for k, v in final_waits:
            if k in self.signaled:
                self.signaled[k].add(v)
        for e in self.ENG:
            rank[e] = {v: i + 1 for i, v in enumerate(sorted(self.signaled[e]))}
        block.tensor(lambda e: run("pe", e))
        block.scalar(lambda e: run("act", e))
        block.vector(lambda e: run("dve", e))
        block.gpsimd(lambda e: run("pool", e))
        block.sync(lambda e: run("sp", e))


class Arena:
    def __init__(self, nc, nbytes, S=None):
        self.S = S
        self.t = nc.alloc_sbuf_tensor("arena", [128, nbytes // 4], F32)
        self.nbytes = nbytes
        self.top = 0
        self.peak = 0

    def alloc(self, free_shape, dtype):
        esz = 2 if dtype == BF16 else 4
        n = int(np.prod(free_shape))
        nb = (n * esz + 63) // 64 * 64
        off = self.top
        self.top += nb
        self.peak = max(self.peak, self.top)
        assert self.top <= self.nbytes, ("arena overflow", self.top, self.nbytes)
        ap = self.t[:, off // 4:(off + nb) // 4]
        if dtype != F32:
            ap = ap.bitcast(dtype)
        ap = ap[:, 0:n]
        if len(free_shape) == 2:
            ap = ap.rearrange("p (a b) -> p a b", b=free_shape[1])
        elif len(free_shape) == 3:
            ap = ap.rearrange("p (a b c) -> p a b c", b=free_shape[1], c=free_shape[2])
        return ap

    def mark(self):
        return self.top

    def release(self, m):
        self.top = m
        self.S.fence()


D = 1024
NT = 18
NL = 16
DEPTH = 4
DFF = 2816
DFE = 3584
NE = 8
ALPHA = float((2 * DEPTH) ** 0.25)
NEG = -1.0e30
SLAB = 512


class B:
    def __init__(self, layers, dbg=None):
        self.layers = layers
        self.dbg = dbg or {}
        nc = self.nc = bass.Bass("TRN2", target_bir_lowering=False)
        self.S = Sched(nc)
        self.A = Arena(nc, 212800, self.S)
        self.fin = []
        self.din = {}
        self.psall = nc.alloc_psum_tensor("psall", [128, 8 * 512], F32)
        self.ps = [self.psall[:, i * 512:(i + 1) * 512] for i in range(8)]
        self.tp = [Tk() for _ in range(8)]

    SHAPES = {
        "hx": ([NT * 128, D], F32), "cv": ([128, 8, 2], F32), "c_ident_f": ([128, 128], F32), "c_ident_b": ([128, 128], BF16),
        "c_rope": ([128, NT, 96], F32), "c_tri": ([128, 2, 128], BF16), "c_namask": ([128, 2688], BF16), "c_tau": ([128, 130], F32),
        "ada_w": ([DEPTH, D, 6 * D], F32), "ada_b": ([DEPTH, 6 * D], F32), "w_in": ([DEPTH, D, 2048], F32), "w_out": ([DEPTH, D, D], F32),
        "rpbh": ([DEPTH, 4, 18, 128], F32), "gqk": ([DEPTH, 2, 64], F32), "ssm_vec": ([DEPTH, 128, 16, 3], F32),
        "ssm_bs": ([DEPTH, 128, 16, 2, 16], F32), "ssm_cs": ([DEPTH, 128, 16, 2, 32], F32), "ssm_dd": ([DEPTH, 128, 8, 32], F32),
        "ssm_wglu": ([DEPTH, 256, 256], F32), "sink": ([DEPTH, 4], F32), "lngb": ([DEPTH, 4, D], F32),
        "ffn_g": ([2, D, DFF], F32), "ffn_u": ([2, D, DFF], F32), "ffn_d": ([2, DFF, D], F32),
        "moe_r": ([2, D, NE], F32), "moe_rb": ([2, NE], F32), "moe_g": ([2, NE, D, DFE], F32), "moe_u": ([2, NE, D, DFE], F32),
        "moe_d": ([2, NE, DFE, D], F32),
    }

    def __getattr__(self, name):
        sh = B.SHAPES.get(name)
        if sh is None:
            raise AttributeError(name)
        ap = self.inp(name, sh[0], sh[1])
        self.__dict__[name] = ap
        return ap

    def dt_(self, name):
        getattr(self, name)
        return self.din[name]

    def inp(self, name, shape, dt=F32):
        t = self.nc.dram_tensor(name, list(shape), dt, kind="ExternalInput")
        self.din[name] = t
        return t.ap()

    def outp(self, name, shape, dt=F32):
        return self.nc.dram_tensor(name, list(shape), dt, kind="ExternalOutput").ap()

    def store(self, dst, src, R):
        S = self.S
        S.dma("sp", dst, src, R=R)
        j = (S.dma_rr["sp"] - 1) % S.NDMA["sp"]
        key = ("d", "sp", j)
        self.fin.append((key, S.dma_tot[key]))

    def pe(self, fn, R=(), W=()): self.S.op("pe", fn, R, W)
    def act(self, fn, R=(), W=()): self.S.op("act", fn, R, W)
    def dve(self, fn, R=(), W=()): self.S.op("dve", fn, R, W)
    def pool(self, fn, R=(), W=()): self.S.op("pool", fn, R, W)

    def mm(self, out, lhsT, rhs, start, stop, R, W):
        self.pe(lambda e: e.matmul(out, lhsT=lhsT, rhs=rhs, start=start, stop=stop), R, W)

    def tr(self, out, in_, R, W):
        self.pe(lambda e: e.transpose(out=out, in_=in_, identity=self.ident_f), R + [self.tconst], W)

    def rstd_from(self, out, var_ap, scale, eps, R, tk):
        self.act(lambda e: e.activation(out=out, in_=var_ap, func=AF.Sqrt, bias=self.eps_ap(eps), scale=scale), R, [tk])
        self.dve(lambda e: e.reciprocal(out=out, in_=out), [tk], [tk])

    def eps_ap(self, eps):
        return self.epsc[:, 0:1]

    def setup(self):
        A = self.A
        inp = self.inp
        self.out = self.outp("out", [NL * 128, D])

        S = self.S
        self.tconst = Tk()
        self.H = A.alloc([NT, D], F32)
        self.tH = [Tk() for _ in range(NT)]
        self.ident_f = A.alloc([128], F32)
        self.ident_b = A.alloc([128], BF16)
        self.epsc = A.alloc([2], F32)
        self.csil = A.alloc([8, 2], F32)
        self.modc = A.alloc([DEPTH, 32, 2], F32)
        self.tmodc = Tk()
        for dst, src in ((self.ident_f, self.c_ident_f), (self.ident_b, self.c_ident_b), (self.csil, self.cv)):
            S.dma("sp", dst, src, W=[self.tconst])
        self.dve(lambda e: e.memset(self.epsc, 1e-6), W=[self.tconst])
        for i in range(NT):
            S.dma("sp" if i % 2 == 0 else "act", self.H[:, i, :], self.hx[i * 128:(i + 1) * 128, :], W=[self.tH[i]])
        self.act(lambda e: e.activation(out=self.csil, in_=self.csil, func=AF.Silu), [self.tconst], [self.tconst])

    def ada_cols(self, l):
        A, S = self.A, self.S
        m = A.mark()
        blocks = [0, 1, 3, 4]
        wst = [A.alloc([8, 128], F32) for _ in range(3)]
        tw = [Tk() for _ in range(3)]
        bcol = A.alloc([32], F32)
        tb = Tk()
        for j, blk in enumerate(blocks):
            S.dma("sp", bcol[:, j * 8:(j + 1) * 8], self.ada_b[l, blk * D:(blk + 1) * D].rearrange("(kc p) -> p kc", p=128), W=[tb], slow=True)
        n = 0
        for j, blk in enumerate(blocks):
            for fc in range(8):
                b = n % 3
                col0 = blk * D + fc * 128
                S.dma("sp" if n % 2 == 0 else "act", wst[b], self.ada_w[l, :, col0:col0 + 128].rearrange("(kc p) n -> p kc n", p=128), W=[tw[b]])
                pb = 7
                for kc in range(8):
                    self.mm(self.ps[pb][:, 0:2], wst[b][:, kc, :], self.csil[:, kc, :], kc == 0, kc == 7, [tw[b], self.tconst], [self.tp[pb]])
                idx = j * 8 + fc
                add = 1.0 if blk in (1, 4) else 0.0
                self.dve(lambda e, idx=idx, add=add, pb=pb: e.tensor_scalar(out=self.modc[:, l, idx, :], in0=self.ps[pb][:, 0:2], scalar1=bcol[:, idx:idx + 1],
                                                                           scalar2=add, op0=ALU.add, op1=ALU.add), [self.tp[pb], tb], [self.tmodc])
                n += 1
        A.release(m)

    def ada_gate(self, l, which, G, tG):
        A, S = self.A, self.S
        m = A.mark()
        blk = 2 if which == 0 else 5
        crep = A.alloc([2, 8, 128], F32); tcr = Tk()
        for v in range(2):
            for kc in range(8):
                self.dve(lambda e, v=v, kc=kc: e.tensor_copy(out=crep[:, v, kc, :], in_=self.csil[:, kc, v:v + 1].to_broadcast([128, 128])),
                         [self.tconst], [tcr])
        wst = [A.alloc([8, 512], F32) for _ in range(2)]
        tw = [Tk() for _ in range(2)]
        bb = A.alloc([D], F32)
        tb = Tk()
        S.dma("sp", bb, self.ada_b[l:l + 1, blk * D:(blk + 1) * D].partition_broadcast(128) if False else
              bass.AP(self.dt_("ada_b"), l * 6 * D + blk * D, [[0, 128], [1, D]]), W=[tb])
        for nb in range(2):
            col0 = blk * D + nb * 512
            S.dma("sp", wst[nb], self.ada_w[l, :, col0:col0 + 512].rearrange("(kc p) n -> p kc n", p=128), W=[tw[nb]])
            for v in range(2):
                pb = 5 + v
                for kc in range(8):
                    self.mm(self.ps[pb][:, :], crep[:, v, kc, :], wst[nb][:, kc, :], kc == 0, kc == 7, [tw[nb], tcr], [self.tp[pb]])
                self.dve(lambda e, v=v, nb=nb, pb=pb: e.tensor_tensor(out=G[:, v, nb * 512:(nb + 1) * 512], in0=self.ps[pb][:, :], in1=bb[:, nb * 512:(nb + 1) * 512], op=ALU.add),
                         [self.tp[pb], tb], [tG])
        A.release(m)

    def load_ln(self, l, which, LN, tLN):
        for j in range(2):
            self.S.dma("sp", LN[:, j, :], bass.AP(self.dt_("lngb"), (l * 4 + which * 2 + j) * D, [[0, 128], [1, D]]), W=[tLN])

    def make_aT(self, l, i, which, aT, taT, aT32=None, taT32=None):
        v = 1 if i >= NL else 0
        for half in range(2):
            pb = 5 + half
            for q in range(4):
                kc = half * 4 + q
                self.tr(self.ps[pb][:, q * 128:(q + 1) * 128], self.H[:, i, kc * 128:(kc + 1) * 128], [self.tH[i]], [self.tp[pb]])
            for q in range(4):
                kc = half * 4 + q
                sc = self.modc[:, l, (which * 2 + 1) * 8 + kc, v:v + 1]
                sh = self.modc[:, l, (which * 2) * 8 + kc, v:v + 1]
                self.act(lambda e, kc=kc, q=q, pb=pb, sc=sc, sh=sh: e.activation(out=aT[:, kc, :], in_=self.ps[pb][:, q * 128:(q + 1) * 128], func=AF.Identity, bias=sh, scale=sc),
                         [self.tp[pb], self.tmodc], [taT])
                if aT32 is not None:
                    self.act(lambda e, kc=kc, q=q, pb=pb, sc=sc, sh=sh: e.activation(out=aT32[:, kc, :], in_=self.ps[pb][:, q * 128:(q + 1) * 128], func=AF.Identity, bias=sh, scale=sc),
                             [self.tp[pb], self.tmodc], [taT32])

    def resid_ln(self, i, ys, G, tG, LN, tLN, tmp, ttmp, st, tst):
        v = 1 if i >= NL else 0
        for hf, (yap, ty) in enumerate(ys):
            self.dve(lambda e, hf=hf, yap=yap: e.tensor_tensor(out=tmp[:, hf * 512:(hf + 1) * 512], in0=yap, in1=G[:, v, hf * 512:(hf + 1) * 512], op=ALU.mult),
                     [ty, tG], [ttmp])
        self.ln_tail(i, tmp, ttmp, LN, tLN, st, tst)

    def ln_tail(self, i, tmp, ttmp, LN, tLN, st, tst):
        self.dve(lambda e: e.scalar_tensor_tensor(out=tmp, in0=self.H[:, i, :], scalar=ALPHA, in1=tmp, op0=ALU.mult, op1=ALU.add), [self.tH[i], ttmp], [ttmp])
        for hf in range(2):
            self.dve(lambda e, hf=hf: e.bn_stats(out=st[:, hf * 6:(hf + 1) * 6], in_=tmp[:, hf * 512:(hf + 1) * 512]), [ttmp], [tst])
        self.dve(lambda e: e.bn_aggr(out=st[:, 12:14], in_=st[:, 0:12]), [tst], [tst])
        self.rstd_from(st[:, 14:15], st[:, 13:14], 1.0, 1e-6, [tst], tst)
        self.dve(lambda e: e.tensor_scalar(out=tmp, in0=tmp, scalar1=st[:, 12:13], scalar2=st[:, 14:15], op0=ALU.subtract, op1=ALU.mult), [ttmp, tst], [ttmp])
        self.pool(lambda e: e.tensor_tensor(out=tmp, in0=tmp, in1=LN[:, 0, :], op=ALU.mult), [ttmp, tLN], [ttmp])
        self.pool(lambda e: e.tensor_tensor(out=self.H[:, i, :], in0=tmp, in1=LN[:, 1, :], op=ALU.add), [ttmp, tLN], [self.tH[i]])

    def ffn_phase(self, l):
        A, S = self.A, self.S
        last = (l == DEPTH - 1)
        nt = NL if last else NT
        moe = (l % 2 == 1)
        li = l // 2
        m = A.mark()
        G = A.alloc([2, D], F32); tG = Tk()
        LN = A.alloc([2, D], F32); tLN = Tk()
        self.ada_gate(l, 1, G, tG)
        self.load_ln(l, 1, LN, tLN)
        FT = A.alloc([8, nt * 128], BF16)
        tFT = [Tk() for _ in range(nt)]
        moe_ = (l % 2 == 1)
        if moe_:
            gate = A.alloc([nt, NE], F32); tgate = Tk()
            a32 = A.alloc([8, 128], F32); ta32 = Tk()
            rt = self.router_setup(l // 2)
        for i in range(nt):
            if moe_:
                self.make_aT(l, i, 1, FT[:, :, i * 128:(i + 1) * 128], tFT[i], a32, ta32)
                self.router_tile(rt, i, a32, ta32, gate, tgate)
            else:
                self.make_aT(l, i, 1, FT[:, :, i * 128:(i + 1) * 128], tFT[i])
        experts = range(NE) if moe else [0]
        dff = DFE if moe else DFF
        nsl = dff // SLAB + (1 if dff % SLAB else 0)
        facc = A.alloc([nt, D], F32) if False else None
        tmp = A.alloc([D], F32); ttmp = Tk()
        st = A.alloc([16], F32); tst = Tk()
        for i in range(nt):
            self.pool(lambda e, i=i: e.tensor_scalar(out=self.H[:, i, :], in0=self.H[:, i, :], scalar1=ALPHA, scalar2=None, op0=ALU.mult), [self.tH[i]], [self.tH[i]])
        NB = 2
        wg = [A.alloc([8, SLAB], BF16) for _ in range(NB)]
        wu = [A.alloc([8, SLAB], BF16) for _ in range(NB)]
        wd = [A.alloc([SLAB // 128, D], BF16) for _ in range(NB)]
        tw = [Tk() for _ in range(NB)]
        twu = [Tk() for _ in range(NB)]
        twd = [Tk() for _ in range(NB)]
        h1 = [A.alloc([SLAB // 128, 512], BF16) for _ in range(2)]
        th1 = [Tk() for _ in range(2)]
        sg = [A.alloc([512], F32) for _ in range(2)]
        tsg = [Tk() for _ in range(2)]
        yt = [A.alloc([D], F32)] * 2
        tyt = [Tk()] * 2
        nblk = (nt * 128 + 511) // 512
        cnt = 0
        hcnt = 0
        items = [(e_, s) for e_ in experts for s in range(nsl)]

        def issue(k):
            e_, s = items[k]
            Wg = self.moe_g[li, e_] if moe else self.ffn_g[li]
            Wu = self.moe_u[li, e_] if moe else self.ffn_u[li]
            Wd = self.moe_d[li, e_] if moe else self.ffn_d[li]
            b = k % NB
            c0 = s * SLAB
            w = min(SLAB, dff - c0)
            nch = w // 128
            S.dma("pool", wg[b][:, :, 0:w], Wg[:, c0:c0 + w].rearrange("(kc p) n -> p kc n", p=128), W=[tw[b]])
            S.dma("pool", wu[b][:, :, 0:w], Wu[:, c0:c0 + w].rearrange("(kc p) n -> p kc n", p=128), W=[twu[b]])
            S.dma("pool", wd[b][:, 0:nch, :], Wd[c0:c0 + w, :].rearrange("(fc p) n -> p fc n", p=128), W=[twd[b]])

        issue(0)
        for k, (e_, s) in enumerate(items):
            if True:
                if k + 1 < len(items):
                    issue(k + 1)
                b = k % NB
                c0 = s * SLAB
                w = min(SLAB, dff - c0)
                nch = w // 128
                def gu(tb, hb, b=b, nch=nch):
                    t0 = tb * 512
                    ntok = min(512, nt * 128 - t0)
                    tiles = list(range(t0 // 128, (t0 + ntok) // 128))
                    for fc in range(nch):
                        pg, pu = 0 + (fc % 2) * 2, 1 + (fc % 2) * 2
                        for kc in range(8):
                            self.mm(self.ps[pg][:, 0:ntok], wg[b][:, kc, fc * 128:(fc + 1) * 128], FT[:, kc, t0:t0 + ntok], kc == 0, kc == 7,
                                    [tw[b]] + [tFT[i] for i in tiles], [self.tp[pg]])
                        for kc in range(8):
                            self.mm(self.ps[pu][:, 0:ntok], wu[b][:, kc, fc * 128:(fc + 1) * 128], FT[:, kc, t0:t0 + ntok], kc == 0, kc == 7,
                                    [twu[b]] + [tFT[i] for i in tiles], [self.tp[pu]])
                        sb = fc % 2
                        self.act(lambda e, pg=pg, sb=sb, ntok=ntok: e.activation(out=sg[sb][:, 0:ntok], in_=self.ps[pg][:, 0:ntok], func=AF.Silu), [self.tp[pg]], [tsg[sb]])
                        self.dve(lambda e, pu=pu, sb=sb, hb=hb, fc=fc, ntok=ntok: e.tensor_tensor(out=h1[hb][:, fc, 0:ntok], in0=self.ps[pu][:, 0:ntok], in1=sg[sb][:, 0:ntok], op=ALU.mult),
                                 [self.tp[pu], tsg[sb]], [th1[hb]])

                def down(tb, hb, b=b, nch=nch, e_=e_):
                    t0 = tb * 512
                    ntok = min(512, nt * 128 - t0)
                    tiles = list(range(t0 // 128, (t0 + ntok) // 128))
                    for ti, i in enumerate(tiles):
                        v = 1 if i >= NL else 0
                        yb = i % 2
                        for hf in range(2):
                            pb = 4 + hf + 2 * (i % 2)
                            for fc in range(nch):
                                self.mm(self.ps[pb][:, :], h1[hb][:, fc, ti * 128:(ti + 1) * 128], wd[b][:, fc, hf * 512:(hf + 1) * 512], fc == 0, fc == nch - 1,
                                        [th1[hb], twd[b]], [self.tp[pb]])
                            if moe:
                                self.act(lambda e, pb=pb, hf=hf, yb=yb, i=i, e_=e_: e.activation(out=yt[yb][:, hf * 512:(hf + 1) * 512], in_=self.ps[pb][:, :], func=AF.Copy, scale=gate[:, i, e_:e_ + 1]),
                                         [self.tp[pb], tgate], [tyt[yb]])
                            else:
                                self.dve(lambda e, pb=pb, hf=hf, v=v, yb=yb: e.tensor_tensor(out=yt[yb][:, hf * 512:(hf + 1) * 512], in0=self.ps[pb][:, :], in1=G[:, v, hf * 512:(hf + 1) * 512], op=ALU.mult),
                                         [self.tp[pb], tG], [tyt[yb]])
                        if moe:
                            self.dve(lambda e, v=v, yb=yb: e.tensor_tensor(out=yt[yb], in0=yt[yb], in1=G[:, v, :], op=ALU.mult), [tyt[yb], tG], [tyt[yb]])
                        self.pool(lambda e, i=i, yb=yb: e.tensor_tensor(out=self.H[:, i, :], in0=yt[yb], in1=self.H[:, i, :], op=ALU.add), [tyt[yb], self.tH[i]], [self.tH[i]])

                hbs = []
                for tb in range(nblk):
                    hbs.append(hcnt % 2)
                    hcnt += 1
                gu(0, hbs[0])
                for tb in range(nblk):
                    if tb + 1 < nblk:
                        gu(tb + 1, hbs[tb + 1])
                    down(tb, hbs[tb])
        for i in range(nt):
            self.ln_only(i, LN, tLN, tmp, ttmp, st, tst)
        A.release(m)

    def ln_only(self, i, LN, tLN, tmp, ttmp, st, tst):
        for hf in range(2):
            self.dve(lambda e, hf=hf: e.bn_stats(out=st[:, hf * 6:(hf + 1) * 6], in_=self.H[:, i, hf * 512:(hf + 1) * 512]), [self.tH[i]], [tst])
        self.dve(lambda e: e.bn_aggr(out=st[:, 12:14], in_=st[:, 0:12]), [tst], [tst])
        self.rstd_from(st[:, 14:15], st[:, 13:14], 1.0, 1e-6, [tst], tst)
        self.dve(lambda e: e.tensor_scalar(out=tmp, in0=self.H[:, i, :], scalar1=st[:, 12:13], scalar2=st[:, 14:15], op0=ALU.subtract, op1=ALU.mult), [self.tH[i], tst], [ttmp])
        self.pool(lambda e: e.tensor_tensor(out=tmp, in0=tmp, in1=LN[:, 0, :], op=ALU.mult), [ttmp, tLN], [ttmp])
        self.pool(lambda e: e.tensor_tensor(out=self.H[:, i, :], in0=tmp, in1=LN[:, 1, :], op=ALU.add), [ttmp, tLN], [self.tH[i]])

    def router_setup(self, li):
        A, S = self.A, self.S
        wr = A.alloc([8, NE], F32); twr = Tk()
        rb = A.alloc([NE], F32)
        S.dma("sp", wr, self.moe_r[li].rearrange("(kc p) n -> p kc n", p=128), W=[twr])
        S.dma("sp", rb, bass.AP(self.dt_("moe_rb"), li * NE, [[0, 128], [1, NE]]), W=[twr])
        return dict(wr=wr, twr=twr, rb=rb, lg=A.alloc([NE], F32), tlg=Tk(), m8=A.alloc([8], F32), wk=A.alloc([2, NE], F32))

    def router_tile(self, rt, i, a32, ta32, gate, tgate):
        wr, twr, rb, lg, tlg, m8, wk = rt["wr"], rt["twr"], rt["rb"], rt["lg"], rt["tlg"], rt["m8"], rt["wk"]
        pb = 7
        for kc in range(8):
            self.mm(self.ps[pb][:, 0:NE], a32[:, kc, :], wr[:, kc, :], kc == 0, kc == 7, [ta32, twr], [self.tp[pb]])
        self.dve(lambda e: e.tensor_tensor(out=lg, in0=self.ps[pb][:, 0:NE], in1=rb, op=ALU.add), [self.tp[pb], twr], [tlg])
        self.dve(lambda e: e.max(out=m8, in_=lg), [tlg], [tlg])
        self.dve(lambda e: e.tensor_scalar(out=wk[:, 0, :], in0=lg, scalar1=m8[:, 1:2], scalar2=None, op0=ALU.is_ge), [tlg], [tlg])
        self.dve(lambda e: e.tensor_scalar(out=wk[:, 1, :], in0=lg, scalar1=m8[:, 0:1], scalar2=None, op0=ALU.subtract), [tlg], [tlg])
        self.act(lambda e: e.activation(out=wk[:, 1, :], in_=wk[:, 1, :], func=AF.Exp), [tlg], [tlg])
        self.dve(lambda e: e.tensor_tensor(out=wk[:, 1, :], in0=wk[:, 1, :], in1=wk[:, 0, :], op=ALU.mult), [tlg], [tlg])
        self.dve(lambda e: e.reduce_sum(out=m8[:, 2:3], in_=wk[:, 1, :], axis=AX.X), [tlg], [tlg])
        self.dve(lambda e: e.reciprocal(out=m8[:, 2:3], in_=m8[:, 2:3]), [tlg], [tlg])
        self.dve(lambda e: e.tensor_scalar(out=gate[:, i, :], in0=wk[:, 1, :], scalar1=m8[:, 2:3], scalar2=None, op0=ALU.mult), [tlg], [tgate])


    def rms_rope(self, src, tsrc, nh, dst, tdst, tile, rw, g=None, perm=False, qs=1.0):
        rope, trope = self.rope, self.tmc
        s3 = src.rearrange("p (h d) -> p h d", d=64)
        x, ss, t, tw = rw["x"], rw["ss"], rw["t"], rw["tw"]
        x3 = x[:, 0:nh * 64].rearrange("p (h d) -> p h d", d=64)
        if g is not None:
            for h in range(nh):
                self.act(lambda e, h=h: e.activation(out=x3[:, h, :], in_=s3[:, h, :], func=AF.Square, accum_out=ss[:, h:h + 1]), [tsrc], [tw])
            self.act(lambda e: e.activation(out=ss[:, 0:nh], in_=ss[:, 0:nh], func=AF.Sqrt, bias=self.epsc[:, 0:1], scale=1.0 / 64), [tw, self.tconst], [tw])
            self.dve(lambda e: e.reciprocal(out=ss[:, 0:nh], in_=ss[:, 0:nh]), [tw], [tw])
            for h in range(nh):
                self.dve(lambda e, h=h: e.scalar_tensor_tensor(out=x3[:, h, :], in0=s3[:, h, :], scalar=ss[:, h:h + 1], in1=g, op0=ALU.mult, op1=ALU.mult), [tsrc, tw, self.tmc], [tw])
            cur, tcur, qs = x3, tw, 1.0
        else:
            cur, tcur = s3, tsrc
        C = rope[:, tile, 0:32].unsqueeze(1).unsqueeze(1).to_broadcast([128, nh, 2, 32])
        Sg = rope[:, tile, 32:96].rearrange("p (a d) -> p a d", d=32).unsqueeze(1).to_broadcast([128, nh, 2, 32])
        c4 = cur.rearrange("p h (a d) -> p h a d", d=32)
        sw = c4[:, :, ::-1, :]
        t1 = t[:, 0, 0:nh * 64].rearrange("p (h a d) -> p h a d", a=2, d=32)
        t2 = t[:, 1, 0:nh * 64].rearrange("p (h a d) -> p h a d", a=2, d=32)
        if qs != 1.0:
            self.dve(lambda e: e.scalar_tensor_tensor(out=t1, in0=c4, scalar=qs, in1=C, op0=ALU.mult, op1=ALU.mult), [tcur, trope], [tw])
            self.dve(lambda e: e.scalar_tensor_tensor(out=t2, in0=sw, scalar=qs, in1=Sg, op0=ALU.mult, op1=ALU.mult), [tcur, trope], [tw])
        else:
            self.dve(lambda e: e.tensor_tensor(out=t1, in0=c4, in1=C, op=ALU.mult), [tcur, trope], [tw])
            self.dve(lambda e: e.tensor_tensor(out=t2, in0=sw, in1=Sg, op=ALU.mult), [tcur, trope], [tw])
        f1 = t[:, 0, 0:nh * 64].rearrange("p (h d) -> p h d", d=64)
        f2 = t[:, 1, 0:nh * 64].rearrange("p (h d) -> p h d", d=64)
        if perm:
            dv = dst.rearrange("p (b s d) -> p s b d", b=2, s=2, d=64)
            f1 = f1.rearrange("p (s b) d -> p s b d", b=2)
            f2 = f2.rearrange("p (s b) d -> p s b d", b=2)
        else:
            dv = dst.rearrange("p (h d) -> p h d", d=64)
        self.dve(lambda e: e.tensor_tensor(out=dv, in0=f1, in1=f2, op=ALU.add), [tw], [tdst])

    def mixer_phase(self, l):
        A, S = self.A, self.S
        last = (l == DEPTH - 1)
        nq = NL if last else NT
        m_all = A.mark()
        self.rope = A.alloc([NT, 96], F32)
        gqk = A.alloc([2, 64], F32)
        self.tmc = Tk()
        S.dma("sp", self.rope, self.c_rope, W=[self.tmc])
        S.dma("sp", gqk, bass.AP(self.dt_("gqk"), l * 128, [[0, 128], [1, 128]]), W=[self.tmc])
        OC = A.alloc([NT, 256], BF16); tOC = [Tk() for _ in range(NT)]
        m_s5 = A.mark()
        UT = A.alloc([2, NT * 128], BF16); tUT = [Tk() for _ in range(NT)]
        m0 = A.mark()
        aT = [A.alloc([8, 128], BF16) for _ in range(2)]; taT = [Tk() for _ in range(2)]
        Wu_ = A.alloc([8, 256], BF16); tWu = Tk()
        S.dma("pool", Wu_, self.w_in[l, :, 1280:1536].rearrange("(kc p) n -> p kc n", p=128), W=[tWu])
        for i in range(NT):
            b = i % 2
            self.make_aT(l, i, 0, aT[b], taT[b])
            for c in range(2):
                for kc in range(8):
                    self.mm(self.ps[2 + b][:, c * 128:(c + 1) * 128], Wu_[:, kc, c * 128:(c + 1) * 128], aT[b][:, kc, :], kc == 0, kc == 7, [taT[b], tWu], [self.tp[2 + b]])
            self.act(lambda e, i=i, b=b: e.activation(out=UT[:, :, i * 128:(i + 1) * 128], in_=self.ps[2 + b][:, 0:256].rearrange("p (c t) -> p c t", t=128), func=AF.Copy), [self.tp[2 + b]], [tUT[i]])
        A.release(m0)
        self.s5_phase(l, UT, tUT, OC, tOC, nq)
        A.release(m_s5)
        KT = A.alloc([4, NT * 128], BF16); tKT = [Tk() for _ in range(NT)]
        V = A.alloc([NT, 512], BF16); tV = [Tk() for _ in range(NT)]
        m0 = A.mark()
        rw = dict(x=A.alloc([256], F32), ss=A.alloc([4], F32), t=A.alloc([2, 256], F32), tw=Tk())
        aT = [A.alloc([8, 128], BF16) for _ in range(2)]; taT = [Tk() for _ in range(2)]
        W = A.alloc([8, 1024], BF16); tW = [Tk() for _ in range(6)]
        srcs = [(256, 256), (1024, 128), (1792, 128), (512, 256), (1152, 128), (1920, 128)]
        o = 0
        for k, (c0, w) in enumerate(srcs):
            S.dma("pool", W[:, :, o:o + w], self.w_in[l, :, c0:c0 + w].rearrange("(kc p) n -> p kc n", p=128), W=[tW[k]])
            o += w
        kbf = A.alloc([512], BF16); tkb = Tk()
        for i in range(NT):
            b = i % 2
            self.make_aT(l, i, 0, aT[b], taT[b])
            for kc in range(8):
                self.mm(self.ps[0][:, :], aT[b][:, kc, :], W[:, kc, 0:512], kc == 0, kc == 7, [taT[b]] + tW[0:3], [self.tp[0]])
            for kc in range(8):
                self.mm(self.ps[1][:, :], aT[b][:, kc, :], W[:, kc, 512:1024], kc == 0, kc == 7, [taT[b]] + tW[3:6], [self.tp[1]])
            self.act(lambda e, i=i: e.activation(out=V[:, i, :], in_=self.ps[1][:, :], func=AF.Copy), [self.tp[1]], [tV[i]])
            self.act(lambda e: e.activation(out=kbf[:, 0:256], in_=self.ps[0][:, 0:256], func=AF.Copy), [self.tp[0]], [tkb])
            self.rms_rope(self.ps[0][:, 256:384], self.tp[0], 2, kbf[:, 256:384], tkb, i, rw, g=gqk[:, 1, :])
            self.rms_rope(self.ps[0][:, 384:512], self.tp[0], 2, kbf[:, 384:512], tkb, i, rw)
            for c in range(4):
                self.mm(self.ps[3][:, c * 128:(c + 1) * 128], kbf[:, c * 128:(c + 1) * 128], self.ident_b, True, True, [tkb, self.tconst], [self.tp[3]])
            self.dve(lambda e, i=i: e.tensor_copy(out=KT[:, :, i * 128:(i + 1) * 128], in_=self.ps[3][:, :].rearrange("p (c t) -> p c t", t=128)), [self.tp[3]], [tKT[i]])
        A.release(m0)
        rw = dict(x=A.alloc([256], F32), ss=A.alloc([4], F32), t=A.alloc([2, 256], F32), tw=Tk())
        aT = [A.alloc([8, 128], BF16)] * 2; taT = [Tk()] * 2
        tri = A.alloc([2, 128], BF16); namask = A.alloc([2688], BF16); tmk = Tk()
        S.dma("sp", tri, self.c_tri, W=[tmk]); S.dma("sp", namask, self.c_namask, W=[tmk])
        G = A.alloc([2, D], F32); tG = Tk()
        LN = A.alloc([2, D], F32); tLN = Tk()
        self.ada_gate(l, 0, G, tG)
        self.load_ln(l, 0, LN, tLN)
        Wq = A.alloc([8, 768], BF16); tWq = [Tk() for _ in range(3)]
        for k, c0 in enumerate((0, 768, 1536)):
            S.dma("pool", Wq[:, :, k * 256:(k + 1) * 256], self.w_in[l, :, c0:c0 + 256].rearrange("(kc p) n -> p kc n", p=128), W=[tWq[k]])
        Wo = A.alloc([8, D], BF16); tWo = Tk()
        S.dma("pool", Wo, self.w_out[l].rearrange("(kc p) n -> p kc n", p=128), W=[tWo])
        Traw = A.alloc([4, 14, 64], BF16); tTr = Tk()
        mh = A.mark()
        hk = [A.alloc([16, 64], F32) for _ in range(2)]; thk = [Tk() for _ in range(2)]
        for h in range(4):
            for rl in range(2):
                S.dma("sp", hk[h % 2][rl * 64:(rl + 1) * 64, :, :], bass.AP(self.dt_("rpbh"), ((l * 4 + h) * 18 + (1 - rl)) * 128, [[1, 64], [128, 16], [1, 64]]), W=[thk[h % 2]])
            self.dve(lambda e, h=h: e.tensor_copy(out=Traw[:, h, :, :], in_=hk[h % 2][:, 1:15, ::-1]), [thk[h % 2]], [tTr])
        A.release(mh)
        sk = A.alloc([8], F32); tsk = Tk()
        S.dma("sp", sk[:, 0:4], bass.AP(self.dt_("sink"), l * 4, [[0, 128], [1, 4]]), W=[tsk])
        self.dve(lambda e: e.tensor_scalar(out=sk[:, 4:8], in0=sk[:, 0:4], scalar1=-1.0, scalar2=None, op0=ALU.mult), [tsk], [tsk])
        qbf = A.alloc([768], BF16); tqb = Tk()
        qT = A.alloc([6, 128], BF16); tqT = Tk()
        Ps = [A.alloc([NT * 128], BF16) for _ in range(2)]; tPs = [Tk() for _ in range(2)]
        P, tP = Ps[0], tPs[0]
        PT = [A.alloc([512], BF16) for _ in range(2)]; tPT = [Tk() for _ in range(2)]
        cc = A.alloc([D], BF16); tcc = Tk()
        ccT = A.alloc([8, 128], BF16); tccT = Tk()
        tmp = P[:, 0:2 * D].bitcast(F32); ttmp = tP
        st = A.alloc([16], F32); tst = Tk()
        sms = [A.alloc([16], F32) for _ in range(4)]; tsms = [Tk() for _ in range(4)]
        print('M2 arena top', A.top)
        ocnt = [0]
        acnt = [0]

        jobs = []

        def attention(*a, **kw):
            jobs.append((a, kw, {}))

        def att1(ctx_, i, blk, pb0, segs, vcol, oc0, sc, bias_h=None, s0=0, sink_h=None):
            nseg = len(segs)
            nb = (nseg + 3) // 4
            acnt[0] += 1
            sm, tsm = sms[acnt[0] % 4], tsms[acnt[0] % 4]
            P, tP = Ps[acnt[0] % 2], tPs[acnt[0] % 2]
            ctx_.update(sm=sm, tsm=tsm, P=P, tP=tP)
            b0 = 0 if nb > 2 else 2 * (acnt[0] % 2)
            banks = list(range(b0, b0 + nb))
            tb_ = [self.tp[k] for k in banks]
            ntot = nseg * 128
            Sall = self.psall[:, b0 * 512:b0 * 512 + ntot]
            q_ap = qT[pb0:pb0 + 64, blk, :]
            t = 0
            while t < nseg:
                kt, c, mask = segs[t]
                bk, cb = b0 + t // 4, (t % 4) * 128
                r = 1
                while (mask is None and t + r < nseg and (t + r) // 4 == t // 4 and segs[t + r][2] is None
                       and segs[t + r][0] == kt + r and segs[t + r][1] == c):
                    r += 1
                self.mm(self.ps[bk][:, cb:cb + 128 * r], q_ap, KT[pb0:pb0 + 64, c, kt * 128:(kt + r) * 128], True, mask is None,
                        [tqT] + [tKT[kt + k] for k in range(r)], [self.tp[bk]])
                if mask is not None:
                    self.mm(self.ps[bk][:, cb:cb + 128], self.ident_b, mask, False, True, [self.tconst, tmk], [self.tp[bk]])
                t += r
            if bias_h is not None:
                nloc = nseg - 2
                self.dve(lambda e: e.tensor_tensor(out=self.psall[:, b0 * 512:b0 * 512 + nloc * 128], in0=self.psall[:, b0 * 512:b0 * 512 + nloc * 128],
                                                   in1=Traw[:, bias_h, s0 - 1:s0 - 1 + 2 * nloc, :].rearrange("p s k -> p (s k)"), op=ALU.add), tb_ + [tTr], tb_)
            self.dve(lambda e: e.reduce_max(out=sm[:, 9:10], in_=Sall, axis=AX.X, negate=True), tb_, [tsm])
            if sink_h is not None:
                self.dve(lambda e: e.tensor_tensor(out=sm[:, 9:10], in0=sm[:, 9:10], in1=sk[:, 4 + sink_h:5 + sink_h], op=ALU.min), [tsm, tsk], [tsm])
            self.act(lambda e: e.activation(out=P[:, 0:ntot], in_=Sall, func=AF.Exp, bias=sm[:, 9:10], scale=1.0, accum_out=sm[:, 10:11]), tb_ + [tsm], [tP, tsm])
            if sink_h is not None:
                self.act(lambda e: e.activation(out=sm[:, 12:13], in_=sk[:, sink_h:sink_h + 1], func=AF.Exp, bias=sm[:, 9:10], scale=1.0), [tsk, tsm], [tsm])

        def att1b(ctx_, i, blk, pb0, segs, vcol, oc0, sc, bias_h=None, s0=0, sink_h=None):
            sm, tsm = ctx_["sm"], ctx_["tsm"]
            if sink_h is not None:
                self.dve(lambda e: e.tensor_tensor(out=sm[:, 10:11], in0=sm[:, 10:11], in1=sm[:, 12:13], op=ALU.add), [tsm], [tsm])
            self.dve(lambda e: e.reciprocal(out=sm[:, 11:12], in_=sm[:, 10:11]), [tsm], [tsm])

        def att2(ctx_, i, blk, pb0, segs, vcol, oc0, sc, bias_h=None, s0=0, sink_h=None):
            sm, tsm, P, tP = ctx_["sm"], ctx_["tsm"], ctx_["P"], ctx_["tP"]
            nseg = len(segs)
            ob = oc0 % 512
            for g0 in range(0, nseg, 4):
                gi = ocnt[0] % 2
                ocnt[0] += 1
                pt = 5 + gi
                ng = min(4, nseg - g0)
                for t in range(g0, g0 + ng):
                    self.mm(self.ps[pt][:, (t - g0) * 128:(t - g0 + 1) * 128], P[:, t * 128:(t + 1) * 128], self.ident_b, True, True, [tP, self.tconst], [self.tp[pt]])
                self.dve(lambda e, pt=pt, gi=gi, ng=ng: e.tensor_copy(out=PT[gi][:, 0:ng * 128], in_=self.ps[pt][:, 0:ng * 128]), [self.tp[pt]], [tPT[gi]])
                for t in range(g0, g0 + ng):
                    kt = segs[t][0]
                    self.mm(self.ps[7][:, ob:ob + 64], PT[gi][:, (t - g0) * 128:(t - g0 + 1) * 128], V[:, kt, vcol:vcol + 64], t == 0, t == nseg - 1, [tPT[gi], tV[kt]], [self.tp[7]])
            self.act(lambda e: e.activation(out=cc[:, oc0:oc0 + 64], in_=self.ps[7][:, ob:ob + 64], func=AF.Copy, scale=sm[:, 11:12]), [self.tp[7], tsm], [tcc])

        for i in range(nq):
            b = i % 2
            isctx = i >= NL
            self.make_aT(l, i, 0, aT[b], taT[b])
            for kc in range(8):
                self.mm(self.ps[0][:, :], aT[b][:, kc, :], Wq[:, kc, 0:512], kc == 0, kc == 7, [taT[b]] + tWq[0:2], [self.tp[0]])
            for kc in range(8):
                self.mm(self.ps[1][:, 0:256], aT[b][:, kc, :], Wq[:, kc, 512:768], kc == 0, kc == 7, [taT[b], tWq[2]], [self.tp[1]])
            self.act(lambda e: e.activation(out=qbf[:, 0:256], in_=self.ps[0][:, 0:256], func=AF.Copy), [self.tp[0]], [tqb])
            self.rms_rope(self.ps[0][:, 256:512], self.tp[0], 4, qbf[:, 256:512], tqb, i, rw, g=gqk[:, 0, :], perm=True)
            self.rms_rope(self.ps[1][:, 0:256], self.tp[1], 4, qbf[:, 512:768], tqb, i, rw, perm=True)
            for c in range(6):
                bk = 2 + c // 4
                self.mm(self.ps[bk][:, (c % 4) * 128:(c % 4 + 1) * 128], qbf[:, c * 128:(c + 1) * 128], self.ident_b, True, True, [tqb, self.tconst], [self.tp[bk]])
            self.dve(lambda e: e.tensor_scalar(out=qT[:, 0:4, :], in0=self.ps[2][:, :].rearrange("p (c t) -> p c t", t=128), scalar1=0.125, scalar2=None, op0=ALU.mult), [self.tp[2]], [tqT])
            self.dve(lambda e: e.tensor_scalar(out=qT[:, 4:6, :], in0=self.ps[3][:, 0:256].rearrange("p (c t) -> p c t", t=128), scalar1=0.125, scalar2=None, op0=ALU.mult), [self.tp[3]], [tqT])
            ctxs = [(16, None), (17, None)]
            for h in range(4):
                blk, pb0, c = h // 2, (h % 2) * 64, h // 2
                if isctx:
                    segs = [(kt, c, None) for kt, _ in ctxs]
                    attention(i, blk, pb0, segs, h * 64, h * 64, 0.125)
                else:
                    j = i
                    if 2 <= j <= 13:
                        var, t0, ntl, s0 = 0, j - 2, 5, 3
                    elif j == 0:
                        var, t0, ntl, s0 = 1, 0, 4, 7
                    elif j == 1:
                        var, t0, ntl, s0 = 2, 0, 4, 5
                    elif j == 14:
                        var, t0, ntl, s0 = 3, 12, 4, 3
                    else:
                        var, t0, ntl, s0 = 4, 12, 4, 1
                    mo = 0 if var == 0 else 640 + (var - 1) * 512
                    segs = [(t0 + k, c, namask[:, mo + k * 128:mo + (k + 1) * 128]) for k in range(ntl)] + [(kt, c, None) for kt, _ in ctxs]
                    attention(i, blk, pb0, segs, h * 64, h * 64, 1.0, bias_h=h, s0=s0)
            for h in range(4):
                blk, pb0, kvh = 2 + (h % 2), (h // 2) * 64, h // 2
                kts = [16, 17] if isctx else list(range(NT))
                attention(i, blk, pb0, [(kt, 2, None) for kt in kts], 256 + kvh * 64, 256 + h * 64, 0.125)
            self.pool(lambda e, i=i: e.tensor_copy(out=cc[:, 512:768], in_=OC[:, i, :]), [tOC[i]], [tcc])
            for h in range(4):
                blk, pb0, kvh = 4 + (h % 2), (h // 2) * 64, h // 2
                if isctx:
                    segs = [(16, 3, None), (17, 3, None)]
                else:
                    segs = []
                    if i - 1 >= 0: segs.append((i - 1, 3, tri[:, 0, :]))
                    segs.append((i, 3, None))
                    if i + 1 < NL: segs.append((i + 1, 3, tri[:, 1, :]))
                    segs += [(16, 3, None), (17, 3, None)]
                attention(i, blk, pb0, segs, 384 + kvh * 64, 768 + h * 64, 0.125, sink_h=h)
            att1(jobs[0][2], *jobs[0][0], **jobs[0][1])
            att1b(jobs[0][2], *jobs[0][0], **jobs[0][1])
            for k_ in range(len(jobs)):
                if k_ + 1 < len(jobs):
                    att1(jobs[k_ + 1][2], *jobs[k_ + 1][0], **jobs[k_ + 1][1])
                att2(jobs[k_][2], *jobs[k_][0], **jobs[k_][1])
                if k_ + 1 < len(jobs):
                    att1b(jobs[k_ + 1][2], *jobs[k_ + 1][0], **jobs[k_ + 1][1])
            del jobs[:]
            for c in range(8):
                bk = 5 + c // 4
                self.mm(self.ps[bk][:, (c % 4) * 128:(c % 4 + 1) * 128], cc[:, c * 128:(c + 1) * 128], self.ident_b, True, True, [tcc, self.tconst], [self.tp[bk]])
            for hf in range(2):
                self.act(lambda e, hf=hf: e.activation(out=ccT[:, hf * 4:(hf + 1) * 4, :], in_=self.ps[5 + hf][:, :].rearrange("p (c t) -> p c t", t=128), func=AF.Copy), [self.tp[5 + hf]], [tccT])
            for hf in range(2):
                for kc in range(8):
                    self.mm(self.ps[hf][:, :], ccT[:, kc, :], Wo[:, kc, hf * 512:(hf + 1) * 512], kc == 0, kc == 7, [tccT, tWo], [self.tp[hf]])
            self.resid_ln(i, [(self.ps[0][:, :], self.tp[0]), (self.ps[1][:, :], self.tp[1])], G, tG, LN, tLN, tmp, ttmp, st, tst)
        A.release(m_all)

    def sin_of(self, out, x, shift, wk, tk, R):
        TWO_PI = 2.0 * np.pi
        a, k = wk
        ki = k.bitcast(I32)
        self.dve(lambda e: e.tensor_scalar(out=a, in0=x, scalar1=1.0 / TWO_PI, scalar2=shift / TWO_PI + 0.5, op0=ALU.mult, op1=ALU.add), R, [tk])
        self.dve(lambda e: e.tensor_copy(out=ki, in_=a), [tk], [tk])
        self.dve(lambda e: e.tensor_copy(out=a, in_=ki), [tk], [tk])
        self.dve(lambda e: e.tensor_scalar(out=k, in0=x, scalar1=shift, scalar2=None, op0=ALU.add), R + [tk], [tk])
        self.dve(lambda e: e.scalar_tensor_tensor(out=k, in0=a, scalar=-TWO_PI, in1=k, op0=ALU.mult, op1=ALU.add), [tk], [tk])
        self.dve(lambda e: e.tensor_scalar(out=a, in0=k, scalar1=float(np.pi), scalar2=None, op0=ALU.is_gt), [tk], [tk])
        self.dve(lambda e: e.scalar_tensor_tensor(out=k, in0=a, scalar=-TWO_PI, in1=k, op0=ALU.mult, op1=ALU.add), [tk], [tk])
        self.dve(lambda e: e.tensor_scalar(out=a, in0=k, scalar1=-float(np.pi), scalar2=None, op0=ALU.is_lt), [tk], [tk])
        self.dve(lambda e: e.scalar_tensor_tensor(out=k, in0=a, scalar=TWO_PI, in1=k, op0=ALU.mult, op1=ALU.add), [tk], [tk])
        self.dve(lambda e: e.tensor_scalar(out=k, in0=k, scalar1=3.1415925, scalar2=-3.1415925, op0=ALU.min, op1=ALU.max), [tk], [tk])
        self.act(lambda e: e.activation(out=out, in_=k, func=AF.Sin), [tk], [tk])

    def s5_phase(self, l, UT, tUT, OC, tOC, nq):
        A, S = self.A, self.S
        dve, act, pool = self.dve, self.act, self.pool
        tp_ = Tk()
        vec = A.alloc([16, 3], F32)
        tau = A.alloc([130], F32)
        bs = A.alloc([16, 2, 16], F32)
        S.dma("sp", vec, self.ssm_vec[l], W=[tp_])
        S.dma("sp", tau, self.c_tau, W=[tp_])
        S.dma("sp", bs, self.ssm_bs[l], W=[tp_])
        CSb = A.alloc([16, 2, 32], BF16); tcs = Tk()
        DDb = A.alloc([8, 32], BF16)
        Wg = A.alloc([2, 256], BF16)
        S.dma("pool", CSb, self.ssm_cs[l], W=[tcs])
        S.dma("pool", DDb, self.ssm_dd[l], W=[tcs])
        S.dma("pool", Wg, self.ssm_wglu[l].rearrange("(c p) n -> p c n", p=128), W=[tcs])
        dve(lambda e: e.tensor_scalar(out=CSb[:, :, 1, :], in0=CSb[:, :, 1, :], scalar1=-1.0, scalar2=None, op0=ALU.mult), [tcs], [tcs])
        pv = A.alloc([16, 16], F32)
        def q(k): return pv[:, k, :]
        lre, lim, lst = vec[:, :, 0], vec[:, :, 1], vec[:, :, 2]
        STEP, LR, ANG, MAG, SN, CN, ABR, ABI, DEN, NUM, FRE, FIM, T0, T1 = range(14)
        act(lambda e: e.activation(out=q(STEP), in_=lst, func=AF.Exp), [tp_], [tp_])
        dve(lambda e: e.tensor_tensor(out=q(LR), in0=lre, in1=q(STEP), op=ALU.mult), [tp_], [tp_])
        dve(lambda e: e.tensor_tensor(out=q(ANG), in0=lim, in1=q(STEP), op=ALU.mult), [tp_], [tp_])
        act(lambda e: e.activation(out=q(MAG), in_=q(LR), func=AF.Exp), [tp_], [tp_])
        self.sin_of(q(SN), q(ANG), 0.0, (q(T0), q(T1)), tp_, [tp_])
        self.sin_of(q(CN), q(ANG), float(np.pi / 2), (q(T0), q(T1)), tp_, [tp_])
        dve(lambda e: e.tensor_tensor(out=q(ABR), in0=q(MAG), in1=q(CN), op=ALU.mult), [tp_], [tp_])
        dve(lambda e: e.tensor_tensor(out=q(ABI), in0=q(MAG), in1=q(SN), op=ALU.mult), [tp_], [tp_])
        dve(lambda e: e.tensor_tensor(out=q(DEN), in0=lre, in1=lre, op=ALU.mult), [tp_], [tp_])
        dve(lambda e: e.tensor_tensor(out=q(T0), in0=lim, in1=lim, op=ALU.mult), [tp_], [tp_])
        dve(lambda e: e.tensor_tensor(out=q(DEN), in0=q(DEN), in1=q(T0), op=ALU.add), [tp_], [tp_])
        dve(lambda e: e.reciprocal(out=q(DEN), in_=q(DEN)), [tp_], [tp_])
        dve(lambda e: e.tensor_scalar(out=q(NUM), in0=q(ABR), scalar1=-1.0, scalar2=None, op0=ALU.add), [tp_], [tp_])
        dve(lambda e: e.tensor_tensor(out=q(T0), in0=q(NUM), in1=lre, op=ALU.mult), [tp_], [tp_])
        dve(lambda e: e.tensor_tensor(out=q(T1), in0=q(ABI), in1=lim, op=ALU.mult), [tp_], [tp_])
        dve(lambda e: e.tensor_tensor(out=q(FRE), in0=q(T0), in1=q(T1), op=ALU.add), [tp_], [tp_])
        dve(lambda e: e.tensor_tensor(out=q(FRE), in0=q(FRE), in1=q(DEN), op=ALU.mult), [tp_], [tp_])
        dve(lambda e: e.tensor_tensor(out=q(T0), in0=q(ABI), in1=lre, op=ALU.mult), [tp_], [tp_])
        dve(lambda e: e.tensor_tensor(out=q(T1), in0=q(NUM), in1=lim, op=ALU.mult), [tp_], [tp_])
        dve(lambda e: e.tensor_tensor(out=q(FIM), in0=q(T0), in1=q(T1), op=ALU.subtract), [tp_], [tp_])
        dve(lambda e: e.tensor_tensor(out=q(FIM), in0=q(FIM), in1=q(DEN), op=ALU.mult), [tp_], [tp_])
        EC = A.alloc([16, 129], F32); ES = A.alloc([16, 129], F32); tE = Tk()
        mtab = A.mark()
        X = A.alloc([16, 129], F32); wa = A.alloc([16, 129], F32); wb = A.alloc([16, 129], F32); tX = Tk()
        dve(lambda e: e.tensor_tensor(out=X, in0=q(ANG).unsqueeze(2).to_broadcast([128, 16, 129]), in1=tau[:, 0:129].unsqueeze(1).to_broadcast([128, 16, 129]), op=ALU.mult), [tp_], [tX])
        self.sin_of(ES, X, 0.0, (wa, wb), tE, [tX])
        self.sin_of(EC, X, float(np.pi / 2), (wa, wb), tE, [tX])
        A.release(mtab)
        BT = A.alloc([16, 2, 128], BF16); tBT = Tk()
        mb = A.mark()
        bb = A.alloc([16, 2, 16], F32); tbb = Tk()
        w4 = A.alloc([4, 16, 16], F32)
        fre_b = q(FRE).unsqueeze(2).to_broadcast([128, 16, 16]); fim_b = q(FIM).unsqueeze(2).to_broadcast([128, 16, 16])
        dve(lambda e: e.tensor_tensor(out=w4[:, 0], in0=bs[:, :, 0, :], in1=fre_b, op=ALU.mult), [tp_], [tbb])
        dve(lambda e: e.tensor_tensor(out=w4[:, 1], in0=bs[:, :, 1, :], in1=fim_b, op=ALU.mult), [tp_], [tbb])
        dve(lambda e: e.tensor_tensor(out=w4[:, 2], in0=bs[:, :, 1, :], in1=fre_b, op=ALU.mult), [tp_], [tbb])
        dve(lambda e: e.tensor_tensor(out=w4[:, 3], in0=bs[:, :, 0, :], in1=fim_b, op=ALU.mult), [tp_], [tbb])
        dve(lambda e: e.tensor_tensor(out=bb[:, :, 0, :], in0=w4[:, 0], in1=w4[:, 1], op=ALU.subtract), [tbb], [tbb])
        dve(lambda e: e.tensor_tensor(out=bb[:, :, 1, :], in0=w4[:, 2], in1=w4[:, 3], op=ALU.add), [tbb], [tbb])
        Bp = A.alloc([4, 128], BF16); tBp = [Tk() for _ in range(4)]
        dve(lambda e: e.memset(Bp, 0.0), [], tBp)
        n = 0
        for dg in range(16):
            gc = dg % 8
            band = gc % 4
            for ri in range(2):
                for gl in range(2):
                    dve(lambda e, dg=dg, ri=ri, gl=gl, band=band: e.tensor_copy(out=Bp[gl * 64:(gl + 1) * 64, band, band * 32 + gl * 16:band * 32 + gl * 16 + 16],
                                                                            in_=bb[gl * 64:(gl + 1) * 64, dg, ri, :]), [tbb], [tBp[band]])
                bk = 5 + (n // 4) % 2
                self.mm(self.ps[bk][:, (n % 4) * 128:(n % 4 + 1) * 128], Bp[:, band, :], self.ident_b, True, True, [tBp[band], self.tconst], [self.tp[bk]])
                if n % 4 == 3:
                    dg0 = (n - 3) // 2
                    act(lambda e, bk=bk, dg0=dg0: e.activation(out=BT[:, dg0:dg0 + 2, :, :], in_=self.ps[bk][:, :].rearrange("p (a b t) -> p a b t", a=2, b=2), func=AF.Copy), [self.tp[bk]], [tBT])
                n += 1
        A.release(mb)
        Y = A.alloc([NT, 256], F32); tY = [Tk() for _ in range(NT)]
        z = A.alloc([256], F32); z2 = A.alloc([256], F32); tz = Tk()
        zb = A.alloc([256], BF16); zT = A.alloc([2, 128], BF16); tzT = Tk()
        DB = []
        for d in range(2):
            DB.append(dict(g=A.alloc([8, 2, 128], F32), tg=Tk(), gi=A.alloc([8, 2], F32), tgi=Tk(), giw=A.alloc([4, 8], F32),
                           tt=[A.alloc([2, 128], F32) for _ in range(4)], ttt=Tk(), w=A.alloc([2, 2, 128], F32), tw=Tk(),
                           hs=A.alloc([8, 2, 128], BF16), ths=Tk()))
        orders = [[16, 17] + list(range(16)), [17, 16] + list(range(15, -1, -1))]
        seen_tile = set()
        for n_ in range(NT):
          for d in range(2):
            i = orders[d][n_]
            X_ = DB[d]
            g, tg, gi, tgi, giw, tt, ttt, w, tw, hs, ths = (X_[k] for k in ("g", "tg", "gi", "tgi", "giw", "tt", "ttt", "w", "tw", "hs", "ths"))
            rv = (lambda ap: ap) if d == 0 else (lambda ap: ap[:, ::-1])
            tk = slice(i * 128, (i + 1) * 128)
            if n_ > 0:
                last = 127 if d == 0 else 0
                glr, gli = g[:, :, 0, last], g[:, :, 1, last]
                cT, sT = EC[:, d * 8:(d + 1) * 8, 128], ES[:, d * 8:(d + 1) * 8, 128]
                dve(lambda e, glr=glr, cT=cT, giw=giw: e.tensor_tensor(out=giw[:, 0], in0=glr, in1=cT, op=ALU.mult), [tg, tE], [tgi])
                dve(lambda e, gli=gli, sT=sT, giw=giw: e.tensor_tensor(out=giw[:, 1], in0=gli, in1=sT, op=ALU.mult), [tg, tE], [tgi])
                dve(lambda e, glr=glr, sT=sT, giw=giw: e.tensor_tensor(out=giw[:, 2], in0=glr, in1=sT, op=ALU.mult), [tg, tE], [tgi])
                dve(lambda e, gli=gli, cT=cT, giw=giw: e.tensor_tensor(out=giw[:, 3], in0=gli, in1=cT, op=ALU.mult), [tg, tE], [tgi])
                dve(lambda e, gi=gi, giw=giw: e.tensor_tensor(out=gi[:, :, 0], in0=giw[:, 0], in1=giw[:, 1], op=ALU.subtract), [tgi], [tgi])
                dve(lambda e, gi=gi, giw=giw: e.tensor_tensor(out=gi[:, :, 1], in0=giw[:, 2], in1=giw[:, 3], op=ALU.add), [tgi], [tgi])
            for bq in range(4):
                bk = d * 2 + bq % 2
                for g2 in range(2):
                    gc = bq * 2 + g2
                    for ri in range(2):
                        c0 = g2 * 256 + ri * 128
                        self.mm(self.ps[bk][:, c0:c0 + 128], BT[:, d * 8 + gc, ri, :], UT[:, gc // 4, tk], True, True, [tBT, tUT[i]], [self.tp[bk]])
                pv4 = self.ps[bk][:, :].rearrange("p (a b t) -> p a b t", a=2, b=2)
                bre, bim = pv4[:, :, 0, :], pv4[:, :, 1, :]
                dg0 = d * 8 + bq * 2
                cs_, sn_ = rv_tab(EC, dg0, d), rv_tab(ES, dg0, d)
                dve(lambda e, bre=bre, cs_=cs_, tt=tt: e.tensor_tensor(out=tt[0], in0=bre, in1=cs_, op=ALU.mult), [self.tp[bk], tE], [ttt])
                dve(lambda e, bim=bim, sn_=sn_, tt=tt: e.tensor_tensor(out=tt[1], in0=bim, in1=sn_, op=ALU.mult), [self.tp[bk], tE], [ttt])
                dve(lambda e, bim=bim, cs_=cs_, tt=tt: e.tensor_tensor(out=tt[2], in0=bim, in1=cs_, op=ALU.mult), [self.tp[bk], tE], [ttt])
                dve(lambda e, bre=bre, sn_=sn_, tt=tt: e.tensor_tensor(out=tt[3], in0=bre, in1=sn_, op=ALU.mult), [self.tp[bk], tE], [ttt])
                pool(lambda e, w=w, tt=tt: e.tensor_tensor(out=w[:, :, 0, :], in0=tt[0], in1=tt[1], op=ALU.add), [ttt], [tw])
                pool(lambda e, w=w, tt=tt: e.tensor_tensor(out=w[:, :, 1, :], in0=tt[2], in1=tt[3], op=ALU.subtract), [ttt], [tw])
                for g2 in range(2):
                    gc = bq * 2 + g2
                    for ri in range(2):
                        init = 0.0 if n_ == 0 else gi[:, gc, ri:ri + 1]
                        dve(lambda e, gc=gc, ri=ri, g2=g2, init=init, g=g, w=w, rv=rv, d=d: e.tensor_tensor_scan(out=rv(g[:, gc, ri, :]), data0=q(MAG)[:, d * 8 + gc:d * 8 + gc + 1].to_broadcast([128, 128]),
                                                                                      data1=rv(w[:, g2, ri, :]), initial=init, op0=ALU.mult, op1=ALU.add),
                            [tw, tp_, tgi], [tg])
            for hh in range(4):
                gre, gim = g[:, hh * 2:(hh + 1) * 2, 0, :], g[:, hh * 2:(hh + 1) * 2, 1, :]
                dg0 = d * 8 + hh * 2
                cs_, sn_ = rv_tab(EC, dg0, d), rv_tab(ES, dg0, d)
                dve(lambda e, gre=gre, cs_=cs_, tt=tt: e.tensor_tensor(out=tt[0], in0=gre, in1=cs_, op=ALU.mult), [tg, tE], [ttt])
                dve(lambda e, gim=gim, sn_=sn_, tt=tt: e.tensor_tensor(out=tt[1], in0=gim, in1=sn_, op=ALU.mult), [tg, tE], [ttt])
                dve(lambda e, gre=gre, sn_=sn_, tt=tt: e.tensor_tensor(out=tt[2], in0=gre, in1=sn_, op=ALU.mult), [tg, tE], [ttt])
                dve(lambda e, gim=gim, cs_=cs_, tt=tt: e.tensor_tensor(out=tt[3], in0=gim, in1=cs_, op=ALU.mult), [tg, tE], [ttt])
                pool(lambda e, hh=hh, hs=hs, tt=tt: e.tensor_tensor(out=hs[:, hh * 2:(hh + 1) * 2, 0, :], in0=tt[0], in1=tt[1], op=ALU.subtract), [ttt], [ths])
                pool(lambda e, hh=hh, hs=hs, tt=tt: e.tensor_tensor(out=hs[:, hh * 2:(hh + 1) * 2, 1, :], in0=tt[2], in1=tt[3], op=ALU.add), [ttt], [ths])
            yb_ = 4 + d
            for gc in range(8):
                terms = [(hs[:, gc, 0, :], CSb[:, d * 8 + gc, 0, :], [ths, tcs]), (hs[:, gc, 1, :], CSb[:, d * 8 + gc, 1, :], [ths, tcs])]
                if d == 0:
                    terms.append((UT[:, gc // 4, tk], DDb[:, gc, :], [tUT[i], tcs]))
                for k, (lt, rh, R_) in enumerate(terms):
                    self.mm(self.ps[yb_][:, gc * 32:(gc + 1) * 32], lt, rh, k == 0, k == len(terms) - 1, R_, [self.tp[yb_]])
            if i not in seen_tile:
                seen_tile.add(i)
                act(lambda e, i=i, yb_=yb_: e.activation(out=Y[:, i, :], in_=self.ps[yb_][:, 0:256], func=AF.Copy), [self.tp[yb_]], [tY[i]])
                continue
            if i >= nq:
                continue
            dve(lambda e, i=i, yb_=yb_: e.tensor_tensor(out=z, in0=self.ps[yb_][:, 0:256], in1=Y[:, i, :], op=ALU.add), [self.tp[yb_], tY[i]], [tz])
            pool(lambda e: e.tensor_tensor(out=z2, in0=z, in1=z, op=ALU.mult), [tz], [tz])
            pool(lambda e: e.tensor_scalar(out=z2, in0=z2, scalar1=0.044715, scalar2=1.0, op0=ALU.mult, op1=ALU.add), [tz], [tz])
            pool(lambda e: e.tensor_tensor(out=z2, in0=z2, in1=z, op=ALU.mult), [tz], [tz])
            act(lambda e: e.activation(out=z2, in_=z2, func=AF.Sigmoid, scale=1.5957691216057308), [tz], [tz])
            dve(lambda e: e.tensor_tensor(out=z, in0=z, in1=z2, op=ALU.mult), [tz], [tz])
            dve(lambda e: e.tensor_copy(out=zb, in_=z), [tz], [tz])
            for c in range(2):
                self.mm(self.ps[6][:, c * 128:(c + 1) * 128], zb[:, c * 128:(c + 1) * 128], self.ident_b, True, True, [tz, self.tconst], [self.tp[6]])
            act(lambda e: e.activation(out=zT, in_=self.ps[6][:, 0:256].rearrange("p (c t) -> p c t", t=128), func=AF.Copy), [self.tp[6]], [tzT])
            for c in range(2):
                self.mm(self.ps[7][:, 0:256], zT[:, c, :], Wg[:, c, :], c == 0, c == 1, [tzT, tcs], [self.tp[7]])
            act(lambda e: e.activation(out=z2, in_=self.ps[7][:, 0:256], func=AF.Sigmoid), [self.tp[7]], [tz])
            dve(lambda e, i=i: e.tensor_tensor(out=OC[:, i, :], in0=z, in1=z2, op=ALU.mult), [tz], [tOC[i]])


def rv_tab(E, dg0, d, n=2):
    if d == 0:
        return E[:, dg0:dg0 + n, 0:128]
    return E[:, dg0:dg0 + n, 127::-1]


def _consts():
    c = {}
    c["c_ident_f"] = np.eye(128, dtype=np.float32)
    c["c_ident_b"] = np.eye(128, dtype=np.float32).astype(ml_dtypes.bfloat16)
    pos = np.arange(NL * 128)
    row = (pos // 64).astype(np.float32)
    col = (pos % 64).astype(np.float32)
    inv = (10000.0 ** (-np.arange(16, dtype=np.float32) / 16)).astype(np.float32)
    ang = np.concatenate([row[:, None] * inv, col[:, None] * inv], -1).astype(np.float32)
    cs = np.concatenate([np.cos(ang), -np.sin(ang), np.sin(ang)], -1).astype(np.float32)
    ctxcs = np.concatenate([np.ones((256, 32), np.float32), np.zeros((256, 64), np.float32)], -1)
    cs = np.concatenate([cs, ctxcs], 0).reshape(NT, 128, 96).transpose(1, 0, 2)
    c["c_rope"] = np.ascontiguousarray(cs)
    q = np.arange(128)[:, None]
    k = np.arange(128)[None, :]
    tri = np.stack([np.where(k >= q, 0.0, NEG), np.where(k <= q, 0.0, NEG)], 1).astype(np.float32)
    c["c_tri"] = tri.astype(ml_dtypes.bfloat16)
    nm = np.full((5, 128, 10, 64), NEG, np.float32)
    qc = np.arange(64)
    cstart = np.clip(qc - 8, 0, 48)
    kc = np.arange(64)
    colv = (kc[None, :] >= cstart[:, None]) & (kc[None, :] < cstart[:, None] + 16)
    def fill(var, j, i0, nr):
        for rl in range(2):
            r = 2 * j + rl
            rs = int(np.clip(r - 4, 0, 24))
            for ii in range(nr):
                i = i0 + ii
                if rs <= i < rs + 8:
                    blk = np.where(colv, 0.0, NEG)
                    nm[var, rl * 64:(rl + 1) * 64, ii, :] = blk
    fill(0, 5, 6, 10)
    fill(1, 0, 0, 8); fill(2, 1, 0, 8); fill(3, 14, 24, 8); fill(4, 15, 24, 8)
    nm2 = nm.transpose(1, 0, 2, 3).reshape(128, 5, 640)
    c["c_namask"] = np.ascontiguousarray(np.concatenate([nm2[:, 0, :]] + [nm2[:, v, 0:512] for v in range(1, 5)], 1)).astype(ml_dtypes.bfloat16)
    tau = np.zeros((128, 130), np.float32)
    tau[:, :] = np.arange(130, dtype=np.float32)[None, :]
    c["c_tau"] = tau
    return c


def _prep_shared(I):
    f = np.float32
    d = dict(_consts())
    for k in ("ada_w", "ada_b", "w_in", "w_out"):
        d[k] = np.ascontiguousarray(I[k], dtype=f)
    rp = np.zeros((DEPTH, 4, 18, 128), f)
    rp[:, :, 1:16, 48:79] = I["na_rpb"][:, :, :, ::-1]
    d["rpbh"] = rp
    d["gqk"] = np.ascontiguousarray(np.stack([I["ga_q_norm"], I["ga_k_norm"]], 1), dtype=f)
    def st(a):
        L = a.shape[0]
        rest = a.shape[4:]
        a = a.reshape((L, 2, 8, 2, 64) + rest)
        a = np.moveaxis(a, (3, 4), (1, 2))
        return np.ascontiguousarray(a.reshape((L, 128, 16) + rest))
    ls = np.broadcast_to(I["ssm_log_step"][..., None], I["ssm_lambda_re"].shape)
    d["ssm_vec"] = np.ascontiguousarray(np.stack([st(I["ssm_lambda_re"]), st(I["ssm_lambda_im"]), st(np.ascontiguousarray(ls))], -1), dtype=f)
    d["ssm_bs"] = np.ascontiguousarray(np.stack([st(I["ssm_b_re"]), st(I["ssm_b_im"])], -2), dtype=f)
    cre = st(np.swapaxes(I["ssm_c_re"], -1, -2))
    cim = st(np.swapaxes(I["ssm_c_im"], -1, -2))
    cs = np.zeros((DEPTH, 128, 16, 2, 32), f)
    for gl in range(2):
        cs[:, gl * 64:(gl + 1) * 64, :, 0, gl * 16:(gl + 1) * 16] = cre[:, gl * 64:(gl + 1) * 64]
        cs[:, gl * 64:(gl + 1) * 64, :, 1, gl * 16:(gl + 1) * 16] = cim[:, gl * 64:(gl + 1) * 64]
    d["ssm_cs"] = cs
    dd = np.zeros((DEPTH, 128, 8, 32), f)
    sd = I["ssm_d"].reshape(DEPTH, 2, 128)
    for gc in range(8):
        for j in range(32):
            pl = (gc % 4) * 32 + j
            dd[:, pl, gc, j] = sd[:, gc // 4, pl]
    d["ssm_dd"] = dd
    d["ssm_wglu"] = np.ascontiguousarray(I["ssm_w_glu"], dtype=f)
    d["sink"] = np.ascontiguousarray(I["sw_sink"], dtype=f)
    d["lngb"] = np.ascontiguousarray(np.stack([I["ln1_g"], I["ln1_b"], I["ln2_g"], I["ln2_b"]], 1), dtype=f)
    d["ffn_g"] = I["ffn_w_gate"]; d["ffn_u"] = I["ffn_w_up"]; d["ffn_d"] = I["ffn_w_down"]
    d["moe_r"] = I["moe_w_router"]; d["moe_rb"] = I["moe_b_router"]
    d["moe_g"] = I["moe_w_gate"]; d["moe_u"] = I["moe_w_up"]; d["moe_d"] = I["moe_w_down"]
    return d


def _prep_core(I, b, hx=None):
    if hx is None:
        hx = np.concatenate([I["x"][b], I["ctx"][b]], 0)
    cv = np.stack([I["c"][b].reshape(8, 128).T, I["c_ctx"].reshape(8, 128).T], -1)
    return {"hx": np.ascontiguousarray(hx, dtype=np.float32), "cv": np.ascontiguousarray(cv, dtype=np.float32)}


def build_program(layers=(0, 1, 2, 3)):
    b = B(list(layers))
    b.setup()
    for l in layers:
        b.ada_cols(l)
        b.mixer_phase(l)
        b.ffn_phase(l)
    for i in range(NL):
        b.store(b.out[i * 128:(i + 1) * 128, :], b.H[:, i, :], [b.tH[i]])
    with b.nc.Block() as blk:
        b.S.emit(blk, b.fin)
    return b


def kernel(**inputs):
    I = {k: np.asarray(v) for k, v in inputs.items()}
    n = 8
    b = build_program()
    shared = _prep_shared(I)
    shared = {k: v for k, v in shared.items() if k in b.din}
    in_maps = []
    for c in range(n):
        m = dict(shared)
        m.update(_prep_core(I, c))
        in_maps.append(m)
    res = run_bass_kernel_spmd(b.nc, in_maps, core_ids=list(range(n)))
    out = np.stack([np.asarray(r["out"], dtype=np.float32).reshape(NL * 128, D) for r in res.results], 0)
    return out
```

```python
import numpy as np
import ml_dtypes
import concourse.bass as bass
import concourse.mybir as mybir
from concourse.bass_utils import run_bass_kernel_spmd

F32 = mybir.dt.float32
BF16 = mybir.dt.bfloat16
I32 = mybir.dt.int32
ALU = mybir.AluOpType
AF = mybir.ActivationFunctionType
AX = mybir.AxisListType


class Tk:
    __slots__ = ("w", "r")

    def __init__(self):
        self.w = None
        self.r = []


class Sched:
    ENG = ("pe", "act", "dve", "pool", "sp")
    NDMA = {"sp": 12, "pool": 8, "act": 4}

    def __init__(self, nc):
        self.nc = nc
        self.prog = {e: [] for e in self.ENG}
        self.n = {e: 0 for e in self.ENG}
        self.seen = {e: {} for e in self.ENG}
        self.signaled = {e: set() for e in self.ENG}
        self.dma_rr = {q: 0 for q in self.NDMA}
        self.dma_tot = {}
        self.lastc = {e: 0 for e in self.ENG}
        self.cur_fence = {}

    def fence(self):
        for e in self.ENG:
            if self.lastc[e]:
                self.cur_fence[e] = self.lastc[e]
        for key, tot in self.dma_tot.items():
            self.cur_fence[key] = tot

    def _deps(self, eng, reads, writes):
        deps = dict(self.cur_fence)
        def add(t):
            if t is None:
                return
            k, v = t
            if deps.get(k, 0) < v:
                deps[k] = v
        for t in reads:
            add(t.w)
        for t in writes:
            add(t.w)
            for r in t.r:
                add(r)
        waits = []
        for k, v in deps.items():
            if k == "pe" and eng == "pe":
                continue
            if self.seen[eng].get(k, 0) < v:
                self.seen[eng][k] = v
                waits.append((k, v))
                if k in self.signaled:
                    self.signaled[k].add(v)
        return waits

    def _commit(self, ticket, reads, writes):
        for t in reads:
            t.r.append(ticket)
        for t in writes:
            t.w = ticket
            t.r = []

    def op(self, eng, fn, R=(), W=()):
        waits = self._deps(eng, R, W)
        self.n[eng] += 1
        self.lastc[eng] = self.n[eng]
        ticket = (eng, self.n[eng])
        self.prog[eng].append((waits, fn, ticket, None))
        self._commit(ticket, R, W)

    def dma(self, q, out, in_, R=(), W=(), slow=False):
        j = self.dma_rr[q]
        self.dma_rr[q] = (j + 1) % self.NDMA[q]
        key = ("d", q, j)
        waits = self._deps(q, R, W)
        prev = self.dma_tot.get(key, 0)
        if prev and self.seen[q].get(key, 0) < prev:
            self.seen[q][key] = prev
            waits.append((key, prev))
        self.dma_tot[key] = prev + 16
        ticket = (key, prev + 16)
        self.n[q] += 1
        if slow:
            fn = lambda e, o=out, i=in_: e.dma_start(out=o, in_=i, allow_slow_non_contiguous=True)
        else:
            fn = lambda e, o=out, i=in_: e.dma_start(out=o, in_=i)
        self.prog[q].append((waits, fn, (q, self.n[q]), key))
        self._commit(ticket, R, W)

    def emit(self, block, final_waits):
        nc = self.nc
        sems = {e: nc.alloc_semaphore("s_" + e) for e in self.ENG}
        for key in self.dma_tot:
            sems[key] = nc.alloc_semaphore("d_%s%d" % (key[1], key[2]))
        rank = {}
        for e in self.ENG:
            rank[e] = {v: i + 1 for i, v in enumerate(sorted(self.signaled[e]))}

        def val(k, v):
            return rank[k][v] if k in rank else v

        def run(ename, eng):
            for waits, fn, ticket, dkey in self.prog[ename]:
                for k, v in waits:
                    eng.wait_ge(sems[k], val(k, v))
                ins = fn(eng)
                if dkey is not None:
                    ins.then_inc(sems[dkey], 16)
                elif ticket[1] in self.signaled[ename]:
                    ins.then_inc(sems[ename], 1)
            if ename == "sp":
                for k, v in final_waits:
                    eng.wait_ge(sems[k], val(k, v))

        for k, v in final_waits:
            if k in self.signaled:
                self.signaled[k].add(v)
        for e in self.ENG:
            rank[e] = {v: i + 1 for i, v in enumerate(sorted(self.signaled[e]))}
        block.tensor(lambda e: run("pe", e))
        block.scalar(lambda e: run("act", e))
        block.vector(lambda e: run("dve", e))
        block.gpsimd(lambda e: run("pool", e))
        block.sync(lambda e: run("sp", e))


class Arena:
    def __init__(self, nc, nbytes, S=None):
        self.S = S
        self.t = nc.alloc_sbuf_tensor("arena", [128, nbytes // 4], F32)
        self.nbytes = nbytes
        self.top = 0
        self.peak = 0

    def alloc(self, free_shape, dtype):
        esz = 2 if dtype == BF16 else 4
        n = int(np.prod(free_shape))
        nb = (n * esz + 63) // 64 * 64
        off = self.top
        self.top += nb
        self.peak = max(self.peak, self.top)
        assert self.top <= self.nbytes, ("arena overflow", self.top, self.nbytes)
        ap = self.t[:, off // 4:(off + nb) // 4]
        if dtype != F32:
            ap = ap.bitcast(dtype)
        ap = ap[:, 0:n]
        if len(free_shape) == 2:
            ap = ap.rearrange("p (a b) -> p a b", b=free_shape[1])
        elif len(free_shape) == 3:
            ap = ap.rearrange("p (a b c) -> p a b c", b=free_shape[1], c=free_shape[2])
        return ap

    def mark(self):
        return self.top

    def release(self, m):
        self.top = m
        self.S.fence()


D = 1024
NT = 18
NL = 16
DEPTH = 4
DFF = 2816
DFE = 3584
NE = 8
ALPHA = float((2 * DEPTH) ** 0.25)
NEG = -1.0e30
SLAB = 512


class B:
    def __init__(self, layers, dbg=None):
        self.layers = layers
        self.dbg = dbg or {}
        nc = self.nc = bass.Bass("TRN2", target_bir_lowering=False)
        self.S = Sched(nc)
        self.A = Arena(nc, 212800, self.S)
        self.fin = []
        self.din = {}
        self.psall = nc.alloc_psum_tensor("psall", [128, 8 * 512], F32)
        self.ps = [self.psall[:, i * 512:(i + 1) * 512] for i in range(8)]
        self.tp = [Tk() for _ in range(8)]

    SHAPES = {
        "hx": ([NT * 128, D], F32), "cv": ([128, 8, 2], F32), "c_ident_f": ([128, 128], F32), "c_ident_b": ([128, 128], BF16),
        "c_rope": ([128, NT, 96], F32), "c_tri": ([128, 2, 128], BF16), "c_namask": ([128, 2688], BF16), "c_tau": ([128, 130], F32),
        "ada_w": ([DEPTH, D, 6 * D], F32), "ada_b": ([DEPTH, 6 * D], F32), "w_in": ([DEPTH, D, 2048], F32), "w_out": ([DEPTH, D, D], F32),
        "rpbh": ([DEPTH, 4, 18, 128], F32), "gqk": ([DEPTH, 2, 64], F32), "ssm_vec": ([DEPTH, 128, 16, 3], F32),
        "ssm_bs": ([DEPTH, 128, 16, 2, 16], F32), "ssm_cs": ([DEPTH, 128, 16, 2, 32], F32), "ssm_dd": ([DEPTH, 128, 8, 32], F32),
        "ssm_wglu": ([DEPTH, 256, 256], F32), "sink": ([DEPTH, 4], F32), "lngb": ([DEPTH, 4, D], F32),
        "ffn_g": ([2, D, DFF], F32), "ffn_u": ([2, D, DFF], F32), "ffn_d": ([2, DFF, D], F32),
        "moe_r": ([2, D, NE], F32), "moe_rb": ([2, NE], F32), "moe_g": ([2, NE, D, DFE], F32), "moe_u": ([2, NE, D, DFE], F32),
        "moe_d": ([2, NE, DFE, D], F32),
    }

    def __getattr__(self, name):
        sh = B.SHAPES.get(name)
        if sh is None:
            raise AttributeError(name)
        ap = self.inp(name, sh[0], sh[1])
        self.__dict__[name] = ap
        return ap

    def dt_(self, name):
        getattr(self, name)
        return self.din[name]

    def inp(self, name, shape, dt=F32):
        t = self.nc.dram_tensor(name, list(shape), dt, kind="ExternalInput")
        self.din[name] = t
        return t.ap()

    def outp(self, name, shape, dt=F32):
        return self.nc.dram_tensor(name, list(shape), dt, kind="ExternalOutput").ap()

    def store(self, dst, src, R):
        S = self.S
        S.dma("sp", dst, src, R=R)
        j = (S.dma_rr["sp"] - 1) % S.NDMA["sp"]
        key = ("d", "sp", j)
        self.fin.append((key, S.dma_tot[key]))

    def pe(self, fn, R=(), W=()): self.S.op("pe", fn, R, W)
    def act(self, fn, R=(), W=()): self.S.op("act", fn, R, W)
    def dve(self, fn, R=(), W=()): self.S.op("dve", fn, R, W)
    def pool(self, fn, R=(), W=()): self.S.op("pool", fn, R, W)

    def mm(self, out, lhsT, rhs, start, stop, R, W):
        self.pe(lambda e: e.matmul(out, lhsT=lhsT, rhs=rhs, start=start, stop=stop), R, W)

    def tr(self, out, in_, R, W):
        self.pe(lambda e: e.transpose(out=out, in_=in_, identity=self.ident_f), R + [self.tconst], W)

    def rstd_from(self, out, var_ap, scale, eps, R, tk):
        self.act(lambda e: e.activation(out=out, in_=var_ap, func=AF.Sqrt, bias=self.eps_ap(eps), scale=scale), R, [tk])
        self.dve(lambda e: e.reciprocal(out=out, in_=out), [tk], [tk])

    def eps_ap(self, eps):
        return self.epsc[:, 0:1]

    def setup(self):
        A = self.A
        inp = self.inp
        self.out = self.outp("out", [NL * 128, D])

        S = self.S
        self.tconst = Tk()
        self.H = A.alloc([NT, D], F32)
        self.tH = [Tk() for _ in range(NT)]
        self.ident_f = A.alloc([128], F32)
        self.ident_b = A.alloc([128], BF16)
        self.epsc = A.alloc([2], F32)
        self.csil = A.alloc([8, 2], F32)
        self.modc = A.alloc([DEPTH, 32, 2], F32)
        self.tmodc = Tk()
        for dst, src in ((self.ident_f, self.c_ident_f), (self.ident_b, self.c_ident_b), (self.csil, self.cv)):
            S.dma("sp", dst, src, W=[self.tconst])
        self.dve(lambda e: e.memset(self.epsc, 1e-6), W=[self.tconst])
        for i in range(NT):
            S.dma("sp" if i % 2 == 0 else "act", self.H[:, i, :], self.hx[i * 128:(i + 1) * 128, :], W=[self.tH[i]])
        self.act(lambda e: e.activation(out=self.csil, in_=self.csil, func=AF.Silu), [self.tconst], [self.tconst])

    def ada_cols(self, l):
        A, S = self.A, self.S
        m = A.mark()
        blocks = [0, 1, 3, 4]
        wst = [A.alloc([8, 128], F32) for _ in range(3)]
        tw = [Tk() for _ in range(3)]
        bcol = A.alloc([32], F32)
        tb = Tk()
        for j, blk in enumerate(blocks):
            S.dma("sp", bcol[:, j * 8:(j + 1) * 8], self.ada_b[l, blk * D:(blk + 1) * D].rearrange("(kc p) -> p kc", p=128), W=[tb], slow=True)
        n = 0
        for j, blk in enumerate(blocks):
            for fc in range(8):
                b = n % 3
                col0 = blk * D + fc * 128
                S.dma("sp" if n % 2 == 0 else "act", wst[b], self.ada_w[l, :, col0:col0 + 128].rearrange("(kc p) n -> p kc n", p=128), W=[tw[b]])
                pb = 7
                for kc in range(8):
                    self.mm(self.ps[pb][:, 0:2], wst[b][:, kc, :], self.csil[:, kc, :], kc == 0, kc == 7, [tw[b], self.tconst], [self.tp[pb]])
                idx = j * 8 + fc
                add = 1.0 if blk in (1, 4) else 0.0
                self.dve(lambda e, idx=idx, add=add, pb=pb: e.tensor_scalar(out=self.modc[:, l, idx, :], in0=self.ps[pb][:, 0:2], scalar1=bcol[:, idx:idx + 1],
                                                                           scalar2=add, op0=ALU.add, op1=ALU.add), [self.tp[pb], tb], [self.tmodc])
                n += 1
        A.release(m)

    def ada_gate(self, l, which, G, tG):
        A, S = self.A, self.S
        m = A.mark()
        blk = 2 if which == 0 else 5
        crep = A.alloc([2, 8, 128], F32); tcr = Tk()
        for v in range(2):
            for kc in range(8):
                self.dve(lambda e, v=v, kc=kc: e.tensor_copy(out=crep[:, v, kc, :], in_=self.csil[:, kc, v:v + 1].to_broadcast([128, 128])),
                         [self.tconst], [tcr])
        wst = [A.alloc([8, 512], F32) for _ in range(2)]
        tw = [Tk() for _ in range(2)]
        bb = A.alloc([D], F32)
        tb = Tk()
        S.dma("sp", bb, self.ada_b[l:l + 1, blk * D:(blk + 1) * D].partition_broadcast(128) if False else
              bass.AP(self.dt_("ada_b"), l * 6 * D + blk * D, [[0, 128], [1, D]]), W=[tb])
        for nb in range(2):
            col0 = blk * D + nb * 512
            S.dma("sp", wst[nb], self.ada_w[l, :, col0:col0 + 512].rearrange("(kc p) n -> p kc n", p=128), W=[tw[nb]])
            for v in range(2):
                pb = 5 + v
                for kc in range(8):
                    self.mm(self.ps[pb][:, :], crep[:, v, kc, :], wst[nb][:, kc, :], kc == 0, kc == 7, [tw[nb], tcr], [self.tp[pb]])
                self.dve(lambda e, v=v, nb=nb, pb=pb: e.tensor_tensor(out=G[:, v, nb * 512:(nb + 1) * 512], in0=self.ps[pb][:, :], in1=bb[:, nb * 512:(nb + 1) * 512], op=ALU.add),
                         [self.tp[pb], tb], [tG])
        A.release(m)

    def load_ln(self, l, which, LN, tLN):
        for j in range(2):
            self.S.dma("sp", LN[:, j, :], bass.AP(self.dt_("lngb"), (l * 4 + which * 2 + j) * D, [[0, 128], [1, D]]), W=[tLN])

    def make_aT(self, l, i, which, aT, taT, aT32=None, taT32=None):
        v = 1 if i >= NL else 0
        for half in range(2):
            pb = 5 + half
            for q in range(4):
                kc = half * 4 + q
                self.tr(self.ps[pb][:, q * 128:(q + 1) * 128], self.H[:, i, kc * 128:(kc + 1) * 128], [self.tH[i]], [self.tp[pb]])
            for q in range(4):
                kc = half * 4 + q
                sc = self.modc[:, l, (which * 2 + 1) * 8 + kc, v:v + 1]
                sh = self.modc[:, l, (which * 2) * 8 + kc, v:v + 1]
                self.act(lambda e, kc=kc, q=q, pb=pb, sc=sc, sh=sh: e.activation(out=aT[:, kc, :], in_=self.ps[pb][:, q * 128:(q + 1) * 128], func=AF.Identity, bias=sh, scale=sc),
                         [self.tp[pb], self.tmodc], [taT])
                if aT32 is not None:
                    self.act(lambda e, kc=kc, q=q, pb=pb, sc=sc, sh=sh: e.activation(out=aT32[:, kc, :], in_=self.ps[pb][:, q * 128:(q + 1) * 128], func=AF.Identity, bias=sh, scale=sc),
                             [self.tp[pb], self.tmodc], [taT32])

    def resid_ln(self, i, ys, G, tG, LN, tLN, tmp, ttmp, st, tst):
        v = 1 if i >= NL else 0
        for hf, (yap, ty) in enumerate(ys):
            self.dve(lambda e, hf=hf, yap=yap: e.tensor_tensor(out=tmp[:, hf * 512:(hf + 1) * 512], in0=yap, in1=G[:, v, hf * 512:(hf + 1) * 512], op=ALU.mult),
                     [ty, tG], [ttmp])
        self.ln_tail(i, tmp, ttmp, LN, tLN, st, tst)

    def ln_tail(self, i, tmp, ttmp, LN, tLN, st, tst):
        self.dve(lambda e: e.scalar_tensor_tensor(out=tmp, in0=self.H[:, i, :], scalar=ALPHA, in1=tmp, op0=ALU.mult, op1=ALU.add), [self.tH[i], ttmp], [ttmp])
        for hf in range(2):
            self.dve(lambda e, hf=hf: e.bn_stats(out=st[:, hf * 6:(hf + 1) * 6], in_=tmp[:, hf * 512:(hf + 1) * 512]), [ttmp], [tst])
        self.dve(lambda e: e.bn_aggr(out=st[:, 12:14], in_=st[:, 0:12]), [tst], [tst])
        self.rstd_from(st[:, 14:15], st[:, 13:14], 1.0, 1e-6, [tst], tst)
        self.dve(lambda e: e.tensor_scalar(out=tmp, in0=tmp, scalar1=st[:, 12:13], scalar2=st[:, 14:15], op0=ALU.subtract, op1=ALU.mult), [ttmp, tst], [ttmp])
        self.pool(lambda e: e.tensor_tensor(out=tmp, in0=tmp, in1=LN[:, 0, :], op=ALU.mult), [ttmp, tLN], [ttmp])
        self.pool(lambda e: e.tensor_tensor(out=self.H[:, i, :], in0=tmp, in1=LN[:, 1, :], op=ALU.add), [ttmp, tLN], [self.tH[i]])

    def ffn_phase(self, l):
        A, S = self.A, self.S
        last = (l == DEPTH - 1)
        nt = NL if last else NT
        moe = (l % 2 == 1)
        li = l // 2
        m = A.mark()
        G = A.alloc([2, D], F32); tG = Tk()
        LN = A.alloc([2, D], F32); tLN = Tk()
        self.ada_gate(l, 1, G, tG)
        self.load_ln(l, 1, LN, tLN)
        FT = A.alloc([8, nt * 128], BF16)
        tFT = [Tk() for _ in range(nt)]
        moe_ = (l % 2 == 1)
        if moe_:
            gate = A.alloc([nt, NE], F32); tgate = Tk()
            a32 = A.alloc([8, 128], F32); ta32 = Tk()
            rt = self.router_setup(l // 2)
        for i in range(nt):
            if moe_:
                self.make_aT(l, i, 1, FT[:, :, i * 128:(i + 1) * 128], tFT[i], a32, ta32)
                self.router_tile(rt, i, a32, ta32, gate, tgate)
            else:
                self.make_aT(l, i, 1, FT[:, :, i * 128:(i + 1) * 128], tFT[i])
        experts = range(NE) if moe else [0]
        dff = DFE if moe else DFF
        nsl = dff // SLAB + (1 if dff % SLAB else 0)
        facc = A.alloc([nt, D], F32) if False else None
        tmp = A.alloc([D], F32); ttmp = Tk()
        st = A.alloc([16], F32); tst = Tk()
        for i in range(nt):
            self.pool(lambda e, i=i: e.tensor_scalar(out=self.H[:, i, :], in0=self.H[:, i, :], scalar1=ALPHA, scalar2=None, op0=ALU.mult), [self.tH[i]], [self.tH[i]])
        NB = 2
        wg = [A.alloc([8, SLAB], BF16) for _ in range(NB)]
        wu = [A.alloc([8, SLAB], BF16) for _ in range(NB)]
        wd = [A.alloc([SLAB // 128, D], BF16) for _ in range(NB)]
        tw = [Tk() for _ in range(NB)]
        twu = [Tk() for _ in range(NB)]
        twd = [Tk() for _ in range(NB)]
        h1 = [A.alloc([SLAB // 128, 512], BF16) for _ in range(2)]
        th1 = [Tk() for _ in range(2)]
        sg = [A.alloc([512], F32) for _ in range(2)]
        tsg = [Tk() for _ in range(2)]
        yt = [A.alloc([D], F32)] * 2
        tyt = [Tk()] * 2
        nblk = (nt * 128 + 511) // 512
        cnt = 0
        hcnt = 0
        items = [(e_, s) for e_ in experts for s in range(nsl)]

        def issue(k):
            e_, s = items[k]
            Wg = self.moe_g[li, e_] if moe else self.ffn_g[li]
            Wu = self.moe_u[li, e_] if moe else self.ffn_u[li]
            Wd = self.moe_d[li, e_] if moe else self.ffn_d[li]
            b = k % NB
            c0 = s * SLAB
            w = min(SLAB, dff - c0)
            nch = w // 128
            S.dma("pool", wg[b][:, :, 0:w], Wg[:, c0:c0 + w].rearrange("(kc p) n -> p kc n", p=128), W=[tw[b]])
            S.dma("pool", wu[b][:, :, 0:w], Wu[:, c0:c0 + w].rearrange("(kc p) n -> p kc n", p=128), W=[twu[b]])
            S.dma("pool", wd[b][:, 0:nch, :], Wd[c0:c0 + w, :].rearrange("(fc p) n -> p fc n", p=128), W=[twd[b]])

        issue(0)
        for k, (e_, s) in enumerate(items):
            if True:
                if k + 1 < len(items):
                    issue(k + 1)
                b = k % NB
                c0 = s * SLAB
                w = min(SLAB, dff - c0)
                nch = w // 128
                def gu(tb, hb, b=b, nch=nch):
                    t0 = tb * 512
                    ntok = min(512, nt * 128 - t0)
                    tiles = list(range(t0 // 128, (t0 + ntok) // 128))
                    for fc in range(nch):
                        pg, pu = 0 + (fc % 2) * 2, 1 + (fc % 2) * 2
                        for kc in range(8):
                            self.mm(self.ps[pg][:, 0:ntok], wg[b][:, kc, fc * 128:(fc + 1) * 128], FT[:, kc, t0:t0 + ntok], kc == 0, kc == 7,
                                    [tw[b]] + [tFT[i] for i in tiles], [self.tp[pg]])
                        for kc in range(8):
                            self.mm(self.ps[pu][:, 0:ntok], wu[b][:, kc, fc * 128:(fc + 1) * 128], FT[:, kc, t0:t0 + ntok], kc == 0, kc == 7,
                                    [twu[b]] + [tFT[i] for i in tiles], [self.tp[pu]])
                        sb = fc % 2
                        self.act(lambda e, pg=pg, sb=sb, ntok=ntok: e.activation(out=sg[sb][:, 0:ntok], in_=self.ps[pg][:, 0:ntok], func=AF.Silu), [self.tp[pg]], [tsg[sb]])
                        self.dve(lambda e, pu=pu, sb=sb, hb=hb, fc=fc, ntok=ntok: e.tensor_tensor(out=h1[hb][:, fc, 0:ntok], in0=self.ps[pu][:, 0:ntok], in1=sg[sb][:, 0:ntok], op=ALU.mult),
                                 [self.tp[pu], tsg[sb]], [th1[hb]])

                def down(tb, hb, b=b, nch=nch, e_=e_):
                    t0 = tb * 512
                    ntok = min(512, nt * 128 - t0)
                    tiles = list(range(t0 // 128, (t0 + ntok) // 128))
                    for ti, i in enumerate(tiles):
                        v = 1 if i >= NL else 0
                        yb = i % 2
                        for hf in range(2):
                            pb = 4 + hf + 2 * (i % 2)
                            for fc in range(nch):
                                self.mm(self.ps[pb][:, :], h1[hb][:, fc, ti * 128:(ti + 1) * 128], wd[b][:, fc, hf * 512:(hf + 1) * 512], fc == 0, fc == nch - 1,
                                        [th1[hb], twd[b]], [self.tp[pb]])
                            if moe:
                                self.act(lambda e, pb=pb, hf=hf, yb=yb, i=i, e_=e_: e.activation(out=yt[yb][:, hf * 512:(hf + 1) * 512], in_=self.ps[pb][:, :], func=AF.Copy, scale=gate[:, i, e_:e_ + 1]),
                                         [self.tp[pb], tgate], [tyt[yb]])
                            else:
                                self.dve(lambda e, pb=pb, hf=hf, v=v, yb=yb: e.tensor_tensor(out=yt[yb][:, hf * 512:(hf + 1) * 512], in0=self.ps[pb][:, :], in1=G[:, v, hf * 512:(hf + 1) * 512], op=ALU.mult),
                                         [self.tp[pb], tG], [tyt[yb]])
                        if moe:
                            self.dve(lambda e, v=v, yb=yb: e.tensor_tensor(out=yt[yb], in0=yt[yb], in1=G[:, v, :], op=ALU.mult), [tyt[yb], tG], [tyt[yb]])
                        self.pool(lambda e, i=i, yb=yb: e.tensor_tensor(out=self.H[:, i, :], in0=yt[yb], in1=self.H[:, i, :], op=ALU.add), [tyt[yb], self.tH[i]], [self.tH[i]])

                hbs = []
                for tb in range(nblk):
                    hbs.append(hcnt % 2)
                    hcnt += 1
                gu(0, hbs[0])
                for tb in range(nblk):
                    if tb + 1 < nblk:
                        gu(tb + 1, hbs[tb + 1])
                    down(tb, hbs[tb])
        for i in range(nt):
            self.ln_only(i, LN, tLN, tmp, ttmp, st, tst)
        A.release(m)

    def ln_only(self, i, LN, tLN, tmp, ttmp, st, tst):
        for hf in range(2):
            self.dve(lambda e, hf=hf: e.bn_stats(out=st[:, hf * 6:(hf + 1) * 6], in_=self.H[:, i, hf * 512:(hf + 1) * 512]), [self.tH[i]], [tst])
        self.dve(lambda e: e.bn_aggr(out=st[:, 12:14], in_=st[:, 0:12]), [tst], [tst])
        self.rstd_from(st[:, 14:15], st[:, 13:14], 1.0, 1e-6, [tst], tst)
        self.dve(lambda e: e.tensor_scalar(out=tmp, in0=self.H[:, i, :], scalar1=st[:, 12:13], scalar2=st[:, 14:15], op0=ALU.subtract, op1=ALU.mult), [self.tH[i], tst], [ttmp])
        self.pool(lambda e: e.tensor_tensor(out=tmp, in0=tmp, in1=LN[:, 0, :], op=ALU.mult), [ttmp, tLN], [ttmp])
        self.pool(lambda e: e.tensor_tensor(out=self.H[:, i, :], in0=tmp, in1=LN[:, 1, :], op=ALU.add), [ttmp, tLN], [self.tH[i]])

    def router_setup(self, li):
        A, S = self.A, self.S
        wr = A.alloc([8, NE], F32); twr = Tk()
        rb = A.alloc([NE], F32)
        S.dma("sp", wr, self.moe_r[li].rearrange("(kc p) n -> p kc n", p=128), W=[twr])
        S.dma("sp", rb, bass.AP(self.dt_("moe_rb"), li * NE, [[0, 128], [1, NE]]), W=[twr])
        return dict(wr=wr, twr=twr, rb=rb, lg=A.alloc([NE], F32), tlg=Tk(), m8=A.alloc([8], F32), wk=A.alloc([2, NE], F32))

    def router_tile(self, rt, i, a32, ta32, gate, tgate):
        wr, twr, rb, lg, tlg, m8, wk = rt["wr"], rt["twr"], rt["rb"], rt["lg"], rt["tlg"], rt["m8"], rt["wk"]
        pb = 7
        for kc in range(8):
            self.mm(self.ps[pb][:, 0:NE], a32[:, kc, :], wr[:, kc, :], kc == 0, kc == 7, [ta32, twr], [self.tp[pb]])
        self.dve(lambda e: e.tensor_tensor(out=lg, in0=self.ps[pb][:, 0:NE], in1=rb, op=ALU.add), [self.tp[pb], twr], [tlg])
        self.dve(lambda e: e.max(out=m8, in_=lg), [tlg], [tlg])
        self.dve(lambda e: e.tensor_scalar(out=wk[:, 0, :], in0=lg, scalar1=m8[:, 1:2], scalar2=None, op0=ALU.is_ge), [tlg], [tlg])
        self.dve(lambda e: e.tensor_scalar(out=wk[:, 1, :], in0=lg, scalar1=m8[:, 0:1], scalar2=None, op0=ALU.subtract), [tlg], [tlg])
        self.act(lambda e: e.activation(out=wk[:, 1, :], in_=wk[:, 1, :], func=AF.Exp), [tlg], [tlg])
        self.dve(lambda e: e.tensor_tensor(out=wk[:, 1, :], in0=wk[:, 1, :], in1=wk[:, 0, :], op=ALU.mult), [tlg], [tlg])
        self.dve(lambda e: e.reduce_sum(out=m8[:, 2:3], in_=wk[:, 1, :], axis=AX.X), [tlg], [tlg])
        self.dve(lambda e: e.reciprocal(out=m8[:, 2:3], in_=m8[:, 2:3]), [tlg], [tlg])
        self.dve(lambda e: e.tensor_scalar(out=gate[:, i, :], in0=wk[:, 1, :], scalar1=m8[:, 2:3], scalar2=None, op0=ALU.mult), [tlg], [tgate])


    def rms_rope(self, src, tsrc, nh, dst, tdst, tile, rw, g=None, perm=False, qs=1.0):
        rope, trope = self.rope, self.tmc
        s3 = src.rearrange("p (h d) -> p h d", d=64)
        x, ss, t, tw = rw["x"], rw["ss"], rw["t"], rw["tw"]
        x3 = x[:, 0:nh * 64].rearrange("p (h d) -> p h d", d=64)
        if g is not None:
            for h in range(nh):
                self.act(lambda e, h=h: e.activation(out=x3[:, h, :], in_=s3[:, h, :], func=AF.Square, accum_out=ss[:, h:h + 1]), [tsrc], [tw])
            self.act(lambda e: e.activation(out=ss[:, 0:nh], in_=ss[:, 0:nh], func=AF.Sqrt, bias=self.epsc[:, 0:1], scale=1.0 / 64), [tw, self.tconst], [tw])
            self.dve(lambda e: e.reciprocal(out=ss[:, 0:nh], in_=ss[:, 0:nh]), [tw], [tw])
            for h in range(nh):
                self.dve(lambda e, h=h: e.scalar_tensor_tensor(out=x3[:, h, :], in0=s3[:, h, :], scalar=ss[:, h:h + 1], in1=g, op0=ALU.mult, op1=ALU.mult), [tsrc, tw, self.tmc], [tw])
            cur, tcur, qs = x3, tw, 1.0
        else:
            cur, tcur = s3, tsrc
        C = rope[:, tile, 0:32].unsqueeze(1).unsqueeze(1).to_broadcast([128, nh, 2, 32])
        Sg = rope[:, tile, 32:96].rearrange("p (a d) -> p a d", d=32).unsqueeze(1).to_broadcast([128, nh, 2, 32])
        c4 = cur.rearrange("p h (a d) -> p h a d", d=32)
        sw = c4[:, :, ::-1, :]
        t1 = t[:, 0, 0:nh * 64].rearrange("p (h a d) -> p h a d", a=2, d=32)
        t2 = t[:, 1, 0:nh * 64].rearrange("p (h a d) -> p h a d", a=2, d=32)
        if qs != 1.0:
            self.dve(lambda e: e.scalar_tensor_tensor(out=t1, in0=c4, scalar=qs, in1=C, op0=ALU.mult, op1=ALU.mult), [tcur, trope], [tw])
            self.dve(lambda e: e.scalar_tensor_tensor(out=t2, in0=sw, scalar=qs, in1=Sg, op0=ALU.mult, op1=ALU.mult), [tcur, trope], [tw])
        else:
            self.dve(lambda e: e.tensor_tensor(out=t1, in0=c4, in1=C, op=ALU.mult), [tcur, trope], [tw])
            self.dve(lambda e: e.tensor_tensor(out=t2, in0=sw, in1=Sg, op=ALU.mult), [tcur, trope], [tw])
        f1 = t[:, 0, 0:nh * 64].rearrange("p (h d) -> p h d", d=64)
        f2 = t[:, 1, 0:nh * 64].rearrange("p (h d) -> p h d", d=64)
        if perm:
            dv = dst.rearrange("p (b s d) -> p s b d", b=2, s=2, d=64)
            f1 = f1.rearrange("p (s b) d -> p s b d", b=2)
            f2 = f2.rearrange("p (s b) d -> p s b d", b=2)
        else:
            dv = dst.rearrange("p (h d) -> p h d", d=64)
        self.dve(lambda e: e.tensor_tensor(out=dv, in0=f1, in1=f2, op=ALU.add), [tw], [tdst])

    def mixer_phase(self, l):
        A, S = self.A, self.S
        last = (l == DEPTH - 1)
        nq = NL if last else NT
        m_all = A.mark()
        self.rope = A.alloc([NT, 96], F32)
        gqk = A.alloc([2, 64], F32)
        self.tmc = Tk()
        S.dma("sp", self.rope, self.c_rope, W=[self.tmc])
        S.dma("sp", gqk, bass.AP(self.dt_("gqk"), l * 128, [[0, 128], [1, 128]]), W=[self.tmc])
        OC = A.alloc([NT, 256], BF16); tOC = [Tk() for _ in range(NT)]
        m_s5 = A.mark()
        UT = A.alloc([2, NT * 128], BF16); tUT = [Tk() for _ in range(NT)]
        m0 = A.mark()
        aT = [A.alloc([8, 128], BF16) for _ in range(2)]; taT = [Tk() for _ in range(2)]
        Wu_ = A.alloc([8, 256], BF16); tWu = Tk()
        S.dma("pool", Wu_, self.w_in[l, :, 1280:1536].rearrange("(kc p) n -> p kc n", p=128), W=[tWu])
        for i in range(NT):
            b = i % 2
            self.make_aT(l, i, 0, aT[b], taT[b])
            for c in range(2):
                for kc in range(8):
                    self.mm(self.ps[2 + b][:, c * 128:(c + 1) * 128], Wu_[:, kc, c * 128:(c + 1) * 128], aT[b][:, kc, :], kc == 0, kc == 7, [taT[b], tWu], [self.tp[2 + b]])
            self.act(lambda e, i=i, b=b: e.activation(out=UT[:, :, i * 128:(i + 1) * 128], in_=self.ps[2 + b][:, 0:256].rearrange("p (c t) -> p c t", t=128), func=AF.Copy), [self.tp[2 + b]], [tUT[i]])
        A.release(m0)
        self.s5_phase(l, UT, tUT, OC, tOC, nq)
        if getattr(self, 'dbg_oc', None) is not None:
            for i in range(nq):
                self.store(self.dbg_oc[i * 128:(i + 1) * 128, :], OC[:, i, :], [tOC[i]])
        A.release(m_s5)
        KT = A.alloc([4, NT * 128], BF16); tKT = [Tk() for _ in range(NT)]
        V = A.alloc([NT, 512], BF16); tV = [Tk() for _ in range(NT)]
        m0 = A.mark()
        rw = dict(x=A.alloc([256], F32), ss=A.alloc([4], F32), t=A.alloc([2, 256], F32), tw=Tk())
        aT = [A.alloc([8, 128], BF16) for _ in range(2)]; taT = [Tk() for _ in range(2)]
        W = A.alloc([8, 1024], BF16); tW = [Tk() for _ in range(6)]
        srcs = [(256, 256), (1024, 128), (1792, 128), (512, 256), (1152, 128), (1920, 128)]
        o = 0
        for k, (c0, w) in enumerate(srcs):
            S.dma("pool", W[:, :, o:o + w], self.w_in[l, :, c0:c0 + w].rearrange("(kc p) n -> p kc n", p=128), W=[tW[k]])
            o += w
        kbf = A.alloc([512], BF16); tkb = Tk()
        for i in range(NT):
            b = i % 2
            self.make_aT(l, i, 0, aT[b], taT[b])
            for kc in range(8):
                self.mm(self.ps[0][:, :], aT[b][:, kc, :], W[:, kc, 0:512], kc == 0, kc == 7, [taT[b]] + tW[0:3], [self.tp[0]])
            for kc in range(8):
                self.mm(self.ps[1][:, :], aT[b][:, kc, :], W[:, kc, 512:1024], kc == 0, kc == 7, [taT[b]] + tW[3:6], [self.tp[1]])
            self.act(lambda e, i=i: e.activation(out=V[:, i, :], in_=self.ps[1][:, :], func=AF.Copy), [self.tp[1]], [tV[i]])
            self.act(lambda e: e.activation(out=kbf[:, 0:256], in_=self.ps[0][:, 0:256], func=AF.Copy), [self.tp[0]], [tkb])
            self.rms_rope(self.ps[0][:, 256:384], self.tp[0], 2, kbf[:, 256:384], tkb, i, rw, g=gqk[:, 1, :])
            self.rms_rope(self.ps[0][:, 384:512], self.tp[0], 2, kbf[:, 384:512], tkb, i, rw)
            for c in range(4):
                self.mm(self.ps[3][:, c * 128:(c + 1) * 128], kbf[:, c * 128:(c + 1) * 128], self.ident_b, True, True, [tkb, self.tconst], [self.tp[3]])
            self.dve(lambda e, i=i: e.tensor_copy(out=KT[:, :, i * 128:(i + 1) * 128], in_=self.ps[3][:, :].rearrange("p (c t) -> p c t", t=128)), [self.tp[3]], [tKT[i]])
        A.release(m0)
        rw = dict(x=A.alloc([256], F32), ss=A.alloc([4], F32), t=A.alloc([2, 256], F32), tw=Tk())
        aT = [A.alloc([8, 128], BF16)] * 2; taT = [Tk()] * 2
        tri = A.alloc([2, 128], BF16); namask = A.alloc([2688], BF16); tmk = Tk()
        S.dma("sp", tri, self.c_tri, W=[tmk]); S.dma("sp", namask, self.c_namask, W=[tmk])
        G = A.alloc([2, D], F32); tG = Tk()
        LN = A.alloc([2, D], F32); tLN = Tk()
        self.ada_gate(l, 0, G, tG)
        self.load_ln(l, 0, LN, tLN)
        Wq = A.alloc([8, 768], BF16); tWq = [Tk() for _ in range(3)]
        for k, c0 in enumerate((0, 768, 1536)):
            S.dma("pool", Wq[:, :, k * 256:(k + 1) * 256], self.w_in[l, :, c0:c0 + 256].rearrange("(kc p) n -> p kc n", p=128), W=[tWq[k]])
        Wo = A.alloc([8, D], BF16); tWo = Tk()
        S.dma("pool", Wo, self.w_out[l].rearrange("(kc p) n -> p kc n", p=128), W=[tWo])
        Traw = A.alloc([4, 14, 64], BF16); tTr = Tk()
        mh = A.mark()
        hk = [A.alloc([16, 64], F32) for _ in range(2)]; thk = [Tk() for _ in range(2)]
        for h in range(4):
            for rl in range(2):
                S.dma("sp", hk[h % 2][rl * 64:(rl + 1) * 64, :, :], bass.AP(self.dt_("rpbh"), ((l * 4 + h) * 18 + (1 - rl)) * 128, [[1, 64], [128, 16], [1, 64]]), W=[thk[h % 2]])
            self.dve(lambda e, h=h: e.tensor_copy(out=Traw[:, h, :, :], in_=hk[h % 2][:, 1:15, ::-1]), [thk[h % 2]], [tTr])
        A.release(mh)
        sk = A.alloc([8], F32); tsk = Tk()
        S.dma("sp", sk[:, 0:4], bass.AP(self.dt_("sink"), l * 4, [[0, 128], [1, 4]]), W=[tsk])
        self.dve(lambda e: e.tensor_scalar(out=sk[:, 4:8], in0=sk[:, 0:4], scalar1=-1.0, scalar2=None, op0=ALU.mult), [tsk], [tsk])
        qbf = A.alloc([768], BF16); tqb = Tk()
        qT = A.alloc([6, 128], BF16); tqT = Tk()
        Ps = [A.alloc([NT * 128], BF16) for _ in range(2)]; tPs = [Tk() for _ in range(2)]
        P, tP = Ps[0], tPs[0]
        PT = [A.alloc([512], BF16) for _ in range(2)]; tPT = [Tk() for _ in range(2)]
        cc = A.alloc([D], BF16); tcc = Tk()
        ccT = A.alloc([8, 128], BF16); tccT = Tk()
        tmp = P[:, 0:2 * D].bitcast(F32); ttmp = tP
        st = A.alloc([16], F32); tst = Tk()
        sms = [A.alloc([16], F32) for _ in range(4)]; tsms = [Tk() for _ in range(4)]
        print('M2 arena top', A.top)
        ocnt = [0]
        acnt = [0]

        jobs = []

        def attention(*a, **kw):
            jobs.append((a, kw, {}))

        def att1(ctx_, i, blk, pb0, segs, vcol, oc0, sc, bias_h=None, s0=0, sink_h=None):
            nseg = len(segs)
            nb = (nseg + 3) // 4
            acnt[0] += 1
            sm, tsm = sms[acnt[0] % 4], tsms[acnt[0] % 4]
            P, tP = Ps[acnt[0] % 2], tPs[acnt[0] % 2]
            ctx_.update(sm=sm, tsm=tsm, P=P, tP=tP)
            b0 = 0 if nb > 2 else 2 * (acnt[0] % 2)
            banks = list(range(b0, b0 + nb))
            tb_ = [self.tp[k] for k in banks]
            ntot = nseg * 128
            Sall = self.psall[:, b0 * 512:b0 * 512 + ntot]
            q_ap = qT[pb0:pb0 + 64, blk, :]
            for t, (kt, c, mask) in enumerate(segs):
                bk, cb = b0 + t // 4, (t % 4) * 128
                self.mm(self.ps[bk][:, cb:cb + 128], q_ap, KT[pb0:pb0 + 64, c, kt * 128:(kt + 1) * 128], True, mask is None, [tqT, tKT[kt]], [self.tp[bk]])
                if mask is not None:
                    self.mm(self.ps[bk][:, cb:cb + 128], self.ident_b, mask, False, True, [self.tconst, tmk], [self.tp[bk]])
            if bias_h is not None:
                nloc = nseg - 2
                self.dve(lambda e: e.tensor_tensor(out=self.psall[:, b0 * 512:b0 * 512 + nloc * 128], in0=self.psall[:, b0 * 512:b0 * 512 + nloc * 128],
                                                   in1=Traw[:, bias_h, s0 - 1:s0 - 1 + 2 * nloc, :].rearrange("p s k -> p (s k)"), op=ALU.add), tb_ + [tTr], tb_)
            self.dve(lambda e: e.reduce_max(out=sm[:, 9:10], in_=Sall, axis=AX.X, negate=True), tb_, [tsm])
            if sink_h is not None:
                self.dve(lambda e: e.tensor_tensor(out=sm[:, 9:10], in0=sm[:, 9:10], in1=sk[:, 4 + sink_h:5 + sink_h], op=ALU.min), [tsm, tsk], [tsm])
            self.act(lambda e: e.activation(out=P[:, 0:ntot], in_=Sall, func=AF.Exp, bias=sm[:, 9:10], scale=1.0, accum_out=sm[:, 10:11]), tb_ + [tsm], [tP, tsm])
            if sink_h is not None:
                self.act(lambda e: e.activation(out=sm[:, 12:13], in_=sk[:, sink_h:sink_h + 1], func=AF.Exp, bias=sm[:, 9:10], scale=1.0), [tsk, tsm], [tsm])

        def att1b(ctx_, i, blk, pb0, segs, vcol, oc0, sc, bias_h=None, s0=0, sink_h=None):
            sm, tsm = ctx_["sm"], ctx_["tsm"]
            if sink_h is not None:
                self.dve(lambda e: e.tensor_tensor(out=sm[:, 10:11], in0=sm[:, 10:11], in1=sm[:, 12:13], op=ALU.add), [tsm], [tsm])
            self.dve(lambda e: e.reciprocal(out=sm[:, 11:12], in_=sm[:, 10:11]), [tsm], [tsm])

        def att2(ctx_, i, blk, pb0, segs, vcol, oc0, sc, bias_h=None, s0=0, sink_h=None):
            sm, tsm, P, tP = ctx_["sm"], ctx_["tsm"], ctx_["P"], ctx_["tP"]
            nseg = len(segs)
            ob = oc0 % 512
            for g0 in range(0, nseg, 4):
                gi = ocnt[0] % 2
                ocnt[0] += 1
                pt = 5 + gi
                ng = min(4, nseg - g0)
                for t in range(g0, g0 + ng):
                    self.mm(self.ps[pt][:, (t - g0) * 128:(t - g0 + 1) * 128], P[:, t * 128:(t + 1) * 128], self.ident_b, True, True, [tP, self.tconst], [self.tp[pt]])
                self.dve(lambda e, pt=pt, gi=gi, ng=ng: e.tensor_copy(out=PT[gi][:, 0:ng * 128], in_=self.ps[pt][:, 0:ng * 128]), [self.tp[pt]], [tPT[gi]])
                for t in range(g0, g0 + ng):
                    kt = segs[t][0]
                    self.mm(self.ps[7][:, ob:ob + 64], PT[gi][:, (t - g0) * 128:(t - g0 + 1) * 128], V[:, kt, vcol:vcol + 64], t == 0, t == nseg - 1, [tPT[gi], tV[kt]], [self.tp[7]])
            self.act(lambda e: e.activation(out=cc[:, oc0:oc0 + 64], in_=self.ps[7][:, ob:ob + 64], func=AF.Copy, scale=sm[:, 11:12]), [self.tp[7], tsm], [tcc])

        for i in range(nq):
            b = i % 2
            isctx = i >= NL
            self.make_aT(l, i, 0, aT[b], taT[b])
            for kc in range(8):
                self.mm(self.ps[0][:, :], aT[b][:, kc, :], Wq[:, kc, 0:512], kc == 0, kc == 7, [taT[b]] + tWq[0:2], [self.tp[0]])
            for kc in range(8):
                self.mm(self.ps[1][:, 0:256], aT[b][:, kc, :], Wq[:, kc, 512:768], kc == 0, kc == 7, [taT[b], tWq[2]], [self.tp[1]])
            self.act(lambda e: e.activation(out=qbf[:, 0:256], in_=self.ps[0][:, 0:256], func=AF.Copy), [self.tp[0]], [tqb])
            self.rms_rope(self.ps[0][:, 256:512], self.tp[0], 4, qbf[:, 256:512], tqb, i, rw, g=gqk[:, 0, :], perm=True)
            self.rms_rope(self.ps[1][:, 0:256], self.tp[1], 4, qbf[:, 512:768], tqb, i, rw, perm=True)
            for c in range(6):
                bk = 2 + c // 4
                self.mm(self.ps[bk][:, (c % 4) * 128:(c % 4 + 1) * 128], qbf[:, c * 128:(c + 1) * 128], self.ident_b, True, True, [tqb, self.tconst], [self.tp[bk]])
            self.dve(lambda e: e.tensor_scalar(out=qT[:, 0:4, :], in0=self.ps[2][:, :].rearrange("p (c t) -> p c t", t=128), scalar1=0.125, scalar2=None, op0=ALU.mult), [self.tp[2]], [tqT])
            self.dve(lambda e: e.tensor_scalar(out=qT[:, 4:6, :], in0=self.ps[3][:, 0:256].rearrange("p (c t) -> p c t", t=128), scalar1=0.125, scalar2=None, op0=ALU.mult), [self.tp[3]], [tqT])
            ctxs = [(16, None), (17, None)]
            for h in range(4):
                blk, pb0, c = h // 2, (h % 2) * 64, h // 2
                if isctx:
                    segs = [(kt, c, None) for kt, _ in ctxs]
                    attention(i, blk, pb0, segs, h * 64, h * 64, 0.125)
                else:
                    j = i
                    if 2 <= j <= 13:
                        var, t0, ntl, s0 = 0, j - 2, 5, 3
                    elif j == 0:
                        var, t0, ntl, s0 = 1, 0, 4, 7
                    elif j == 1:
                        var, t0, ntl, s0 = 2, 0, 4, 5
                    elif j == 14:
                        var, t0, ntl, s0 = 3, 12, 4, 3
                    else:
                        var, t0, ntl, s0 = 4, 12, 4, 1
                    mo = 0 if var == 0 else 640 + (var - 1) * 512
                    segs = [(t0 + k, c, namask[:, mo + k * 128:mo + (k + 1) * 128]) for k in range(ntl)] + [(kt, c, None) for kt, _ in ctxs]
                    attention(i, blk, pb0, segs, h * 64, h * 64, 1.0, bias_h=h, s0=s0)
            for h in range(4):
                blk, pb0, kvh = 2 + (h % 2), (h // 2) * 64, h // 2
                kts = [16, 17] if isctx else list(range(NT))
                attention(i, blk, pb0, [(kt, 2, None) for kt in kts], 256 + kvh * 64, 256 + h * 64, 0.125)
            self.pool(lambda e, i=i: e.tensor_copy(out=cc[:, 512:768], in_=OC[:, i, :]), [tOC[i]], [tcc])
            for h in range(4):
                blk, pb0, kvh = 4 + (h % 2), (h // 2) * 64, h // 2
                if isctx:
                    segs = [(16, 3, None), (17, 3, None)]
                else:
                    segs = []
                    if i - 1 >= 0: segs.append((i - 1, 3, tri[:, 0, :]))
                    segs.append((i, 3, None))
                    if i + 1 < NL: segs.append((i + 1, 3, tri[:, 1, :]))
                    segs += [(16, 3, None), (17, 3, None)]
                attention(i, blk, pb0, segs, 384 + kvh * 64, 768 + h * 64, 0.125, sink_h=h)
            att1(jobs[0][2], *jobs[0][0], **jobs[0][1])
            att1b(jobs[0][2], *jobs[0][0], **jobs[0][1])
            for k_ in range(len(jobs)):
                if k_ + 1 < len(jobs):
                    att1(jobs[k_ + 1][2], *jobs[k_ + 1][0], **jobs[k_ + 1][1])
                att2(jobs[k_][2], *jobs[k_][0], **jobs[k_][1])
                if k_ + 1 < len(jobs):
                    att1b(jobs[k_ + 1][2], *jobs[k_ + 1][0], **jobs[k_ + 1][1])
            del jobs[:]
            for c in range(8):
                bk = 5 + c // 4
                self.mm(self.ps[bk][:, (c % 4) * 128:(c % 4 + 1) * 128], cc[:, c * 128:(c + 1) * 128], self.ident_b, True, True, [tcc, self.tconst], [self.tp[bk]])
            for hf in range(2):
                self.act(lambda e, hf=hf: e.activation(out=ccT[:, hf * 4:(hf + 1) * 4, :], in_=self.ps[5 + hf][:, :].rearrange("p (c t) -> p c t", t=128), func=AF.Copy), [self.tp[5 + hf]], [tccT])
            for hf in range(2):
                for kc in range(8):
                    self.mm(self.ps[hf][:, :], ccT[:, kc, :], Wo[:, kc, hf * 512:(hf + 1) * 512], kc == 0, kc == 7, [tccT, tWo], [self.tp[hf]])
            self.resid_ln(i, [(self.ps[0][:, :], self.tp[0]), (self.ps[1][:, :], self.tp[1])], G, tG, LN, tLN, tmp, ttmp, st, tst)
        A.release(m_all)

    def sin_of(self, out, x, shift, wk, tk, R):
        TWO_PI = 2.0 * np.pi
        a, k = wk
        ki = k.bitcast(I32)
        self.dve(lambda e: e.tensor_scalar(out=a, in0=x, scalar1=1.0 / TWO_PI, scalar2=shift / TWO_PI + 0.5, op0=ALU.mult, op1=ALU.add), R, [tk])
        self.dve(lambda e: e.tensor_copy(out=ki, in_=a), [tk], [tk])
        self.dve(lambda e: e.tensor_copy(out=a, in_=ki), [tk], [tk])
        self.dve(lambda e: e.tensor_scalar(out=k, in0=x, scalar1=shift, scalar2=None, op0=ALU.add), R + [tk], [tk])
        self.dve(lambda e: e.scalar_tensor_tensor(out=k, in0=a, scalar=-TWO_PI, in1=k, op0=ALU.mult, op1=ALU.add), [tk], [tk])
        self.dve(lambda e: e.tensor_scalar(out=a, in0=k, scalar1=float(np.pi), scalar2=None, op0=ALU.is_gt), [tk], [tk])
        self.dve(lambda e: e.scalar_tensor_tensor(out=k, in0=a, scalar=-TWO_PI, in1=k, op0=ALU.mult, op1=ALU.add), [tk], [tk])
        self.dve(lambda e: e.tensor_scalar(out=a, in0=k, scalar1=-float(np.pi), scalar2=None, op0=ALU.is_lt), [tk], [tk])
        self.dve(lambda e: e.scalar_tensor_tensor(out=k, in0=a, scalar=TWO_PI, in1=k, op0=ALU.mult, op1=ALU.add), [tk], [tk])
        self.dve(lambda e: e.tensor_scalar(out=k, in0=k, scalar1=3.1415925, scalar2=-3.1415925, op0=ALU.min, op1=ALU.max), [tk], [tk])
        self.act(lambda e: e.activation(out=out, in_=k, func=AF.Sin), [tk], [tk])

    def s5_phase(self, l, UT, tUT, OC, tOC, nq):
        A, S = self.A, self.S
        dve, act, pool = self.dve, self.act, self.pool
        tp_ = Tk()
        vec = A.alloc([16, 3], F32)
        tau = A.alloc([130], F32)
        bs = A.alloc([16, 2, 16], F32)
        S.dma("sp", vec, self.ssm_vec[l], W=[tp_])
        S.dma("sp", tau, self.c_tau, W=[tp_])
        S.dma("sp", bs, self.ssm_bs[l], W=[tp_])
        CSb = A.alloc([16, 2, 32], BF16); tcs = Tk()
        DDb = A.alloc([8, 32], BF16)
        Wg = A.alloc([2, 256], BF16)
        S.dma("pool", CSb, self.ssm_cs[l], W=[tcs])
        S.dma("pool", DDb, self.ssm_dd[l], W=[tcs])
        S.dma("pool", Wg, self.ssm_wglu[l].rearrange("(c p) n -> p c n", p=128), W=[tcs])
        dve(lambda e: e.tensor_scalar(out=CSb[:, :, 1, :], in0=CSb[:, :, 1, :], scalar1=-1.0, scalar2=None, op0=ALU.mult), [tcs], [tcs])
        pv = A.alloc([16, 16], F32)
        def q(k): return pv[:, k, :]
        lre, lim, lst = vec[:, :, 0], vec[:, :, 1], vec[:, :, 2]
        STEP, LR, ANG, MAG, SN, CN, ABR, ABI, DEN, NUM, FRE, FIM, T0, T1 = range(14)
        act(lambda e: e.activation(out=q(STEP), in_=lst, func=AF.Exp), [tp_], [tp_])
        dve(lambda e: e.tensor_tensor(out=q(LR), in0=lre, in1=q(STEP), op=ALU.mult), [tp_], [tp_])
        dve(lambda e: e.tensor_tensor(out=q(ANG), in0=lim, in1=q(STEP), op=ALU.mult), [tp_], [tp_])
        act(lambda e: e.activation(out=q(MAG), in_=q(LR), func=AF.Exp), [tp_], [tp_])
        self.sin_of(q(SN), q(ANG), 0.0, (q(T0), q(T1)), tp_, [tp_])
        self.sin_of(q(CN), q(ANG), float(np.pi / 2), (q(T0), q(T1)), tp_, [tp_])
        dve(lambda e: e.tensor_tensor(out=q(ABR), in0=q(MAG), in1=q(CN), op=ALU.mult), [tp_], [tp_])
        dve(lambda e: e.tensor_tensor(out=q(ABI), in0=q(MAG), in1=q(SN), op=ALU.mult), [tp_], [tp_])
        dve(lambda e: e.tensor_tensor(out=q(DEN), in0=lre, in1=lre, op=ALU.mult), [tp_], [tp_])
        dve(lambda e: e.tensor_tensor(out=q(T0), in0=lim, in1=lim, op=ALU.mult), [tp_], [tp_])
        dve(lambda e: e.tensor_tensor(out=q(DEN), in0=q(DEN), in1=q(T0), op=ALU.add), [tp_], [tp_])
        dve(lambda e: e.reciprocal(out=q(DEN), in_=q(DEN)), [tp_], [tp_])
        dve(lambda e: e.tensor_scalar(out=q(NUM), in0=q(ABR), scalar1=-1.0, scalar2=None, op0=ALU.add), [tp_], [tp_])
        dve(lambda e: e.tensor_tensor(out=q(T0), in0=q(NUM), in1=lre, op=ALU.mult), [tp_], [tp_])
        dve(lambda e: e.tensor_tensor(out=q(T1), in0=q(ABI), in1=lim, op=ALU.mult), [tp_], [tp_])
        dve(lambda e: e.tensor_tensor(out=q(FRE), in0=q(T0), in1=q(T1), op=ALU.add), [tp_], [tp_])
        dve(lambda e: e.tensor_tensor(out=q(FRE), in0=q(FRE), in1=q(DEN), op=ALU.mult), [tp_], [tp_])
        dve(lambda e: e.tensor_tensor(out=q(T0), in0=q(ABI), in1=lre, op=ALU.mult), [tp_], [tp_])
        dve(lambda e: e.tensor_tensor(out=q(T1), in0=q(NUM), in1=lim, op=ALU.mult), [tp_], [tp_])
        dve(lambda e: e.tensor_tensor(out=q(FIM), in0=q(T0), in1=q(T1), op=ALU.subtract), [tp_], [tp_])
        dve(lambda e: e.tensor_tensor(out=q(FIM), in0=q(FIM), in1=q(DEN), op=ALU.mult), [tp_], [tp_])
        EC = A.alloc([16, 129], F32); ES = A.alloc([16, 129], F32); tE = Tk()
        mtab = A.mark()
        X = A.alloc([16, 129], F32); wa = A.alloc([16, 129], F32); wb = A.alloc([16, 129], F32); tX = Tk()
        dve(lambda e: e.tensor_tensor(out=X, in0=q(ANG).unsqueeze(2).to_broadcast([128, 16, 129]), in1=tau[:, 0:129].unsqueeze(1).to_broadcast([128, 16, 129]), op=ALU.mult), [tp_], [tX])
        self.sin_of(ES, X, 0.0, (wa, wb), tE, [tX])
        self.sin_of(EC, X, float(np.pi / 2), (wa, wb), tE, [tX])
        A.release(mtab)
        BT = A.alloc([16, 2, 128], BF16); tBT = Tk()
        mb = A.mark()
        bb = A.alloc([16, 2, 16], F32); tbb = Tk()
        w4 = A.alloc([4, 16, 16], F32)
        fre_b = q(FRE).unsqueeze(2).to_broadcast([128, 16, 16]); fim_b = q(FIM).unsqueeze(2).to_broadcast([128, 16, 16])
        dve(lambda e: e.tensor_tensor(out=w4[:, 0], in0=bs[:, :, 0, :], in1=fre_b, op=ALU.mult), [tp_], [tbb])
        dve(lambda e: e.tensor_tensor(out=w4[:, 1], in0=bs[:, :, 1, :], in1=fim_b, op=ALU.mult), [tp_], [tbb])
        dve(lambda e: e.tensor_tensor(out=w4[:, 2], in0=bs[:, :, 1, :], in1=fre_b, op=ALU.mult), [tp_], [tbb])
        dve(lambda e: e.tensor_tensor(out=w4[:, 3], in0=bs[:, :, 0, :], in1=fim_b, op=ALU.mult), [tp_], [tbb])
        dve(lambda e: e.tensor_tensor(out=bb[:, :, 0, :], in0=w4[:, 0], in1=w4[:, 1], op=ALU.subtract), [tbb], [tbb])
        dve(lambda e: e.tensor_tensor(out=bb[:, :, 1, :], in0=w4[:, 2], in1=w4[:, 3], op=ALU.add), [tbb], [tbb])
        Bp = A.alloc([4, 128], BF16); tBp = [Tk() for _ in range(4)]
        dve(lambda e: e.memset(Bp, 0.0), [], tBp)
        n = 0
        for dg in range(16):
            gc = dg % 8
            band = gc % 4
            for ri in range(2):
                for gl in range(2):
                    dve(lambda e, dg=dg, ri=ri, gl=gl, band=band: e.tensor_copy(out=Bp[gl * 64:(gl + 1) * 64, band, band * 32 + gl * 16:band * 32 + gl * 16 + 16],
                                                                            in_=bb[gl * 64:(gl + 1) * 64, dg, ri, :]), [tbb], [tBp[band]])
                bk = 5 + (n // 4) % 2
                self.mm(self.ps[bk][:, (n % 4) * 128:(n % 4 + 1) * 128], Bp[:, band, :], self.ident_b, True, True, [tBp[band], self.tconst], [self.tp[bk]])
                if n % 4 == 3:
                    dg0 = (n - 3) // 2
                    act(lambda e, bk=bk, dg0=dg0: e.activation(out=BT[:, dg0:dg0 + 2, :, :], in_=self.ps[bk][:, :].rearrange("p (a b t) -> p a b t", a=2, b=2), func=AF.Copy), [self.tp[bk]], [tBT])
                n += 1
        A.release(mb)
        Y = A.alloc([NT, 256], F32); tY = [Tk() for _ in range(NT)]
        z = A.alloc([256], F32); z2 = A.alloc([256], F32); tz = Tk()
        zb = A.alloc([256], BF16); zT = A.alloc([2, 128], BF16); tzT = Tk()
        RD = A.alloc([2, 16, 128], F32); tRD = Tk()
        for d in range(2):
            dve(lambda e, d=d: e.tensor_copy(out=RD[:, d].rearrange("p (g r) t -> p g r t", r=2),
                                             in_=q(MAG)[:, d * 8:(d + 1) * 8].unsqueeze(2).unsqueeze(3).to_broadcast([128, 8, 2, 128])), [tp_], [tRD])
            first = 0 if d == 0 else 127
            dve(lambda e, d=d, first=first: e.memset(RD[:, d, :, first:first + 1], 0.0), [tRD], [tRD])
        g = A.alloc([8, 2, 128], F32); tg = Tk()
        gi = A.alloc([8, 2], F32); tgi = Tk()
        giw = A.alloc([4, 8], F32)
        tt = [A.alloc([8, 128], F32) for _ in range(4)]; ttt = Tk()
        w = A.alloc([8, 2, 128], F32); tw = Tk()
        hs = A.alloc([8, 2, 128], BF16); ths = Tk()
        bu4 = self.psall[:, 0:2048].rearrange("p (g r t) -> p g r t", r=2, t=128)
        tb4 = [self.tp[k] for k in range(4)]
        for d in range(2):
            order = [16, 17] + list(range(16)) if d == 0 else [17, 16] + list(range(15, -1, -1))
            first, last = (0, 127) if d == 0 else (127, 0)
            cs_, sn_ = rv_tab(EC, d * 8, d, 8), rv_tab(ES, d * 8, d, 8)
            for n_, i in enumerate(order):
                tk = slice(i * 128, (i + 1) * 128)
                for gc in range(8):
                    for ri in range(2):
                        bk = gc // 2
                        c0 = (gc % 2) * 256 + ri * 128
                        self.mm(self.ps[bk][:, c0:c0 + 128], BT[:, d * 8 + gc, ri, :], UT[:, gc // 4, tk], True, True, [tBT, tUT[i]], [self.tp[bk]])
                if n_ > 0:
                    glr, gli = g[:, :, 0, last], g[:, :, 1, last]
                    cT, sT = EC[:, d * 8:(d + 1) * 8, 128], ES[:, d * 8:(d + 1) * 8, 128]
                    dve(lambda e, glr=glr, cT=cT: e.tensor_tensor(out=giw[:, 0], in0=glr, in1=cT, op=ALU.mult), [tg, tE], [tgi])
                    dve(lambda e, gli=gli, sT=sT: e.tensor_tensor(out=giw[:, 1], in0=gli, in1=sT, op=ALU.mult), [tg, tE], [tgi])
                    dve(lambda e, glr=glr, sT=sT: e.tensor_tensor(out=giw[:, 2], in0=glr, in1=sT, op=ALU.mult), [tg, tE], [tgi])
                    dve(lambda e, gli=gli, cT=cT: e.tensor_tensor(out=giw[:, 3], in0=gli, in1=cT, op=ALU.mult), [tg, tE], [tgi])
                    dve(lambda e: e.tensor_tensor(out=gi[:, :, 0], in0=giw[:, 0], in1=giw[:, 1], op=ALU.subtract), [tgi], [tgi])
                    dve(lambda e: e.tensor_tensor(out=gi[:, :, 1], in0=giw[:, 2], in1=giw[:, 3], op=ALU.add), [tgi], [tgi])
                    dve(lambda e, d=d: e.tensor_tensor(out=gi, in0=gi, in1=q(MAG)[:, d * 8:(d + 1) * 8].unsqueeze(2).to_broadcast([128, 8, 2]), op=ALU.mult), [tgi, tp_], [tgi])
                bre, bim = bu4[:, :, 0, :], bu4[:, :, 1, :]
                dve(lambda e, cs_=cs_, sn_=sn_: e.tensor_tensor(out=tt[0], in0=bre, in1=cs_, op=ALU.mult), tb4 + [tE], [ttt])
                dve(lambda e, cs_=cs_, sn_=sn_: e.tensor_tensor(out=tt[1], in0=bim, in1=sn_, op=ALU.mult), tb4 + [tE], [ttt])
                dve(lambda e, cs_=cs_, sn_=sn_: e.tensor_tensor(out=tt[2], in0=bim, in1=cs_, op=ALU.mult), tb4 + [tE], [ttt])
                dve(lambda e, cs_=cs_, sn_=sn_: e.tensor_tensor(out=tt[3], in0=bre, in1=sn_, op=ALU.mult), tb4 + [tE], [ttt])
                pool(lambda e: e.tensor_tensor(out=w[:, :, 0, :], in0=tt[0], in1=tt[1], op=ALU.add), [ttt], [tw])
                pool(lambda e: e.tensor_tensor(out=w[:, :, 1, :], in0=tt[2], in1=tt[3], op=ALU.subtract), [ttt], [tw])
                if n_ > 0:
                    pool(lambda e, first=first: e.tensor_tensor(out=w[:, :, :, first], in0=w[:, :, :, first], in1=gi, op=ALU.add), [tw, tgi], [tw])
                gf = g.rearrange("p g r t -> p (g r t)")
                wf = w.rearrange("p g r t -> p (g r t)")
                rf = RD[:, d].rearrange("p k t -> p (k t)")
                if d == 0:
                    dve(lambda e, rf=rf: e.tensor_tensor_scan(out=gf, data0=rf, data1=wf, initial=0.0, op0=ALU.mult, op1=ALU.add), [tw, tRD], [tg])
                else:
                    dve(lambda e, rf=rf: e.tensor_tensor_scan(out=gf[:, ::-1], data0=rf[:, ::-1], data1=wf[:, ::-1], initial=0.0, op0=ALU.mult, op1=ALU.add), [tw, tRD], [tg])
                gre, gim = g[:, :, 0, :], g[:, :, 1, :]
                dve(lambda e, cs_=cs_, sn_=sn_: e.tensor_tensor(out=tt[0], in0=gre, in1=cs_, op=ALU.mult), [tg, tE], [ttt])
                dve(lambda e, cs_=cs_, sn_=sn_: e.tensor_tensor(out=tt[1], in0=gim, in1=sn_, op=ALU.mult), [tg, tE], [ttt])
                dve(lambda e, cs_=cs_, sn_=sn_: e.tensor_tensor(out=tt[2], in0=gre, in1=sn_, op=ALU.mult), [tg, tE], [ttt])
                dve(lambda e, cs_=cs_, sn_=sn_: e.tensor_tensor(out=tt[3], in0=gim, in1=cs_, op=ALU.mult), [tg, tE], [ttt])
                pool(lambda e: e.tensor_tensor(out=hs[:, :, 0, :], in0=tt[0], in1=tt[1], op=ALU.subtract), [ttt], [ths])
                pool(lambda e: e.tensor_tensor(out=hs[:, :, 1, :], in0=tt[2], in1=tt[3], op=ALU.add), [ttt], [ths])
                for gc in range(8):
                    terms = [(hs[:, gc, 0, :], CSb[:, d * 8 + gc, 0, :], [ths, tcs]), (hs[:, gc, 1, :], CSb[:, d * 8 + gc, 1, :], [ths, tcs])]
                    if d == 0:
                        terms.append((UT[:, gc // 4, tk], DDb[:, gc, :], [tUT[i], tcs]))
                    for k, (lt, rh, R_) in enumerate(terms):
                        self.mm(self.ps[4][:, gc * 32:(gc + 1) * 32], lt, rh, k == 0, k == len(terms) - 1, R_, [self.tp[4]])
                if d == 0:
                    act(lambda e, i=i: e.activation(out=Y[:, i, :], in_=self.ps[4][:, 0:256], func=AF.Copy), [self.tp[4]], [tY[i]])
                    if getattr(self, 'dbg_y', None) is not None:
                        self.store(self.dbg_y[i * 128:(i + 1) * 128, :], Y[:, i, :], [tY[i]])
                    continue
                if i >= nq:
                    continue
                dve(lambda e, i=i: e.tensor_tensor(out=z, in0=self.ps[4][:, 0:256], in1=Y[:, i, :], op=ALU.add), [self.tp[4], tY[i]], [tz])
                pool(lambda e: e.tensor_tensor(out=z2, in0=z, in1=z, op=ALU.mult), [tz], [tz])
                pool(lambda e: e.tensor_scalar(out=z2, in0=z2, scalar1=0.044715, scalar2=1.0, op0=ALU.mult, op1=ALU.add), [tz], [tz])
                pool(lambda e: e.tensor_tensor(out=z2, in0=z2, in1=z, op=ALU.mult), [tz], [tz])
                act(lambda e: e.activation(out=z2, in_=z2, func=AF.Sigmoid, scale=1.5957691216057308), [tz], [tz])
                dve(lambda e: e.tensor_tensor(out=z, in0=z, in1=z2, op=ALU.mult), [tz], [tz])
                dve(lambda e: e.tensor_copy(out=zb, in_=z), [tz], [tz])
                for c in range(2):
                    self.mm(self.ps[5][:, c * 128:(c + 1) * 128], zb[:, c * 128:(c + 1) * 128], self.ident_b, True, True, [tz, self.tconst], [self.tp[5]])
                act(lambda e: e.activation(out=zT, in_=self.ps[5][:, 0:256].rearrange("p (c t) -> p c t", t=128), func=AF.Copy), [self.tp[5]], [tzT])
                for c in range(2):
                    self.mm(self.ps[6][:, 0:256], zT[:, c, :], Wg[:, c, :], c == 0, c == 1, [tzT, tcs], [self.tp[6]])
                act(lambda e: e.activation(out=z2, in_=self.ps[6][:, 0:256], func=AF.Sigmoid), [self.tp[6]], [tz])
                dve(lambda e, i=i: e.tensor_tensor(out=OC[:, i, :], in0=z, in1=z2, op=ALU.mult), [tz], [tOC[i]])


def rv_tab(E, dg0, d, n=2):
    if d == 0:
        return E[:, dg0:dg0 + n, 0:128]
    return E[:, dg0:dg0 + n, 127::-1]


def _consts():
    c = {}
    c["c_ident_f"] = np.eye(128, dtype=np.float32)
    c["c_ident_b"] = np.eye(128, dtype=np.float32).astype(ml_dtypes.bfloat16)
    pos = np.arange(NL * 128)
    row = (pos // 64).astype(np.float32)
    col = (pos % 64).astype(np.float32)
    inv = (10000.0 ** (-np.arange(16, dtype=np.float32) / 16)).astype(np.float32)
    ang = np.concatenate([row[:, None] * inv, col[:, None] * inv], -1).astype(np.float32)
    cs = np.concatenate([np.cos(ang), -np.sin(ang), np.sin(ang)], -1).astype(np.float32)
    ctxcs = np.concatenate([np.ones((256, 32), np.float32), np.zeros((256, 64), np.float32)], -1)
    cs = np.concatenate([cs, ctxcs], 0).reshape(NT, 128, 96).transpose(1, 0, 2)
    c["c_rope"] = np.ascontiguousarray(cs)
    q = np.arange(128)[:, None]
    k = np.arange(128)[None, :]
    tri = np.stack([np.where(k >= q, 0.0, NEG), np.where(k <= q, 0.0, NEG)], 1).astype(np.float32)
    c["c_tri"] = tri.astype(ml_dtypes.bfloat16)
    nm = np.full((5, 128, 10, 64), NEG, np.float32)
    qc = np.arange(64)
    cstart = np.clip(qc - 8, 0, 48)
    kc = np.arange(64)
    colv = (kc[None, :] >= cstart[:, None]) & (kc[None, :] < cstart[:, None] + 16)
    def fill(var, j, i0, nr):
        for rl in range(2):
            r = 2 * j + rl
            rs = int(np.clip(r - 4, 0, 24))
            for ii in range(nr):
                i = i0 + ii
                if rs <= i < rs + 8:
                    blk = np.where(colv, 0.0, NEG)
                    nm[var, rl * 64:(rl + 1) * 64, ii, :] = blk
    fill(0, 5, 6, 10)
    fill(1, 0, 0, 8); fill(2, 1, 0, 8); fill(3, 14, 24, 8); fill(4, 15, 24, 8)
    nm2 = nm.transpose(1, 0, 2, 3).reshape(128, 5, 640)
    c["c_namask"] = np.ascontiguousarray(np.concatenate([nm2[:, 0, :]] + [nm2[:, v, 0:512] for v in range(1, 5)], 1)).astype(ml_dtypes.bfloat16)
    tau = np.zeros((128, 130), np.float32)
    tau[:, :] = np.arange(130, dtype=np.float32)[None, :]
    c["c_tau"] = tau
    return c


def _prep_shared(I):
    f = np.float32
    d = dict(_consts())
    for k in ("ada_w", "ada_b", "w_in", "w_out"):
        d[k] = np.ascontiguousarray(I[k], dtype=f)
    rp = np.zeros((DEPTH, 4, 18, 128), f)
    rp[:, :, 1:16, 48:79] = I["na_rpb"][:, :, :, ::-1]
    d["rpbh"] = rp
    d["gqk"] = np.ascontiguousarray(np.stack([I["ga_q_norm"], I["ga_k_norm"]], 1), dtype=f)
    def st(a):
        L = a.shape[0]
        rest = a.shape[4:]
        a = a.reshape((L, 2, 8, 2, 64) + rest)
        a = np.moveaxis(a, (3, 4), (1, 2))
        return np.ascontiguousarray(a.reshape((L, 128, 16) + rest))
    ls = np.broadcast_to(I["ssm_log_step"][..., None], I["ssm_lambda_re"].shape)
    d["ssm_vec"] = np.ascontiguousarray(np.stack([st(I["ssm_lambda_re"]), st(I["ssm_lambda_im"]), st(np.ascontiguousarray(ls))], -1), dtype=f)
    d["ssm_bs"] = np.ascontiguousarray(np.stack([st(I["ssm_b_re"]), st(I["ssm_b_im"])], -2), dtype=f)
    cre = st(np.swapaxes(I["ssm_c_re"], -1, -2))
    cim = st(np.swapaxes(I["ssm_c_im"], -1, -2))
    cs = np.zeros((DEPTH, 128, 16, 2, 32), f)
    for gl in range(2):
        cs[:, gl * 64:(gl + 1) * 64, :, 0, gl * 16:(gl + 1) * 16] = cre[:, gl * 64:(gl + 1) * 64]
        cs[:, gl * 64:(gl + 1) * 64, :, 1, gl * 16:(gl + 1) * 16] = cim[:, gl * 64:(gl + 1) * 64]
    d["ssm_cs"] = cs
    dd = np.zeros((DEPTH, 128, 8, 32), f)
    sd = I["ssm_d"].reshape(DEPTH, 2, 128)
    for gc in range(8):
        for j in range(32):
            pl = (gc % 4) * 32 + j
            dd[:, pl, gc, j] = sd[:, gc // 4, pl]
    d["ssm_dd"] = dd
    d["ssm_wglu"] = np.ascontiguousarray(I["ssm_w_glu"], dtype=f)
    d["sink"] = np.ascontiguousarray(I["sw_sink"], dtype=f)
    d["lngb"] = np.ascontiguousarray(np.stack([I["ln1_g"], I["ln1_b"], I["ln2_g"], I["ln2_b"]], 1), dtype=f)
    d["ffn_g"] = I["ffn_w_gate"]; d["ffn_u"] = I["ffn_w_up"]; d["ffn_d"] = I["ffn_w_down"]
    d["moe_r"] = I["moe_w_router"]; d["moe_rb"] = I["moe_b_router"]
    d["moe_g"] = I["moe_w_gate"]; d["moe_u"] = I["moe_w_up"]; d["moe_d"] = I["moe_w_down"]
    return d


def _prep_core(I, b, hx=None):
    if hx is None:
        hx = np.concatenate([I["x"][b], I["ctx"][b]], 0)
    cv = np.stack([I["c"][b].reshape(8, 128).T, I["c_ctx"].reshape(8, 128).T], -1)
    return {"hx": np.ascontiguousarray(hx, dtype=np.float32), "cv": np.ascontiguousarray(cv, dtype=np.float32)}


def build_program(layers=(0, 1, 2, 3)):
    b = B(list(layers))
    b.setup()
    for l in layers:
        b.ada_cols(l)
        b.mixer_phase(l)
        b.ffn_phase(l)
    for i in range(NL):
        b.store(b.out[i * 128:(i + 1) * 128, :], b.H[:, i, :], [b.tH[i]])
    with b.nc.Block() as blk:
        b.S.emit(blk, b.fin)
    return b


def kernel(**inputs):
    I = {k: np.asarray(v) for k, v in inputs.items()}
    n = 8
    b = build_program()
    shared = _prep_shared(I)
    shared = {k: v for k, v in shared.items() if k in b.din}
    in_maps = []
    for c in range(n):
        m = dict(shared)
        m.update(_prep_core(I, c))
        in_maps.append(m)
    res = run_bass_kernel_spmd(b.nc, in_maps, core_ids=list(range(n)))
    out = np.stack([np.asarray(r["out"], dtype=np.float32).reshape(NL * 128, D) for r in res.results], 0)
    return out
```

```python
import numpy as np
import ml_dtypes
import concourse.bass as bass
import concourse.mybir as mybir
from concourse.bass_utils import run_bass_kernel_spmd

F32 = mybir.dt.float32
BF16 = mybir.dt.bfloat16
I32 = mybir.dt.int32
ALU = mybir.AluOpType
AF = mybir.ActivationFunctionType
AX = mybir.AxisListType


class Tk:
    __slots__ = ("w", "r")

    def __init__(self):
        self.w = None
        self.r = []


class Sched:
    ENG = ("pe", "act", "dve", "pool", "sp")
    NDMA = {"sp": 12, "pool": 8, "act": 4}

    def __init__(self, nc):
        self.nc = nc
        self.prog = {e: [] for e in self.ENG}
        self.n = {e: 0 for e in self.ENG}
        self.seen = {e: {} for e in self.ENG}
        self.signaled = {e: set() for e in self.ENG}
        self.dma_rr = {q: 0 for q in self.NDMA}
        self.dma_tot = {}
        self.lastc = {e: 0 for e in self.ENG}
        self.cur_fence = {}

    def fence(self):
        for e in self.ENG:
            if self.lastc[e]:
                self.cur_fence[e] = self.lastc[e]
        for key, tot in self.dma_tot.items():
            self.cur_fence[key] = tot

    def _deps(self, eng, reads, writes):
        deps = dict(self.cur_fence)
        def add(t):
            if t is None:
                return
            k, v = t
            if deps.get(k, 0) < v:
                deps[k] = v
        for t in reads:
            add(t.w)
        for t in writes:
            add(t.w)
            for r in t.r:
                add(r)
        waits = []
        for k, v in deps.items():
            if k == "pe" and eng == "pe":
                continue
            if self.seen[eng].get(k, 0) < v:
                self.seen[eng][k] = v
                waits.append((k, v))
                if k in self.signaled:
                    self.signaled[k].add(v)
        return waits

    def _commit(self, ticket, reads, writes):
        for t in reads:
            t.r.append(ticket)
        for t in writes:
            t.w = ticket
            t.r = []

    def op(self, eng, fn, R=(), W=()):
        waits = self._deps(eng, R, W)
        self.n[eng] += 1
        self.lastc[eng] = self.n[eng]
        ticket = (eng, self.n[eng])
        self.prog[eng].append((waits, fn, ticket, None))
        self._commit(ticket, R, W)

    def dma(self, q, out, in_, R=(), W=(), slow=False):
        j = self.dma_rr[q]
        self.dma_rr[q] = (j + 1) % self.NDMA[q]
        key = ("d", q, j)
        waits = self._deps(q, R, W)
        prev = self.dma_tot.get(key, 0)
        if prev and self.seen[q].get(key, 0) < prev:
            self.seen[q][key] = prev
            waits.append((key, prev))
        self.dma_tot[key] = prev + 16
        ticket = (key, prev + 16)
        self.n[q] += 1
        if slow:
            fn = lambda e, o=out, i=in_: e.dma_start(out=o, in_=i, allow_slow_non_contiguous=True)
        else:
            fn = lambda e, o=out, i=in_: e.dma_start(out=o, in_=i)
        self.prog[q].append((waits, fn, (q, self.n[q]), key))
        self._commit(ticket, R, W)

    def emit(self, block, final_waits):
        nc = self.nc
        sems = {e: nc.alloc_semaphore("s_" + e) for e in self.ENG}
        for key in self.dma_tot:
            sems[key] = nc.alloc_semaphore("d_%s%d" % (key[1], key[2]))
        rank = {}
        for e in self.ENG:
            rank[e] = {v: i + 1 for i, v in enumerate(sorted(self.signaled[e]))}

        def val(k, v):
            return rank[k][v] if k in rank else v

        def run(ename, eng):
            for waits, fn, ticket, dkey in self.prog[ename]:
                for k, v in waits:
                    eng.wait_ge(sems[k], val(k, v))
                ins = fn(eng)
                if dkey is not None:
                    ins.then_inc(sems[dkey], 16)
                elif ticket[1] in self.signaled[ename]:
                    ins.then_inc(sems[ename], 1)
            if ename == "sp":
                for k, v in final_waits:
                    eng.wait_ge(sems[k], val(k, v))

        for k, v in final_waits:
            if k in self.signaled:
                self.signaled[k].add(v)
        for e in self.ENG:
            rank[e] = {v: i + 1 for i, v in enumerate(sorted(self.signaled[e]))}
        block.tensor(lambda e: run("pe", e))
        block.scalar(lambda e: run("act", e))
        block.vector(lambda e: run("dve", e))
        block.gpsimd(lambda e: run("pool", e))
        block.sync(lambda e: run("sp", e))


class Arena:
    def __init__(self, nc, nbytes, S=None):
        self.S = S
        self.t = nc.alloc_sbuf_tensor("arena", [128, nbytes // 4], F32)
        self.nbytes = nbytes
        self.top = 0
        self.peak = 0

    def alloc(self, free_shape, dtype):
        esz = 2 if dtype == BF16 else 4
        n = int(np.prod(free_shape))
        nb = (n * esz + 63) // 64 * 64
        off = self.top
        self.top += nb
        self.peak = max(self.peak, self.top)
        assert self.top <= self.nbytes, ("arena overflow", self.top, self.nbytes)
        ap = self.t[:, off // 4:(off + nb) // 4]
        if dtype != F32:
            ap = ap.bitcast(dtype)
        ap = ap[:, 0:n]
        if len(free_shape) == 2:
            ap = ap.rearrange("p (a b) -> p a b", b=free_shape[1])
        elif len(free_shape) == 3:
            ap = ap.rearrange("p (a b c) -> p a b c", b=free_shape[1], c=free_shape[2])
        return ap

    def mark(self):
        return self.top

    def release(self, m):
        self.top = m
        self.S.fence()


D = 1024
NT = 18
NL = 16
DEPTH = 4
DFF = 2816
DFE = 3584
NE = 8
ALPHA = float((2 * DEPTH) ** 0.25)
NEG = -1.0e30
SLAB = 512


class B:
    def __init__(self, layers, dbg=None):
        self.layers = layers
        self.dbg = dbg or {}
        nc = self.nc = bass.Bass("TRN2", target_bir_lowering=False)
        self.S = Sched(nc)
        self.A = Arena(nc, 212800, self.S)
        self.fin = []
        self.din = {}
        self.psall = nc.alloc_psum_tensor("psall", [128, 8 * 512], F32)
        self.ps = [self.psall[:, i * 512:(i + 1) * 512] for i in range(8)]
        self.tp = [Tk() for _ in range(8)]

    SHAPES = {
        "hx": ([NT * 128, D], F32), "cv": ([128, 8, 2], F32), "c_ident_f": ([128, 128], F32), "c_ident_b": ([128, 128], BF16),
        "c_rope": ([128, NT, 96], F32), "c_tri": ([128, 2, 128], BF16), "c_namask": ([128, 2688], BF16), "c_tau": ([128, 130], F32),
        "ada_w": ([DEPTH, D, 6 * D], F32), "ada_b": ([DEPTH, 6 * D], F32), "w_in": ([DEPTH, D, 2048], F32), "w_out": ([DEPTH, D, D], F32),
        "rpbh": ([DEPTH, 4, 18, 128], F32), "gqk": ([DEPTH, 2, 64], F32), "ssm_vec": ([DEPTH, 128, 16, 3], F32),
        "ssm_bs": ([DEPTH, 128, 16, 2, 16], F32), "ssm_cs": ([DEPTH, 128, 16, 2, 32], F32), "ssm_dd": ([DEPTH, 128, 8, 32], F32),
        "ssm_wglu": ([DEPTH, 256, 256], F32), "sink": ([DEPTH, 4], F32), "lngb": ([DEPTH, 4, D], F32),
        "ffn_g": ([2, D, DFF], F32), "ffn_u": ([2, D, DFF], F32), "ffn_d": ([2, DFF, D], F32),
        "moe_r": ([2, D, NE], F32), "moe_rb": ([2, NE], F32), "moe_g": ([2, NE, D, DFE], F32), "moe_u": ([2, NE, D, DFE], F32),
        "moe_d": ([2, NE, DFE, D], F32),
    }

    def __getattr__(self, name):
        sh = B.SHAPES.get(name)
        if sh is None:
            raise AttributeError(name)
        ap = self.inp(name, sh[0], sh[1])
        self.__dict__[name] = ap
        return ap

    def dt_(self, name):
        getattr(self, name)
        return self.din[name]

    def inp(self, name, shape, dt=F32):
        t = self.nc.dram_tensor(name, list(shape), dt, kind="ExternalInput")
        self.din[name] = t
        return t.ap()

    def outp(self, name, shape, dt=F32):
        return self.nc.dram_tensor(name, list(shape), dt, kind="ExternalOutput").ap()

    def store(self, dst, src, R):
        S = self.S
        S.dma("sp", dst, src, R=R)
        j = (S.dma_rr["sp"] - 1) % S.NDMA["sp"]
        key = ("d", "sp", j)
        self.fin.append((key, S.dma_tot[key]))

    def pe(self, fn, R=(), W=()): self.S.op("pe", fn, R, W)
    def act(self, fn, R=(), W=()): self.S.op("act", fn, R, W)
    def dve(self, fn, R=(), W=()): self.S.op("dve", fn, R, W)
    def pool(self, fn, R=(), W=()): self.S.op("pool", fn, R, W)

    def mm(self, out, lhsT, rhs, start, stop, R, W):
        self.pe(lambda e: e.matmul(out, lhsT=lhsT, rhs=rhs, start=start, stop=stop), R, W)

    def tr(self, out, in_, R, W):
        self.pe(lambda e: e.transpose(out=out, in_=in_, identity=self.ident_f), R + [self.tconst], W)

    def rstd_from(self, out, var_ap, scale, eps, R, tk):
        self.act(lambda e: e.activation(out=out, in_=var_ap, func=AF.Sqrt, bias=self.eps_ap(eps), scale=scale), R, [tk])
        self.dve(lambda e: e.reciprocal(out=out, in_=out), [tk], [tk])

    def eps_ap(self, eps):
        return self.epsc[:, 0:1]

    def setup(self):
        A = self.A
        inp = self.inp
        self.out = self.outp("out", [NL * 128, D])

        S = self.S
        self.tconst = Tk()
        self.H = A.alloc([NT, D], F32)
        self.tH = [Tk() for _ in range(NT)]
        self.ident_f = A.alloc([128], F32)
        self.ident_b = A.alloc([128], BF16)
        self.epsc = A.alloc([2], F32)
        self.csil = A.alloc([8, 2], F32)
        self.modc = A.alloc([DEPTH, 32, 2], F32)
        self.tmodc = Tk()
        for dst, src in ((self.ident_f, self.c_ident_f), (self.ident_b, self.c_ident_b), (self.csil, self.cv)):
            S.dma("sp", dst, src, W=[self.tconst])
        self.dve(lambda e: e.memset(self.epsc, 1e-6), W=[self.tconst])
        for i in range(NT):
            S.dma("sp" if i % 2 == 0 else "act", self.H[:, i, :], self.hx[i * 128:(i + 1) * 128, :], W=[self.tH[i]])
        self.act(lambda e: e.activation(out=self.csil, in_=self.csil, func=AF.Silu), [self.tconst], [self.tconst])

    def ada_cols(self, l):
        A, S = self.A, self.S
        m = A.mark()
        blocks = [0, 1, 3, 4]
        wst = [A.alloc([8, 128], F32) for _ in range(3)]
        tw = [Tk() for _ in range(3)]
        bcol = A.alloc([32], F32)
        tb = Tk()
        for j, blk in enumerate(blocks):
            S.dma("sp", bcol[:, j * 8:(j + 1) * 8], self.ada_b[l, blk * D:(blk + 1) * D].rearrange("(kc p) -> p kc", p=128), W=[tb], slow=True)
        n = 0
        for j, blk in enumerate(blocks):
            for fc in range(8):
                b = n % 3
                col0 = blk * D + fc * 128
                S.dma("sp" if n % 2 == 0 else "act", wst[b], self.ada_w[l, :, col0:col0 + 128].rearrange("(kc p) n -> p kc n", p=128), W=[tw[b]])
                pb = 7
                for kc in range(8):
                    self.mm(self.ps[pb][:, 0:2], wst[b][:, kc, :], self.csil[:, kc, :], kc == 0, kc == 7, [tw[b], self.tconst], [self.tp[pb]])
                idx = j * 8 + fc
                add = 1.0 if blk in (1, 4) else 0.0
                self.dve(lambda e, idx=idx, add=add, pb=pb: e.tensor_scalar(out=self.modc[:, l, idx, :], in0=self.ps[pb][:, 0:2], scalar1=bcol[:, idx:idx + 1],
                                                                           scalar2=add, op0=ALU.add, op1=ALU.add), [self.tp[pb], tb], [self.tmodc])
                n += 1
        A.release(m)

    def ada_gate(self, l, which, G, tG):
        A, S = self.A, self.S
        m = A.mark()
        blk = 2 if which == 0 else 5
        crep = A.alloc([2, 8, 128], F32); tcr = Tk()
        for v in range(2):
            for kc in range(8):
                self.dve(lambda e, v=v, kc=kc: e.tensor_copy(out=crep[:, v, kc, :], in_=self.csil[:, kc, v:v + 1].to_broadcast([128, 128])),
                         [self.tconst], [tcr])
        wst = [A.alloc([8, 512], F32) for _ in range(2)]
        tw = [Tk() for _ in range(2)]
        bb = A.alloc([D], F32)
        tb = Tk()
        S.dma("sp", bb, self.ada_b[l:l + 1, blk * D:(blk + 1) * D].partition_broadcast(128) if False else
              bass.AP(self.dt_("ada_b"), l * 6 * D + blk * D, [[0, 128], [1, D]]), W=[tb])
        for nb in range(2):
            col0 = blk * D + nb * 512
            S.dma("sp", wst[nb], self.ada_w[l, :, col0:col0 + 512].rearrange("(kc p) n -> p kc n", p=128), W=[tw[nb]])
            for v in range(2):
                pb = 5 + v
                for kc in range(8):
                    self.mm(self.ps[pb][:, :], crep[:, v, kc, :], wst[nb][:, kc, :], kc == 0, kc == 7, [tw[nb], tcr], [self.tp[pb]])
                self.dve(lambda e, v=v, nb=nb, pb=pb: e.tensor_tensor(out=G[:, v, nb * 512:(nb + 1) * 512], in0=self.ps[pb][:, :], in1=bb[:, nb * 512:(nb + 1) * 512], op=ALU.add),
                         [self.tp[pb], tb], [tG])
        A.release(m)

    def load_ln(self, l, which, LN, tLN):
        for j in range(2):
            self.S.dma("sp", LN[:, j, :], bass.AP(self.dt_("lngb"), (l * 4 + which * 2 + j) * D, [[0, 128], [1, D]]), W=[tLN])

    def make_aT(self, l, i, which, aT, taT, aT32=None, taT32=None):
        v = 1 if i >= NL else 0
        for half in range(2):
            pb = 5 + half
            for q in range(4):
                kc = half * 4 + q
                self.tr(self.ps[pb][:, q * 128:(q + 1) * 128], self.H[:, i, kc * 128:(kc + 1) * 128], [self.tH[i]], [self.tp[pb]])
            for q in range(4):
                kc = half * 4 + q
                sc = self.modc[:, l, (which * 2 + 1) * 8 + kc, v:v + 1]
                sh = self.modc[:, l, (which * 2) * 8 + kc, v:v + 1]
                self.act(lambda e, kc=kc, q=q, pb=pb, sc=sc, sh=sh: e.activation(out=aT[:, kc, :], in_=self.ps[pb][:, q * 128:(q + 1) * 128], func=AF.Identity, bias=sh, scale=sc),
                         [self.tp[pb], self.tmodc], [taT])
                if aT32 is not None:
                    self.act(lambda e, kc=kc, q=q, pb=pb, sc=sc, sh=sh: e.activation(out=aT32[:, kc, :], in_=self.ps[pb][:, q * 128:(q + 1) * 128], func=AF.Identity, bias=sh, scale=sc),
                             [self.tp[pb], self.tmodc], [taT32])

    def resid_ln(self, i, ys, G, tG, LN, tLN, tmp, ttmp, st, tst):
        v = 1 if i >= NL else 0
        for hf, (yap, ty) in enumerate(ys):
            self.dve(lambda e, hf=hf, yap=yap: e.tensor_tensor(out=tmp[:, hf * 512:(hf + 1) * 512], in0=yap, in1=G[:, v, hf * 512:(hf + 1) * 512], op=ALU.mult),
                     [ty, tG], [ttmp])
        self.ln_tail(i, tmp, ttmp, LN, tLN, st, tst)

    def ln_tail(self, i, tmp, ttmp, LN, tLN, st, tst):
        self.dve(lambda e: e.scalar_tensor_tensor(out=tmp, in0=self.H[:, i, :], scalar=ALPHA, in1=tmp, op0=ALU.mult, op1=ALU.add), [self.tH[i], ttmp], [ttmp])
        for hf in range(2):
            self.dve(lambda e, hf=hf: e.bn_stats(out=st[:, hf * 6:(hf + 1) * 6], in_=tmp[:, hf * 512:(hf + 1) * 512]), [ttmp], [tst])
        self.dve(lambda e: e.bn_aggr(out=st[:, 12:14], in_=st[:, 0:12]), [tst], [tst])
        self.rstd_from(st[:, 14:15], st[:, 13:14], 1.0, 1e-6, [tst], tst)
        self.dve(lambda e: e.tensor_scalar(out=tmp, in0=tmp, scalar1=st[:, 12:13], scalar2=st[:, 14:15], op0=ALU.subtract, op1=ALU.mult), [ttmp, tst], [ttmp])
        self.pool(lambda e: e.tensor_tensor(out=tmp, in0=tmp, in1=LN[:, 0, :], op=ALU.mult), [ttmp, tLN], [ttmp])
        self.pool(lambda e: e.tensor_tensor(out=self.H[:, i, :], in0=tmp, in1=LN[:, 1, :], op=ALU.add), [ttmp, tLN], [self.tH[i]])

    def ffn_phase(self, l):
        A, S = self.A, self.S
        last = (l == DEPTH - 1)
        nt = NL if last else NT
        moe = (l % 2 == 1)
        li = l // 2
        m = A.mark()
        G = A.alloc([2, D], F32); tG = Tk()
        LN = A.alloc([2, D], F32); tLN = Tk()
        self.ada_gate(l, 1, G, tG)
        self.load_ln(l, 1, LN, tLN)
        FT = A.alloc([8, nt * 128], BF16)
        tFT = [Tk() for _ in range(nt)]
        moe_ = (l % 2 == 1)
        if moe_:
            gate = A.alloc([nt, NE], F32); tgate = Tk()
            a32 = A.alloc([8, 128], F32); ta32 = Tk()
            rt = self.router_setup(l // 2)
        for i in range(nt):
            if moe_:
                self.make_aT(l, i, 1, FT[:, :, i * 128:(i + 1) * 128], tFT[i], a32, ta32)
                self.router_tile(rt, i, a32, ta32, gate, tgate)
            else:
                self.make_aT(l, i, 1, FT[:, :, i * 128:(i + 1) * 128], tFT[i])
        experts = range(NE) if moe else [0]
        dff = DFE if moe else DFF
        nsl = dff // SLAB + (1 if dff % SLAB else 0)
        facc = A.alloc([nt, D], F32) if False else None
        tmp = A.alloc([D], F32); ttmp = Tk()
        st = A.alloc([16], F32); tst = Tk()
        for i in range(nt):
            self.pool(lambda e, i=i: e.tensor_scalar(out=self.H[:, i, :], in0=self.H[:, i, :], scalar1=ALPHA, scalar2=None, op0=ALU.mult), [self.tH[i]], [self.tH[i]])
        NB = 2
        wg = [A.alloc([8, SLAB], BF16) for _ in range(NB)]
        wu = [A.alloc([8, SLAB], BF16) for _ in range(NB)]
        wd = [A.alloc([SLAB // 128, D], BF16) for _ in range(NB)]
        tw = [Tk() for _ in range(NB)]
        twu = [Tk() for _ in range(NB)]
        twd = [Tk() for _ in range(NB)]
        h1 = [A.alloc([SLAB // 128, 512], BF16) for _ in range(2)]
        th1 = [Tk() for _ in range(2)]
        sg = [A.alloc([512], F32) for _ in range(2)]
        tsg = [Tk() for _ in range(2)]
        yt = [A.alloc([D], F32)] * 2
        tyt = [Tk()] * 2
        nblk = (nt * 128 + 511) // 512
        cnt = 0
        hcnt = 0
        items = [(e_, s) for e_ in experts for s in range(nsl)]

        def issue(k):
            e_, s = items[k]
            Wg = self.moe_g[li, e_] if moe else self.ffn_g[li]
            Wu = self.moe_u[li, e_] if moe else self.ffn_u[li]
            Wd = self.moe_d[li, e_] if moe else self.ffn_d[li]
            b = k % NB
            c0 = s * SLAB
            w = min(SLAB, dff - c0)
            nch = w // 128
            S.dma("pool", wg[b][:, :, 0:w], Wg[:, c0:c0 + w].rearrange("(kc p) n -> p kc n", p=128), W=[tw[b]])
            S.dma("pool", wu[b][:, :, 0:w], Wu[:, c0:c0 + w].rearrange("(kc p) n -> p kc n", p=128), W=[twu[b]])
            S.dma("pool", wd[b][:, 0:nch, :], Wd[c0:c0 + w, :].rearrange("(fc p) n -> p fc n", p=128), W=[twd[b]])

        def mk(k):
            e_, s = items[k]
            b = k % NB
            c0 = s * SLAB
            w = min(SLAB, dff - c0)
            nch = w // 128
            def gu(tb, hb, fcs=None, b=b, nch=nch):
                t0 = tb * 512
                ntok = min(512, nt * 128 - t0)
                tiles = list(range(t0 // 128, (t0 + ntok) // 128))
                for fc in (range(nch) if fcs is None else fcs):
                    if fc >= nch:
                        continue
                    pg, pu = 0 + (fc % 2) * 2, 1 + (fc % 2) * 2
                    for kc in range(8):
                        self.mm(self.ps[pg][:, 0:ntok], wg[b][:, kc, fc * 128:(fc + 1) * 128], FT[:, kc, t0:t0 + ntok], kc == 0, kc == 7,
                                [tw[b]] + [tFT[i] for i in tiles], [self.tp[pg]])
                    for kc in range(8):
                        self.mm(self.ps[pu][:, 0:ntok], wu[b][:, kc, fc * 128:(fc + 1) * 128], FT[:, kc, t0:t0 + ntok], kc == 0, kc == 7,
                                [twu[b]] + [tFT[i] for i in tiles], [self.tp[pu]])
                    sb = fc % 2
                    self.act(lambda e, pg=pg, sb=sb, ntok=ntok: e.activation(out=sg[sb][:, 0:ntok], in_=self.ps[pg][:, 0:ntok], func=AF.Silu), [self.tp[pg]], [tsg[sb]])
                    self.dve(lambda e, pu=pu, sb=sb, hb=hb, fc=fc, ntok=ntok: e.tensor_tensor(out=h1[hb][:, fc, 0:ntok], in0=self.ps[pu][:, 0:ntok], in1=sg[sb][:, 0:ntok], op=ALU.mult),
                             [self.tp[pu], tsg[sb]], [th1[hb]])

            def down(tb, hb, tis=None, b=b, nch=nch, e_=e_):
                t0 = tb * 512
                ntok = min(512, nt * 128 - t0)
                tiles = list(range(t0 // 128, (t0 + ntok) // 128))
                for ti, i in enumerate(tiles):
                    if tis is not None and ti not in tis:
                        continue
                    v = 1 if i >= NL else 0
                    yb = i % 2
                    for hf in range(2):
                        pb = 4 + hf + 2 * (i % 2)
                        for fc in range(nch):
                            self.mm(self.ps[pb][:, :], h1[hb][:, fc, ti * 128:(ti + 1) * 128], wd[b][:, fc, hf * 512:(hf + 1) * 512], fc == 0, fc == nch - 1,
                                    [th1[hb], twd[b]], [self.tp[pb]])
                        if moe:
                            self.act(lambda e, pb=pb, hf=hf, yb=yb, i=i, e_=e_: e.activation(out=yt[yb][:, hf * 512:(hf + 1) * 512], in_=self.ps[pb][:, :], func=AF.Copy, scale=gate[:, i, e_:e_ + 1]),
                                     [self.tp[pb], tgate], [tyt[yb]])
                        else:
                            self.dve(lambda e, pb=pb, hf=hf, v=v, yb=yb: e.tensor_tensor(out=yt[yb][:, hf * 512:(hf + 1) * 512], in0=self.ps[pb][:, :], in1=G[:, v, hf * 512:(hf + 1) * 512], op=ALU.mult),
                                     [self.tp[pb], tG], [tyt[yb]])
                    if moe:
                        self.dve(lambda e, v=v, yb=yb: e.tensor_tensor(out=yt[yb], in0=yt[yb], in1=G[:, v, :], op=ALU.mult), [tyt[yb], tG], [tyt[yb]])
                    self.pool(lambda e, i=i, yb=yb: e.tensor_tensor(out=self.H[:, i, :], in0=yt[yb], in1=self.H[:, i, :], op=ALU.add), [tyt[yb], self.tH[i]], [self.tH[i]])

            return gu, down

        seq = [(k, tb) for k in range(len(items)) for tb in range(nblk)]
        fns = {0: mk(0)}
        hbl = [j % 2 for j in range(len(seq))]
        issue(0)
        fns[0][0](0, hbl[0])
        for j, (k, tb) in enumerate(seq):
            if tb == 0 and k + 1 < len(items):
                issue(k + 1)
            gu_k, down_k = fns[k]
            if j + 1 < len(seq):
                k2, tb2 = seq[j + 1]
                if k2 not in fns:
                    fns[k2] = mk(k2)
                gu_n = fns[k2][0]
                gu_n(tb2, hbl[j + 1], [0, 1])
                down_k(tb, hbl[j], [0])
                gu_n(tb2, hbl[j + 1], [2])
                down_k(tb, hbl[j], [1])
                gu_n(tb2, hbl[j + 1], [3])
                down_k(tb, hbl[j], [2, 3])
            else:
                down_k(tb, hbl[j])
            if tb == nblk - 1:
                fns.pop(k, None)
        for i in range(nt):
            self.ln_only(i, LN, tLN, tmp, ttmp, st, tst)
        A.release(m)

    def ln_only(self, i, LN, tLN, tmp, ttmp, st, tst):
        for hf in range(2):
            self.dve(lambda e, hf=hf: e.bn_stats(out=st[:, hf * 6:(hf + 1) * 6], in_=self.H[:, i, hf * 512:(hf + 1) * 512]), [self.tH[i]], [tst])
        self.dve(lambda e: e.bn_aggr(out=st[:, 12:14], in_=st[:, 0:12]), [tst], [tst])
        self.rstd_from(st[:, 14:15], st[:, 13:14], 1.0, 1e-6, [tst], tst)
        self.dve(lambda e: e.tensor_scalar(out=tmp, in0=self.H[:, i, :], scalar1=st[:, 12:13], scalar2=st[:, 14:15], op0=ALU.subtract, op1=ALU.mult), [self.tH[i], tst], [ttmp])
        self.pool(lambda e: e.tensor_tensor(out=tmp, in0=tmp, in1=LN[:, 0, :], op=ALU.mult), [ttmp, tLN], [ttmp])
        self.pool(lambda e: e.tensor_tensor(out=self.H[:, i, :], in0=tmp, in1=LN[:, 1, :], op=ALU.add), [ttmp, tLN], [self.tH[i]])

    def router_setup(self, li):
        A, S = self.A, self.S
        wr = A.alloc([8, NE], F32); twr = Tk()
        rb = A.alloc([NE], F32)
        S.dma("sp", wr, self.moe_r[li].rearrange("(kc p) n -> p kc n", p=128), W=[twr])
        S.dma("sp", rb, bass.AP(self.dt_("moe_rb"), li * NE, [[0, 128], [1, NE]]), W=[twr])
        return dict(wr=wr, twr=twr, rb=rb, lg=A.alloc([NE], F32), tlg=Tk(), m8=A.alloc([8], F32), wk=A.alloc([2, NE], F32))

    def router_tile(self, rt, i, a32, ta32, gate, tgate):
        wr, twr, rb, lg, tlg, m8, wk = rt["wr"], rt["twr"], rt["rb"], rt["lg"], rt["tlg"], rt["m8"], rt["wk"]
        pb = 7
        for kc in range(8):
            self.mm(self.ps[pb][:, 0:NE], a32[:, kc, :], wr[:, kc, :], kc == 0, kc == 7, [ta32, twr], [self.tp[pb]])
        self.dve(lambda e: e.tensor_tensor(out=lg, in0=self.ps[pb][:, 0:NE], in1=rb, op=ALU.add), [self.tp[pb], twr], [tlg])
        self.dve(lambda e: e.max(out=m8, in_=lg), [tlg], [tlg])
        self.dve(lambda e: e.tensor_scalar(out=wk[:, 0, :], in0=lg, scalar1=m8[:, 1:2], scalar2=None, op0=ALU.is_ge), [tlg], [tlg])
        self.dve(lambda e: e.tensor_scalar(out=wk[:, 1, :], in0=lg, scalar1=m8[:, 0:1], scalar2=None, op0=ALU.subtract), [tlg], [tlg])
        self.act(lambda e: e.activation(out=wk[:, 1, :], in_=wk[:, 1, :], func=AF.Exp), [tlg], [tlg])
        self.dve(lambda e: e.tensor_tensor(out=wk[:, 1, :], in0=wk[:, 1, :], in1=wk[:, 0, :], op=ALU.mult), [tlg], [tlg])
        self.dve(lambda e: e.reduce_sum(out=m8[:, 2:3], in_=wk[:, 1, :], axis=AX.X), [tlg], [tlg])
        self.dve(lambda e: e.reciprocal(out=m8[:, 2:3], in_=m8[:, 2:3]), [tlg], [tlg])
        self.dve(lambda e: e.tensor_scalar(out=gate[:, i, :], in0=wk[:, 1, :], scalar1=m8[:, 2:3], scalar2=None, op0=ALU.mult), [tlg], [tgate])


    def rms_rope(self, src, tsrc, nh, dst, tdst, tile, rw, g=None, perm=False, qs=1.0):
        rope, trope = self.rope, self.tmc
        s3 = src.rearrange("p (h d) -> p h d", d=64)
        x, ss, t, tw = rw["x"], rw["ss"], rw["t"], rw["tw"]
        x3 = x[:, 0:nh * 64].rearrange("p (h d) -> p h d", d=64)
        if g is not None:
            for h in range(nh):
                self.act(lambda e, h=h: e.activation(out=x3[:, h, :], in_=s3[:, h, :], func=AF.Square, accum_out=ss[:, h:h + 1]), [tsrc], [tw])
            self.act(lambda e: e.activation(out=ss[:, 0:nh], in_=ss[:, 0:nh], func=AF.Sqrt, bias=self.epsc[:, 0:1], scale=1.0 / 64), [tw, self.tconst], [tw])
            self.dve(lambda e: e.reciprocal(out=ss[:, 0:nh], in_=ss[:, 0:nh]), [tw], [tw])
            for h in range(nh):
                self.dve(lambda e, h=h: e.scalar_tensor_tensor(out=x3[:, h, :], in0=s3[:, h, :], scalar=ss[:, h:h + 1], in1=g, op0=ALU.mult, op1=ALU.mult), [tsrc, tw, self.tmc], [tw])
            cur, tcur, qs = x3, tw, 1.0
        else:
            cur, tcur = s3, tsrc
        C = rope[:, tile, 0:32].unsqueeze(1).unsqueeze(1).to_broadcast([128, nh, 2, 32])
        Sg = rope[:, tile, 32:96].rearrange("p (a d) -> p a d", d=32).unsqueeze(1).to_broadcast([128, nh, 2, 32])
        c4 = cur.rearrange("p h (a d) -> p h a d", d=32)
        sw = c4[:, :, ::-1, :]
        t1 = t[:, 0, 0:nh * 64].rearrange("p (h a d) -> p h a d", a=2, d=32)
        t2 = t[:, 1, 0:nh * 64].rearrange("p (h a d) -> p h a d", a=2, d=32)
        if qs != 1.0:
            self.dve(lambda e: e.scalar_tensor_tensor(out=t1, in0=c4, scalar=qs, in1=C, op0=ALU.mult, op1=ALU.mult), [tcur, trope], [tw])
            self.dve(lambda e: e.scalar_tensor_tensor(out=t2, in0=sw, scalar=qs, in1=Sg, op0=ALU.mult, op1=ALU.mult), [tcur, trope], [tw])
        else:
            self.dve(lambda e: e.tensor_tensor(out=t1, in0=c4, in1=C, op=ALU.mult), [tcur, trope], [tw])
            self.dve(lambda e: e.tensor_tensor(out=t2, in0=sw, in1=Sg, op=ALU.mult), [tcur, trope], [tw])
        f1 = t[:, 0, 0:nh * 64].rearrange("p (h d) -> p h d", d=64)
        f2 = t[:, 1, 0:nh * 64].rearrange("p (h d) -> p h d", d=64)
        if perm:
            dv = dst.rearrange("p (b s d) -> p s b d", b=2, s=2, d=64)
            f1 = f1.rearrange("p (s b) d -> p s b d", b=2)
            f2 = f2.rearrange("p (s b) d -> p s b d", b=2)
        else:
            dv = dst.rearrange("p (h d) -> p h d", d=64)
        self.dve(lambda e: e.tensor_tensor(out=dv, in0=f1, in1=f2, op=ALU.add), [tw], [tdst])

    def mixer_phase(self, l):
        A, S = self.A, self.S
        last = (l == DEPTH - 1)
        nq = NL if last else NT
        m_all = A.mark()
        self.rope = A.alloc([NT, 96], F32)
        gqk = A.alloc([2, 64], F32)
        self.tmc = Tk()
        S.dma("sp", self.rope, self.c_rope, W=[self.tmc])
        S.dma("sp", gqk, bass.AP(self.dt_("gqk"), l * 128, [[0, 128], [1, 128]]), W=[self.tmc])
        OC = A.alloc([NT, 256], BF16); tOC = [Tk() for _ in range(NT)]
        m_s5 = A.mark()
        UT = A.alloc([2, NT * 128], BF16); tUT = [Tk() for _ in range(NT)]
        m0 = A.mark()
        aT = [A.alloc([8, 128], BF16) for _ in range(2)]; taT = [Tk() for _ in range(2)]
        Wu_ = A.alloc([8, 256], BF16); tWu = Tk()
        S.dma("pool", Wu_, self.w_in[l, :, 1280:1536].rearrange("(kc p) n -> p kc n", p=128), W=[tWu])
        for i in range(NT):
            b = i % 2
            self.make_aT(l, i, 0, aT[b], taT[b])
            for c in range(2):
                for kc in range(8):
                    self.mm(self.ps[2 + b][:, c * 128:(c + 1) * 128], Wu_[:, kc, c * 128:(c + 1) * 128], aT[b][:, kc, :], kc == 0, kc == 7, [taT[b], tWu], [self.tp[2 + b]])
            self.act(lambda e, i=i, b=b: e.activation(out=UT[:, :, i * 128:(i + 1) * 128], in_=self.ps[2 + b][:, 0:256].rearrange("p (c t) -> p c t", t=128), func=AF.Copy), [self.tp[2 + b]], [tUT[i]])
        A.release(m0)
        self.s5_phase(l, UT, tUT, OC, tOC, nq)
        if getattr(self, 'dbg_oc', None) is not None:
            for i in range(nq):
                self.store(self.dbg_oc[i * 128:(i + 1) * 128, :], OC[:, i, :], [tOC[i]])
        A.release(m_s5)
        KT = A.alloc([4, NT * 128], BF16); tKT = [Tk() for _ in range(NT)]
        V = A.alloc([NT, 512], BF16); tV = [Tk() for _ in range(NT)]
        m0 = A.mark()
        rw = dict(x=A.alloc([256], F32), ss=A.alloc([4], F32), t=A.alloc([2, 256], F32), tw=Tk())
        aT = [A.alloc([8, 128], BF16) for _ in range(2)]; taT = [Tk() for _ in range(2)]
        W = A.alloc([8, 1024], BF16); tW = [Tk() for _ in range(6)]
        srcs = [(256, 256), (1024, 128), (1792, 128), (512, 256), (1152, 128), (1920, 128)]
        o = 0
        for k, (c0, w) in enumerate(srcs):
            S.dma("pool", W[:, :, o:o + w], self.w_in[l, :, c0:c0 + w].rearrange("(kc p) n -> p kc n", p=128), W=[tW[k]])
            o += w
        kbf = A.alloc([512], BF16); tkb = Tk()
        for i in range(NT):
            b = i % 2
            self.make_aT(l, i, 0, aT[b], taT[b])
            for kc in range(8):
                self.mm(self.ps[0][:, :], aT[b][:, kc, :], W[:, kc, 0:512], kc == 0, kc == 7, [taT[b]] + tW[0:3], [self.tp[0]])
            for kc in range(8):
                self.mm(self.ps[1][:, :], aT[b][:, kc, :], W[:, kc, 512:1024], kc == 0, kc == 7, [taT[b]] + tW[3:6], [self.tp[1]])
            self.act(lambda e, i=i: e.activation(out=V[:, i, :], in_=self.ps[1][:, :], func=AF.Copy), [self.tp[1]], [tV[i]])
            self.act(lambda e: e.activation(out=kbf[:, 0:256], in_=self.ps[0][:, 0:256], func=AF.Copy), [self.tp[0]], [tkb])
            self.rms_rope(self.ps[0][:, 256:384], self.tp[0], 2, kbf[:, 256:384], tkb, i, rw, g=gqk[:, 1, :])
            self.rms_rope(self.ps[0][:, 384:512], self.tp[0], 2, kbf[:, 384:512], tkb, i, rw)
            for c in range(4):
                self.mm(self.ps[3][:, c * 128:(c + 1) * 128], kbf[:, c * 128:(c + 1) * 128], self.ident_b, True, True, [tkb, self.tconst], [self.tp[3]])
            self.dve(lambda e, i=i: e.tensor_copy(out=KT[:, :, i * 128:(i + 1) * 128], in_=self.ps[3][:, :].rearrange("p (c t) -> p c t", t=128)), [self.tp[3]], [tKT[i]])
        A.release(m0)
        rw = dict(x=A.alloc([256], F32), ss=A.alloc([4], F32), t=A.alloc([2, 256], F32), tw=Tk())
        aT = [A.alloc([8, 128], BF16)] * 2; taT = [Tk()] * 2
        tri = A.alloc([2, 128], BF16); namask = A.alloc([2688], BF16); tmk = Tk()
        S.dma("sp", tri, self.c_tri, W=[tmk]); S.dma("sp", namask, self.c_namask, W=[tmk])
        G = A.alloc([2, D], F32); tG = Tk()
        LN = A.alloc([2, D], F32); tLN = Tk()
        self.ada_gate(l, 0, G, tG)
        self.load_ln(l, 0, LN, tLN)
        Wq = A.alloc([8, 768], BF16); tWq = [Tk() for _ in range(3)]
        for k, c0 in enumerate((0, 768, 1536)):
            S.dma("pool", Wq[:, :, k * 256:(k + 1) * 256], self.w_in[l, :, c0:c0 + 256].rearrange("(kc p) n -> p kc n", p=128), W=[tWq[k]])
        Wo = A.alloc([8, D], BF16); tWo = Tk()
        S.dma("pool", Wo, self.w_out[l].rearrange("(kc p) n -> p kc n", p=128), W=[tWo])
        Traw = A.alloc([4, 14, 64], BF16); tTr = Tk()
        mh = A.mark()
        hk = [A.alloc([16, 64], F32) for _ in range(2)]; thk = [Tk() for _ in range(2)]
        for h in range(4):
            for rl in range(2):
                S.dma("sp", hk[h % 2][rl * 64:(rl + 1) * 64, :, :], bass.AP(self.dt_("rpbh"), ((l * 4 + h) * 18 + (1 - rl)) * 128, [[1, 64], [128, 16], [1, 64]]), W=[thk[h % 2]])
            self.dve(lambda e, h=h: e.tensor_copy(out=Traw[:, h, :, :], in_=hk[h % 2][:, 1:15, ::-1]), [thk[h % 2]], [tTr])
        A.release(mh)
        sk = A.alloc([8], F32); tsk = Tk()
        S.dma("sp", sk[:, 0:4], bass.AP(self.dt_("sink"), l * 4, [[0, 128], [1, 4]]), W=[tsk])
        self.dve(lambda e: e.tensor_scalar(out=sk[:, 4:8], in0=sk[:, 0:4], scalar1=-1.0, scalar2=None, op0=ALU.mult), [tsk], [tsk])
        qbf = A.alloc([768], BF16); tqb = Tk()
        qT = A.alloc([6, 128], BF16); tqT = Tk()
        Ps = [A.alloc([NT * 128], BF16) for _ in range(2)]; tPs = [Tk() for _ in range(2)]
        P, tP = Ps[0], tPs[0]
        PT = [A.alloc([512], BF16) for _ in range(2)]; tPT = [Tk() for _ in range(2)]
        cc = A.alloc([D], BF16); tcc = Tk()
        ccT = A.alloc([8, 128], BF16); tccT = Tk()
        tmp = P[:, 0:2 * D].bitcast(F32); ttmp = tP
        st = A.alloc([16], F32); tst = Tk()
        sms = [A.alloc([16], F32) for _ in range(4)]; tsms = [Tk() for _ in range(4)]
        print('M2 arena top', A.top)
        ocnt = [0]
        acnt = [0]

        jobs = []

        def attention(*a, **kw):
            jobs.append((a, kw, {}))

        def att1(ctx_, i, blk, pb0, segs, vcol, oc0, sc, bias_h=None, s0=0, sink_h=None):
            nseg = len(segs)
            nb = (nseg + 3) // 4
            acnt[0] += 1
            sm, tsm = sms[acnt[0] % 4], tsms[acnt[0] % 4]
            P, tP = Ps[acnt[0] % 2], tPs[acnt[0] % 2]
            ctx_.update(sm=sm, tsm=tsm, P=P, tP=tP)
            b0 = 0 if nb > 2 else 2 * (acnt[0] % 2)
            banks = list(range(b0, b0 + nb))
            tb_ = [self.tp[k] for k in banks]
            ntot = nseg * 128
            Sall = self.psall[:, b0 * 512:b0 * 512 + ntot]
            q_ap = qT[pb0:pb0 + 64, blk, :]
            for t, (kt, c, mask) in enumerate(segs):
                bk, cb = b0 + t // 4, (t % 4) * 128
                self.mm(self.ps[bk][:, cb:cb + 128], q_ap, KT[pb0:pb0 + 64, c, kt * 128:(kt + 1) * 128], True, mask is None, [tqT, tKT[kt]], [self.tp[bk]])
                if mask is not None:
                    self.mm(self.ps[bk][:, cb:cb + 128], self.ident_b, mask, False, True, [self.tconst, tmk], [self.tp[bk]])
            if bias_h is not None:
                nloc = nseg - 2
                self.dve(lambda e: e.tensor_tensor(out=self.psall[:, b0 * 512:b0 * 512 + nloc * 128], in0=self.psall[:, b0 * 512:b0 * 512 + nloc * 128],
                                                   in1=Traw[:, bias_h, s0 - 1:s0 - 1 + 2 * nloc, :].rearrange("p s k -> p (s k)"), op=ALU.add), tb_ + [tTr], tb_)
            self.dve(lambda e: e.reduce_max(out=sm[:, 9:10], in_=Sall, axis=AX.X, negate=True), tb_, [tsm])
            if sink_h is not None:
                self.dve(lambda e: e.tensor_tensor(out=sm[:, 9:10], in0=sm[:, 9:10], in1=sk[:, 4 + sink_h:5 + sink_h], op=ALU.min), [tsm, tsk], [tsm])
            self.act(lambda e: e.activation(out=P[:, 0:ntot], in_=Sall, func=AF.Exp, bias=sm[:, 9:10], scale=1.0, accum_out=sm[:, 10:11]), tb_ + [tsm], [tP, tsm])
            if sink_h is not None:
                self.act(lambda e: e.activation(out=sm[:, 12:13], in_=sk[:, sink_h:sink_h + 1], func=AF.Exp, bias=sm[:, 9:10], scale=1.0), [tsk, tsm], [tsm])

        def att1b(ctx_, i, blk, pb0, segs, vcol, oc0, sc, bias_h=None, s0=0, sink_h=None):
            sm, tsm = ctx_["sm"], ctx_["tsm"]
            if sink_h is not None:
                self.dve(lambda e: e.tensor_tensor(out=sm[:, 10:11], in0=sm[:, 10:11], in1=sm[:, 12:13], op=ALU.add), [tsm], [tsm])
            self.dve(lambda e: e.reciprocal(out=sm[:, 11:12], in_=sm[:, 10:11]), [tsm], [tsm])

        def att2(ctx_, i, blk, pb0, segs, vcol, oc0, sc, bias_h=None, s0=0, sink_h=None):
            sm, tsm, P, tP = ctx_["sm"], ctx_["tsm"], ctx_["P"], ctx_["tP"]
            nseg = len(segs)
            ob = oc0 % 512
            for g0 in range(0, nseg, 4):
                gi = ocnt[0] % 2
                ocnt[0] += 1
                pt = 5 + gi
                ng = min(4, nseg - g0)
                for t in range(g0, g0 + ng):
                    self.mm(self.ps[pt][:, (t - g0) * 128:(t - g0 + 1) * 128], P[:, t * 128:(t + 1) * 128], self.ident_b, True, True, [tP, self.tconst], [self.tp[pt]])
                self.dve(lambda e, pt=pt, gi=gi, ng=ng: e.tensor_copy(out=PT[gi][:, 0:ng * 128], in_=self.ps[pt][:, 0:ng * 128]), [self.tp[pt]], [tPT[gi]])
                for t in range(g0, g0 + ng):
                    kt = segs[t][0]
                    self.mm(self.ps[7][:, ob:ob + 64], PT[gi][:, (t - g0) * 128:(t - g0 + 1) * 128], V[:, kt, vcol:vcol + 64], t == 0, t == nseg - 1, [tPT[gi], tV[kt]], [self.tp[7]])
            self.act(lambda e: e.activation(out=cc[:, oc0:oc0 + 64], in_=self.ps[7][:, ob:ob + 64], func=AF.Copy, scale=sm[:, 11:12]), [self.tp[7], tsm], [tcc])

        for i in range(nq):
            b = i % 2
            isctx = i >= NL
            self.make_aT(l, i, 0, aT[b], taT[b])
            for kc in range(8):
                self.mm(self.ps[0][:, :], aT[b][:, kc, :], Wq[:, kc, 0:512], kc == 0, kc == 7, [taT[b]] + tWq[0:2], [self.tp[0]])
            for kc in range(8):
                self.mm(self.ps[1][:, 0:256], aT[b][:, kc, :], Wq[:, kc, 512:768], kc == 0, kc == 7, [taT[b], tWq[2]], [self.tp[1]])
            self.act(lambda e: e.activation(out=qbf[:, 0:256], in_=self.ps[0][:, 0:256], func=AF.Copy), [self.tp[0]], [tqb])
            self.rms_rope(self.ps[0][:, 256:512], self.tp[0], 4, qbf[:, 256:512], tqb, i, rw, g=gqk[:, 0, :], perm=True)
            self.rms_rope(self.ps[1][:, 0:256], self.tp[1], 4, qbf[:, 512:768], tqb, i, rw, perm=True)
            for c in range(6):
                bk = 2 + c // 4
                self.mm(self.ps[bk][:, (c % 4) * 128:(c % 4 + 1) * 128], qbf[:, c * 128:(c + 1) * 128], self.ident_b, True, True, [tqb, self.tconst], [self.tp[bk]])
            self.dve(lambda e: e.tensor_scalar(out=qT[:, 0:4, :], in0=self.ps[2][:, :].rearrange("p (c t) -> p c t", t=128), scalar1=0.125, scalar2=None, op0=ALU.mult), [self.tp[2]], [tqT])
            self.dve(lambda e: e.tensor_scalar(out=qT[:, 4:6, :], in0=self.ps[3][:, 0:256].rearrange("p (c t) -> p c t", t=128), scalar1=0.125, scalar2=None, op0=ALU.mult), [self.tp[3]], [tqT])
            ctxs = [(16, None), (17, None)]
            for h in range(4):
                blk, pb0, c = h // 2, (h % 2) * 64, h // 2
                if isctx:
                    segs = [(kt, c, None) for kt, _ in ctxs]
                    attention(i, blk, pb0, segs, h * 64, h * 64, 0.125)
                else:
                    j = i
                    if 2 <= j <= 13:
                        var, t0, ntl, s0 = 0, j - 2, 5, 3
                    elif j == 0:
                        var, t0, ntl, s0 = 1, 0, 4, 7
                    elif j == 1:
                        var, t0, ntl, s0 = 2, 0, 4, 5
                    elif j == 14:
                        var, t0, ntl, s0 = 3, 12, 4, 3
                    else:
                        var, t0, ntl, s0 = 4, 12, 4, 1
                    mo = 0 if var == 0 else 640 + (var - 1) * 512
                    segs = [(t0 + k, c, namask[:, mo + k * 128:mo + (k + 1) * 128]) for k in range(ntl)] + [(kt, c, None) for kt, _ in ctxs]
                    attention(i, blk, pb0, segs, h * 64, h * 64, 1.0, bias_h=h, s0=s0)
            for h in range(4):
                blk, pb0, kvh = 2 + (h % 2), (h // 2) * 64, h // 2
                kts = [16, 17] if isctx else list(range(NT))
                attention(i, blk, pb0, [(kt, 2, None) for kt in kts], 256 + kvh * 64, 256 + h * 64, 0.125)
            self.pool(lambda e, i=i: e.tensor_copy(out=cc[:, 512:768], in_=OC[:, i, :]), [tOC[i]], [tcc])
            for h in range(4):
                blk, pb0, kvh = 4 + (h % 2), (h // 2) * 64, h // 2
                if isctx:
                    segs = [(16, 3, None), (17, 3, None)]
                else:
                    segs = []
                    if i - 1 >= 0: segs.append((i - 1, 3, tri[:, 0, :]))
                    segs.append((i, 3, None))
                    if i + 1 < NL: segs.append((i + 1, 3, tri[:, 1, :]))
                    segs += [(16, 3, None), (17, 3, None)]
                attention(i, blk, pb0, segs, 384 + kvh * 64, 768 + h * 64, 0.125, sink_h=h)
            att1(jobs[0][2], *jobs[0][0], **jobs[0][1])
            att1b(jobs[0][2], *jobs[0][0], **jobs[0][1])
            for k_ in range(len(jobs)):
                if k_ + 1 < len(jobs):
                    att1(jobs[k_ + 1][2], *jobs[k_ + 1][0], **jobs[k_ + 1][1])
                att2(jobs[k_][2], *jobs[k_][0], **jobs[k_][1])
                if k_ + 1 < len(jobs):
                    att1b(jobs[k_ + 1][2], *jobs[k_ + 1][0], **jobs[k_ + 1][1])
            del jobs[:]
            for c in range(8):
                bk = 5 + c // 4
                self.mm(self.ps[bk][:, (c % 4) * 128:(c % 4 + 1) * 128], cc[:, c * 128:(c + 1) * 128], self.ident_b, True, True, [tcc, self.tconst], [self.tp[bk]])
            for hf in range(2):
                self.act(lambda e, hf=hf: e.activation(out=ccT[:, hf * 4:(hf + 1) * 4, :], in_=self.ps[5 + hf][:, :].rearrange("p (c t) -> p c t", t=128), func=AF.Copy), [self.tp[5 + hf]], [tccT])
            for hf in range(2):
                for kc in range(8):
                    self.mm(self.ps[hf][:, :], ccT[:, kc, :], Wo[:, kc, hf * 512:(hf + 1) * 512], kc == 0, kc == 7, [tccT, tWo], [self.tp[hf]])
            self.resid_ln(i, [(self.ps[0][:, :], self.tp[0]), (self.ps[1][:, :], self.tp[1])], G, tG, LN, tLN, tmp, ttmp, st, tst)
        A.release(m_all)

    def sin_of(self, out, x, shift, wk, tk, R):
        TWO_PI = 2.0 * np.pi
        a, k = wk
        ki = k.bitcast(I32)
        self.dve(lambda e: e.tensor_scalar(out=a, in0=x, scalar1=1.0 / TWO_PI, scalar2=shift / TWO_PI + 0.5, op0=ALU.mult, op1=ALU.add), R, [tk])
        self.dve(lambda e: e.tensor_copy(out=ki, in_=a), [tk], [tk])
        self.dve(lambda e: e.tensor_copy(out=a, in_=ki), [tk], [tk])
        self.dve(lambda e: e.tensor_scalar(out=k, in0=x, scalar1=shift, scalar2=None, op0=ALU.add), R + [tk], [tk])
        self.dve(lambda e: e.scalar_tensor_tensor(out=k, in0=a, scalar=-TWO_PI, in1=k, op0=ALU.mult, op1=ALU.add), [tk], [tk])
        self.dve(lambda e: e.tensor_scalar(out=a, in0=k, scalar1=float(np.pi), scalar2=None, op0=ALU.is_gt), [tk], [tk])
        self.dve(lambda e: e.scalar_tensor_tensor(out=k, in0=a, scalar=-TWO_PI, in1=k, op0=ALU.mult, op1=ALU.add), [tk], [tk])
        self.dve(lambda e: e.tensor_scalar(out=a, in0=k, scalar1=-float(np.pi), scalar2=None, op0=ALU.is_lt), [tk], [tk])
        self.dve(lambda e: e.scalar_tensor_tensor(out=k, in0=a, scalar=TWO_PI, in1=k, op0=ALU.mult, op1=ALU.add), [tk], [tk])
        self.dve(lambda e: e.tensor_scalar(out=k, in0=k, scalar1=3.1415925, scalar2=-3.1415925, op0=ALU.min, op1=ALU.max), [tk], [tk])
        self.act(lambda e: e.activation(out=out, in_=k, func=AF.Sin), [tk], [tk])

    def s5_phase(self, l, UT, tUT, OC, tOC, nq):
        A, S = self.A, self.S
        dve, act, pool = self.dve, self.act, self.pool
        tp_ = Tk()
        vec = A.alloc([16, 3], F32)
        tau = A.alloc([130], F32)
        bs = A.alloc([16, 2, 16], F32)
        S.dma("sp", vec, self.ssm_vec[l], W=[tp_])
        S.dma("sp", tau, self.c_tau, W=[tp_])
        S.dma("sp", bs, self.ssm_bs[l], W=[tp_])
        CSb = A.alloc([16, 2, 32], BF16); tcs = Tk()
        DDb = A.alloc([8, 32], BF16)
        Wg = A.alloc([2, 256], BF16)
        S.dma("pool", CSb, self.ssm_cs[l], W=[tcs])
        S.dma("pool", DDb, self.ssm_dd[l], W=[tcs])
        S.dma("pool", Wg, self.ssm_wglu[l].rearrange("(c p) n -> p c n", p=128), W=[tcs])
        dve(lambda e: e.tensor_scalar(out=CSb[:, :, 1, :], in0=CSb[:, :, 1, :], scalar1=-1.0, scalar2=None, op0=ALU.mult), [tcs], [tcs])
        pv = A.alloc([16, 16], F32)
        def q(k): return pv[:, k, :]
        lre, lim, lst = vec[:, :, 0], vec[:, :, 1], vec[:, :, 2]
        STEP, LR, ANG, MAG, SN, CN, ABR, ABI, DEN, NUM, FRE, FIM, T0, T1 = range(14)
        act(lambda e: e.activation(out=q(STEP), in_=lst, func=AF.Exp), [tp_], [tp_])
        dve(lambda e: e.tensor_tensor(out=q(LR), in0=lre, in1=q(STEP), op=ALU.mult), [tp_], [tp_])
        dve(lambda e: e.tensor_tensor(out=q(ANG), in0=lim, in1=q(STEP), op=ALU.mult), [tp_], [tp_])
        act(lambda e: e.activation(out=q(MAG), in_=q(LR), func=AF.Exp), [tp_], [tp_])
        self.sin_of(q(SN), q(ANG), 0.0, (q(T0), q(T1)), tp_, [tp_])
        self.sin_of(q(CN), q(ANG), float(np.pi / 2), (q(T0), q(T1)), tp_, [tp_])
        dve(lambda e: e.tensor_tensor(out=q(ABR), in0=q(MAG), in1=q(CN), op=ALU.mult), [tp_], [tp_])
        dve(lambda e: e.tensor_tensor(out=q(ABI), in0=q(MAG), in1=q(SN), op=ALU.mult), [tp_], [tp_])
        dve(lambda e: e.tensor_tensor(out=q(DEN), in0=lre, in1=lre, op=ALU.mult), [tp_], [tp_])
        dve(lambda e: e.tensor_tensor(out=q(T0), in0=lim, in1=lim, op=ALU.mult), [tp_], [tp_])
        dve(lambda e: e.tensor_tensor(out=q(DEN), in0=q(DEN), in1=q(T0), op=ALU.add), [tp_], [tp_])
        dve(lambda e: e.reciprocal(out=q(DEN), in_=q(DEN)), [tp_], [tp_])
        dve(lambda e: e.tensor_scalar(out=q(NUM), in0=q(ABR), scalar1=-1.0, scalar2=None, op0=ALU.add), [tp_], [tp_])
        dve(lambda e: e.tensor_tensor(out=q(T0), in0=q(NUM), in1=lre, op=ALU.mult), [tp_], [tp_])
        dve(lambda e: e.tensor_tensor(out=q(T1), in0=q(ABI), in1=lim, op=ALU.mult), [tp_], [tp_])
        dve(lambda e: e.tensor_tensor(out=q(FRE), in0=q(T0), in1=q(T1), op=ALU.add), [tp_], [tp_])
        dve(lambda e: e.tensor_tensor(out=q(FRE), in0=q(FRE), in1=q(DEN), op=ALU.mult), [tp_], [tp_])
        dve(lambda e: e.tensor_tensor(out=q(T0), in0=q(ABI), in1=lre, op=ALU.mult), [tp_], [tp_])
        dve(lambda e: e.tensor_tensor(out=q(T1), in0=q(NUM), in1=lim, op=ALU.mult), [tp_], [tp_])
        dve(lambda e: e.tensor_tensor(out=q(FIM), in0=q(T0), in1=q(T1), op=ALU.subtract), [tp_], [tp_])
        dve(lambda e: e.tensor_tensor(out=q(FIM), in0=q(FIM), in1=q(DEN), op=ALU.mult), [tp_], [tp_])
        EC = A.alloc([16, 129], F32); ES = A.alloc([16, 129], F32); tE = Tk()
        mtab = A.mark()
        X = A.alloc([16, 129], F32); wa = A.alloc([16, 129], F32); wb = A.alloc([16, 129], F32); tX = Tk()
        dve(lambda e: e.tensor_tensor(out=X, in0=q(ANG).unsqueeze(2).to_broadcast([128, 16, 129]), in1=tau[:, 0:129].unsqueeze(1).to_broadcast([128, 16, 129]), op=ALU.mult), [tp_], [tX])
        self.sin_of(ES, X, 0.0, (wa, wb), tE, [tX])
        self.sin_of(EC, X, float(np.pi / 2), (wa, wb), tE, [tX])
        A.release(mtab)
        BT = A.alloc([16, 2, 128], BF16); tBT = Tk()
        mb = A.mark()
        bb = A.alloc([16, 2, 16], F32); tbb = Tk()
        w4 = A.alloc([4, 16, 16], F32)
        fre_b = q(FRE).unsqueeze(2).to_broadcast([128, 16, 16]); fim_b = q(FIM).unsqueeze(2).to_broadcast([128, 16, 16])
        dve(lambda e: e.tensor_tensor(out=w4[:, 0], in0=bs[:, :, 0, :], in1=fre_b, op=ALU.mult), [tp_], [tbb])
        dve(lambda e: e.tensor_tensor(out=w4[:, 1], in0=bs[:, :, 1, :], in1=fim_b, op=ALU.mult), [tp_], [tbb])
        dve(lambda e: e.tensor_tensor(out=w4[:, 2], in0=bs[:, :, 1, :], in1=fre_b, op=ALU.mult), [tp_], [tbb])
        dve(lambda e: e.tensor_tensor(out=w4[:, 3], in0=bs[:, :, 0, :], in1=fim_b, op=ALU.mult), [tp_], [tbb])
        dve(lambda e: e.tensor_tensor(out=bb[:, :, 0, :], in0=w4[:, 0], in1=w4[:, 1], op=ALU.subtract), [tbb], [tbb])
        dve(lambda e: e.tensor_tensor(out=bb[:, :, 1, :], in0=w4[:, 2], in1=w4[:, 3], op=ALU.add), [tbb], [tbb])
        Bp = A.alloc([4, 128], BF16); tBp = [Tk() for _ in range(4)]
        dve(lambda e: e.memset(Bp, 0.0), [], tBp)
        n = 0
        for dg in range(16):
            gc = dg % 8
            band = gc % 4
            for ri in range(2):
                for gl in range(2):
                    dve(lambda e, dg=dg, ri=ri, gl=gl, band=band: e.tensor_copy(out=Bp[gl * 64:(gl + 1) * 64, band, band * 32 + gl * 16:band * 32 + gl * 16 + 16],
                                                                            in_=bb[gl * 64:(gl + 1) * 64, dg, ri, :]), [tbb], [tBp[band]])
                bk = 5 + (n // 4) % 2
                self.mm(self.ps[bk][:, (n % 4) * 128:(n % 4 + 1) * 128], Bp[:, band, :], self.ident_b, True, True, [tBp[band], self.tconst], [self.tp[bk]])
                if n % 4 == 3:
                    dg0 = (n - 3) // 2
                    act(lambda e, bk=bk, dg0=dg0: e.activation(out=BT[:, dg0:dg0 + 2, :, :], in_=self.ps[bk][:, :].rearrange("p (a b t) -> p a b t", a=2, b=2), func=AF.Copy), [self.tp[bk]], [tBT])
                n += 1
        A.release(mb)
        Y = A.alloc([NT, 256], F32); tY = [Tk() for _ in range(NT)]
        z = A.alloc([256], F32); z2 = A.alloc([256], F32); tz = Tk()
        zb = A.alloc([256], BF16); zT = A.alloc([2, 128], BF16); tzT = Tk()
        RD = A.alloc([2, 16, 128], F32); tRD = Tk()
        for d in range(2):
            dve(lambda e, d=d: e.tensor_copy(out=RD[:, d].rearrange("p (g r) t -> p g r t", r=2),
                                             in_=q(MAG)[:, d * 8:(d + 1) * 8].unsqueeze(2).unsqueeze(3).to_broadcast([128, 8, 2, 128])), [tp_], [tRD])
            first = 0 if d == 0 else 127
            dve(lambda e, d=d, first=first: e.memset(RD[:, d, :, first:first + 1], 0.0), [tRD], [tRD])
        g = A.alloc([8, 2, 128], F32); tg = Tk()
        gi = A.alloc([8, 2], F32); tgi = Tk()
        giw = A.alloc([4, 8], F32)
        tt = [A.alloc([8, 128], F32) for _ in range(4)]; ttt = Tk()
        w = A.alloc([8, 2, 128], F32); tw = Tk()
        hs = A.alloc([8, 2, 128], BF16); ths = Tk()
        bu4 = self.psall[:, 0:2048].rearrange("p (g r t) -> p g r t", r=2, t=128)
        tb4 = [self.tp[k] for k in range(4)]
        for d in range(2):
            order = [16, 17] + list(range(16)) if d == 0 else [17, 16] + list(range(15, -1, -1))
            first, last = (0, 127) if d == 0 else (127, 0)
            cs_, sn_ = rv_tab(EC, d * 8, d, 8), rv_tab(ES, d * 8, d, 8)
            for n_, i in enumerate(order):
                tk = slice(i * 128, (i + 1) * 128)
                for gc in range(8):
                    for ri in range(2):
                        bk = gc // 2
                        c0 = (gc % 2) * 256 + ri * 128
                        self.mm(self.ps[bk][:, c0:c0 + 128], BT[:, d * 8 + gc, ri, :], UT[:, gc // 4, tk], True, True, [tBT, tUT[i]], [self.tp[bk]])
                if n_ > 0:
                    glr, gli = g[:, :, 0, last], g[:, :, 1, last]
                    cT, sT = EC[:, d * 8:(d + 1) * 8, 128], ES[:, d * 8:(d + 1) * 8, 128]
                    dve(lambda e, glr=glr, cT=cT: e.tensor_tensor(out=giw[:, 0], in0=glr, in1=cT, op=ALU.mult), [tg, tE], [tgi])
                    dve(lambda e, gli=gli, sT=sT: e.tensor_tensor(out=giw[:, 1], in0=gli, in1=sT, op=ALU.mult), [tg, tE], [tgi])
                    dve(lambda e, glr=glr, sT=sT: e.tensor_tensor(out=giw[:, 2], in0=glr, in1=sT, op=ALU.mult), [tg, tE], [tgi])
                    dve(lambda e, gli=gli, cT=cT: e.tensor_tensor(out=giw[:, 3], in0=gli, in1=cT, op=ALU.mult), [tg, tE], [tgi])
                    dve(lambda e: e.tensor_tensor(out=gi[:, :, 0], in0=giw[:, 0], in1=giw[:, 1], op=ALU.subtract), [tgi], [tgi])
                    dve(lambda e: e.tensor_tensor(out=gi[:, :, 1], in0=giw[:, 2], in1=giw[:, 3], op=ALU.add), [tgi], [tgi])
                    dve(lambda e, d=d: e.tensor_tensor(out=gi, in0=gi, in1=q(MAG)[:, d * 8:(d + 1) * 8].unsqueeze(2).to_broadcast([128, 8, 2]), op=ALU.mult), [tgi, tp_], [tgi])
                bre, bim = bu4[:, :, 0, :], bu4[:, :, 1, :]
                dve(lambda e, cs_=cs_, sn_=sn_: e.tensor_tensor(out=tt[0], in0=bre, in1=cs_, op=ALU.mult), tb4 + [tE], [ttt])
                dve(lambda e, cs_=cs_, sn_=sn_: e.tensor_tensor(out=tt[1], in0=bim, in1=sn_, op=ALU.mult), tb4 + [tE], [ttt])
                dve(lambda e, cs_=cs_, sn_=sn_: e.tensor_tensor(out=tt[2], in0=bim, in1=cs_, op=ALU.mult), tb4 + [tE], [ttt])
                dve(lambda e, cs_=cs_, sn_=sn_: e.tensor_tensor(out=tt[3], in0=bre, in1=sn_, op=ALU.mult), tb4 + [tE], [ttt])
                pool(lambda e: e.tensor_tensor(out=w[:, :, 0, :], in0=tt[0], in1=tt[1], op=ALU.add), [ttt], [tw])
                pool(lambda e: e.tensor_tensor(out=w[:, :, 1, :], in0=tt[2], in1=tt[3], op=ALU.subtract), [ttt], [tw])
                if n_ > 0:
                    pool(lambda e, first=first: e.tensor_tensor(out=w[:, :, :, first], in0=w[:, :, :, first], in1=gi, op=ALU.add), [tw, tgi], [tw])
                gf = g.rearrange("p g r t -> p (g r t)")
                wf = w.rearrange("p g r t -> p (g r t)")
                rf = RD[:, d].rearrange("p k t -> p (k t)")
                if d == 0:
                    dve(lambda e, rf=rf: e.tensor_tensor_scan(out=gf, data0=rf, data1=wf, initial=0.0, op0=ALU.mult, op1=ALU.add), [tw, tRD], [tg])
                else:
                    dve(lambda e, rf=rf: e.tensor_tensor_scan(out=gf[:, ::-1], data0=rf[:, ::-1], data1=wf[:, ::-1], initial=0.0, op0=ALU.mult, op1=ALU.add), [tw, tRD], [tg])
                gre, gim = g[:, :, 0, :], g[:, :, 1, :]
                dve(lambda e, cs_=cs_, sn_=sn_: e.tensor_tensor(out=tt[0], in0=gre, in1=cs_, op=ALU.mult), [tg, tE], [ttt])
                dve(lambda e, cs_=cs_, sn_=sn_: e.tensor_tensor(out=tt[1], in0=gim, in1=sn_, op=ALU.mult), [tg, tE], [ttt])
                dve(lambda e, cs_=cs_, sn_=sn_: e.tensor_tensor(out=tt[2], in0=gre, in1=sn_, op=ALU.mult), [tg, tE], [ttt])
                dve(lambda e, cs_=cs_, sn_=sn_: e.tensor_tensor(out=tt[3], in0=gim, in1=cs_, op=ALU.mult), [tg, tE], [ttt])
                pool(lambda e: e.tensor_tensor(out=hs[:, :, 0, :], in0=tt[0], in1=tt[1], op=ALU.subtract), [ttt], [ths])
                pool(lambda e: e.tensor_tensor(out=hs[:, :, 1, :], in0=tt[2], in1=tt[3], op=ALU.add), [ttt], [ths])
                for gc in range(8):
                    terms = [(hs[:, gc, 0, :], CSb[:, d * 8 + gc, 0, :], [ths, tcs]), (hs[:, gc, 1, :], CSb[:, d * 8 + gc, 1, :], [ths, tcs])]
                    if d == 0:
                        terms.append((UT[:, gc // 4, tk], DDb[:, gc, :], [tUT[i], tcs]))
                    for k, (lt, rh, R_) in enumerate(terms):
                        self.mm(self.ps[4][:, gc * 32:(gc + 1) * 32], lt, rh, k == 0, k == len(terms) - 1, R_, [self.tp[4]])
                if d == 0:
                    act(lambda e, i=i: e.activation(out=Y[:, i, :], in_=self.ps[4][:, 0:256], func=AF.Copy), [self.tp[4]], [tY[i]])
                    if getattr(self, 'dbg_y', None) is not None:
                        self.store(self.dbg_y[i * 128:(i + 1) * 128, :], Y[:, i, :], [tY[i]])
                    continue
                if i >= nq:
                    continue
                dve(lambda e, i=i: e.tensor_tensor(out=z, in0=self.ps[4][:, 0:256], in1=Y[:, i, :], op=ALU.add), [self.tp[4], tY[i]], [tz])
                pool(lambda e: e.tensor_tensor(out=z2, in0=z, in1=z, op=ALU.mult), [tz], [tz])
                pool(lambda e: e.tensor_scalar(out=z2, in0=z2, scalar1=0.044715, scalar2=1.0, op0=ALU.mult, op1=ALU.add), [tz], [tz])
                pool(lambda e: e.tensor_tensor(out=z2, in0=z2, in1=z, op=ALU.mult), [tz], [tz])
                act(lambda e: e.activation(out=z2, in_=z2, func=AF.Sigmoid, scale=1.5957691216057308), [tz], [tz])
                dve(lambda e: e.tensor_tensor(out=z, in0=z, in1=z2, op=ALU.mult), [tz], [tz])
                dve(lambda e: e.tensor_copy(out=zb, in_=z), [tz], [tz])
                for c in range(2):
                    self.mm(self.ps[5][:, c * 128:(c + 1) * 128], zb[:, c * 128:(c + 1) * 128], self.ident_b, True, True, [tz, self.tconst], [self.tp[5]])
                act(lambda e: e.activation(out=zT, in_=self.ps[5][:, 0:256].rearrange("p (c t) -> p c t", t=128), func=AF.Copy), [self.tp[5]], [tzT])
                for c in range(2):
                    self.mm(self.ps[6][:, 0:256], zT[:, c, :], Wg[:, c, :], c == 0, c == 1, [tzT, tcs], [self.tp[6]])
                act(lambda e: e.activation(out=z2, in_=self.ps[6][:, 0:256], func=AF.Sigmoid), [self.tp[6]], [tz])
                dve(lambda e, i=i: e.tensor_tensor(out=OC[:, i, :], in0=z, in1=z2, op=ALU.mult), [tz], [tOC[i]])


def rv_tab(E, dg0, d, n=2):
    if d == 0:
        return E[:, dg0:dg0 + n, 0:128]
    return E[:, dg0:dg0 + n, 127::-1]


def _consts():
    c = {}
    c["c_ident_f"] = np.eye(128, dtype=np.float32)
    c["c_ident_b"] = np.eye(128, dtype=np.float32).astype(ml_dtypes.bfloat16)
    pos = np.arange(NL * 128)
    row = (pos // 64).astype(np.float32)
    col = (pos % 64).astype(np.float32)
    inv = (10000.0 ** (-np.arange(16, dtype=np.float32) / 16)).astype(np.float32)
    ang = np.concatenate([row[:, None] * inv, col[:, None] * inv], -1).astype(np.float32)
    cs = np.concatenate([np.cos(ang), -np.sin(ang), np.sin(ang)], -1).astype(np.float32)
    ctxcs = np.concatenate([np.ones((256, 32), np.float32), np.zeros((256, 64), np.float32)], -1)
    cs = np.concatenate([cs, ctxcs], 0).reshape(NT, 128, 96).transpose(1, 0, 2)
    c["c_rope"] = np.ascontiguousarray(cs)
    q = np.arange(128)[:, None]
    k = np.arange(128)[None, :]
    tri = np.stack([np.where(k >= q, 0.0, NEG), np.where(k <= q, 0.0, NEG)], 1).astype(np.float32)
    c["c_tri"] = tri.astype(ml_dtypes.bfloat16)
    nm = np.full((5, 128, 10, 64), NEG, np.float32)
    qc = np.arange(64)
    cstart = np.clip(qc - 8, 0, 48)
    kc = np.arange(64)
    colv = (kc[None, :] >= cstart[:, None]) & (kc[None, :] < cstart[:, None] + 16)
    def fill(var, j, i0, nr):
        for rl in range(2):
            r = 2 * j + rl
            rs = int(np.clip(r - 4, 0, 24))
            for ii in range(nr):
                i = i0 + ii
                if rs <= i < rs + 8:
                    blk = np.where(colv, 0.0, NEG)
                    nm[var, rl * 64:(rl + 1) * 64, ii, :] = blk
    fill(0, 5, 6, 10)
    fill(1, 0, 0, 8); fill(2, 1, 0, 8); fill(3, 14, 24, 8); fill(4, 15, 24, 8)
    nm2 = nm.transpose(1, 0, 2, 3).reshape(128, 5, 640)
    c["c_namask"] = np.ascontiguousarray(np.concatenate([nm2[:, 0, :]] + [nm2[:, v, 0:512] for v in range(1, 5)], 1)).astype(ml_dtypes.bfloat16)
    tau = np.zeros((128, 130), np.float32)
    tau[:, :] = np.arange(130, dtype=np.float32)[None, :]
    c["c_tau"] = tau
    return c


def _prep_shared(I):
    f = np.float32
    d = dict(_consts())
    for k in ("ada_w", "ada_b", "w_in", "w_out"):
        d[k] = np.ascontiguousarray(I[k], dtype=f)
    rp = np.zeros((DEPTH, 4, 18, 128), f)
    rp[:, :, 1:16, 48:79] = I["na_rpb"][:, :, :, ::-1]
    d["rpbh"] = rp
    d["gqk"] = np.ascontiguousarray(np.stack([I["ga_q_norm"], I["ga_k_norm"]], 1), dtype=f)
    def st(a):
        L = a.shape[0]
        rest = a.shape[4:]
        a = a.reshape((L, 2, 8, 2, 64) + rest)
        a = np.moveaxis(a, (3, 4), (1, 2))
        return np.ascontiguousarray(a.reshape((L, 128, 16) + rest))
    ls = np.broadcast_to(I["ssm_log_step"][..., None], I["ssm_lambda_re"].shape)
    d["ssm_vec"] = np.ascontiguousarray(np.stack([st(I["ssm_lambda_re"]), st(I["ssm_lambda_im"]), st(np.ascontiguousarray(ls))], -1), dtype=f)
    d["ssm_bs"] = np.ascontiguousarray(np.stack([st(I["ssm_b_re"]), st(I["ssm_b_im"])], -2), dtype=f)
    cre = st(np.swapaxes(I["ssm_c_re"], -1, -2))
    cim = st(np.swapaxes(I["ssm_c_im"], -1, -2))
    cs = np.zeros((DEPTH, 128, 16, 2, 32), f)
    for gl in range(2):
        cs[:, gl * 64:(gl + 1) * 64, :, 0, gl * 16:(gl + 1) * 16] = cre[:, gl * 64:(gl + 1) * 64]
        cs[:, gl * 64:(gl + 1) * 64, :, 1, gl * 16:(gl + 1) * 16] = cim[:, gl * 64:(gl + 1) * 64]
    d["ssm_cs"] = cs
    dd = np.zeros((DEPTH, 128, 8, 32), f)
    sd = I["ssm_d"].reshape(DEPTH, 2, 128)
    for gc in range(8):
        for j in range(32):
            pl = (gc % 4) * 32 + j
            dd[:, pl, gc, j] = sd[:, gc // 4, pl]
    d["ssm_dd"] = dd
    d["ssm_wglu"] = np.ascontiguousarray(I["ssm_w_glu"], dtype=f)
    d["sink"] = np.ascontiguousarray(I["sw_sink"], dtype=f)
    d["lngb"] = np.ascontiguousarray(np.stack([I["ln1_g"], I["ln1_b"], I["ln2_g"], I["ln2_b"]], 1), dtype=f)
    d["ffn_g"] = I["ffn_w_gate"]; d["ffn_u"] = I["ffn_w_up"]; d["ffn_d"] = I["ffn_w_down"]
    d["moe_r"] = I["moe_w_router"]; d["moe_rb"] = I["moe_b_router"]
    d["moe_g"] = I["moe_w_gate"]; d["moe_u"] = I["moe_w_up"]; d["moe_d"] = I["moe_w_down"]
    return d


def _prep_core(I, b, hx=None):
    if hx is None:
        hx = np.concatenate([I["x"][b], I["ctx"][b]], 0)
    cv = np.stack([I["c"][b].reshape(8, 128).T, I["c_ctx"].reshape(8, 128).T], -1)
    return {"hx": np.ascontiguousarray(hx, dtype=np.float32), "cv": np.ascontiguousarray(cv, dtype=np.float32)}


def build_program(layers=(0, 1, 2, 3)):
    b = B(list(layers))
    b.setup()
    for l in layers:
        b.ada_cols(l)
        b.mixer_phase(l)
        b.ffn_phase(l)
    for i in range(NL):
        b.store(b.out[i * 128:(i + 1) * 128, :], b.H[:, i, :], [b.tH[i]])
    with b.nc.Block() as blk:
        b.S.emit(blk, b.fin)
    return b


def kernel(**inputs):
    I = {k: np.asarray(v) for k, v in inputs.items()}
    n = 8
    b = build_program()
    shared = _prep_shared(I)
    shared = {k: v for k, v in shared.items() if k in b.din}
    in_maps = []
    for c in range(n):
        m = dict(shared)
        m.update(_prep_core(I, c))
        in_maps.append(m)
    res = run_bass_kernel_spmd(b.nc, in_maps, core_ids=list(range(n)))
    out = np.stack([np.asarray(r["out"], dtype=np.float32).reshape(NL * 128, D) for r in res.results], 0)
    return out
```

```python
import numpy as np
import ml_dtypes
import concourse.bass as bass
import concourse.mybir as mybir
from concourse.bass_utils import run_bass_kernel_spmd

F32 = mybir.dt.float32
BF16 = mybir.dt.bfloat16
I32 = mybir.dt.int32
ALU = mybir.AluOpType
AF = mybir.ActivationFunctionType
AX = mybir.AxisListType


class Tk:
    __slots__ = ("w", "r")

    def __init__(self):
        self.w = None
        self.r = []


class Sched:
    ENG = ("pe", "act", "dve", "pool", "sp")
    NDMA = {"sp": 12, "pool": 8, "act": 4}

    def __init__(self, nc):
        self.nc = nc
        self.prog = {e: [] for e in self.ENG}
        self.n = {e: 0 for e in self.ENG}
        self.seen = {e: {} for e in self.ENG}
        self.signaled = {e: set() for e in self.ENG}
        self.dma_rr = {q: 0 for q in self.NDMA}
        self.dma_tot = {}
        self.lastc = {e: 0 for e in self.ENG}
        self.cur_fence = {}

    def fence(self):
        for e in self.ENG:
            if self.lastc[e]:
                self.cur_fence[e] = self.lastc[e]
        for key, tot in self.dma_tot.items():
            self.cur_fence[key] = tot

    def _deps(self, eng, reads, writes):
        deps = dict(self.cur_fence)
        def add(t):
            if t is None:
                return
            k, v = t
            if deps.get(k, 0) < v:
                deps[k] = v
        for t in reads:
            add(t.w)
        for t in writes:
            add(t.w)
            for r in t.r:
                add(r)
        waits = []
        for k, v in deps.items():
            if k == "pe" and eng == "pe":
                continue
            if self.seen[eng].get(k, 0) < v:
                self.seen[eng][k] = v
                waits.append((k, v))
                if k in self.signaled:
                    self.signaled[k].add(v)
        return waits

    def _commit(self, ticket, reads, writes):
        for t in reads:
            t.r.append(ticket)
        for t in writes:
            t.w = ticket
            t.r = []

    def op(self, eng, fn, R=(), W=()):
        waits = self._deps(eng, R, W)
        self.n[eng] += 1
        self.lastc[eng] = self.n[eng]
        ticket = (eng, self.n[eng])
        self.prog[eng].append((waits, fn, ticket, None))
        self._commit(ticket, R, W)

    def dma(self, q, out, in_, R=(), W=(), slow=False):
        j = self.dma_rr[q]
        self.dma_rr[q] = (j + 1) % self.NDMA[q]
        key = ("d", q, j)
        waits = self._deps(q, R, W)
        prev = self.dma_tot.get(key, 0)
        if prev and self.seen[q].get(key, 0) < prev:
            self.seen[q][key] = prev
            waits.append((key, prev))
        self.dma_tot[key] = prev + 16
        ticket = (key, prev + 16)
        self.n[q] += 1
        if slow:
            fn = lambda e, o=out, i=in_: e.dma_start(out=o, in_=i, allow_slow_non_contiguous=True)
        else:
            fn = lambda e, o=out, i=in_: e.dma_start(out=o, in_=i)
        self.prog[q].append((waits, fn, (q, self.n[q]), key))
        self._commit(ticket, R, W)

    def emit(self, block, final_waits):
        nc = self.nc
        sems = {e: nc.alloc_semaphore("s_" + e) for e in self.ENG}
        for key in self.dma_tot:
            sems[key] = nc.alloc_semaphore("d_%s%d" % (key[1], key[2]))
        rank = {}
        for e in self.ENG:
            rank[e] = {v: i + 1 for i, v in enumerate(sorted(self.signaled[e]))}

        def val(k, v):
            return rank[k][v] if k in rank else v

        def run(ename, eng):
            for waits, fn, ticket, dkey in self.prog[ename]:
                for k, v in waits:
                    eng.wait_ge(sems[k], val(k, v))
                ins = fn(eng)
                if dkey is not None:
                    ins.then_inc(sems[dkey], 16)
                elif ticket[1] in self.signaled[ename]:
                    ins.then_inc(sems[ename], 1)
            if ename == "sp":
                for k, v in final_waits:
                    eng.wait_ge(sems[k], val(k, v))

        for k, v in final_waits:
            if k in self.signaled:
                self.signaled[k].add(v)
        for e in self.ENG:
            rank[e] = {v: i + 1 for i, v in enumerate(sorted(self.signaled[e]))}
        block.tensor(lambda e: run("pe", e))
        block.scalar(lambda e: run("act", e))
        block.vector(lambda e: run("dve", e))
        block.gpsimd(lambda e: run("pool", e))
        block.sync(lambda e: run("sp", e))


class Arena:
    def __init__(self, nc, nbytes, S=None):
        self.S = S
        self.t = nc.alloc_sbuf_tensor("arena", [128, nbytes // 4], F32)
        self.nbytes = nbytes
        self.top = 0
        self.peak = 0

    def alloc(self, free_shape, dtype):
        esz = 2 if dtype == BF16 else 4
        n = int(np.prod(free_shape))
        nb = (n * esz + 63) // 64 * 64
        off = self.top
        self.top += nb
        self.peak = max(self.peak, self.top)
        assert self.top <= self.nbytes, ("arena overflow", self.top, self.nbytes)
        ap = self.t[:, off // 4:(off + nb) // 4]
        if dtype != F32:
            ap = ap.bitcast(dtype)
        ap = ap[:, 0:n]
        if len(free_shape) == 2:
            ap = ap.rearrange("p (a b) -> p a b", b=free_shape[1])
        elif len(free_shape) == 3:
            ap = ap.rearrange("p (a b c) -> p a b c", b=free_shape[1], c=free_shape[2])
        return ap

    def mark(self):
        return self.top

    def release(self, m):
        self.top = m
        self.S.fence()


D = 1024
NT = 18
NL = 16
DEPTH = 4
DFF = 2816
DFE = 3584
NE = 8
ALPHA = float((2 * DEPTH) ** 0.25)
NEG = -1.0e30
SLAB = 512


class B:
    def __init__(self, layers, dbg=None):
        self.layers = layers
        self.dbg = dbg or {}
        nc = self.nc = bass.Bass("TRN2", target_bir_lowering=False)
        self.S = Sched(nc)
        self.A = Arena(nc, 212800, self.S)
        self.fin = []
        self.din = {}
        self.psall = nc.alloc_psum_tensor("psall", [128, 8 * 512], F32)
        self.ps = [self.psall[:, i * 512:(i + 1) * 512] for i in range(8)]
        self.tp = [Tk() for _ in range(8)]

    SHAPES = {
        "hx": ([NT * 128, D], F32), "cv": ([128, 8, 2], F32), "c_ident_f": ([128, 128], F32), "c_ident_b": ([128, 128], BF16),
        "c_rope": ([128, NT, 96], F32), "c_tri": ([128, 2, 128], BF16), "c_namask": ([128, 2688], BF16), "c_tau": ([128, 130], F32),
        "ada_w": ([DEPTH, D, 6 * D], F32), "ada_b": ([DEPTH, 6 * D], F32), "w_in": ([DEPTH, D, 2048], F32), "w_out": ([DEPTH, D, D], F32),
        "rpbh": ([DEPTH, 4, 18, 128], F32), "gqk": ([DEPTH, 2, 64], F32), "ssm_vec": ([DEPTH, 128, 16, 3], F32),
        "ssm_bs": ([DEPTH, 128, 16, 2, 16], F32), "ssm_cs": ([DEPTH, 128, 16, 2, 32], F32), "ssm_dd": ([DEPTH, 128, 8, 32], F32),
        "ssm_wglu": ([DEPTH, 256, 256], F32), "sink": ([DEPTH, 4], F32), "lngb": ([DEPTH, 4, D], F32),
        "ffn_g": ([2, D, DFF], F32), "ffn_u": ([2, D, DFF], F32), "ffn_d": ([2, DFF, D], F32),
        "moe_r": ([2, D, NE], F32), "moe_rb": ([2, NE], F32), "moe_g": ([2, NE, D, DFE], F32), "moe_u": ([2, NE, D, DFE], F32),
        "moe_d": ([2, NE, DFE, D], F32),
    }

    def __getattr__(self, name):
        sh = B.SHAPES.get(name)
        if sh is None:
            raise AttributeError(name)
        ap = self.inp(name, sh[0], sh[1])
        self.__dict__[name] = ap
        return ap

    def dt_(self, name):
        getattr(self, name)
        return self.din[name]

    def inp(self, name, shape, dt=F32):
        t = self.nc.dram_tensor(name, list(shape), dt, kind="ExternalInput")
        self.din[name] = t
        return t.ap()

    def outp(self, name, shape, dt=F32):
        return self.nc.dram_tensor(name, list(shape), dt, kind="ExternalOutput").ap()

    def store(self, dst, src, R):
        S = self.S
        S.dma("sp", dst, src, R=R)
        j = (S.dma_rr["sp"] - 1) % S.NDMA["sp"]
        key = ("d", "sp", j)
        self.fin.append((key, S.dma_tot[key]))

    def pe(self, fn, R=(), W=()): self.S.op("pe", fn, R, W)
    def act(self, fn, R=(), W=()): self.S.op("act", fn, R, W)
    def dve(self, fn, R=(), W=()): self.S.op("dve", fn, R, W)
    def pool(self, fn, R=(), W=()): self.S.op("pool", fn, R, W)

    def mm(self, out, lhsT, rhs, start, stop, R, W):
        self.pe(lambda e: e.matmul(out, lhsT=lhsT, rhs=rhs, start=start, stop=stop), R, W)

    def tr(self, out, in_, R, W):
        self.pe(lambda e: e.transpose(out=out, in_=in_, identity=self.ident_f), R + [self.tconst], W)

    def rstd_from(self, out, var_ap, scale, eps, R, tk):
        self.act(lambda e: e.activation(out=out, in_=var_ap, func=AF.Sqrt, bias=self.eps_ap(eps), scale=scale), R, [tk])
        self.dve(lambda e: e.reciprocal(out=out, in_=out), [tk], [tk])

    def eps_ap(self, eps):
        return self.epsc[:, 0:1]

    def setup(self):
        A = self.A
        inp = self.inp
        self.out = self.outp("out", [NL * 128, D])

        S = self.S
        self.tconst = Tk()
        self.H = A.alloc([NT, D], F32)
        self.tH = [Tk() for _ in range(NT)]
        self.ident_f = A.alloc([128], F32)
        self.ident_b = A.alloc([128], BF16)
        self.epsc = A.alloc([2], F32)
        self.csil = A.alloc([8, 2], F32)
        self.modc = A.alloc([DEPTH, 32, 2], F32)
        self.tmodc = Tk()
        for dst, src in ((self.ident_f, self.c_ident_f), (self.ident_b, self.c_ident_b), (self.csil, self.cv)):
            S.dma("sp", dst, src, W=[self.tconst])
        self.dve(lambda e: e.memset(self.epsc, 1e-6), W=[self.tconst])
        for i in range(NT):
            S.dma("sp" if i % 2 == 0 else "act", self.H[:, i, :], self.hx[i * 128:(i + 1) * 128, :], W=[self.tH[i]])
        self.act(lambda e: e.activation(out=self.csil, in_=self.csil, func=AF.Silu), [self.tconst], [self.tconst])

    def ada_cols(self, l):
        A, S = self.A, self.S
        m = A.mark()
        blocks = [0, 1, 3, 4]
        wst = [A.alloc([8, 128], F32) for _ in range(3)]
        tw = [Tk() for _ in range(3)]
        bcol = A.alloc([32], F32)
        tb = Tk()
        for j, blk in enumerate(blocks):
            S.dma("sp", bcol[:, j * 8:(j + 1) * 8], self.ada_b[l, blk * D:(blk + 1) * D].rearrange("(kc p) -> p kc", p=128), W=[tb], slow=True)
        n = 0
        for j, blk in enumerate(blocks):
            for fc in range(8):
                b = n % 3
                col0 = blk * D + fc * 128
                S.dma("sp" if n % 2 == 0 else "act", wst[b], self.ada_w[l, :, col0:col0 + 128].rearrange("(kc p) n -> p kc n", p=128), W=[tw[b]])
                pb = 7
                for kc in range(8):
                    self.mm(self.ps[pb][:, 0:2], wst[b][:, kc, :], self.csil[:, kc, :], kc == 0, kc == 7, [tw[b], self.tconst], [self.tp[pb]])
                idx = j * 8 + fc
                add = 1.0 if blk in (1, 4) else 0.0
                self.dve(lambda e, idx=idx, add=add, pb=pb: e.tensor_scalar(out=self.modc[:, l, idx, :], in0=self.ps[pb][:, 0:2], scalar1=bcol[:, idx:idx + 1],
                                                                           scalar2=add, op0=ALU.add, op1=ALU.add), [self.tp[pb], tb], [self.tmodc])
                n += 1
        A.release(m)

    def ada_gate(self, l, which, G, tG):
        A, S = self.A, self.S
        m = A.mark()
        blk = 2 if which == 0 else 5
        crep = A.alloc([2, 8, 128], F32); tcr = Tk()
        for v in range(2):
            for kc in range(8):
                self.dve(lambda e, v=v, kc=kc: e.tensor_copy(out=crep[:, v, kc, :], in_=self.csil[:, kc, v:v + 1].to_broadcast([128, 128])),
                         [self.tconst], [tcr])
        wst = [A.alloc([8, 512], F32) for _ in range(2)]
        tw = [Tk() for _ in range(2)]
        bb = A.alloc([D], F32)
        tb = Tk()
        S.dma("sp", bb, self.ada_b[l:l + 1, blk * D:(blk + 1) * D].partition_broadcast(128) if False else
              bass.AP(self.dt_("ada_b"), l * 6 * D + blk * D, [[0, 128], [1, D]]), W=[tb])
        for nb in range(2):
            col0 = blk * D + nb * 512
            S.dma("sp", wst[nb], self.ada_w[l, :, col0:col0 + 512].rearrange("(kc p) n -> p kc n", p=128), W=[tw[nb]])
            for v in range(2):
                pb = 5 + v
                for kc in range(8):
                    self.mm(self.ps[pb][:, :], crep[:, v, kc, :], wst[nb][:, kc, :], kc == 0, kc == 7, [tw[nb], tcr], [self.tp[pb]])
                self.dve(lambda e, v=v, nb=nb, pb=pb: e.tensor_tensor(out=G[:, v, nb * 512:(nb + 1) * 512], in0=self.ps[pb][:, :], in1=bb[:, nb * 512:(nb + 1) * 512], op=ALU.add),
                         [self.tp[pb], tb], [tG])
        A.release(m)

    def load_ln(self, l, which, LN, tLN):
        for j in range(2):
            self.S.dma("sp", LN[:, j, :], bass.AP(self.dt_("lngb"), (l * 4 + which * 2 + j) * D, [[0, 128], [1, D]]), W=[tLN])

    def make_aT(self, l, i, which, aT, taT, aT32=None, taT32=None):
        v = 1 if i >= NL else 0
        for half in range(2):
            pb = 5 + half
            for q in range(4):
                kc = half * 4 + q
                self.tr(self.ps[pb][:, q * 128:(q + 1) * 128], self.H[:, i, kc * 128:(kc + 1) * 128], [self.tH[i]], [self.tp[pb]])
            for q in range(4):
                kc = half * 4 + q
                sc = self.modc[:, l, (which * 2 + 1) * 8 + kc, v:v + 1]
                sh = self.modc[:, l, (which * 2) * 8 + kc, v:v + 1]
                self.act(lambda e, kc=kc, q=q, pb=pb, sc=sc, sh=sh: e.activation(out=aT[:, kc, :], in_=self.ps[pb][:, q * 128:(q + 1) * 128], func=AF.Identity, bias=sh, scale=sc),
                         [self.tp[pb], self.tmodc], [taT])
                if aT32 is not None:
                    self.act(lambda e, kc=kc, q=q, pb=pb, sc=sc, sh=sh: e.activation(out=aT32[:, kc, :], in_=self.ps[pb][:, q * 128:(q + 1) * 128], func=AF.Identity, bias=sh, scale=sc),
                             [self.tp[pb], self.tmodc], [taT32])

    def resid_ln(self, i, ys, G, tG, LN, tLN, tmp, ttmp, st, tst):
        v = 1 if i >= NL else 0
        for hf, (yap, ty) in enumerate(ys):
            self.dve(lambda e, hf=hf, yap=yap: e.tensor_tensor(out=tmp[:, hf * 512:(hf + 1) * 512], in0=yap, in1=G[:, v, hf * 512:(hf + 1) * 512], op=ALU.mult),
                     [ty, tG], [ttmp])
        self.ln_tail(i, tmp, ttmp, LN, tLN, st, tst)

    def ln_tail(self, i, tmp, ttmp, LN, tLN, st, tst):
        self.dve(lambda e: e.scalar_tensor_tensor(out=tmp, in0=self.H[:, i, :], scalar=ALPHA, in1=tmp, op0=ALU.mult, op1=ALU.add), [self.tH[i], ttmp], [ttmp])
        for hf in range(2):
            self.dve(lambda e, hf=hf: e.bn_stats(out=st[:, hf * 6:(hf + 1) * 6], in_=tmp[:, hf * 512:(hf + 1) * 512]), [ttmp], [tst])
        self.dve(lambda e: e.bn_aggr(out=st[:, 12:14], in_=st[:, 0:12]), [tst], [tst])
        self.rstd_from(st[:, 14:15], st[:, 13:14], 1.0, 1e-6, [tst], tst)
        self.dve(lambda e: e.tensor_scalar(out=tmp, in0=tmp, scalar1=st[:, 12:13], scalar2=st[:, 14:15], op0=ALU.subtract, op1=ALU.mult), [ttmp, tst], [ttmp])
        self.pool(lambda e: e.tensor_tensor(out=tmp, in0=tmp, in1=LN[:, 0, :], op=ALU.mult), [ttmp, tLN], [ttmp])
        self.pool(lambda e: e.tensor_tensor(out=self.H[:, i, :], in0=tmp, in1=LN[:, 1, :], op=ALU.add), [ttmp, tLN], [self.tH[i]])

    def ffn_phase(self, l):
        A, S = self.A, self.S
        last = (l == DEPTH - 1)
        nt = NL if last else NT
        moe = (l % 2 == 1)
        li = l // 2
        m = A.mark()
        G = A.alloc([2, D], F32); tG = Tk()
        LN = A.alloc([2, D], F32); tLN = Tk()
        self.ada_gate(l, 1, G, tG)
        self.load_ln(l, 1, LN, tLN)
        FT = A.alloc([8, nt * 128], BF16)
        tFT = [Tk() for _ in range(nt)]
        moe_ = (l % 2 == 1)
        if moe_:
            gate = A.alloc([nt, NE], F32); tgate = Tk()
            a32 = A.alloc([8, 128], F32); ta32 = Tk()
            rt = self.router_setup(l // 2)
        for i in range(nt):
            if moe_:
                self.make_aT(l, i, 1, FT[:, :, i * 128:(i + 1) * 128], tFT[i], a32, ta32)
                self.router_tile(rt, i, a32, ta32, gate, tgate)
            else:
                self.make_aT(l, i, 1, FT[:, :, i * 128:(i + 1) * 128], tFT[i])
        experts = range(NE) if moe else [0]
        dff = DFE if moe else DFF
        nsl = dff // SLAB + (1 if dff % SLAB else 0)
        facc = A.alloc([nt, D], F32) if False else None
        tmp = A.alloc([D], F32); ttmp = Tk()
        st = A.alloc([16], F32); tst = Tk()
        for i in range(nt):
            self.pool(lambda e, i=i: e.tensor_scalar(out=self.H[:, i, :], in0=self.H[:, i, :], scalar1=ALPHA, scalar2=None, op0=ALU.mult), [self.tH[i]], [self.tH[i]])
        NB = 2
        wg = [A.alloc([8, SLAB], BF16) for _ in range(NB)]
        wu = [A.alloc([8, SLAB], BF16) for _ in range(NB)]
        wd = [A.alloc([SLAB // 128, D], BF16) for _ in range(NB)]
        tw = [Tk() for _ in range(NB)]
        twu = [Tk() for _ in range(NB)]
        twd = [Tk() for _ in range(NB)]
        h1 = [A.alloc([SLAB // 128, 512], BF16) for _ in range(2)]
        th1 = [Tk() for _ in range(2)]
        sg = [A.alloc([512], F32) for _ in range(2)]
        tsg = [Tk() for _ in range(2)]
        yt = [A.alloc([D], F32)] * 2
        tyt = [Tk()] * 2
        nblk = (nt * 128 + 511) // 512
        cnt = 0
        hcnt = 0
        items = [(e_, s) for e_ in experts for s in range(nsl)]

        def issue(k):
            e_, s = items[k]
            Wg = self.moe_g[li, e_] if moe else self.ffn_g[li]
            Wu = self.moe_u[li, e_] if moe else self.ffn_u[li]
            Wd = self.moe_d[li, e_] if moe else self.ffn_d[li]
            b = k % NB
            c0 = s * SLAB
            w = min(SLAB, dff - c0)
            nch = w // 128
            S.dma("pool", wg[b][:, :, 0:w], Wg[:, c0:c0 + w].rearrange("(kc p) n -> p kc n", p=128), W=[tw[b]])
            S.dma("pool", wu[b][:, :, 0:w], Wu[:, c0:c0 + w].rearrange("(kc p) n -> p kc n", p=128), W=[twu[b]])
            S.dma("pool", wd[b][:, 0:nch, :], Wd[c0:c0 + w, :].rearrange("(fc p) n -> p fc n", p=128), W=[twd[b]])

        def mk(k):
            e_, s = items[k]
            b = k % NB
            c0 = s * SLAB
            w = min(SLAB, dff - c0)
            nch = w // 128
            def gu(tb, hb, fcs=None, b=b, nch=nch):
                t0 = tb * 512
                ntok = min(512, nt * 128 - t0)
                tiles = list(range(t0 // 128, (t0 + ntok) // 128))
                for fc in (range(nch) if fcs is None else fcs):
                    if fc >= nch:
                        continue
                    pg, pu = 0 + (fc % 2) * 2, 1 + (fc % 2) * 2
                    for kc in range(8):
                        self.mm(self.ps[pg][:, 0:ntok], wg[b][:, kc, fc * 128:(fc + 1) * 128], FT[:, kc, t0:t0 + ntok], kc == 0, kc == 7,
                                [tw[b]] + [tFT[i] for i in tiles], [self.tp[pg]])
                    for kc in range(8):
                        self.mm(self.ps[pu][:, 0:ntok], wu[b][:, kc, fc * 128:(fc + 1) * 128], FT[:, kc, t0:t0 + ntok], kc == 0, kc == 7,
                                [twu[b]] + [tFT[i] for i in tiles], [self.tp[pu]])
                    sb = fc % 2
                    self.act(lambda e, pg=pg, sb=sb, ntok=ntok: e.activation(out=sg[sb][:, 0:ntok], in_=self.ps[pg][:, 0:ntok], func=AF.Silu), [self.tp[pg]], [tsg[sb]])
                    self.dve(lambda e, pu=pu, sb=sb, hb=hb, fc=fc, ntok=ntok: e.tensor_tensor(out=h1[hb][:, fc, 0:ntok], in0=self.ps[pu][:, 0:ntok], in1=sg[sb][:, 0:ntok], op=ALU.mult),
                             [self.tp[pu], tsg[sb]], [th1[hb]])

            def down(tb, hb, tis=None, b=b, nch=nch, e_=e_):
                t0 = tb * 512
                ntok = min(512, nt * 128 - t0)
                tiles = list(range(t0 // 128, (t0 + ntok) // 128))
                for ti, i in enumerate(tiles):
                    if tis is not None and ti not in tis:
                        continue
                    v = 1 if i >= NL else 0
                    yb = i % 2
                    for hf in range(2):
                        pb = 4 + hf + 2 * (i % 2)
                        for fc in range(nch):
                            self.mm(self.ps[pb][:, :], h1[hb][:, fc, ti * 128:(ti + 1) * 128], wd[b][:, fc, hf * 512:(hf + 1) * 512], fc == 0, fc == nch - 1,
                                    [th1[hb], twd[b]], [self.tp[pb]])
                        if moe:
                            self.act(lambda e, pb=pb, hf=hf, yb=yb, i=i, e_=e_: e.activation(out=yt[yb][:, hf * 512:(hf + 1) * 512], in_=self.ps[pb][:, :], func=AF.Copy, scale=gate[:, i, e_:e_ + 1]),
                                     [self.tp[pb], tgate], [tyt[yb]])
                        else:
                            self.dve(lambda e, pb=pb, hf=hf, v=v, yb=yb: e.tensor_tensor(out=yt[yb][:, hf * 512:(hf + 1) * 512], in0=self.ps[pb][:, :], in1=G[:, v, hf * 512:(hf + 1) * 512], op=ALU.mult),
                                     [self.tp[pb], tG], [tyt[yb]])
                    if moe:
                        self.dve(lambda e, v=v, yb=yb: e.tensor_tensor(out=yt[yb], in0=yt[yb], in1=G[:, v, :], op=ALU.mult), [tyt[yb], tG], [tyt[yb]])
                    self.pool(lambda e, i=i, yb=yb: e.tensor_tensor(out=self.H[:, i, :], in0=yt[yb], in1=self.H[:, i, :], op=ALU.add), [tyt[yb], self.tH[i]], [self.tH[i]])

            return gu, down

        seq = [(k, tb) for k in range(len(items)) for tb in range(nblk)]
        fns = {0: mk(0)}
        hbl = [j % 2 for j in range(len(seq))]
        issue(0)
        fns[0][0](0, hbl[0])
        for j, (k, tb) in enumerate(seq):
            if tb == 0 and k + 1 < len(items):
                issue(k + 1)
            gu_k, down_k = fns[k]
            if j + 1 < len(seq):
                k2, tb2 = seq[j + 1]
                if k2 not in fns:
                    fns[k2] = mk(k2)
                gu_n = fns[k2][0]
                gu_n(tb2, hbl[j + 1], [0, 1])
                down_k(tb, hbl[j], [0])
                gu_n(tb2, hbl[j + 1], [2])
                down_k(tb, hbl[j], [1])
                gu_n(tb2, hbl[j + 1], [3])
                down_k(tb, hbl[j], [2, 3])
            else:
                down_k(tb, hbl[j])
            if tb == nblk - 1:
                fns.pop(k, None)
        for i in range(nt):
            self.ln_only(i, LN, tLN, tmp, ttmp, st, tst)
        A.release(m)

    def ln_only(self, i, LN, tLN, tmp, ttmp, st, tst):
        for hf in range(2):
            self.dve(lambda e, hf=hf: e.bn_stats(out=st[:, hf * 6:(hf + 1) * 6], in_=self.H[:, i, hf * 512:(hf + 1) * 512]), [self.tH[i]], [tst])
        self.dve(lambda e: e.bn_aggr(out=st[:, 12:14], in_=st[:, 0:12]), [tst], [tst])
        self.rstd_from(st[:, 14:15], st[:, 13:14], 1.0, 1e-6, [tst], tst)
        self.dve(lambda e: e.tensor_scalar(out=tmp, in0=self.H[:, i, :], scalar1=st[:, 12:13], scalar2=st[:, 14:15], op0=ALU.subtract, op1=ALU.mult), [self.tH[i], tst], [ttmp])
        self.pool(lambda e: e.tensor_tensor(out=tmp, in0=tmp, in1=LN[:, 0, :], op=ALU.mult), [ttmp, tLN], [ttmp])
        self.pool(lambda e: e.tensor_tensor(out=self.H[:, i, :], in0=tmp, in1=LN[:, 1, :], op=ALU.add), [ttmp, tLN], [self.tH[i]])

    def router_setup(self, li):
        A, S = self.A, self.S
        wr = A.alloc([8, NE], F32); twr = Tk()
        rb = A.alloc([NE], F32)
        S.dma("sp", wr, self.moe_r[li].rearrange("(kc p) n -> p kc n", p=128), W=[twr])
        S.dma("sp", rb, bass.AP(self.dt_("moe_rb"), li * NE, [[0, 128], [1, NE]]), W=[twr])
        return dict(wr=wr, twr=twr, rb=rb, lg=A.alloc([NE], F32), tlg=Tk(), m8=A.alloc([8], F32), wk=A.alloc([2, NE], F32))

    def router_tile(self, rt, i, a32, ta32, gate, tgate):
        wr, twr, rb, lg, tlg, m8, wk = rt["wr"], rt["twr"], rt["rb"], rt["lg"], rt["tlg"], rt["m8"], rt["wk"]
        pb = 7
        for kc in range(8):
            self.mm(self.ps[pb][:, 0:NE], a32[:, kc, :], wr[:, kc, :], kc == 0, kc == 7, [ta32, twr], [self.tp[pb]])
        self.dve(lambda e: e.tensor_tensor(out=lg, in0=self.ps[pb][:, 0:NE], in1=rb, op=ALU.add), [self.tp[pb], twr], [tlg])
        self.dve(lambda e: e.max(out=m8, in_=lg), [tlg], [tlg])
        self.dve(lambda e: e.tensor_scalar(out=wk[:, 0, :], in0=lg, scalar1=m8[:, 1:2], scalar2=None, op0=ALU.is_ge), [tlg], [tlg])
        self.dve(lambda e: e.tensor_scalar(out=wk[:, 1, :], in0=lg, scalar1=m8[:, 0:1], scalar2=None, op0=ALU.subtract), [tlg], [tlg])
        self.act(lambda e: e.activation(out=wk[:, 1, :], in_=wk[:, 1, :], func=AF.Exp), [tlg], [tlg])
        self.dve(lambda e: e.tensor_tensor(out=wk[:, 1, :], in0=wk[:, 1, :], in1=wk[:, 0, :], op=ALU.mult), [tlg], [tlg])
        self.dve(lambda e: e.reduce_sum(out=m8[:, 2:3], in_=wk[:, 1, :], axis=AX.X), [tlg], [tlg])
        self.dve(lambda e: e.reciprocal(out=m8[:, 2:3], in_=m8[:, 2:3]), [tlg], [tlg])
        self.dve(lambda e: e.tensor_scalar(out=gate[:, i, :], in0=wk[:, 1, :], scalar1=m8[:, 2:3], scalar2=None, op0=ALU.mult), [tlg], [tgate])


    def rms_rope(self, src, tsrc, nh, dst, tdst, tile, rw, g=None, perm=False, qs=1.0):
        rope, trope = self.rope, self.tmc
        s3 = src.rearrange("p (h d) -> p h d", d=64)
        x, ss, t, tw = rw["x"], rw["ss"], rw["t"], rw["tw"]
        x3 = x[:, 0:nh * 64].rearrange("p (h d) -> p h d", d=64)
        if g is not None:
            for h in range(nh):
                self.act(lambda e, h=h: e.activation(out=x3[:, h, :], in_=s3[:, h, :], func=AF.Square, accum_out=ss[:, h:h + 1]), [tsrc], [tw])
            self.act(lambda e: e.activation(out=ss[:, 0:nh], in_=ss[:, 0:nh], func=AF.Sqrt, bias=self.epsc[:, 0:1], scale=1.0 / 64), [tw, self.tconst], [tw])
            self.dve(lambda e: e.reciprocal(out=ss[:, 0:nh], in_=ss[:, 0:nh]), [tw], [tw])
            for h in range(nh):
                self.dve(lambda e, h=h: e.scalar_tensor_tensor(out=x3[:, h, :], in0=s3[:, h, :], scalar=ss[:, h:h + 1], in1=g, op0=ALU.mult, op1=ALU.mult), [tsrc, tw, self.tmc], [tw])
            cur, tcur, qs = x3, tw, 1.0
        else:
            cur, tcur = s3, tsrc
        C = rope[:, tile, 0:32].unsqueeze(1).unsqueeze(1).to_broadcast([128, nh, 2, 32])
        Sg = rope[:, tile, 32:96].rearrange("p (a d) -> p a d", d=32).unsqueeze(1).to_broadcast([128, nh, 2, 32])
        c4 = cur.rearrange("p h (a d) -> p h a d", d=32)
        sw = c4[:, :, ::-1, :]
        t1 = t[:, 0, 0:nh * 64].rearrange("p (h a d) -> p h a d", a=2, d=32)
        t2 = t[:, 1, 0:nh * 64].rearrange("p (h a d) -> p h a d", a=2, d=32)
        if qs != 1.0:
            self.dve(lambda e: e.scalar_tensor_tensor(out=t1, in0=c4, scalar=qs, in1=C, op0=ALU.mult, op1=ALU.mult), [tcur, trope], [tw])
            self.dve(lambda e: e.scalar_tensor_tensor(out=t2, in0=sw, scalar=qs, in1=Sg, op0=ALU.mult, op1=ALU.mult), [tcur, trope], [tw])
        else:
            self.dve(lambda e: e.tensor_tensor(out=t1, in0=c4, in1=C, op=ALU.mult), [tcur, trope], [tw])
            self.dve(lambda e: e.tensor_tensor(out=t2, in0=sw, in1=Sg, op=ALU.mult), [tcur, trope], [tw])
        f1 = t[:, 0, 0:nh * 64].rearrange("p (h d) -> p h d", d=64)
        f2 = t[:, 1, 0:nh * 64].rearrange("p (h d) -> p h d", d=64)
        if perm:
            dv = dst.rearrange("p (b s d) -> p s b d", b=2, s=2, d=64)
            f1 = f1.rearrange("p (s b) d -> p s b d", b=2)
            f2 = f2.rearrange("p (s b) d -> p s b d", b=2)
        else:
            dv = dst.rearrange("p (h d) -> p h d", d=64)
        self.dve(lambda e: e.tensor_tensor(out=dv, in0=f1, in1=f2, op=ALU.add), [tw], [tdst])

    def mixer_phase(self, l):
        A, S = self.A, self.S
        last = (l == DEPTH - 1)
        nq = NL if last else NT
        m_all = A.mark()
        self.rope = A.alloc([NT, 96], F32)
        gqk = A.alloc([2, 64], F32)
        self.tmc = Tk()
        S.dma("sp", self.rope, self.c_rope, W=[self.tmc])
        S.dma("sp", gqk, bass.AP(self.dt_("gqk"), l * 128, [[0, 128], [1, 128]]), W=[self.tmc])
        OC = A.alloc([NT, 256], BF16); tOC = [Tk() for _ in range(NT)]
        m_s5 = A.mark()
        UT = A.alloc([2, NT * 128], BF16); tUT = [Tk() for _ in range(NT)]
        m0 = A.mark()
        aT = [A.alloc([8, 128], BF16) for _ in range(2)]; taT = [Tk() for _ in range(2)]
        Wu_ = A.alloc([8, 256], BF16); tWu = Tk()
        S.dma("pool", Wu_, self.w_in[l, :, 1280:1536].rearrange("(kc p) n -> p kc n", p=128), W=[tWu])
        for i in range(NT):
            b = i % 2
            self.make_aT(l, i, 0, aT[b], taT[b])
            for c in range(2):
                for kc in range(8):
                    self.mm(self.ps[2 + b][:, c * 128:(c + 1) * 128], Wu_[:, kc, c * 128:(c + 1) * 128], aT[b][:, kc, :], kc == 0, kc == 7, [taT[b], tWu], [self.tp[2 + b]])
            self.act(lambda e, i=i, b=b: e.activation(out=UT[:, :, i * 128:(i + 1) * 128], in_=self.ps[2 + b][:, 0:256].rearrange("p (c t) -> p c t", t=128), func=AF.Copy), [self.tp[2 + b]], [tUT[i]])
        A.release(m0)
        self.s5_phase(l, UT, tUT, OC, tOC, nq)
        if getattr(self, 'dbg_oc', None) is not None:
            for i in range(nq):
                self.store(self.dbg_oc[i * 128:(i + 1) * 128, :], OC[:, i, :], [tOC[i]])
        A.release(m_s5)
        KT = A.alloc([4, NT * 128], BF16); tKT = [Tk() for _ in range(NT)]
        V = A.alloc([NT, 512], BF16); tV = [Tk() for _ in range(NT)]
        m0 = A.mark()
        rw = dict(x=A.alloc([256], F32), ss=A.alloc([4], F32), t=A.alloc([2, 256], F32), tw=Tk())
        aT = [A.alloc([8, 128], BF16) for _ in range(2)]; taT = [Tk() for _ in range(2)]
        W = A.alloc([8, 1024], BF16); tW = [Tk() for _ in range(6)]
        srcs = [(256, 256), (1024, 128), (1792, 128), (512, 256), (1152, 128), (1920, 128)]
        o = 0
        for k, (c0, w) in enumerate(srcs):
            S.dma("pool", W[:, :, o:o + w], self.w_in[l, :, c0:c0 + w].rearrange("(kc p) n -> p kc n", p=128), W=[tW[k]])
            o += w
        kbf = A.alloc([512], BF16); tkb = Tk()
        for i in range(NT):
            b = i % 2
            self.make_aT(l, i, 0, aT[b], taT[b])
            for kc in range(8):
                self.mm(self.ps[0][:, :], aT[b][:, kc, :], W[:, kc, 0:512], kc == 0, kc == 7, [taT[b]] + tW[0:3], [self.tp[0]])
            for kc in range(8):
                self.mm(self.ps[1][:, :], aT[b][:, kc, :], W[:, kc, 512:1024], kc == 0, kc == 7, [taT[b]] + tW[3:6], [self.tp[1]])
            self.act(lambda e, i=i: e.activation(out=V[:, i, :], in_=self.ps[1][:, :], func=AF.Copy), [self.tp[1]], [tV[i]])
            self.act(lambda e: e.activation(out=kbf[:, 0:256], in_=self.ps[0][:, 0:256], func=AF.Copy), [self.tp[0]], [tkb])
            self.rms_rope(self.ps[0][:, 256:384], self.tp[0], 2, kbf[:, 256:384], tkb, i, rw, g=gqk[:, 1, :])
            self.rms_rope(self.ps[0][:, 384:512], self.tp[0], 2, kbf[:, 384:512], tkb, i, rw)
            for c in range(4):
                self.mm(self.ps[3][:, c * 128:(c + 1) * 128], kbf[:, c * 128:(c + 1) * 128], self.ident_b, True, True, [tkb, self.tconst], [self.tp[3]])
            self.dve(lambda e, i=i: e.tensor_copy(out=KT[:, :, i * 128:(i + 1) * 128], in_=self.ps[3][:, :].rearrange("p (c t) -> p c t", t=128)), [self.tp[3]], [tKT[i]])
        A.release(m0)
        rw = dict(x=A.alloc([256], F32), ss=A.alloc([4], F32), t=A.alloc([2, 256], F32), tw=Tk())
        aT = [A.alloc([8, 128], BF16)] * 2; taT = [Tk()] * 2
        tri = A.alloc([2, 128], BF16); namask = A.alloc([2688], BF16); tmk = Tk()
        S.dma("sp", tri, self.c_tri, W=[tmk]); S.dma("sp", namask, self.c_namask, W=[tmk])
        G = A.alloc([2, D], F32); tG = Tk()
        LN = A.alloc([2, D], F32); tLN = Tk()
        self.ada_gate(l, 0, G, tG)
        self.load_ln(l, 0, LN, tLN)
        Wq = A.alloc([8, 768], BF16); tWq = [Tk() for _ in range(3)]
        for k, c0 in enumerate((0, 768, 1536)):
            S.dma("pool", Wq[:, :, k * 256:(k + 1) * 256], self.w_in[l, :, c0:c0 + 256].rearrange("(kc p) n -> p kc n", p=128), W=[tWq[k]])
        Wo = A.alloc([8, D], BF16); tWo = Tk()
        S.dma("pool", Wo, self.w_out[l].rearrange("(kc p) n -> p kc n", p=128), W=[tWo])
        Traw = A.alloc([4, 14, 64], BF16); tTr = Tk()
        mh = A.mark()
        hk = [A.alloc([16, 64], F32) for _ in range(2)]; thk = [Tk() for _ in range(2)]
        for h in range(4):
            for rl in range(2):
                S.dma("sp", hk[h % 2][rl * 64:(rl + 1) * 64, :, :], bass.AP(self.dt_("rpbh"), ((l * 4 + h) * 18 + (1 - rl)) * 128, [[1, 64], [128, 16], [1, 64]]), W=[thk[h % 2]])
            self.dve(lambda e, h=h: e.tensor_copy(out=Traw[:, h, :, :], in_=hk[h % 2][:, 1:15, ::-1]), [thk[h % 2]], [tTr])
        A.release(mh)
        sk = A.alloc([8], F32); tsk = Tk()
        S.dma("sp", sk[:, 0:4], bass.AP(self.dt_("sink"), l * 4, [[0, 128], [1, 4]]), W=[tsk])
        self.dve(lambda e: e.tensor_scalar(out=sk[:, 4:8], in0=sk[:, 0:4], scalar1=-1.0, scalar2=None, op0=ALU.mult), [tsk], [tsk])
        qbf = A.alloc([768], BF16); tqb = Tk()
        qT = A.alloc([6, 128], BF16); tqT = Tk()
        Ps = [A.alloc([NT * 128], BF16) for _ in range(2)]; tPs = [Tk() for _ in range(2)]
        P, tP = Ps[0], tPs[0]
        PT = [A.alloc([512], BF16) for _ in range(2)]; tPT = [Tk() for _ in range(2)]
        cc = A.alloc([D], BF16); tcc = Tk()
        ccT = A.alloc([8, 128], BF16); tccT = Tk()
        tmp = P[:, 0:2 * D].bitcast(F32); ttmp = tP
        st = A.alloc([16], F32); tst = Tk()
        sms = [A.alloc([16], F32) for _ in range(4)]; tsms = [Tk() for _ in range(4)]
        print('M2 arena top', A.top)
        ocnt = [0]
        acnt = [0]

        jobs = []

        def attention(*a, **kw):
            jobs.append((a, kw, {}))

        def att1(ctx_, i, blk, pb0, segs, vcol, oc0, sc, bias_h=None, s0=0, sink_h=None):
            nseg = len(segs)
            nb = (nseg + 3) // 4
            acnt[0] += 1
            sm, tsm = sms[acnt[0] % 4], tsms[acnt[0] % 4]
            P, tP = Ps[acnt[0] % 2], tPs[acnt[0] % 2]
            ctx_.update(sm=sm, tsm=tsm, P=P, tP=tP)
            b0 = 0 if nb > 2 else 2 * (acnt[0] % 2)
            banks = list(range(b0, b0 + nb))
            tb_ = [self.tp[k] for k in banks]
            ntot = nseg * 128
            Sall = self.psall[:, b0 * 512:b0 * 512 + ntot]
            q_ap = qT[pb0:pb0 + 64, blk, :]
            for t, (kt, c, mask) in enumerate(segs):
                bk, cb = b0 + t // 4, (t % 4) * 128
                self.mm(self.ps[bk][:, cb:cb + 128], q_ap, KT[pb0:pb0 + 64, c, kt * 128:(kt + 1) * 128], True, mask is None, [tqT, tKT[kt]], [self.tp[bk]])
                if mask is not None:
                    self.mm(self.ps[bk][:, cb:cb + 128], self.ident_b, mask, False, True, [self.tconst, tmk], [self.tp[bk]])
            if bias_h is not None:
                nloc = nseg - 2
                self.dve(lambda e: e.tensor_tensor(out=self.psall[:, b0 * 512:b0 * 512 + nloc * 128], in0=self.psall[:, b0 * 512:b0 * 512 + nloc * 128],
                                                   in1=Traw[:, bias_h, s0 - 1:s0 - 1 + 2 * nloc, :].rearrange("p s k -> p (s k)"), op=ALU.add), tb_ + [tTr], tb_)
            self.dve(lambda e: e.reduce_max(out=sm[:, 9:10], in_=Sall, axis=AX.X, negate=True), tb_, [tsm])
            if sink_h is not None:
                self.dve(lambda e: e.tensor_tensor(out=sm[:, 9:10], in0=sm[:, 9:10], in1=sk[:, 4 + sink_h:5 + sink_h], op=ALU.min), [tsm, tsk], [tsm])
            self.act(lambda e: e.activation(out=P[:, 0:ntot], in_=Sall, func=AF.Exp, bias=sm[:, 9:10], scale=1.0, accum_out=sm[:, 10:11]), tb_ + [tsm], [tP, tsm])
            if sink_h is not None:
                self.act(lambda e: e.activation(out=sm[:, 12:13], in_=sk[:, sink_h:sink_h + 1], func=AF.Exp, bias=sm[:, 9:10], scale=1.0), [tsk, tsm], [tsm])

        def att1b(ctx_, i, blk, pb0, segs, vcol, oc0, sc, bias_h=None, s0=0, sink_h=None):
            sm, tsm = ctx_["sm"], ctx_["tsm"]
            if sink_h is not None:
                self.dve(lambda e: e.tensor_tensor(out=sm[:, 10:11], in0=sm[:, 10:11], in1=sm[:, 12:13], op=ALU.add), [tsm], [tsm])
            self.dve(lambda e: e.reciprocal(out=sm[:, 11:12], in_=sm[:, 10:11]), [tsm], [tsm])

        def att2(ctx_, i, blk, pb0, segs, vcol, oc0, sc, bias_h=None, s0=0, sink_h=None):
            sm, tsm, P, tP = ctx_["sm"], ctx_["tsm"], ctx_["P"], ctx_["tP"]
            nseg = len(segs)
            ob = oc0 % 512
            def pt_(g0):
                gi = ocnt[0] % 2
                ocnt[0] += 1
                pt = 5 + gi
                ng = min(4, nseg - g0)
                for t in range(g0, g0 + ng):
                    self.mm(self.ps[pt][:, (t - g0) * 128:(t - g0 + 1) * 128], P[:, t * 128:(t + 1) * 128], self.ident_b, True, True, [tP, self.tconst], [self.tp[pt]])
                self.dve(lambda e, pt=pt, gi=gi, ng=ng: e.tensor_copy(out=PT[gi][:, 0:ng * 128], in_=self.ps[pt][:, 0:ng * 128]), [self.tp[pt]], [tPT[gi]])
                return gi, ng

            def pv_(g0, gi, ng):
                for t in range(g0, g0 + ng):
                    kt = segs[t][0]
                    self.mm(self.ps[7][:, ob:ob + 64], PT[gi][:, (t - g0) * 128:(t - g0 + 1) * 128], V[:, kt, vcol:vcol + 64], t == 0, t == nseg - 1, [tPT[gi], tV[kt]], [self.tp[7]])

            groups = list(range(0, nseg, 4))
            info = pt_(groups[0])
            for gx, g0 in enumerate(groups):
                nxt = pt_(groups[gx + 1]) if gx + 1 < len(groups) else None
                pv_(g0, *info)
                info = nxt
            self.act(lambda e: e.activation(out=cc[:, oc0:oc0 + 64], in_=self.ps[7][:, ob:ob + 64], func=AF.Copy, scale=sm[:, 11:12]), [self.tp[7], tsm], [tcc])

        for i in range(nq):
            b = i % 2
            isctx = i >= NL
            self.make_aT(l, i, 0, aT[b], taT[b])
            for kc in range(8):
                self.mm(self.ps[0][:, :], aT[b][:, kc, :], Wq[:, kc, 0:512], kc == 0, kc == 7, [taT[b]] + tWq[0:2], [self.tp[0]])
            for kc in range(8):
                self.mm(self.ps[1][:, 0:256], aT[b][:, kc, :], Wq[:, kc, 512:768], kc == 0, kc == 7, [taT[b], tWq[2]], [self.tp[1]])
            self.act(lambda e: e.activation(out=qbf[:, 0:256], in_=self.ps[0][:, 0:256], func=AF.Copy), [self.tp[0]], [tqb])
            self.rms_rope(self.ps[0][:, 256:512], self.tp[0], 4, qbf[:, 256:512], tqb, i, rw, g=gqk[:, 0, :], perm=True)
            self.rms_rope(self.ps[1][:, 0:256], self.tp[1], 4, qbf[:, 512:768], tqb, i, rw, perm=True)
            for c in range(6):
                bk = 2 + c // 4
                self.mm(self.ps[bk][:, (c % 4) * 128:(c % 4 + 1) * 128], qbf[:, c * 128:(c + 1) * 128], self.ident_b, True, True, [tqb, self.tconst], [self.tp[bk]])
            self.dve(lambda e: e.tensor_scalar(out=qT[:, 0:4, :], in0=self.ps[2][:, :].rearrange("p (c t) -> p c t", t=128), scalar1=0.125, scalar2=None, op0=ALU.mult), [self.tp[2]], [tqT])
            self.dve(lambda e: e.tensor_scalar(out=qT[:, 4:6, :], in0=self.ps[3][:, 0:256].rearrange("p (c t) -> p c t", t=128), scalar1=0.125, scalar2=None, op0=ALU.mult), [self.tp[3]], [tqT])
            ctxs = [(16, None), (17, None)]
            for h in range(4):
                blk, pb0, c = h // 2, (h % 2) * 64, h // 2
                if isctx:
                    segs = [(kt, c, None) for kt, _ in ctxs]
                    attention(i, blk, pb0, segs, h * 64, h * 64, 0.125)
                else:
                    j = i
                    if 2 <= j <= 13:
                        var, t0, ntl, s0 = 0, j - 2, 5, 3
                    elif j == 0:
                        var, t0, ntl, s0 = 1, 0, 4, 7
                    elif j == 1:
                        var, t0, ntl, s0 = 2, 0, 4, 5
                    elif j == 14:
                        var, t0, ntl, s0 = 3, 12, 4, 3
                    else:
                        var, t0, ntl, s0 = 4, 12, 4, 1
                    mo = 0 if var == 0 else 640 + (var - 1) * 512
                    segs = [(t0 + k, c, namask[:, mo + k * 128:mo + (k + 1) * 128]) for k in range(ntl)] + [(kt, c, None) for kt, _ in ctxs]
                    attention(i, blk, pb0, segs, h * 64, h * 64, 1.0, bias_h=h, s0=s0)
            for h in range(4):
                blk, pb0, kvh = 2 + (h % 2), (h // 2) * 64, h // 2
                kts = [16, 17] if isctx else list(range(NT))
                attention(i, blk, pb0, [(kt, 2, None) for kt in kts], 256 + kvh * 64, 256 + h * 64, 0.125)
            self.pool(lambda e, i=i: e.tensor_copy(out=cc[:, 512:768], in_=OC[:, i, :]), [tOC[i]], [tcc])
            for h in range(4):
                blk, pb0, kvh = 4 + (h % 2), (h // 2) * 64, h // 2
                if isctx:
                    segs = [(16, 3, None), (17, 3, None)]
                else:
                    segs = []
                    if i - 1 >= 0: segs.append((i - 1, 3, tri[:, 0, :]))
                    segs.append((i, 3, None))
                    if i + 1 < NL: segs.append((i + 1, 3, tri[:, 1, :]))
                    segs += [(16, 3, None), (17, 3, None)]
                attention(i, blk, pb0, segs, 384 + kvh * 64, 768 + h * 64, 0.125, sink_h=h)
            att1(jobs[0][2], *jobs[0][0], **jobs[0][1])
            att1b(jobs[0][2], *jobs[0][0], **jobs[0][1])
            for k_ in range(len(jobs)):
                if k_ + 1 < len(jobs):
                    att1(jobs[k_ + 1][2], *jobs[k_ + 1][0], **jobs[k_ + 1][1])
                att2(jobs[k_][2], *jobs[k_][0], **jobs[k_][1])
                if k_ + 1 < len(jobs):
                    att1b(jobs[k_ + 1][2], *jobs[k_ + 1][0], **jobs[k_ + 1][1])
            del jobs[:]
            for c in range(8):
                bk = 5 + c // 4
                self.mm(self.ps[bk][:, (c % 4) * 128:(c % 4 + 1) * 128], cc[:, c * 128:(c + 1) * 128], self.ident_b, True, True, [tcc, self.tconst], [self.tp[bk]])
            for hf in range(2):
                self.act(lambda e, hf=hf: e.activation(out=ccT[:, hf * 4:(hf + 1) * 4, :], in_=self.ps[5 + hf][:, :].rearrange("p (c t) -> p c t", t=128), func=AF.Copy), [self.tp[5 + hf]], [tccT])
            for hf in range(2):
                for kc in range(8):
                    self.mm(self.ps[hf][:, :], ccT[:, kc, :], Wo[:, kc, hf * 512:(hf + 1) * 512], kc == 0, kc == 7, [tccT, tWo], [self.tp[hf]])
            self.resid_ln(i, [(self.ps[0][:, :], self.tp[0]), (self.ps[1][:, :], self.tp[1])], G, tG, LN, tLN, tmp, ttmp, st, tst)
        A.release(m_all)

    def sin_of(self, out, x, shift, wk, tk, R):
        TWO_PI = 2.0 * np.pi
        a, k = wk
        ki = k.bitcast(I32)
        self.dve(lambda e: e.tensor_scalar(out=a, in0=x, scalar1=1.0 / TWO_PI, scalar2=shift / TWO_PI + 0.5, op0=ALU.mult, op1=ALU.add), R, [tk])
        self.dve(lambda e: e.tensor_copy(out=ki, in_=a), [tk], [tk])
        self.dve(lambda e: e.tensor_copy(out=a, in_=ki), [tk], [tk])
        self.dve(lambda e: e.tensor_scalar(out=k, in0=x, scalar1=shift, scalar2=None, op0=ALU.add), R + [tk], [tk])
        self.dve(lambda e: e.scalar_tensor_tensor(out=k, in0=a, scalar=-TWO_PI, in1=k, op0=ALU.mult, op1=ALU.add), [tk], [tk])
        self.dve(lambda e: e.tensor_scalar(out=a, in0=k, scalar1=float(np.pi), scalar2=None, op0=ALU.is_gt), [tk], [tk])
        self.dve(lambda e: e.scalar_tensor_tensor(out=k, in0=a, scalar=-TWO_PI, in1=k, op0=ALU.mult, op1=ALU.add), [tk], [tk])
        self.dve(lambda e: e.tensor_scalar(out=a, in0=k, scalar1=-float(np.pi), scalar2=None, op0=ALU.is_lt), [tk], [tk])
        self.dve(lambda e: e.scalar_tensor_tensor(out=k, in0=a, scalar=TWO_PI, in1=k, op0=ALU.mult, op1=ALU.add), [tk], [tk])
        self.dve(lambda e: e.tensor_scalar(out=k, in0=k, scalar1=3.1415925, scalar2=-3.1415925, op0=ALU.min, op1=ALU.max), [tk], [tk])
        self.act(lambda e: e.activation(out=out, in_=k, func=AF.Sin), [tk], [tk])

    def s5_phase(self, l, UT, tUT, OC, tOC, nq):
        A, S = self.A, self.S
        dve, act, pool = self.dve, self.act, self.pool
        tp_ = Tk()
        vec = A.alloc([16, 3], F32)
        tau = A.alloc([130], F32)
        bs = A.alloc([16, 2, 16], F32)
        S.dma("sp", vec, self.ssm_vec[l], W=[tp_])
        S.dma("sp", tau, self.c_tau, W=[tp_])
        S.dma("sp", bs, self.ssm_bs[l], W=[tp_])
        CSb = A.alloc([16, 2, 32], BF16); tcs = Tk()
        DDb = A.alloc([8, 32], BF16)
        Wg = A.alloc([2, 256], BF16)
        S.dma("pool", CSb, self.ssm_cs[l], W=[tcs])
        S.dma("pool", DDb, self.ssm_dd[l], W=[tcs])
        S.dma("pool", Wg, self.ssm_wglu[l].rearrange("(c p) n -> p c n", p=128), W=[tcs])
        dve(lambda e: e.tensor_scalar(out=CSb[:, :, 1, :], in0=CSb[:, :, 1, :], scalar1=-1.0, scalar2=None, op0=ALU.mult), [tcs], [tcs])
        pv = A.alloc([16, 16], F32)
        def q(k): return pv[:, k, :]
        lre, lim, lst = vec[:, :, 0], vec[:, :, 1], vec[:, :, 2]
        STEP, LR, ANG, MAG, SN, CN, ABR, ABI, DEN, NUM, FRE, FIM, T0, T1 = range(14)
        act(lambda e: e.activation(out=q(STEP), in_=lst, func=AF.Exp), [tp_], [tp_])
        dve(lambda e: e.tensor_tensor(out=q(LR), in0=lre, in1=q(STEP), op=ALU.mult), [tp_], [tp_])
        dve(lambda e: e.tensor_tensor(out=q(ANG), in0=lim, in1=q(STEP), op=ALU.mult), [tp_], [tp_])
        act(lambda e: e.activation(out=q(MAG), in_=q(LR), func=AF.Exp), [tp_], [tp_])
        self.sin_of(q(SN), q(ANG), 0.0, (q(T0), q(T1)), tp_, [tp_])
        self.sin_of(q(CN), q(ANG), float(np.pi / 2), (q(T0), q(T1)), tp_, [tp_])
        dve(lambda e: e.tensor_tensor(out=q(ABR), in0=q(MAG), in1=q(CN), op=ALU.mult), [tp_], [tp_])
        dve(lambda e: e.tensor_tensor(out=q(ABI), in0=q(MAG), in1=q(SN), op=ALU.mult), [tp_], [tp_])
        dve(lambda e: e.tensor_tensor(out=q(DEN), in0=lre, in1=lre, op=ALU.mult), [tp_], [tp_])
        dve(lambda e: e.tensor_tensor(out=q(T0), in0=lim, in1=lim, op=ALU.mult), [tp_], [tp_])
        dve(lambda e: e.tensor_tensor(out=q(DEN), in0=q(DEN), in1=q(T0), op=ALU.add), [tp_], [tp_])
        dve(lambda e: e.reciprocal(out=q(DEN), in_=q(DEN)), [tp_], [tp_])
        dve(lambda e: e.tensor_scalar(out=q(NUM), in0=q(ABR), scalar1=-1.0, scalar2=None, op0=ALU.add), [tp_], [tp_])
        dve(lambda e: e.tensor_tensor(out=q(T0), in0=q(NUM), in1=lre, op=ALU.mult), [tp_], [tp_])
        dve(lambda e: e.tensor_tensor(out=q(T1), in0=q(ABI), in1=lim, op=ALU.mult), [tp_], [tp_])
        dve(lambda e: e.tensor_tensor(out=q(FRE), in0=q(T0), in1=q(T1), op=ALU.add), [tp_], [tp_])
        dve(lambda e: e.tensor_tensor(out=q(FRE), in0=q(FRE), in1=q(DEN), op=ALU.mult), [tp_], [tp_])
        dve(lambda e: e.tensor_tensor(out=q(T0), in0=q(ABI), in1=lre, op=ALU.mult), [tp_], [tp_])
        dve(lambda e: e.tensor_tensor(out=q(T1), in0=q(NUM), in1=lim, op=ALU.mult), [tp_], [tp_])
        dve(lambda e: e.tensor_tensor(out=q(FIM), in0=q(T0), in1=q(T1), op=ALU.subtract), [tp_], [tp_])
        dve(lambda e: e.tensor_tensor(out=q(FIM), in0=q(FIM), in1=q(DEN), op=ALU.mult), [tp_], [tp_])
        EC = A.alloc([16, 129], F32); ES = A.alloc([16, 129], F32); tE = Tk()
        mtab = A.mark()
        X = A.alloc([16, 129], F32); wa = A.alloc([16, 129], F32); wb = A.alloc([16, 129], F32); tX = Tk()
        dve(lambda e: e.tensor_tensor(out=X, in0=q(ANG).unsqueeze(2).to_broadcast([128, 16, 129]), in1=tau[:, 0:129].unsqueeze(1).to_broadcast([128, 16, 129]), op=ALU.mult), [tp_], [tX])
        self.sin_of(ES, X, 0.0, (wa, wb), tE, [tX])
        self.sin_of(EC, X, float(np.pi / 2), (wa, wb), tE, [tX])
        A.release(mtab)
        BT = A.alloc([16, 2, 128], BF16); tBT = Tk()
        mb = A.mark()
        bb = A.alloc([16, 2, 16], F32); tbb = Tk()
        w4 = A.alloc([4, 16, 16], F32)
        fre_b = q(FRE).unsqueeze(2).to_broadcast([128, 16, 16]); fim_b = q(FIM).unsqueeze(2).to_broadcast([128, 16, 16])
        dve(lambda e: e.tensor_tensor(out=w4[:, 0], in0=bs[:, :, 0, :], in1=fre_b, op=ALU.mult), [tp_], [tbb])
        dve(lambda e: e.tensor_tensor(out=w4[:, 1], in0=bs[:, :, 1, :], in1=fim_b, op=ALU.mult), [tp_], [tbb])
        dve(lambda e: e.tensor_tensor(out=w4[:, 2], in0=bs[:, :, 1, :], in1=fre_b, op=ALU.mult), [tp_], [tbb])
        dve(lambda e: e.tensor_tensor(out=w4[:, 3], in0=bs[:, :, 0, :], in1=fim_b, op=ALU.mult), [tp_], [tbb])
        dve(lambda e: e.tensor_tensor(out=bb[:, :, 0, :], in0=w4[:, 0], in1=w4[:, 1], op=ALU.subtract), [tbb], [tbb])
        dve(lambda e: e.tensor_tensor(out=bb[:, :, 1, :], in0=w4[:, 2], in1=w4[:, 3], op=ALU.add), [tbb], [tbb])
        Bp = A.alloc([4, 128], BF16); tBp = [Tk() for _ in range(4)]
        dve(lambda e: e.memset(Bp, 0.0), [], tBp)
        n = 0
        for dg in range(16):
            gc = dg % 8
            band = gc % 4
            for ri in range(2):
                for gl in range(2):
                    dve(lambda e, dg=dg, ri=ri, gl=gl, band=band: e.tensor_copy(out=Bp[gl * 64:(gl + 1) * 64, band, band * 32 + gl * 16:band * 32 + gl * 16 + 16],
                                                                            in_=bb[gl * 64:(gl + 1) * 64, dg, ri, :]), [tbb], [tBp[band]])
                bk = 5 + (n // 4) % 2
                self.mm(self.ps[bk][:, (n % 4) * 128:(n % 4 + 1) * 128], Bp[:, band, :], self.ident_b, True, True, [tBp[band], self.tconst], [self.tp[bk]])
                if n % 4 == 3:
                    dg0 = (n - 3) // 2
                    act(lambda e, bk=bk, dg0=dg0: e.activation(out=BT[:, dg0:dg0 + 2, :, :], in_=self.ps[bk][:, :].rearrange("p (a b t) -> p a b t", a=2, b=2), func=AF.Copy), [self.tp[bk]], [tBT])
                n += 1
        A.release(mb)
        Y = A.alloc([NT, 256], F32); tY = [Tk() for _ in range(NT)]
        z = A.alloc([256], F32); z2 = A.alloc([256], F32); tz = Tk()
        zb = A.alloc([256], BF16); zT = A.alloc([2, 128], BF16); tzT = Tk()
        RD = A.alloc([2, 16, 128], F32); tRD = Tk()
        for d in range(2):
            dve(lambda e, d=d: e.tensor_copy(out=RD[:, d].rearrange("p (g r) t -> p g r t", r=2),
                                             in_=q(MAG)[:, d * 8:(d + 1) * 8].unsqueeze(2).unsqueeze(3).to_broadcast([128, 8, 2, 128])), [tp_], [tRD])
            first = 0 if d == 0 else 127
            dve(lambda e, d=d, first=first: e.memset(RD[:, d, :, first:first + 1], 0.0), [tRD], [tRD])
        g = A.alloc([8, 2, 128], F32); tg = Tk()
        gi = A.alloc([8, 2], F32); tgi = Tk()
        giw = A.alloc([4, 8], F32)
        tt = [A.alloc([8, 128], F32) for _ in range(4)]; ttt = Tk()
        w = A.alloc([8, 2, 128], F32); tw = Tk()
        hs = A.alloc([8, 2, 128], BF16); ths = Tk()
        bu4 = self.psall[:, 0:2048].rearrange("p (g r t) -> p g r t", r=2, t=128)
        tb4 = [self.tp[k] for k in range(4)]
        for d in range(2):
            order = [16, 17] + list(range(16)) if d == 0 else [17, 16] + list(range(15, -1, -1))
            first, last = (0, 127) if d == 0 else (127, 0)
            cs_, sn_ = rv_tab(EC, d * 8, d, 8), rv_tab(ES, d * 8, d, 8)
            for n_, i in enumerate(order):
                tk = slice(i * 128, (i + 1) * 128)
                for gc in range(8):
                    for ri in range(2):
                        bk = gc // 2
                        c0 = (gc % 2) * 256 + ri * 128
                        self.mm(self.ps[bk][:, c0:c0 + 128], BT[:, d * 8 + gc, ri, :], UT[:, gc // 4, tk], True, True, [tBT, tUT[i]], [self.tp[bk]])
                if n_ > 0:
                    glr, gli = g[:, :, 0, last], g[:, :, 1, last]
                    cT, sT = EC[:, d * 8:(d + 1) * 8, 128], ES[:, d * 8:(d + 1) * 8, 128]
                    dve(lambda e, glr=glr, cT=cT: e.tensor_tensor(out=giw[:, 0], in0=glr, in1=cT, op=ALU.mult), [tg, tE], [tgi])
                    dve(lambda e, gli=gli, sT=sT: e.tensor_tensor(out=giw[:, 1], in0=gli, in1=sT, op=ALU.mult), [tg, tE], [tgi])
                    dve(lambda e, glr=glr, sT=sT: e.tensor_tensor(out=giw[:, 2], in0=glr, in1=sT, op=ALU.mult), [tg, tE], [tgi])
                    dve(lambda e, gli=gli, cT=cT: e.tensor_tensor(out=giw[:, 3], in0=gli, in1=cT, op=ALU.mult), [tg, tE], [tgi])
                    dve(lambda e: e.tensor_tensor(out=gi[:, :, 0], in0=giw[:, 0], in1=giw[:, 1], op=ALU.subtract), [tgi], [tgi])
                    dve(lambda e: e.tensor_tensor(out=gi[:, :, 1], in0=giw[:, 2], in1=giw[:, 3], op=ALU.add), [tgi], [tgi])
                    dve(lambda e, d=d: e.tensor_tensor(out=gi, in0=gi, in1=q(MAG)[:, d * 8:(d + 1) * 8].unsqueeze(2).to_broadcast([128, 8, 2]), op=ALU.mult), [tgi, tp_], [tgi])
                bre, bim = bu4[:, :, 0, :], bu4[:, :, 1, :]
                dve(lambda e, cs_=cs_, sn_=sn_: e.tensor_tensor(out=tt[0], in0=bre, in1=cs_, op=ALU.mult), tb4 + [tE], [ttt])
                dve(lambda e, cs_=cs_, sn_=sn_: e.tensor_tensor(out=tt[1], in0=bim, in1=sn_, op=ALU.mult), tb4 + [tE], [ttt])
                dve(lambda e, cs_=cs_, sn_=sn_: e.tensor_tensor(out=tt[2], in0=bim, in1=cs_, op=ALU.mult), tb4 + [tE], [ttt])
                dve(lambda e, cs_=cs_, sn_=sn_: e.tensor_tensor(out=tt[3], in0=bre, in1=sn_, op=ALU.mult), tb4 + [tE], [ttt])
                pool(lambda e: e.tensor_tensor(out=w[:, :, 0, :], in0=tt[0], in1=tt[1], op=ALU.add), [ttt], [tw])
                pool(lambda e: e.tensor_tensor(out=w[:, :, 1, :], in0=tt[2], in1=tt[3], op=ALU.subtract), [ttt], [tw])
                if n_ > 0:
                    pool(lambda e, first=first: e.tensor_tensor(out=w[:, :, :, first], in0=w[:, :, :, first], in1=gi, op=ALU.add), [tw, tgi], [tw])
                gf = g.rearrange("p g r t -> p (g r t)")
                wf = w.rearrange("p g r t -> p (g r t)")
                rf = RD[:, d].rearrange("p k t -> p (k t)")
                if d == 0:
                    dve(lambda e, rf=rf: e.tensor_tensor_scan(out=gf, data0=rf, data1=wf, initial=0.0, op0=ALU.mult, op1=ALU.add), [tw, tRD], [tg])
                else:
                    dve(lambda e, rf=rf: e.tensor_tensor_scan(out=gf[:, ::-1], data0=rf[:, ::-1], data1=wf[:, ::-1], initial=0.0, op0=ALU.mult, op1=ALU.add), [tw, tRD], [tg])
                gre, gim = g[:, :, 0, :], g[:, :, 1, :]
                dve(lambda e, cs_=cs_, sn_=sn_: e.tensor_tensor(out=tt[0], in0=gre, in1=cs_, op=ALU.mult), [tg, tE], [ttt])
                dve(lambda e, cs_=cs_, sn_=sn_: e.tensor_tensor(out=tt[1], in0=gim, in1=sn_, op=ALU.mult), [tg, tE], [ttt])
                dve(lambda e, cs_=cs_, sn_=sn_: e.tensor_tensor(out=tt[2], in0=gre, in1=sn_, op=ALU.mult), [tg, tE], [ttt])
                dve(lambda e, cs_=cs_, sn_=sn_: e.tensor_tensor(out=tt[3], in0=gim, in1=cs_, op=ALU.mult), [tg, tE], [ttt])
                pool(lambda e: e.tensor_tensor(out=hs[:, :, 0, :], in0=tt[0], in1=tt[1], op=ALU.subtract), [ttt], [ths])
                pool(lambda e: e.tensor_tensor(out=hs[:, :, 1, :], in0=tt[2], in1=tt[3], op=ALU.add), [ttt], [ths])
                for gc in range(8):
                    terms = [(hs[:, gc, 0, :], CSb[:, d * 8 + gc, 0, :], [ths, tcs]), (hs[:, gc, 1, :], CSb[:, d * 8 + gc, 1, :], [ths, tcs])]
                    if d == 0:
                        terms.append((UT[:, gc // 4, tk], DDb[:, gc, :], [tUT[i], tcs]))
                    for k, (lt, rh, R_) in enumerate(terms):
                        self.mm(self.ps[4][:, gc * 32:(gc + 1) * 32], lt, rh, k == 0, k == len(terms) - 1, R_, [self.tp[4]])
                if d == 0:
                    act(lambda e, i=i: e.activation(out=Y[:, i, :], in_=self.ps[4][:, 0:256], func=AF.Copy), [self.tp[4]], [tY[i]])
                    if getattr(self, 'dbg_y', None) is not None:
                        self.store(self.dbg_y[i * 128:(i + 1) * 128, :], Y[:, i, :], [tY[i]])
                    continue
                if i >= nq:
                    continue
                dve(lambda e, i=i: e.tensor_tensor(out=z, in0=self.ps[4][:, 0:256], in1=Y[:, i, :], op=ALU.add), [self.tp[4], tY[i]], [tz])
                pool(lambda e: e.tensor_tensor(out=z2, in0=z, in1=z, op=ALU.mult), [tz], [tz])
                pool(lambda e: e.tensor_scalar(out=z2, in0=z2, scalar1=0.044715, scalar2=1.0, op0=ALU.mult, op1=ALU.add), [tz], [tz])
                pool(lambda e: e.tensor_tensor(out=z2, in0=z2, in1=z, op=ALU.mult), [tz], [tz])
                act(lambda e: e.activation(out=z2, in_=z2, func=AF.Sigmoid, scale=1.5957691216057308), [tz], [tz])
                dve(lambda e: e.tensor_tensor(out=z, in0=z, in1=z2, op=ALU.mult), [tz], [tz])
                dve(lambda e: e.tensor_copy(out=zb, in_=z), [tz], [tz])
                for c in range(2):
                    self.mm(self.ps[5][:, c * 128:(c + 1) * 128], zb[:, c * 128:(c + 1) * 128], self.ident_b, True, True, [tz, self.tconst], [self.tp[5]])
                act(lambda e: e.activation(out=zT, in_=self.ps[5][:, 0:256].rearrange("p (c t) -> p c t", t=128), func=AF.Copy), [self.tp[5]], [tzT])
                for c in range(2):
                    self.mm(self.ps[6][:, 0:256], zT[:, c, :], Wg[:, c, :], c == 0, c == 1, [tzT, tcs], [self.tp[6]])
                act(lambda e: e.activation(out=z2, in_=self.ps[6][:, 0:256], func=AF.Sigmoid), [self.tp[6]], [tz])
                dve(lambda e, i=i: e.tensor_tensor(out=OC[:, i, :], in0=z, in1=z2, op=ALU.mult), [tz], [tOC[i]])


def rv_tab(E, dg0, d, n=2):
    if d == 0:
        return E[:, dg0:dg0 + n, 0:128]
    return E[:, dg0:dg0 + n, 127::-1]


def _consts():
    c = {}
    c["c_ident_f"] = np.eye(128, dtype=np.float32)
    c["c_ident_b"] = np.eye(128, dtype=np.float32).astype(ml_dtypes.bfloat16)
    pos = np.arange(NL * 128)
    row = (pos // 64).astype(np.float32)
    col = (pos % 64).astype(np.float32)
    inv = (10000.0 ** (-np.arange(16, dtype=np.float32) / 16)).astype(np.float32)
    ang = np.concatenate([row[:, None] * inv, col[:, None] * inv], -1).astype(np.float32)
    cs = np.concatenate([np.cos(ang), -np.sin(ang), np.sin(ang)], -1).astype(np.float32)
    ctxcs = np.concatenate([np.ones((256, 32), np.float32), np.zeros((256, 64), np.float32)], -1)
    cs = np.concatenate([cs, ctxcs], 0).reshape(NT, 128, 96).transpose(1, 0, 2)
    c["c_rope"] = np.ascontiguousarray(cs)
    q = np.arange(128)[:, None]
    k = np.arange(128)[None, :]
    tri = np.stack([np.where(k >= q, 0.0, NEG), np.where(k <= q, 0.0, NEG)], 1).astype(np.float32)
    c["c_tri"] = tri.astype(ml_dtypes.bfloat16)
    nm = np.full((5, 128, 10, 64), NEG, np.float32)
    qc = np.arange(64)
    cstart = np.clip(qc - 8, 0, 48)
    kc = np.arange(64)
    colv = (kc[None, :] >= cstart[:, None]) & (kc[None, :] < cstart[:, None] + 16)
    def fill(var, j, i0, nr):
        for rl in range(2):
            r = 2 * j + rl
            rs = int(np.clip(r - 4, 0, 24))
            for ii in range(nr):
                i = i0 + ii
                if rs <= i < rs + 8:
                    blk = np.where(colv, 0.0, NEG)
                    nm[var, rl * 64:(rl + 1) * 64, ii, :] = blk
    fill(0, 5, 6, 10)
    fill(1, 0, 0, 8); fill(2, 1, 0, 8); fill(3, 14, 24, 8); fill(4, 15, 24, 8)
    nm2 = nm.transpose(1, 0, 2, 3).reshape(128, 5, 640)
    c["c_namask"] = np.ascontiguousarray(np.concatenate([nm2[:, 0, :]] + [nm2[:, v, 0:512] for v in range(1, 5)], 1)).astype(ml_dtypes.bfloat16)
    tau = np.zeros((128, 130), np.float32)
    tau[:, :] = np.arange(130, dtype=np.float32)[None, :]
    c["c_tau"] = tau
    return c


def _prep_shared(I):
    f = np.float32
    d = dict(_consts())
    for k in ("ada_w", "ada_b", "w_in", "w_out"):
        d[k] = np.ascontiguousarray(I[k], dtype=f)
    rp = np.zeros((DEPTH, 4, 18, 128), f)
    rp[:, :, 1:16, 48:79] = I["na_rpb"][:, :, :, ::-1]
    d["rpbh"] = rp
    d["gqk"] = np.ascontiguousarray(np.stack([I["ga_q_norm"], I["ga_k_norm"]], 1), dtype=f)
    def st(a):
        L = a.shape[0]
        rest = a.shape[4:]
        a = a.reshape((L, 2, 8, 2, 64) + rest)
        a = np.moveaxis(a, (3, 4), (1, 2))
        return np.ascontiguousarray(a.reshape((L, 128, 16) + rest))
    ls = np.broadcast_to(I["ssm_log_step"][..., None], I["ssm_lambda_re"].shape)
    d["ssm_vec"] = np.ascontiguousarray(np.stack([st(I["ssm_lambda_re"]), st(I["ssm_lambda_im"]), st(np.ascontiguousarray(ls))], -1), dtype=f)
    d["ssm_bs"] = np.ascontiguousarray(np.stack([st(I["ssm_b_re"]), st(I["ssm_b_im"])], -2), dtype=f)
    cre = st(np.swapaxes(I["ssm_c_re"], -1, -2))
    cim = st(np.swapaxes(I["ssm_c_im"], -1, -2))
    cs = np.zeros((DEPTH, 128, 16, 2, 32), f)
    for gl in range(2):
        cs[:, gl * 64:(gl + 1) * 64, :, 0, gl * 16:(gl + 1) * 16] = cre[:, gl * 64:(gl + 1) * 64]
        cs[:, gl * 64:(gl + 1) * 64, :, 1, gl * 16:(gl + 1) * 16] = cim[:, gl * 64:(gl + 1) * 64]
    d["ssm_cs"] = cs
    dd = np.zeros((DEPTH, 128, 8, 32), f)
    sd = I["ssm_d"].reshape(DEPTH, 2, 128)
    for gc in range(8):
        for j in range(32):
            pl = (gc % 4) * 32 + j
            dd[:, pl, gc, j] = sd[:, gc // 4, pl]
    d["ssm_dd"] = dd
    d["ssm_wglu"] = np.ascontiguousarray(I["ssm_w_glu"], dtype=f)
    d["sink"] = np.ascontiguousarray(I["sw_sink"], dtype=f)
    d["lngb"] = np.ascontiguousarray(np.stack([I["ln1_g"], I["ln1_b"], I["ln2_g"], I["ln2_b"]], 1), dtype=f)
    d["ffn_g"] = I["ffn_w_gate"]; d["ffn_u"] = I["ffn_w_up"]; d["ffn_d"] = I["ffn_w_down"]
    d["moe_r"] = I["moe_w_router"]; d["moe_rb"] = I["moe_b_router"]
    d["moe_g"] = I["moe_w_gate"]; d["moe_u"] = I["moe_w_up"]; d["moe_d"] = I["moe_w_down"]
    return d


def _prep_core(I, b, hx=None):
    if hx is None:
        hx = np.concatenate([I["x"][b], I["ctx"][b]], 0)
    cv = np.stack([I["c"][b].reshape(8, 128).T, I["c_ctx"].reshape(8, 128).T], -1)
    return {"hx": np.ascontiguousarray(hx, dtype=np.float32), "cv": np.ascontiguousarray(cv, dtype=np.float32)}


def build_program(layers=(0, 1, 2, 3)):
    b = B(list(layers))
    b.setup()
    for l in layers:
        b.ada_cols(l)
        b.mixer_phase(l)
        b.ffn_phase(l)
    for i in range(NL):
        b.store(b.out[i * 128:(i + 1) * 128, :], b.H[:, i, :], [b.tH[i]])
    with b.nc.Block() as blk:
        b.S.emit(blk, b.fin)
    return b


def kernel(**inputs):
    I = {k: np.asarray(v) for k, v in inputs.items()}
    n = 8
    b = build_program()
    shared = _prep_shared(I)
    shared = {k: v for k, v in shared.items() if k in b.din}
    in_maps = []
    for c in range(n):
        m = dict(shared)
        m.update(_prep_core(I, c))
        in_maps.append(m)
    res = run_bass_kernel_spmd(b.nc, in_maps, core_ids=list(range(n)))
    out = np.stack([np.asarray(r["out"], dtype=np.float32).reshape(NL * 128, D) for r in res.results], 0)
    return out
```
